# Optimizing a Trainium2 kernel written in Bass

```python
import math
import jax, jax.numpy as jnp
from jax import lax
import numpy as np

D_MODEL = 1024
BATCH = 4
SEQ = 8192
DEPTH = 1

GRID_W = 64
DA_HEADS = 4
DA_HEAD_DIM = 64
DA_V_DIM = 2 * DA_HEAD_DIM
DA_WIDTH = DA_HEADS * DA_V_DIM
Q_BLOCK = 128
ROPE_THETA = 10000.0
NA_HEADS = 8
NA_HEAD_DIM = 64
NA_WIDTH = NA_HEADS * NA_HEAD_DIM
NA_KH_MAX = 8
NA_KW = 16
N_BRANCHES = 2
IN_COLS = 3 * DA_WIDTH + 3 * NA_WIDTH + N_BRANCHES * D_MODEL
IN_SPLITS = (DA_WIDTH, 2 * DA_WIDTH, 3 * DA_WIDTH,
             3 * DA_WIDTH + NA_WIDTH, 3 * DA_WIDTH + 2 * NA_WIDTH,
             3 * DA_WIDTH + 3 * NA_WIDTH)
N_EXPERTS = 32
TOP_K = 4
D_EXPERT = 1024
SWIGLU_ALPHA = 1.702
SWIGLU_LIMIT = 7.0
MOE_BLOCK = 256
LN_EPS = 1e-5
RMS_EPS = 1e-5
DEEPNORM_ALPHA = (2 * DEPTH) ** 0.25
DEEPNORM_BETA = (8 * DEPTH) ** -0.25

kernel_name = "hybrid_diffattn_natten_moe_deepnorm"


def layer_norm(x, g, b):
    xf = x.astype(jnp.float32)
    mu = jnp.mean(xf, axis=-1, keepdims=True)
    var = jnp.mean(jnp.square(xf - mu), axis=-1, keepdims=True)
    return ((xf - mu) * lax.rsqrt(var + LN_EPS) * g + b).astype(x.dtype)


def rope_tables(seq, dim):
    pos = jnp.arange(seq, dtype=jnp.float32)
    inv = ROPE_THETA ** (-jnp.arange(0, dim, 2, dtype=jnp.float32) / dim)
    ang = pos[:, None] * inv[None, :]
    ang = jnp.concatenate([ang, ang], axis=-1)
    return jnp.cos(ang), jnp.sin(ang)


def apply_rope(x, cos, sin):
    c = cos[None, :, None, None, :]
    s = sin[None, :, None, None, :]
    x1, x2 = jnp.split(x, 2, axis=-1)
    rot = jnp.concatenate([-x2, x1], axis=-1)
    return (x.astype(jnp.float32) * c + rot.astype(jnp.float32) * s).astype(x.dtype)


def diff_attention(q, k, v, lam, lam_init, subln_g):
    B, S, H, _, d = q.shape
    nb = S // Q_BLOCK
    qb = (q * d ** -0.5).reshape(B, nb, Q_BLOCK, H, 2, d).transpose(1, 0, 2, 3, 4, 5)

    def block(q_blk):
        s = jnp.einsum('bqhcd,bkhcd->bhcqk', q_blk, k).astype(jnp.float32)
        p = jax.nn.softmax(s, axis=-1)
        w = p[:, :, 0] - lam * p[:, :, 1]
        return jnp.einsum('bhqk,bkhe->bqhe', w.astype(v.dtype), v)

    o = lax.map(block, qb)
    o = o.transpose(1, 0, 2, 3, 4).reshape(B, S, H, 2 * d).astype(jnp.float32)
    o = o * lax.rsqrt(jnp.mean(jnp.square(o), axis=-1, keepdims=True) + RMS_EPS) * subln_g
    return (o * (1.0 - lam_init)).reshape(B, S, H * 2 * d).astype(q.dtype)


def neighbourhood_attention(q, k, v, rpb):
    B, S, H, hd = q.shape
    rows = S // GRID_W
    kh = min(NA_KH_MAX, rows)
    q = (q * hd ** -0.5).reshape(B, rows, GRID_W, H, hd)
    k = k.reshape(B, rows, GRID_W, H, hd)
    v = v.reshape(B, rows, GRID_W, H, hd)
    cols = jnp.arange(GRID_W)
    col_start = jnp.clip(cols - NA_KW // 2, 0, GRID_W - NA_KW)
    col_idx = col_start[:, None] + jnp.arange(NA_KW)[None, :]
    col_off = col_idx - cols[:, None] + (NA_KW - 1)

    def row(r):
        rs = jnp.clip(r - kh // 2, 0, rows - kh)
        q_r = lax.dynamic_index_in_dim(q, r, axis=1, keepdims=False)
        k_rows = lax.dynamic_slice_in_dim(k, rs, kh, axis=1)
        v_rows = lax.dynamic_slice_in_dim(v, rs, kh, axis=1)
        k_win = k_rows[:, :, col_idx]
        v_win = v_rows[:, :, col_idx]
        row_off = rs + jnp.arange(kh) - r + (NA_KH_MAX - 1)
        bias = rpb[:, row_off[:, None, None], col_off[None, :, :]]
        s = jnp.einsum('bchd,bicjhd->bhcij', q_r, k_win).astype(jnp.float32)
        s = s + bias.transpose(0, 2, 1, 3)[None].astype(jnp.float32)
        p = jax.nn.softmax(s.reshape(B, H, GRID_W, kh * NA_KW), axis=-1).reshape(s.shape)
        return jnp.einsum('bhcij,bicjhd->bchd', p.astype(v.dtype), v_win)

    o = lax.map(row, jnp.arange(rows))
    return o.transpose(1, 0, 2, 3, 4).reshape(B, S, H * hd)


def hybrid_mixer(x, cos, sin, lam_init, w_in, b_in, lambda_q1, lambda_k1, lambda_q2,
                 lambda_k2, subln_g, rpb, w_branch_da, w_branch_na, w_out):
    B, S, D = x.shape
    proj = jnp.einsum('bsd,de->bse', x, w_in) + b_in
    q_da, k_da, v_da, q_na, k_na, v_na, gate_pre = jnp.split(proj, IN_SPLITS, axis=-1)
    q_da = apply_rope(q_da.reshape(B, S, DA_HEADS, 2, DA_HEAD_DIM), cos, sin)
    k_da = apply_rope(k_da.reshape(B, S, DA_HEADS, 2, DA_HEAD_DIM), cos, sin)
    v_da = v_da.reshape(B, S, DA_HEADS, DA_V_DIM)
    f32 = jnp.float32
    lam = (jnp.exp(jnp.sum(lambda_q1.astype(f32) * lambda_k1.astype(f32)))
           - jnp.exp(jnp.sum(lambda_q2.astype(f32) * lambda_k2.astype(f32))) + lam_init)
    a = diff_attention(q_da, k_da, v_da, lam, lam_init, subln_g)
    nb = neighbourhood_attention(q_na.reshape(B, S, NA_HEADS, NA_HEAD_DIM),
                                 k_na.reshape(B, S, NA_HEADS, NA_HEAD_DIM),
                                 v_na.reshape(B, S, NA_HEADS, NA_HEAD_DIM), rpb)
    ya = jnp.einsum('bse,ed->bsd', a, w_branch_da)
    yb = jnp.einsum('bse,ed->bsd', nb, w_branch_na)
    g = jax.nn.sigmoid(gate_pre.astype(f32)).reshape(B, S, N_BRANCHES, D)
    merged = (g[:, :, 0] * ya + g[:, :, 1] * yb).astype(x.dtype)
    return jnp.einsum('bsd,de->bse', merged, w_out)


def moe(x2, w_router, b_router, w_mlp1, b_mlp1, w_mlp2, b_mlp2):
    T, D = x2.shape
    TK = T * TOP_K
    logits = (x2 @ w_router + b_router).astype(jnp.float32)
    top_v, top_i = lax.top_k(logits, TOP_K)
    gates = jax.nn.softmax(top_v, axis=-1)
    flat_e = top_i.reshape(-1)
    flat_tok = jnp.arange(TK, dtype=jnp.int32) // TOP_K
    order = jnp.argsort(flat_e, stable=True)
    sorted_e = flat_e[order]
    counts = jnp.bincount(flat_e, length=N_EXPERTS)
    padded = ((counts + MOE_BLOCK - 1) // MOE_BLOCK) * MOE_BLOCK
    padded_end = jnp.cumsum(padded)
    padded_start = padded_end - padded
    group_start = jnp.cumsum(counts) - counts
    rank = jnp.arange(TK, dtype=jnp.int32) - group_start[sorted_e]
    dest = (padded_start[sorted_e] + rank).astype(jnp.int32)
    n_blocks = -(-TK // MOE_BLOCK) + N_EXPERTS
    P = n_blocks * MOE_BLOCK
    buf_tok = jnp.full((P,), T, dtype=jnp.int32).at[dest].set(flat_tok[order])
    x_pad = jnp.concatenate([x2, jnp.zeros((1, D), x2.dtype)], axis=0)
    xs = jnp.take(x_pad, buf_tok, axis=0).reshape(n_blocks, MOE_BLOCK, D)
    block_expert = jnp.clip(jnp.searchsorted(padded_end, jnp.arange(n_blocks) * MOE_BLOCK,
                                             side='right'), 0, N_EXPERTS - 1)

    def expert_block(args):
        xb, e = args
        h = xb @ w_mlp1[e] + b_mlp1[e]
        x_glu = jnp.minimum(h[:, ::2], SWIGLU_LIMIT)
        x_lin = jnp.clip(h[:, 1::2], -SWIGLU_LIMIT, SWIGLU_LIMIT)
        act = x_glu * jax.nn.sigmoid(SWIGLU_ALPHA * x_glu) * (x_lin + 1.0)
        return act @ w_mlp2[e] + b_mlp2[e]

    ys = lax.map(expert_block, (xs, block_expert)).reshape(P, D)
    dest_orig = jnp.zeros((TK,), jnp.int32).at[order].set(dest)
    y_slots = jnp.take(ys, dest_orig, axis=0).reshape(T, TOP_K, D)
    return jnp.sum(y_slots * gates[..., None].astype(ys.dtype), axis=1)


def setup_inputs(seed: int = 0) -> dict:
    key = jax.random.key(seed)
    ks = jax.random.split(key, 24)
    L = DEPTH
    f32 = jnp.float32

    def nrm(k, shape, s):
        return jax.random.normal(k, shape, f32) * s

    col_scale = jnp.concatenate([
        jnp.ones((2 * DA_WIDTH,), f32), jnp.full((DA_WIDTH,), DEEPNORM_BETA, f32),
        jnp.ones((2 * NA_WIDTH,), f32), jnp.full((NA_WIDTH,), DEEPNORM_BETA, f32),
        jnp.ones((N_BRANCHES * D_MODEL,), f32)])
    return {
        "x": nrm(ks[0], (BATCH, SEQ, D_MODEL), 1.0),
        "w_in": nrm(ks[1], (L, D_MODEL, IN_COLS), D_MODEL ** -0.5) * col_scale,
        "b_in": nrm(ks[2], (L, IN_COLS), 0.02),
        "lambda_q1": nrm(ks[3], (L, DA_HEAD_DIM), 0.1),
        "lambda_k1": nrm(ks[4], (L, DA_HEAD_DIM), 0.1),
        "lambda_q2": nrm(ks[5], (L, DA_HEAD_DIM), 0.1),
        "lambda_k2": nrm(ks[6], (L, DA_HEAD_DIM), 0.1),
        "subln_g": 1.0 + nrm(ks[7], (L, DA_V_DIM), 0.02),
        "rpb": nrm(ks[8], (L, NA_HEADS, 2 * NA_KH_MAX - 1, 2 * NA_KW - 1), 0.02),
        "w_branch_da": nrm(ks[9], (L, DA_WIDTH, D_MODEL), DA_WIDTH ** -0.5),
        "w_branch_na": nrm(ks[10], (L, NA_WIDTH, D_MODEL), NA_WIDTH ** -0.5),
        "w_out": nrm(ks[11], (L, D_MODEL, D_MODEL), D_MODEL ** -0.5) * DEEPNORM_BETA,
        "ln1_g": 1.0 + nrm(ks[12], (L, D_MODEL), 0.02),
        "ln1_b": nrm(ks[13], (L, D_MODEL), 0.02),
        "w_router": nrm(ks[14], (L, D_MODEL, N_EXPERTS), D_MODEL ** -0.5),
        "b_router": nrm(ks[15], (L, N_EXPERTS), 0.01),
        "w_mlp1": nrm(ks[16], (L, N_EXPERTS, D_MODEL, 2 * D_EXPERT), D_MODEL ** -0.5),
        "b_mlp1": nrm(ks[17], (L, N_EXPERTS, 2 * D_EXPERT), 0.02),
        "w_mlp2": nrm(ks[18], (L, N_EXPERTS, D_EXPERT, D_MODEL), D_EXPERT ** -0.5) * DEEPNORM_BETA,
        "b_mlp2": nrm(ks[19], (L, N_EXPERTS, D_MODEL), 0.02),
        "ln2_g": 1.0 + nrm(ks[20], (L, D_MODEL), 0.02),
        "ln2_b": nrm(ks[21], (L, D_MODEL), 0.02),
    }


def reference(x, w_in, b_in, lambda_q1, lambda_k1, lambda_q2, lambda_k2, subln_g, rpb,
              w_branch_da, w_branch_na, w_out, ln1_g, ln1_b, w_router, b_router,
              w_mlp1, b_mlp1, w_mlp2, b_mlp2, ln2_g, ln2_b):
    B, S, D = x.shape
    cos, sin = rope_tables(S, DA_HEAD_DIM)
    for l in range(DEPTH):
        lam_init = 0.8 - 0.6 * math.exp(-0.3 * l)
        mix = hybrid_mixer(x, cos, sin, lam_init, w_in[l], b_in[l], lambda_q1[l], lambda_k1[l],
                           lambda_q2[l], lambda_k2[l], subln_g[l], rpb[l],
                           w_branch_da[l], w_branch_na[l], w_out[l])
        x = layer_norm(DEEPNORM_ALPHA * x + mix, ln1_g[l], ln1_b[l])
        ffn = moe(x.reshape(B * S, D), w_router[l], b_router[l], w_mlp1[l], b_mlp1[l],
                  w_mlp2[l], b_mlp2[l]).reshape(B, S, D)
        x = layer_norm(DEEPNORM_ALPHA * x + ffn, ln2_g[l], ln2_b[l])
    return x
```

```python
import numpy as np
import concourse.bass as bass
import concourse.mybir as mybir

F32 = mybir.dt.float32
BF16 = mybir.dt.bfloat16
I32 = mybir.dt.int32
U32 = mybir.dt.uint32
AF = mybir.ActivationFunctionType
ALU = mybir.AluOpType
AX = mybir.AxisListType

ENGS = ("pe", "act", "dve", "pool", "sp")
KDMA = 8


class Tok:
    __slots__ = ("eng", "seq", "dma")

    def __init__(self, eng, seq, dma=None):
        self.eng = eng
        self.seq = seq
        self.dma = dma


class Buf:
    __slots__ = ("w", "r", "name")

    def __init__(self, name=""):
        self.w = None
        self.r = []
        self.name = name


class Prog:
    def __init__(self, nc):
        self.nc = nc
        self.ops = {e: [] for e in ENGS}
        self.ndma = {e: 0 for e in ENGS}
        self.all_dma = []
        self.sb_off = self.SB_BASE
        self.sb_hi = 0
        self.uid = 0

    SB_BASE = 16640
    SB_END = 229376

    def sb_reset(self, off=None):
        self.sb_off = self.SB_BASE if off is None else off

    def sb(self, shape, dtype, name=None):
        self.uid += 1
        nm = "%s_%d" % (name or "t", self.uid)
        nbytes = int(np.prod(shape[1:])) * mybir.dt.size(dtype)
        off = (self.sb_off + 63) // 64 * 64
        t = self.nc.alloc_sbuf_tensor_at(nm, list(shape), dtype, offset=off)
        self.sb_off = off + nbytes
        self.sb_hi = max(self.sb_hi, self.sb_off)
        assert self.sb_off <= self.SB_END, ("SBUF overflow", nm, self.sb_off)
        return t

    def _deps(self, reads, writes, extra):
        deps = {}
        for b in reads:
            if b.w is not None:
                deps[id(b.w)] = b.w
        for b in writes:
            if b.w is not None:
                deps[id(b.w)] = b.w
            for t in b.r:
                deps[id(t)] = t
        for t in extra:
            if t is not None:
                deps[id(t)] = t
        return list(deps.values())

    def _commit(self, tok, reads, writes):
        for b in writes:
            b.w = tok
            b.r = []
        for b in reads:
            if tok.dma is None:
                b.r = [t for t in b.r if not (t.dma is None and t.eng == tok.eng)]
            b.r.append(tok)

    def op(self, eng, fn, reads=(), writes=(), extra=()):
        deps = self._deps(reads, writes, extra)
        if eng == "pe":
            deps = [t for t in deps if not (t.eng == "pe" and t.dma is None)]
        tok = Tok(eng, len(self.ops[eng]))
        self.ops[eng].append(dict(fn=fn, waits=deps, tok=tok, dma=False))
        self._commit(tok, reads, writes)
        return tok

    def dma(self, eng, fn, reads=(), writes=(), extra=()):
        deps = self._deps(reads, writes, extra)
        i = self.ndma[eng]
        self.ndma[eng] += 1
        tok = Tok(eng, len(self.ops[eng]), dma=(i % KDMA, 16 * (i // KDMA + 1)))
        self.ops[eng].append(dict(fn=fn, waits=deps, tok=tok, dma=True, idx=i))
        self.all_dma.append(tok)
        self._commit(tok, reads, writes)
        return tok

    def barrier(self):
        lasts = []
        for e in ENGS:
            for o in reversed(self.ops[e]):
                if not o["dma"]:
                    lasts.append(o["tok"])
                    break
        dm = list(self.all_dma)
        self.all_dma = []
        for e in ENGS:
            if e == "sp":
                self.op(e, None, extra=lasts + dm)
            else:
                self.op(e, None, extra=lasts + dm)

    def emit(self):
        nc = self.nc
        needed = {e: set() for e in ENGS}
        for e in ENGS:
            for o in self.ops[e]:
                for t in o["waits"]:
                    if t.dma is None:
                        needed[t.eng].add(t.seq)
        tokval = {e: {} for e in ENGS}
        for e in ENGS:
            c = 0
            for o in self.ops[e]:
                if o["dma"]:
                    continue
                if o["tok"].seq in needed[e]:
                    c += 1
                    tokval[e][o["tok"].seq] = c
            self.maxcount = getattr(self, "maxcount", {})
            self.maxcount[e] = c
        engobj = {"pe": nc.tensor, "act": nc.scalar, "dve": nc.vector, "pool": nc.gpsimd, "sp": nc.sync}
        import contextlib
        with contextlib.ExitStack() as st:
            csem = {e: st.enter_context(nc.semaphore("c_" + e)) for e in ENGS}
            dsem = {e: [st.enter_context(nc.semaphore("d_%s%d" % (e, k))) for k in range(KDMA)]
                    for e in ENGS if self.ndma[e] > 0}
            block = st.enter_context(nc.Block())

            def run(e, engine):
                waited = {}
                for o in self.ops[e]:
                    for t in o["waits"]:
                        if t.dma is not None:
                            sem = dsem[t.eng][t.dma[0]]
                            val = t.dma[1]
                            key = ("d", t.eng, t.dma[0])
                        else:
                            sem = csem[t.eng]
                            val = tokval[t.eng][t.seq]
                            key = ("c", t.eng)
                        if waited.get(key, 0) >= val:
                            continue
                        waited[key] = val
                        engine.wait_ge(sem, val)
                    if o["dma"]:
                        i = o["idx"]
                        slot = i % KDMA
                        if i >= KDMA:
                            key = ("d", e, slot)
                            val = 16 * (i // KDMA)
                            if waited.get(key, 0) < val:
                                waited[key] = val
                                engine.wait_ge(dsem[e][slot], val)
                        ins = o["fn"](engine)
                        ins.then_inc(dsem[e][slot], 16)
                    else:
                        if o["fn"] is None:
                            if o["tok"].seq in needed[e]:
                                ins = engine.nop() if hasattr(engine, "nop") else None
                                ins.then_inc(csem[e], 1)
                            continue
                        ins = o["fn"](engine)
                        if o["tok"].seq in needed[e]:
                            ins.then_inc(csem[e], 1)

            @block.tensor
            def _(eng):
                run("pe", eng)

            @block.scalar
            def _(eng):
                run("act", eng)

            @block.vector
            def _(eng):
                run("dve", eng)

            @block.gpsimd
            def _(eng):
                run("pool", eng)

            @block.sync
            def _(eng):
                run("sp", eng)
D = 1024
SEQ = 8192
NOWN = 4096
NCORES = 8
CAP = 768
NEXP = 32
LAM_INIT = 0.2
ALPHA = 2.0 ** 0.25


class Ring:
    def __init__(self, items):
        self.items = list(items)
        self.i = 0

    def next(self):
        it = self.items[self.i % len(self.items)]
        self.i += 1
        return it


def phase1(P, nc, T):
    P.sb_reset()
    wfm = P.sb([128, 8, 3072], BF16, "wfm")
    wtm = P.sb([128, 8, 1024], BF16, "wtm")
    bfm = P.sb([128, 24], F32, "bfm")
    btm = P.sb([128, 1024], F32, "btm")
    b_w = Buf()
    wtoks = []
    for c in range(8):
        for g in range(6):
            wtoks.append(P.dma("pool", lambda e, c=c, g=g: e.dma_start(
                out=wfm[:, c, g * 512:(g + 1) * 512], in_=T["w_fm"][c * 128:(c + 1) * 128, g * 512:(g + 1) * 512])))
        for g in range(2):
            wtoks.append(P.dma("pool", lambda e, c=c, g=g: e.dma_start(
                out=wtm[:, c, g * 512:(g + 1) * 512], in_=T["w_tm"][c * 128:(c + 1) * 128, g * 512:(g + 1) * 512])))
    wtoks.append(P.dma("sp", lambda e: e.dma_start(out=bfm[:], in_=T["b_fm"])))
    wtoks.append(P.dma("sp", lambda e: e.dma_start(out=btm[:], in_=T["b_tm"])))
    for eng in ("pe", "act", "dve"):
        P.op(eng, None, extra=wtoks)

    xblk = Ring([(P.sb([128, 8, 512], BF16, "xblk"), Buf()) for _ in range(2)])
    csb = Ring([(P.sb([128, 512], F32, "cos"), P.sb([128, 512], F32, "sin"), Buf()) for _ in range(2)])
    t1r = Ring([(P.sb([128, 512], F32, "t1"), Buf()) for _ in range(2)])
    t2r = Ring([(P.sb([128, 512], F32, "t2"), Buf()) for _ in range(2)])
    stg = Ring([(P.sb([128, 512], BF16, "stg"), Buf()) for _ in range(6)])
    psr = Ring([(T["ps"][i], T["psb"][i]) for i in range(8)])
    xT3 = T["xT"].rearrange("(c p) t -> p c t", p=128)

    def do_block(i):
        own = i < 8
        xb, xbB = xblk.next()
        P.dma("pool", lambda e, xb=xb, i=i: e.dma_start(out=xb[:], in_=xT3[:, :, i * 512:(i + 1) * 512]), writes=[xbB])
        cb, sb_, csB = csb.next()
        P.dma("sp", lambda e, cb=cb, i=i: e.dma_start(out=cb[:], in_=T["cosT"][:, i * 512:(i + 1) * 512]), writes=[csB])
        P.dma("sp", lambda e, sb_=sb_, i=i: e.dma_start(out=sb_[:], in_=T["sinT"][:, i * 512:(i + 1) * 512]), writes=[csB])

        def mm_fm(ps, psB, col):
            for k in range(8):
                P.op("pe", lambda e, k=k: e.matmul(ps[:], wfm[:, k, col:col + 128], xb[:, k, :],
                                                   start=(k == 0), stop=(k == 7)),
                     reads=[xbB], writes=[psB])

        def rope_tile(col0, col1, dst):
            psA, psAB = psr.next()
            psC, psCB = psr.next()
            mm_fm(psA, psAB, col0)
            mm_fm(psC, psCB, col1)
            t1, t1B = t1r.next()
            t2, t2B = t2r.next()
            P.op("dve", lambda e: e.scalar_tensor_tensor(out=t1[:], in0=psA[:], scalar=bfm[:, col0 // 128:col0 // 128 + 1],
                                                          in1=cb[:], op0=ALU.add, op1=ALU.mult),
                 reads=[psAB, csB], writes=[t1B])
            P.op("dve", lambda e: e.scalar_tensor_tensor(out=t2[:], in0=psC[:], scalar=bfm[:, col1 // 128:col1 // 128 + 1],
                                                          in1=sb_[:], op0=ALU.add, op1=ALU.mult),
                 reads=[psCB, csB], writes=[t2B])
            st, stB = stg.next()
            P.op("pool", lambda e: e.tensor_tensor(out=st[:], in0=t1[:], in1=t2[:], op=ALU.add),
                 reads=[t1B, t2B], writes=[stB])
            P.dma("sp", lambda e: e.dma_start(out=dst, in_=st[:]), reads=[stB])

        def plain_tile(col, dst):
            ps, psB = psr.next()
            mm_fm(ps, psB, col)
            st, stB = stg.next()
            P.op("act", lambda e: e.activation(out=st[:], in_=ps[:], func=AF.Identity,
                                               bias=bfm[:, col // 128:col // 128 + 1], scale=1.0),
                 reads=[psB], writes=[stB])
            P.dma("sp", lambda e: e.dma_start(out=dst, in_=st[:]), reads=[stB])

        sl = slice(i * 512, (i + 1) * 512)
        for h in range(4):
            if own:
                rope_tile(h * 128, 512 + h * 128, T["qT_da"][h, :, sl])
            rope_tile(1024 + h * 128, 1536 + h * 128, T["kT_da"][h, :, sl])
        for j in range(4):
            if own:
                plain_tile(2048 + j * 128, T["qT_na"][j, :, sl])
            plain_tile(2560 + j * 128, T["kT_na"][j, :, sl])
        for tt in range(4):
            for g, dst in ((0, T["v_da"]), (1, T["v_na"])):
                ps, psB = psr.next()
                for k in range(8):
                    P.op("pe", lambda e, k=k, ps=ps, g=g, tt=tt: e.matmul(
                        ps[:], xb[:, k, tt * 128:(tt + 1) * 128], wtm[:, k, g * 512:(g + 1) * 512],
                        start=(k == 0), stop=(k == 7)), reads=[xbB], writes=[psB])
                st, stB = stg.next()
                P.op("dve", lambda e, ps=ps, st=st, g=g: e.tensor_tensor(out=st[:], in0=ps[:], in1=btm[:, g * 512:(g + 1) * 512],
                                                                         op=ALU.add), reads=[psB], writes=[stB])
                r0 = i * 512 + tt * 128
                P.dma("sp", lambda e, st=st, dst=dst, r0=r0: e.dma_start(out=dst[r0:r0 + 128, :], in_=st[:]), reads=[stB])

    for i in range(16):
        do_block(i)
    P.barrier()
def phase2(P, nc, T, a_sb, a_B):
    P.sb_reset(T["arena0"])
    lamv = P.sb([128, 256], F32, "lamv")
    gbc = P.sb([128, 128], F32, "gbc")
    tmp64 = P.sb([128, 128], F32, "tmp64")
    sc = P.sb([128, 8], F32, "sc")
    neglam = P.sb([128, 1], F32, "neglam")
    cB = Buf()
    P.dma("sp", lambda e: e.dma_start(out=lamv[:], in_=T["lamv"]), writes=[cB])
    P.dma("sp", lambda e: e.dma_start(out=gbc[:], in_=T["subln_bc"]), writes=[cB])
    P.op("dve", lambda e: e.tensor_tensor(out=tmp64[:, 0:64], in0=lamv[:, 0:64], in1=lamv[:, 64:128], op=ALU.mult), reads=[cB], writes=[cB])
    P.op("dve", lambda e: e.tensor_tensor(out=tmp64[:, 64:128], in0=lamv[:, 128:192], in1=lamv[:, 192:256], op=ALU.mult), reads=[cB], writes=[cB])
    P.op("dve", lambda e: e.reduce_sum(out=sc[:, 0:1], in_=tmp64[:, 0:64], axis=AX.X), reads=[cB], writes=[cB])
    P.op("dve", lambda e: e.reduce_sum(out=sc[:, 1:2], in_=tmp64[:, 64:128], axis=AX.X), reads=[cB], writes=[cB])
    P.op("act", lambda e: e.activation(out=sc[:, 2:4], in_=sc[:, 0:2], func=AF.Exp), reads=[cB], writes=[cB])
    P.op("dve", lambda e: e.tensor_tensor(out=sc[:, 4:5], in0=sc[:, 3:4], in1=sc[:, 2:3], op=ALU.subtract), reads=[cB], writes=[cB])
    P.op("dve", lambda e: e.tensor_scalar(out=neglam[:], in0=sc[:, 4:5], scalar1=-LAM_INIT, scalar2=None, op0=ALU.add), reads=[cB], writes=[cB])
    P.op("dve", lambda e: e.tensor_scalar(out=gbc[:], in0=gbc[:], scalar1=1.0 - LAM_INIT, scalar2=None, op0=ALU.mult), reads=[cB], writes=[cB])

    hb = []
    for _ in range(2):
        KT = P.sb([128, 8192], BF16, "KT")
        V = P.sb([128, 64, 129], BF16, "V")
        QT = P.sb([128, 2, 4096], BF16, "QT")
        oB = Buf()
        P.op("pool", lambda e, V=V: e.memset(V[:, :, 128:129], 1.0), writes=[oB])
        P.op("pool", lambda e, QT=QT: e.memset(QT[64:128, 0, :], 0.0), writes=[oB])
        P.op("pool", lambda e, QT=QT: e.memset(QT[0:64, 1, :], 0.0), writes=[oB])
        hb.append((KT, V, QT, Buf(), Buf(), Buf(), oB))
    ptr = Ring([(P.sb([128, 512], BF16, "pt"), Buf()) for _ in range(4)])
    sps = Ring([(T["ps"][i], T["psb"][i]) for i in range(4, 8)])
    accs = {}
    lay = [(0, 0), (0, 1), (0, 2), (1, 0), (2, 0), (2, 1), (2, 2), (3, 0)]
    n = 0
    for c in range(2):
        for qt in range(4):
            bk, pos = lay[n]
            n += 1
            accs[(c, qt)] = (T["ps"][bk][:, pos * 129:(pos + 1) * 129], T["psb"][bk])
    small = Ring([(P.sb([128, 8], F32, "sm"), P.sb([128, 128], F32, "tt"), P.sb([128, 128], F32, "oo"),
                   P.sb([128, 128], F32, "sq"), Buf()) for _ in range(3)])
    v_da3 = T["v_da"].rearrange("(t p) e -> p t e", p=128)

    def load_head(h):
        KT, V, QT, kB, vB, qB, oB = hb[h % 2]
        for s4 in range(4):
            P.dma("sp", lambda e, s4=s4: e.dma_start(out=KT[:, s4 * 2048:(s4 + 1) * 2048],
                                                      in_=T["kT_da"][h, :, s4 * 2048:(s4 + 1) * 2048]), writes=[kB])
        for s8 in range(8):
            P.dma("sp", lambda e, s8=s8: e.dma_start(out=V[:, s8 * 8:(s8 + 1) * 8, 0:128],
                                                      in_=v_da3[:, s8 * 8:(s8 + 1) * 8, h * 128:(h + 1) * 128]), writes=[vB])
        P.dma("sp", lambda e: e.dma_start(out=QT[0:64, 0, :], in_=T["qT_da"][h, 0:64, :]), writes=[qB])
        P.dma("sp", lambda e: e.dma_start(out=QT[64:128, 1, :], in_=T["qT_da"][h, 64:128, :]), writes=[qB])

    def epilogue(h, qb, qt):
        O1, O1B = accs[(0, qt)]
        O2, O2B = accs[(1, qt)]
        sm, tt, oo, sq, sB = small.next()
        P.op("dve", lambda e: e.reciprocal(out=sm[:, 0:1], in_=O1[:, 128:129]), reads=[O1B], writes=[sB])
        P.op("dve", lambda e: e.reciprocal(out=sm[:, 1:2], in_=O2[:, 128:129]), reads=[O2B], writes=[sB])
        P.op("dve", lambda e: e.tensor_tensor(out=sm[:, 2:3], in0=sm[:, 1:2], in1=neglam[:], op=ALU.mult), reads=[sB, cB], writes=[sB])
        P.op("dve", lambda e: e.tensor_scalar(out=tt[:], in0=O2[:, 0:128], scalar1=sm[:, 2:3], scalar2=None, op0=ALU.mult),
             reads=[O2B, sB], writes=[sB])
        P.op("dve", lambda e: e.scalar_tensor_tensor(out=oo[:], in0=O1[:, 0:128], scalar=sm[:, 0:1], in1=tt[:],
                                                      op0=ALU.mult, op1=ALU.add), reads=[O1B, sB], writes=[sB])
        P.op("act", lambda e: e.activation(out=sq[:], in_=oo[:], func=AF.Square, accum_out=sm[:, 3:4]), reads=[sB], writes=[sB])
        P.op("act", lambda e: e.activation(out=sm[:, 4:5], in_=sm[:, 3:4], func=AF.Ln, scale=1.0 / 128.0, bias=1e-5),
             reads=[sB], writes=[sB])
        P.op("act", lambda e: e.activation(out=sm[:, 5:6], in_=sm[:, 4:5], func=AF.Exp, scale=-0.5), reads=[sB], writes=[sB])
        P.op("dve", lambda e: e.scalar_tensor_tensor(out=a_sb[:, qb * 4 + qt, h * 128:(h + 1) * 128], in0=oo[:], scalar=sm[:, 5:6],
                                                      in1=gbc[:], op0=ALU.mult, op1=ALU.mult), reads=[sB, cB], writes=[a_B])

    def qk(h, qb, c, kt):
        KT, V, QT, kB, vB, qB, oB = hb[h % 2]
        S, SB = sps.next()
        P.op("pe", lambda e: e.matmul(S[:], KT[:, kt * 128:(kt + 1) * 128],
                                      QT[:, c, qb * 512:(qb + 1) * 512],
                                      start=True, stop=True), reads=[kB, qB, oB], writes=[SB])
        Pt, PtB = ptr.next()
        P.op("act", lambda e: e.activation(out=Pt[:], in_=S[:], func=AF.Exp, scale=0.125),
             reads=[SB], writes=[PtB])
        return Pt, PtB

    def pv(h, qb, c, kt, Pt, PtB):
        KT, V, QT, kB, vB, qB, oB = hb[h % 2]
        for qt in range(4):
            O, OB = accs[(c, qt)]
            P.op("pe", lambda e, O=O, qt=qt: e.matmul(O, Pt[:, qt * 128:(qt + 1) * 128], V[:, kt, :],
                                                      start=(kt == 0 and qt in (0, 3)), stop=(kt == 63), skip_group_check=True),
                 reads=[PtB, vB, oB], writes=[OB])
        if c == 1 and kt == 63:
            for qt in range(4):
                epilogue(h, qb, qt)

    iters = [(h, qb, c, kt) for h in range(T.get("da_heads", 4)) for qb in range(8) for c in range(2) for kt in range(64)]
    LA = 2
    pend = {}
    NH = T.get("da_heads", 4)
    load_head(0)
    if NH > 1:
        load_head(1)
    for n in range(len(iters) + LA):
        if n < len(iters):
            h, qb, c, kt = iters[n]
            pend[n] = qk(h, qb, c, kt)
        m = n - LA
        if m >= 0:
            h, qb, c, kt = iters[m]
            Pt, PtB = pend.pop(m)
            pv(h, qb, c, kt, Pt, PtB)
            if qb == 7 and c == 1 and kt == 63 and h + 2 < NH:
                load_head(h + 2)
    P.barrier()
def phase3(P, nc, T, nb_sb, nb_B):
    P.sb_reset(T["arena0"])
    Mt = P.sb([128, 18 * 256], BF16, "Mt")
    mB = Buf()
    for s in range(3):
        P.dma("pool", lambda e, s=s: e.dma_start(out=Mt[:, s * 1536:(s + 1) * 1536], in_=T["na_M"][:, s * 1536:(s + 1) * 1536]), writes=[mB])
    Rt = P.sb([128, 18 * 256], F32, "Rt")
    rB = Buf()
    hb = []
    for _ in range(2):
        KT = P.sb([64, 4608], BF16, "KTn")
        V = P.sb([128, 36, 65], BF16, "Vn")
        QT = P.sb([64, 4096], BF16, "QTn")
        E = P.sb([128, 18 * 256], BF16, "En")
        oB = Buf()
        P.op("pool", lambda e, V=V: e.memset(V[:, :, 64:65], 1.0), writes=[oB])
        hb.append((KT, V, QT, E, Buf(), Buf(), Buf(), Buf(), oB))
    per = Ring([(P.sb([128, 256], BF16, "pe_"), Buf()) for _ in range(4)])
    pmr = Ring([(P.sb([128, 256], BF16, "pm_"), Buf()) for _ in range(4)])
    sps = Ring([(T["ps"][i], T["psb"][i]) for i in range(2, 8)])
    accs = [(T["ps"][0][:, 0:65], T["psb"][0]), (T["ps"][0][:, 128:193], T["psb"][0]),
            (T["ps"][1][:, 0:65], T["psb"][1]), (T["ps"][1][:, 128:193], T["psb"][1])]
    smr = Ring([(P.sb([128, 2], F32, "smn"), Buf()) for _ in range(4)])
    v_na3 = T["v_na"].rearrange("(t p) e -> p t e", p=128)

    def load_head(hn):
        KT, V, QT, E, kB, vB, qB, eB, oB = hb[hn % 2]
        j, po = hn // 2, (hn % 2) * 64
        P.dma("sp", lambda e: e.dma_start(out=KT[:, 0:256], in_=T["kT_na"][j, po:po + 64, 7936:8192]), writes=[kB])
        P.dma("sp", lambda e: e.dma_start(out=KT[:, 256:4608], in_=T["kT_na"][j, po:po + 64, 0:4352]), writes=[kB])
        P.dma("sp", lambda e: e.dma_start(out=QT[:], in_=T["qT_na"][j, po:po + 64, :]), writes=[qB])
        P.dma("sp", lambda e: e.dma_start(out=V[:, 0:2, 0:64], in_=v_na3[:, 62:64, hn * 64:(hn + 1) * 64]), writes=[vB])
        for s in range(2):
            P.dma("sp", lambda e, s=s: e.dma_start(out=V[:, 2 + s * 17:2 + (s + 1) * 17, 0:64],
                                                    in_=v_na3[:, s * 17:(s + 1) * 17, hn * 64:(hn + 1) * 64]), writes=[vB])
        P.dma("sp", lambda e: e.dma_start(out=Rt[:], in_=T["na_R"][hn, :, :]), writes=[rB])
        for s in range(3):
            sl = slice(s * 1536, (s + 1) * 1536)
            P.op("act", lambda e, sl=sl: e.activation(out=Rt[:, sl], in_=Rt[:, sl], func=AF.Exp), reads=[rB], writes=[rB])
            P.op("dve", lambda e, sl=sl: e.tensor_tensor(out=E[:, sl], in0=Rt[:, sl], in1=Mt[:, sl], op=ALU.mult),
                 reads=[rB, mB], writes=[eB])

    def qk(hn, g, j):
        KT, V, QT, E, kB, vB, qB, eB, oB = hb[hn % 2]
        S, SB = sps.next()
        ti = 2 * g + j
        P.op("pe", lambda e: e.matmul(S[:, 0:256], KT[:, ti * 128:(ti + 1) * 128], QT[:, g * 256:(g + 1) * 256],
                                      start=True, stop=True), reads=[kB, qB], writes=[SB])
        Pe, PeB = per.next()
        P.op("act", lambda e: e.activation(out=Pe[:], in_=S[:, 0:256], func=AF.Exp, scale=0.125), reads=[SB], writes=[PeB])
        Pm, PmB = pmr.next()
        cls = 0 if g == 0 else (2 if g == 15 else 1)
        ei = (cls * 6 + j) * 256
        P.op("dve", lambda e: e.tensor_tensor(out=Pm[:], in0=Pe[:], in1=E[:, ei:ei + 256], op=ALU.mult),
             reads=[PeB, eB], writes=[PmB])
        return Pm, PmB

    def pv(hn, g, j, Pm, PmB):
        KT, V, QT, E, kB, vB, qB, eB, oB = hb[hn % 2]
        ti = 2 * g + j
        for qt in range(2):
            O, OB = accs[(g % 2) * 2 + qt]
            P.op("pe", lambda e, O=O, qt=qt: e.matmul(O, Pm[:, qt * 128:(qt + 1) * 128], V[:, ti, :],
                                                      start=(j == 0 and qt == 0), stop=(j == 5), skip_group_check=True), reads=[PmB, vB, oB], writes=[OB])
        if j == 5:
            for qt in range(2):
                O, OB = accs[(g % 2) * 2 + qt]
                sm, sB = smr.next()
                P.op("dve", lambda e, O=O, sm=sm: e.reciprocal(out=sm[:, 0:1], in_=O[:, 64:65]), reads=[OB], writes=[sB])
                P.op("dve", lambda e, O=O, sm=sm, qt=qt: e.tensor_scalar(
                    out=nb_sb[:, g * 2 + qt, hn * 64:(hn + 1) * 64], in0=O[:, 0:64], scalar1=sm[:, 0:1], scalar2=None,
                    op0=ALU.mult), reads=[OB, sB], writes=[nb_B])

    iters = [(hn, g, j) for hn in range(T.get("na_heads", 8)) for g in range(16) for j in range(6)]
    LA = 3
    pend = {}
    NH = T.get("na_heads", 8)
    load_head(0)
    if NH > 1:
        load_head(1)
    for n in range(len(iters) + LA):
        if n < len(iters):
            hn, g, j = iters[n]
            pend[n] = qk(hn, g, j)
        m = n - LA
        if m >= 0:
            hn, g, j = iters[m]
            Pm, PmB = pend.pop(m)
            pv(hn, g, j, Pm, PmB)
            if g == 15 and j == 5 and hn + 2 < NH:
                load_head(hn + 2)
    P.barrier()
def layer_norm_tile(P, y, yB, out, outB, gbc, bbc, cB, junk, jB, sm, sB):
    class _W:
        def __init__(self, t):
            self.t = t

        def __getitem__(self, k):
            return self.t if isinstance(self.t, bass.AP) else self.t[k]
    y = _W(y)
    out = _W(out)
    junk = _W(junk)
    P.op("act", lambda e: e.activation(out=junk[:], in_=y[:], func=AF.Identity, accum_out=sm[:, 0:1]), reads=[yB], writes=[jB, sB])
    P.op("act", lambda e: e.activation(out=junk[:], in_=y[:], func=AF.Square, accum_out=sm[:, 1:2]), reads=[yB], writes=[jB, sB])
    P.op("dve", lambda e: e.tensor_scalar(out=sm[:, 2:3], in0=sm[:, 0:1], scalar1=1.0 / 1024.0, scalar2=None, op0=ALU.mult), reads=[sB], writes=[sB])
    P.op("dve", lambda e: e.tensor_tensor(out=sm[:, 3:4], in0=sm[:, 2:3], in1=sm[:, 2:3], op=ALU.mult), reads=[sB], writes=[sB])
    P.op("dve", lambda e: e.scalar_tensor_tensor(out=sm[:, 4:5], in0=sm[:, 1:2], scalar=1.0 / 1024.0, in1=sm[:, 3:4],
                                                  op0=ALU.mult, op1=ALU.subtract), reads=[sB], writes=[sB])
    P.op("act", lambda e: e.activation(out=sm[:, 5:6], in_=sm[:, 4:5], func=AF.Ln, scale=1.0, bias=1e-5), reads=[sB], writes=[sB])
    P.op("act", lambda e: e.activation(out=sm[:, 6:7], in_=sm[:, 5:6], func=AF.Exp, scale=-0.5), reads=[sB], writes=[sB])
    P.op("dve", lambda e: e.tensor_scalar(out=out[:], in0=y[:], scalar1=sm[:, 2:3], scalar2=sm[:, 6:7], op0=ALU.subtract, op1=ALU.mult),
         reads=[yB, sB], writes=[outB])
    P.op("dve", lambda e: e.tensor_tensor(out=out[:], in0=out[:], in1=gbc[:], op=ALU.mult), reads=[outB, cB], writes=[outB])
    P.op("dve", lambda e: e.tensor_tensor(out=out[:], in0=out[:], in1=bbc[:], op=ALU.add), reads=[outB, cB], writes=[outB])


def phase4(P, nc, T, a_sb, a_B, nb_sb, nb_B, GT, GT_B, G_all, G_allB):
    P.sb_reset(T["arena0"])
    wg = P.sb([128, 8, 2048], BF16, "wg")
    wbd = P.sb([128, 4, 1024], BF16, "wbd")
    wbn = P.sb([128, 4, 1024], BF16, "wbn")
    wo = P.sb([128, 8, 1024], BF16, "wo")
    wr = P.sb([128, 8, 32], F32, "wr")
    bg = P.sb([128, 16], F32, "bg")
    g1bc = P.sb([128, 1024], F32, "g1bc")
    b1bc = P.sb([128, 1024], F32, "b1bc")
    brbc = P.sb([128, 32], F32, "brbc")
    identb = P.sb([128, 128], BF16, "identb")
    identf = P.sb([128, 128], F32, "identf")
    cB = Buf()
    toks = []
    for c in range(8):
        for g in range(4):
            toks.append(P.dma("pool", lambda e, c=c, g=g: e.dma_start(out=wg[:, c, g * 512:(g + 1) * 512],
                                                                      in_=T["w_gate"][c * 128:(c + 1) * 128, g * 512:(g + 1) * 512])))
        toks.append(P.dma("pool", lambda e, c=c: e.dma_start(out=wo[:, c, :], in_=T["w_out"][c * 128:(c + 1) * 128, :])))
        toks.append(P.dma("sp", lambda e, c=c: e.dma_start(out=wr[:, c, :], in_=T["w_router"][c * 128:(c + 1) * 128, :])))
    for c in range(4):
        toks.append(P.dma("pool", lambda e, c=c: e.dma_start(out=wbd[:, c, :], in_=T["w_bda"][c * 128:(c + 1) * 128, :])))
        toks.append(P.dma("pool", lambda e, c=c: e.dma_start(out=wbn[:, c, :], in_=T["w_bna"][c * 128:(c + 1) * 128, :])))
    for dst, src in ((bg, "b_gate"), (g1bc, "ln1_g_bc"), (b1bc, "ln1_b_bc"), (brbc, "b_router_bc"), (identf, "ident")):
        toks.append(P.dma("sp", lambda e, dst=dst, src=src: e.dma_start(out=dst[:], in_=T[src])))
    toks.append(P.dma("pool", lambda e: e.dma_start(out=identb[:], in_=T["ident"])))
    for eng in ("pe", "act", "dve", "pool"):
        P.op(eng, None, extra=toks)

    xblk = Ring([(P.sb([128, 8, 512], BF16, "xblk4"), Buf()) for _ in range(1)])
    aT = P.sb([128, 4, 512], BF16, "aT"); aTB = Buf()
    nT = P.sb([128, 4, 512], BF16, "nT"); nTB = Buf()
    g_r = Ring([(P.sb([128, 2, 512], BF16, "g01"), Buf()) for _ in range(2)])
    mT = P.sb([128, 8, 512], BF16, "mT"); mTB = Buf()
    tr = Ring([(P.sb([128, 512], F32, "t4"), P.sb([128, 512], F32, "u4"), Buf()) for _ in range(1)])
    xt_r = Ring([(P.sb([128, 1024], F32, "xt"), Buf()) for _ in range(1)])
    y_r = Ring([(P.sb([128, 1024], F32, "y4"), Buf()) for _ in range(1)])
    x1_r = Ring([(P.sb([128, 1024], F32, "x1"), Buf()) for _ in range(2)])
    junk = P.sb([128, 1024], BF16, "junk"); jB = Buf()
    sm_r = Ring([(P.sb([128, 8], F32, "sm4"), Buf()) for _ in range(2)])
    x1Tf_r = Ring([(P.sb([128, 1024], F32, "x1Tf"), P.sb([128, 1024], BF16, "x1Tb"), Buf()) for _ in range(1)])
    rt_r = Ring([(P.sb([128, 32], F32, "lg"), P.sb([128, 8], F32, "t8"), P.sb([128, 32], F32, "mk"), P.sb([128, 32], F32, "ex"),
                  P.sb([128, 4], F32, "rs"), P.sb([128, 32], F32, "G"), Buf()) for _ in range(2)])
    psr = Ring([(T["ps"][i], T["psb"][i]) for i in range(8)])
    xT3 = T["xT"].rearrange("(c p) t -> p c t", p=128)
    x1T_d3 = T["x1T_d"].rearrange("(c p) t -> p c t", p=128)

    def do_block(tb):
        xb, xbB = xblk.next()
        P.dma("pool", lambda e: e.dma_start(out=xb[:], in_=xT3[:, :, tb * 512:(tb + 1) * 512]), writes=[xbB])
        for src, sB_, dst, dB in ((a_sb, a_B, aT, aTB), (nb_sb, nb_B, nT, nTB)):
            for hc in range(4):
                ps, psB = psr.next()
                psb16 = ps.bitcast(BF16)
                for tt in range(4):
                    P.op("pe", lambda e, tt=tt, psb16=psb16, src=src, hc=hc: e.transpose(
                        psb16[:, tt * 128:(tt + 1) * 128], src[:, tb * 4 + tt, hc * 128:(hc + 1) * 128], identb[:]),
                        reads=[sB_], writes=[psB])
                P.op("act", lambda e, psb16=psb16, dst=dst, hc=hc: e.activation(out=dst[:, hc, :], in_=psb16[:, 0:512], func=AF.Identity),
                     reads=[psB], writes=[dB])
        for dt in range(8):
            g01, g01B = g_r.next()
            for gi, j in enumerate((dt, 8 + dt)):
                ps, psB = psr.next()
                for k in range(8):
                    P.op("pe", lambda e, k=k, ps=ps, j=j: e.matmul(ps[:], wg[:, k, j * 128:(j + 1) * 128], xb[:, k, :],
                                                                   start=(k == 0), stop=(k == 7)), reads=[xbB], writes=[psB])
                P.op("act", lambda e, ps=ps, j=j, gi=gi, g01=g01: e.activation(out=g01[:, gi, :], in_=ps[:], func=AF.Sigmoid,
                                                                              bias=bg[:, j:j + 1], scale=1.0),
                     reads=[psB], writes=[g01B])
            psa, psaB = psr.next()
            psn, psnB = psr.next()
            for ec in range(4):
                P.op("pe", lambda e, ec=ec, psa=psa, dt=dt: e.matmul(psa[:], wbd[:, ec, dt * 128:(dt + 1) * 128], aT[:, ec, :],
                                                                    start=(ec == 0), stop=(ec == 3)), reads=[aTB], writes=[psaB])
            for ec in range(4):
                P.op("pe", lambda e, ec=ec, psn=psn, dt=dt: e.matmul(psn[:], wbn[:, ec, dt * 128:(dt + 1) * 128], nT[:, ec, :],
                                                                    start=(ec == 0), stop=(ec == 3)), reads=[nTB], writes=[psnB])
            t4, u4, tB = tr.next()
            P.op("dve", lambda e, t4=t4, psa=psa, g01=g01: e.tensor_tensor(out=t4[:], in0=psa[:], in1=g01[:, 0, :], op=ALU.mult),
                 reads=[psaB, g01B], writes=[tB])
            P.op("dve", lambda e, u4=u4, psn=psn, g01=g01: e.tensor_tensor(out=u4[:], in0=psn[:], in1=g01[:, 1, :], op=ALU.mult),
                 reads=[psnB, g01B], writes=[tB])
            P.op("pool", lambda e, t4=t4, u4=u4, dt=dt: e.tensor_tensor(out=mT[:, dt, :], in0=t4[:], in1=u4[:], op=ALU.add),
                 reads=[tB], writes=[mTB])
        for tt in range(4):
            tok0 = tb * 512 + tt * 128
            xt, xtB = xt_r.next()
            P.dma("sp", lambda e, xt=xt, tok0=tok0: e.dma_start(out=xt[:], in_=T["x_own"][tok0:tok0 + 128, :]), writes=[xtB])
            y, yB = y_r.next()
            for half in range(2):
                ps, psB = psr.next()
                for k in range(8):
                    P.op("pe", lambda e, k=k, ps=ps, half=half, tt=tt: e.matmul(
                        ps[:], mT[:, k, tt * 128:(tt + 1) * 128], wo[:, k, half * 512:(half + 1) * 512],
                        start=(k == 0), stop=(k == 7)), reads=[mTB], writes=[psB])
                P.op("dve", lambda e, ps=ps, half=half, y=y, xt=xt: e.scalar_tensor_tensor(
                    out=y[:, half * 512:(half + 1) * 512], in0=xt[:, half * 512:(half + 1) * 512], scalar=ALPHA, in1=ps[:],
                    op0=ALU.mult, op1=ALU.add), reads=[psB, xtB], writes=[yB])
            x1, x1B = x1_r.next()
            sm, sB = sm_r.next()
            layer_norm_tile(P, y, yB, x1, x1B, g1bc, b1bc, cB, junk, jB, sm, sB)
            P.dma("sp", lambda e, x1=x1, tok0=tok0: e.dma_start(out=T["x1_d"][tok0:tok0 + 128, :], in_=x1[:]), reads=[x1B])
            x1Tf, x1Tb, xTB = x1Tf_r.next()
            for k2 in range(2):
                ps, psB = psr.next()
                for k4 in range(4):
                    k = k2 * 4 + k4
                    P.op("pe", lambda e, ps=ps, k=k, k4=k4, x1=x1: e.transpose(ps[:, k4 * 128:(k4 + 1) * 128], x1[:, k * 128:(k + 1) * 128],
                                                                        identf[:]), reads=[x1B], writes=[psB])
                P.op("act", lambda e, ps=ps, x1Tf=x1Tf, k2=k2: e.activation(out=x1Tf[:, k2 * 512:(k2 + 1) * 512], in_=ps[:], func=AF.Identity),
                     reads=[psB], writes=[xTB])
                P.op("dve", lambda e, ps=ps, x1Tb=x1Tb, k2=k2: e.tensor_copy(out=x1Tb[:, k2 * 512:(k2 + 1) * 512], in_=ps[:]),
                     reads=[psB], writes=[xTB])
            P.dma("sp", lambda e, x1Tb=x1Tb, tok0=tok0: e.dma_start(out=x1T_d3[:, :, tok0:tok0 + 128], in_=x1Tb[:].rearrange("p (c t) -> p c t", c=8)), reads=[xTB])
            ps, psB = psr.next()
            for k in range(8):
                P.op("pe", lambda e, ps=ps, k=k, x1Tf=x1Tf: e.matmul(ps[:, 0:32], x1Tf[:, k * 128:(k + 1) * 128], wr[:, k, :], start=(k == 0), stop=(k == 7)),
                     reads=[xTB], writes=[psB])
            lg, t8, mk, ex, rs, G, rB = rt_r.next()
            P.op("dve", lambda e, ps=ps, lg=lg: e.tensor_tensor(out=lg[:], in0=ps[:, 0:32], in1=brbc[:], op=ALU.add), reads=[psB], writes=[rB])
            P.op("dve", lambda e, lg=lg, t8=t8: e.max(out=t8[:], in_=lg[:]), reads=[rB], writes=[rB])
            P.op("dve", lambda e, lg=lg, t8=t8, mk=mk: e.tensor_scalar(out=mk[:], in0=lg[:], scalar1=t8[:, 3:4], scalar2=None, op0=ALU.is_ge),
                 reads=[rB], writes=[rB])
            P.op("dve", lambda e, t8=t8, rs=rs: e.tensor_scalar(out=rs[:, 0:1], in0=t8[:, 0:1], scalar1=-1.0, scalar2=None, op0=ALU.mult),
                 reads=[rB], writes=[rB])
            P.op("act", lambda e, lg=lg, ex=ex, rs=rs: e.activation(out=ex[:], in_=lg[:], func=AF.Exp, bias=rs[:, 0:1], scale=1.0),
                 reads=[rB], writes=[rB])
            P.op("dve", lambda e, ex=ex, mk=mk: e.tensor_tensor(out=ex[:], in0=ex[:], in1=mk[:], op=ALU.mult), reads=[rB], writes=[rB])
            P.op("dve", lambda e, ex=ex, rs=rs: e.reduce_sum(out=rs[:, 1:2], in_=ex[:], axis=AX.X), reads=[rB], writes=[rB])
            P.op("dve", lambda e, rs=rs: e.reciprocal(out=rs[:, 2:3], in_=rs[:, 1:2]), reads=[rB], writes=[rB])
            G = G_all[:, tb * 4 + tt, :]
            P.op("dve", lambda e, ex=ex, rs=rs, G=G: e.tensor_scalar(out=G, in0=ex[:], scalar1=rs[:, 2:3], scalar2=None, op0=ALU.mult),
                 reads=[rB], writes=[rB, G_allB])
            ps2, ps2B = psr.next()
            P.op("pe", lambda e, ps2=ps2, G=G: e.transpose(ps2[0:32, 0:128], G, identf[:]), reads=[rB, G_allB], writes=[ps2B])
            P.op("act", lambda e, ps2=ps2, tok0=tok0: e.activation(out=GT[:, tok0:tok0 + 128], in_=ps2[0:32, 0:128], func=AF.Identity),
                 reads=[ps2B], writes=[GT_B])
            if T.get("dbg_G") is not None:
                P.dma("sp", lambda e, G=G, tok0=tok0: e.dma_start(out=T["dbg_G"][tok0:tok0 + 128, :], in_=G), reads=[rB, G_allB])

    for tb in range(8):
        do_block(tb)
    P.barrier()
def phase5(P, nc, T, GT, GT_B, G_all, G_allB):
    P.sb_reset(T["arena5"])
    NQ = 4
    QT_ = 1024
    acc = P.sb([128, 8, 1024], F32, "acc")
    accB = [Buf() for _ in range(8)]
    x1T = P.sb([128, 8, QT_], BF16, "x1T5"); x1TB = Buf()
    b1a = P.sb([128, 32, 16], F32, "b1a")
    b2b = P.sb([32, 1024], BF16, "b2b")
    g2bc = P.sb([128, 1024], F32, "g2bc")
    b2bc = P.sb([128, 1024], F32, "b2bc")
    cB = Buf()
    toks = []
    toks.append(P.dma("sp", lambda e: e.dma_start(out=b1a[:], in_=T["b_mlp1"])))
    toks.append(P.dma("pool", lambda e: e.dma_start(out=b2b[:], in_=T["b_mlp2"])))
    toks.append(P.dma("sp", lambda e: e.dma_start(out=g2bc[:], in_=T["ln2_g_bc"])))
    toks.append(P.dma("sp", lambda e: e.dma_start(out=b2bc[:], in_=T["ln2_b_bc"])))
    for eng in ("pe", "act", "dve", "pool"):
        P.op(eng, None, extra=toks)
    wr_ = Ring([(P.sb([128, 8, 1024], BF16, "w1g"), P.sb([128, 8, 1024], BF16, "w1l"), P.sb([128, 8, 1024], BF16, "w2"),
                 Buf(), Buf(), Buf()) for _ in range(2)])
    actT_r = Ring([(P.sb([128, 8, 512], BF16, "actT"), Buf()) for _ in range(2)])
    tmp_r = Ring([(P.sb([128, 512], F32, "g5"), P.sb([128, 512], F32, "s5"), P.sb([128, 512], F32, "l5"), Buf()) for _ in range(2)])
    psr = Ring([(T["ps"][i], T["psb"][i]) for i in range(8)])
    x1T_d3 = T["x1T_d"].rearrange("(c p) t -> p c t", p=128)
    fin_x = P.sb([128, 1024], F32, "finx"); fxB = Buf()
    fin_y = P.sb([128, 1024], F32, "finy"); fyB = Buf()
    junk = fin_x
    sm_r = Ring([(P.sb([128, 8], F32, "sm5"), Buf()) for _ in range(2)])

    def load_w(e_):
        w1g, w1l, w2, gB, lB, wB = wr_.next()
        for c in range(8):
            P.dma("pool", lambda e, c=c: e.dma_start(out=w1g[:, c, :], in_=T["w1g"][e_, c * 128:(c + 1) * 128, :]), writes=[gB])
            P.dma("pool", lambda e, c=c: e.dma_start(out=w1l[:, c, :], in_=T["w1l"][e_, c * 128:(c + 1) * 128, :]), writes=[lB])
        for c in range(8):
            P.dma("pool", lambda e, c=c: e.dma_start(out=w2[:, c, :], in_=T["w2"][e_, c * 128:(c + 1) * 128, :]), writes=[wB])
        return w1g, w1l, w2, gB, lB, wB

    def expert(q, e_, W):
        w1g, w1l, w2, gB, lB, wB = W
        for tb in range(2):
            expert_tb(q, e_, W, tb)

    def expert_tb(q, e_, W, tb):
        w1g, w1l, w2, gB, lB, wB = W
        if True:
            tsl = slice(tb * 512, (tb + 1) * 512)
            tok0 = q * QT_ + tb * 512
            actT, aB = actT_r.next()
            for f in range(8):
                pg, pgB = psr.next()
                pl, plB = psr.next()
                for k in range(8):
                    P.op("pe", lambda e, k=k, pg=pg, f=f: e.matmul(pg[:], w1g[:, k, f * 128:(f + 1) * 128], x1T[:, k, tsl],
                                                                   start=(k == 0), stop=(k == 7)), reads=[gB, x1TB], writes=[pgB])
                for k in range(8):
                    P.op("pe", lambda e, k=k, pl=pl, f=f: e.matmul(pl[:], w1l[:, k, f * 128:(f + 1) * 128], x1T[:, k, tsl],
                                                                   start=(k == 0), stop=(k == 7)), reads=[lB, x1TB], writes=[plB])
                g5, s5, l5, tB = tmp_r.next()
                P.op("act", lambda e, l5=l5, pl=pl, f=f: e.activation(out=l5[:], in_=pl[:], func=AF.Identity,
                                                                      bias=b1a[:, e_, 8 + f:9 + f], scale=1.0), reads=[plB], writes=[tB])
                P.op("dve", lambda e, g5=g5, pg=pg, f=f: e.tensor_scalar(out=g5[:], in0=pg[:], scalar1=b1a[:, e_, f:f + 1], scalar2=7.0,
                                                                         op0=ALU.add, op1=ALU.min), reads=[pgB], writes=[tB])
                P.op("act", lambda e, g5=g5, s5=s5: e.activation(out=s5[:], in_=g5[:], func=AF.Sigmoid, scale=1.702), reads=[tB], writes=[tB])
                P.op("dve", lambda e, l5=l5: e.tensor_scalar(out=l5[:], in0=l5[:], scalar1=-7.0, scalar2=7.0, op0=ALU.max, op1=ALU.min),
                     reads=[tB], writes=[tB])
                P.op("dve", lambda e, g5=g5, s5=s5: e.tensor_tensor(out=g5[:], in0=g5[:], in1=s5[:], op=ALU.mult), reads=[tB], writes=[tB])
                P.op("dve", lambda e, g5=g5, l5=l5, actT=actT, f=f: e.scalar_tensor_tensor(out=actT[:, f, :], in0=l5[:], scalar=1.0, in1=g5[:],
                                                                                          op0=ALU.add, op1=ALU.mult),
                     reads=[tB], writes=[aB])
            for tt in range(4):
                ti = tb * 4 + tt
                for half in range(2):
                    ps, psB = psr.next()
                    for k in range(8):
                        P.op("pe", lambda e, k=k, ps=ps, half=half, tt=tt, actT=actT: e.matmul(
                            ps[:], actT[:, k, tt * 128:(tt + 1) * 128], w2[:, k, half * 512:(half + 1) * 512],
                            start=(k == 0), stop=(k == 7)), reads=[aB, wB], writes=[psB])
                    dst = acc[:, ti, half * 512:(half + 1) * 512]
                    gcol = G_all[:, q * 8 + ti, e_:e_ + 1]
                    if e_ == 0:
                        P.op("dve", lambda e, ps=ps, dst=dst, gcol=gcol: e.tensor_scalar(out=dst, in0=ps[:], scalar1=gcol, scalar2=None,
                                                                                          op0=ALU.mult), reads=[psB, G_allB], writes=[accB[ti]])
                    else:
                        P.op("dve", lambda e, ps=ps, dst=dst, gcol=gcol: e.scalar_tensor_tensor(out=dst, in0=ps[:], scalar=gcol, in1=dst,
                                                                                                 op0=ALU.mult, op1=ALU.add),
                             reads=[psB, accB[ti], G_allB], writes=[accB[ti]])

    def finalize(q):
        for ti in range(8):
            fin_tile(q, ti)

    def fin_tile(q, ti):
        if True:
            tok0 = q * QT_ + ti * 128
            for half in range(2):
                ps, psB = psr.next()
                P.op("pe", lambda e, ps=ps, half=half: e.matmul(ps[:], GT[:, tok0:tok0 + 128], b2b[:, half * 512:(half + 1) * 512],
                                                                start=True, stop=True), reads=[GT_B], writes=[psB])
                dst = acc[:, ti, half * 512:(half + 1) * 512]
                P.op("dve", lambda e, ps=ps, dst=dst: e.tensor_tensor(out=dst, in0=dst, in1=ps[:], op=ALU.add),
                     reads=[psB, accB[ti]], writes=[accB[ti]])
            P.dma("sp", lambda e: e.dma_start(out=fin_x[:], in_=T["x1_d"][tok0:tok0 + 128, :]), writes=[fxB])
            P.op("dve", lambda e, ti=ti: e.scalar_tensor_tensor(out=acc[:, ti, :], in0=fin_x[:], scalar=ALPHA, in1=acc[:, ti, :],
                                                                 op0=ALU.mult, op1=ALU.add), reads=[fxB, accB[ti]], writes=[accB[ti]])
            sm, sB = sm_r.next()
            layer_norm_tile(P, acc[:, ti, :], accB[ti], fin_y, fyB, g2bc, b2bc, cB, junk, fxB, sm, sB)
            P.dma("sp", lambda e: e.dma_start(out=T["out"][tok0:tok0 + 128, :], in_=fin_y[:]), reads=[fyB])

    NE = T.get("n_exp", 32)
    seq = [(q, e_) for q in range(NQ) for e_ in range(NE)]
    W = load_w(seq[0][1])
    for i, (q, e_) in enumerate(seq):
        if e_ == 0:
            P.dma("sp", lambda e, q=q: e.dma_start(out=x1T[:], in_=x1T_d3[:, :, q * QT_:(q + 1) * QT_]), writes=[x1TB])
        Wn = load_w(seq[i + 1][1]) if i + 1 < len(seq) else None
        expert(q, e_, W)
        W = Wn
        if e_ == NE - 1:
            finalize(q)
    P.barrier()
def build(upto=99, debug=False):
    nc = bass.Bass("TRN2", target_bir_lowering=False)
    T = {}

    def inp(name, shape, dt=F32):
        T[name] = nc.dram_tensor(name, list(shape), dt, kind="ExternalInput").ap()

    def scr(name, shape, dt=BF16):
        T[name] = nc.dram_tensor(name, list(shape), dt, kind=("ExternalOutput" if (debug and debug.get("dump_scr")) else "Internal")).ap()

    def outp(name, shape, dt=F32):
        T[name] = nc.dram_tensor(name, list(shape), dt, kind="ExternalOutput").ap()

    inp("xT", [1024, 8192]); inp("x_own", [4096, 1024])
    inp("w_fm", [1024, 3072]); inp("w_tm", [1024, 1024]); inp("b_fm", [128, 24]); inp("b_tm", [128, 1024])
    inp("cosT", [128, 8192]); inp("sinT", [128, 8192]); inp("lamv", [128, 256]); inp("subln_bc", [128, 128])
    scr("qT_da", [4, 128, 4096]); scr("kT_da", [4, 128, 8192]); scr("qT_na", [4, 128, 4096]); scr("kT_na", [4, 128, 8192])
    scr("v_da", [8192, 512]); scr("v_na", [8192, 512])
    if upto >= 3:
        inp("na_R", [8, 128, 18 * 256]); inp("na_M", [128, 18 * 256])
    if upto >= 4:
        inp("w_gate", [1024, 2048]); inp("b_gate", [128, 16]); inp("w_bda", [512, 1024]); inp("w_bna", [512, 1024])
        inp("w_out", [1024, 1024]); inp("w_router", [1024, 32]); inp("ln1_g_bc", [128, 1024]); inp("ln1_b_bc", [128, 1024])
        inp("b_router_bc", [128, 32]); inp("ident", [128, 128])
        scr("x1_d", [4096, 1024], F32); scr("x1T_d", [1024, 4096], BF16)
        if debug and debug.get("dump_scr"):
            outp("dbg_G", [4096, 32])
    if upto >= 5:
        inp("b_mlp1", [128, 32, 16]); inp("b_mlp2", [32, 1024])
        inp("ln2_g_bc", [128, 1024]); inp("ln2_b_bc", [128, 1024])
        inp("w1g", [32, 1024, 1024]); inp("w1l", [32, 1024, 1024]); inp("w2", [32, 1024, 1024])
        outp("out", [4096, 1024])
    T["ps"] = [nc.alloc_psum_tensor("ps%d" % i, [128, 512], F32) for i in range(8)]
    T["psb"] = [Buf() for _ in range(8)]
    P = Prog(nc)
    GT = P.sb([32, 4096], BF16, "GT")
    GT_B = Buf()
    G_all = P.sb([128, 32, 32], F32, "G_all")
    G_allB = Buf()
    T["arena5"] = P.sb_off
    a_sb = P.sb([128, 32, 512], BF16, "a_sb")
    nb_sb = P.sb([128, 32, 512], BF16, "nb_sb")
    a_B, nb_B = Buf(), Buf()
    T["arena0"] = P.sb_off
    if debug:
        T["da_heads"] = debug.get("da_heads", 4)
        T["n_exp"] = debug.get("n_exp", 32)
    phase1(P, nc, T)
    if upto >= 2:
        phase2(P, nc, T, a_sb, a_B)
    if upto >= 3:
        phase3(P, nc, T, nb_sb, nb_B)
    if upto >= 4:
        phase4(P, nc, T, a_sb, a_B, nb_sb, nb_B, GT, GT_B, G_all, G_allB)
    if upto >= 5:
        phase5(P, nc, T, GT, GT_B, G_all, G_allB)
    if upto < 4:
        outp("dbg_a", [4096, 512], BF16); outp("dbg_nb", [4096, 512], BF16)
        P.dma("sp", lambda e: e.dma_start(out=T["dbg_a"].rearrange("(t p) e -> p t e", p=128), in_=a_sb[:]), reads=[a_B])
        P.dma("sp", lambda e: e.dma_start(out=T["dbg_nb"].rearrange("(t p) e -> p t e", p=128), in_=nb_sb[:]), reads=[nb_B])
        if debug and debug.get("dump_scr"):
            for nm in ("qT_da", "kT_da", "v_da", "qT_na", "kT_na", "v_na"):
                pass
    P.barrier()
    P.emit()
    return nc, P


def rope_tables(pos):
    inv = (10000.0 ** (-np.arange(0, 64, 2, dtype=np.float32) / np.float32(64))).astype(np.float32)
    ang = pos.astype(np.float32)[:, None] * inv[None, :]
    ang = np.concatenate([ang, ang], axis=-1)
    cos = np.cos(ang).astype(np.float32)
    sin = np.sin(ang).astype(np.float32)
    sgn = np.concatenate([-np.ones(32, np.float32), np.ones(32, np.float32)])
    sin_s = sin * sgn[None, :]
    cosT = np.ascontiguousarray(np.concatenate([cos, cos], axis=1).T)
    sinT = np.ascontiguousarray(np.concatenate([sin_s, sin_s], axis=1).T)
    return cosT, sinT


def na_tables(rpb, h):
    R = np.zeros((8, 3, 6, 2, 64, 4, 64), np.float32)
    M = np.zeros((3, 6, 2, 64, 4, 64), np.float32)
    cc = np.arange(64)
    cs = np.clip(cc - 8, 0, 48)
    colvalid = (cc[:, None] >= cs[None, :]) & (cc[:, None] <= cs[None, :] + 15)
    coloff = np.clip(cc[:, None] - cc[None, :] + 15, 0, 30)
    for cls, g in ((0, 0), (1, 1 if h == 0 else 14), (2, 15)):
        for j in range(6):
            for a in range(2):
                for i in range(4):
                    r = 64 * h + 4 * g + i
                    kr = 64 * h + 4 * g + 2 * j - 4 + a
                    rs = min(max(r - 4, 0), 120)
                    if kr < rs or kr > rs + 7:
                        continue
                    M[cls, j, a, :, i, :] = colvalid
                    R[:, cls, j, a, :, i, :] = rpb[:, kr - r + 7][:, coloff] * colvalid[None]
    M2 = np.ascontiguousarray(M.transpose(2, 3, 0, 1, 4, 5).reshape(128, 18 * 256))
    R2 = np.ascontiguousarray(R.transpose(0, 3, 4, 1, 2, 5, 6).reshape(8, 128, 18 * 256))
    return R2, M2


def host_prep(inputs, upto=99):
    x = np.asarray(inputs["x"], np.float32)
    w_in = np.asarray(inputs["w_in"], np.float32)[0]
    b_in = np.asarray(inputs["b_in"], np.float32)[0]
    d = np.arange(64)
    swap = np.concatenate([(hh * 128 + c * 64 + (d + 32) % 64) for hh in range(4) for c in range(2)])
    qda, kda, vda = np.arange(0, 512), np.arange(512, 1024), np.arange(1024, 1536)
    qna, kna, vna = np.arange(1536, 2048), np.arange(2048, 2560), np.arange(2560, 3072)
    fm_cols = np.concatenate([qda, qda[swap], kda, kda[swap], qna, kna])
    tm_cols = np.concatenate([vda, vna])
    w_fm = np.ascontiguousarray(w_in[:, fm_cols])
    w_tm = np.ascontiguousarray(w_in[:, tm_cols])
    b_fm = np.ascontiguousarray(b_in[fm_cols].reshape(24, 128).T)
    b_tm = np.ascontiguousarray(np.broadcast_to(b_in[tm_cols][None, :], (128, 1024)))
    lamv = np.concatenate([np.asarray(inputs[k], np.float32)[0] for k in ("lambda_q1", "lambda_k1", "lambda_q2", "lambda_k2")])
    lamv = np.ascontiguousarray(np.broadcast_to(lamv[None, :], (128, 256)))
    subln_bc = np.ascontiguousarray(np.broadcast_to(np.asarray(inputs["subln_g"], np.float32)[0][None, :], (128, 128)))
    rpb = np.asarray(inputs["rpb"], np.float32)[0]
    shared = dict(w_fm=w_fm, w_tm=w_tm, b_fm=b_fm, b_tm=b_tm, lamv=lamv, subln_bc=subln_bc)
    f32 = lambda k: np.asarray(inputs[k], np.float32)[0]
    bc = lambda v, n=128: np.ascontiguousarray(np.broadcast_to(v[None, :], (n, v.shape[0])))
    if upto >= 4:
        shared.update(w_gate=np.ascontiguousarray(w_in[:, 3072:5120]), b_gate=np.ascontiguousarray(b_in[3072:5120].reshape(16, 128).T),
                      w_bda=f32("w_branch_da"), w_bna=f32("w_branch_na"), w_out=f32("w_out"), w_router=f32("w_router"),
                      ln1_g_bc=bc(f32("ln1_g")), ln1_b_bc=bc(f32("ln1_b")), b_router_bc=bc(f32("b_router")),
                      ident=np.eye(128, dtype=np.float32))
    if upto >= 5:
        w1 = f32("w_mlp1"); b1 = f32("b_mlp1")
        b1g = b1[:, 0::2].reshape(32, 8, 128).transpose(2, 0, 1)
        b1l = b1[:, 1::2].reshape(32, 8, 128).transpose(2, 0, 1)
        shared.update(b_mlp1=np.ascontiguousarray(np.concatenate([b1g, b1l], axis=2)),
                      b_mlp2=f32("b_mlp2"), ln2_g_bc=bc(f32("ln2_g")), ln2_b_bc=bc(f32("ln2_b")),
                      w1g=np.ascontiguousarray(w1[:, :, 0::2]), w1l=np.ascontiguousarray(w1[:, :, 1::2]), w2=f32("w_mlp2"))
    natab = [na_tables(rpb, h) for h in range(2)] if upto >= 3 else None
    maps = []
    for c in range(NCORES):
        b, h = c // 2, c % 2
        perm = np.concatenate([np.arange(h * 4096, (h + 1) * 4096), np.arange((1 - h) * 4096, (2 - h) * 4096)])
        xb = x[b]
        cosT, sinT = rope_tables(perm)
        m = dict(shared)
        m["xT"] = np.ascontiguousarray(xb[perm].T)
        m["x_own"] = np.ascontiguousarray(xb[h * 4096:(h + 1) * 4096])
        m["cosT"] = cosT
        m["sinT"] = sinT
        if upto >= 3:
            m["na_R"], m["na_M"] = natab[h]
        maps.append(m)
    return maps


def kernel(**inputs):
    from concourse.bass_utils import run_bass_kernel_spmd
    maps = host_prep(inputs)
    nc, P = build()
    res = run_bass_kernel_spmd(nc, maps, core_ids=list(range(NCORES)))
    out = np.zeros((4, SEQ, D), np.float32)
    for c in range(NCORES):
        b, h = c // 2, c % 2
        out[b, h * 4096:(h + 1) * 4096] = np.asarray(res.results[c]["out"], np.float32)
    return out
```

```python
import numpy as np
import concourse.bass as bass
import concourse.mybir as mybir

F32 = mybir.dt.float32
BF16 = mybir.dt.bfloat16
I32 = mybir.dt.int32
U32 = mybir.dt.uint32
AF = mybir.ActivationFunctionType
ALU = mybir.AluOpType
AX = mybir.AxisListType

ENGS = ("pe", "act", "dve", "pool", "sp")
KDMA = 8


class Tok:
    __slots__ = ("eng", "seq", "dma")

    def __init__(self, eng, seq, dma=None):
        self.eng = eng
        self.seq = seq
        self.dma = dma


class Buf:
    __slots__ = ("w", "r", "name")

    def __init__(self, name=""):
        self.w = None
        self.r = []
        self.name = name


class Prog:
    def __init__(self, nc):
        self.nc = nc
        self.ops = {e: [] for e in ENGS}
        self.ndma = {e: 0 for e in ENGS}
        self.all_dma = []
        self.sb_off = self.SB_BASE
        self.sb_hi = 0
        self.uid = 0

    SB_BASE = 16640
    SB_END = 229376

    def sb_reset(self, off=None):
        self.sb_off = self.SB_BASE if off is None else off

    def sb(self, shape, dtype, name=None):
        self.uid += 1
        nm = "%s_%d" % (name or "t", self.uid)
        nbytes = int(np.prod(shape[1:])) * mybir.dt.size(dtype)
        off = (self.sb_off + 63) // 64 * 64
        t = self.nc.alloc_sbuf_tensor_at(nm, list(shape), dtype, offset=off)
        self.sb_off = off + nbytes
        self.sb_hi = max(self.sb_hi, self.sb_off)
        assert self.sb_off <= self.SB_END, ("SBUF overflow", nm, self.sb_off)
        return t

    def _deps(self, reads, writes, extra):
        deps = {}
        for b in reads:
            if b.w is not None:
                deps[id(b.w)] = b.w
        for b in writes:
            if b.w is not None:
                deps[id(b.w)] = b.w
            for t in b.r:
                deps[id(t)] = t
        for t in extra:
            if t is not None:
                deps[id(t)] = t
        return list(deps.values())

    def _commit(self, tok, reads, writes):
        for b in writes:
            b.w = tok
            b.r = []
        for b in reads:
            if tok.dma is None:
                b.r = [t for t in b.r if not (t.dma is None and t.eng == tok.eng)]
            b.r.append(tok)

    def op(self, eng, fn, reads=(), writes=(), extra=()):
        deps = self._deps(reads, writes, extra)
        if eng == "pe":
            deps = [t for t in deps if not (t.eng == "pe" and t.dma is None)]
        tok = Tok(eng, len(self.ops[eng]))
        self.ops[eng].append(dict(fn=fn, waits=deps, tok=tok, dma=False))
        self._commit(tok, reads, writes)
        return tok

    def dma(self, eng, fn, reads=(), writes=(), extra=()):
        deps = self._deps(reads, writes, extra)
        i = self.ndma[eng]
        self.ndma[eng] += 1
        tok = Tok(eng, len(self.ops[eng]), dma=(i % KDMA, 16 * (i // KDMA + 1)))
        self.ops[eng].append(dict(fn=fn, waits=deps, tok=tok, dma=True, idx=i))
        self.all_dma.append(tok)
        self._commit(tok, reads, writes)
        return tok

    def barrier(self):
        lasts = []
        for e in ENGS:
            for o in reversed(self.ops[e]):
                if not o["dma"]:
                    lasts.append(o["tok"])
                    break
        dm = list(self.all_dma)
        self.all_dma = []
        for e in ENGS:
            if e == "sp":
                self.op(e, None, extra=lasts + dm)
            else:
                self.op(e, None, extra=lasts + dm)

    def emit(self):
        nc = self.nc
        needed = {e: set() for e in ENGS}
        for e in ENGS:
            for o in self.ops[e]:
                for t in o["waits"]:
                    if t.dma is None:
                        needed[t.eng].add(t.seq)
        tokval = {e: {} for e in ENGS}
        for e in ENGS:
            c = 0
            for o in self.ops[e]:
                if o["dma"]:
                    continue
                if o["tok"].seq in needed[e]:
                    c += 1
                    tokval[e][o["tok"].seq] = c
            self.maxcount = getattr(self, "maxcount", {})
            self.maxcount[e] = c
        engobj = {"pe": nc.tensor, "act": nc.scalar, "dve": nc.vector, "pool": nc.gpsimd, "sp": nc.sync}
        import contextlib
        with contextlib.ExitStack() as st:
            csem = {e: st.enter_context(nc.semaphore("c_" + e)) for e in ENGS}
            dsem = {e: [st.enter_context(nc.semaphore("d_%s%d" % (e, k))) for k in range(KDMA)]
                    for e in ENGS if self.ndma[e] > 0}
            block = st.enter_context(nc.Block())

            def run(e, engine):
                waited = {}
                for o in self.ops[e]:
                    for t in o["waits"]:
                        if t.dma is not None:
                            sem = dsem[t.eng][t.dma[0]]
                            val = t.dma[1]
                            key = ("d", t.eng, t.dma[0])
                        else:
                            sem = csem[t.eng]
                            val = tokval[t.eng][t.seq]
                            key = ("c", t.eng)
                        if waited.get(key, 0) >= val:
                            continue
                        waited[key] = val
                        engine.wait_ge(sem, val)
                    if o["dma"]:
                        i = o["idx"]
                        slot = i % KDMA
                        if i >= KDMA:
                            key = ("d", e, slot)
                            val = 16 * (i // KDMA)
                            if waited.get(key, 0) < val:
                                waited[key] = val
                                engine.wait_ge(dsem[e][slot], val)
                        ins = o["fn"](engine)
                        ins.then_inc(dsem[e][slot], 16)
                    else:
                        if o["fn"] is None:
                            if o["tok"].seq in needed[e]:
                                ins = engine.nop() if hasattr(engine, "nop") else None
                                ins.then_inc(csem[e], 1)
                            continue
                        ins = o["fn"](engine)
                        if o["tok"].seq in needed[e]:
                            ins.then_inc(csem[e], 1)

            @block.tensor
            def _(eng):
                run("pe", eng)

            @block.scalar
            def _(eng):
                run("act", eng)

            @block.vector
            def _(eng):
                run("dve", eng)

            @block.gpsimd
            def _(eng):
                run("pool", eng)

            @block.sync
            def _(eng):
                run("sp", eng)
D = 1024
SEQ = 8192
NOWN = 4096
NCORES = 8
CAP = 1280
NEXP = 32
LAM_INIT = 0.2
ALPHA = 2.0 ** 0.25


class Ring:
    def __init__(self, items):
        self.items = list(items)
        self.i = 0

    def next(self):
        it = self.items[self.i % len(self.items)]
        self.i += 1
        return it


def phase1(P, nc, T):
    P.sb_reset()
    wfm = P.sb([128, 8, 3072], BF16, "wfm")
    wtm = P.sb([128, 8, 1024], BF16, "wtm")
    bfm = P.sb([128, 24], F32, "bfm")
    btm = P.sb([128, 1024], F32, "btm")
    b_w = Buf()
    wtoks = []
    for c in range(8):
        for g in range(6):
            wtoks.append(P.dma("pool", lambda e, c=c, g=g: e.dma_start(
                out=wfm[:, c, g * 512:(g + 1) * 512], in_=T["w_fm"][c * 128:(c + 1) * 128, g * 512:(g + 1) * 512])))
        for g in range(2):
            wtoks.append(P.dma("pool", lambda e, c=c, g=g: e.dma_start(
                out=wtm[:, c, g * 512:(g + 1) * 512], in_=T["w_tm"][c * 128:(c + 1) * 128, g * 512:(g + 1) * 512])))
    wtoks.append(P.dma("sp", lambda e: e.dma_start(out=bfm[:], in_=T["b_fm"])))
    wtoks.append(P.dma("sp", lambda e: e.dma_start(out=btm[:], in_=T["b_tm"])))
    for eng in ("pe", "act", "dve"):
        P.op(eng, None, extra=wtoks)

    xblk = Ring([(P.sb([128, 8, 512], BF16, "xblk"), Buf()) for _ in range(2)])
    csb = Ring([(P.sb([128, 512], F32, "cos"), P.sb([128, 512], F32, "sin"), Buf()) for _ in range(2)])
    t1r = Ring([(P.sb([128, 512], F32, "t1"), Buf()) for _ in range(2)])
    t2r = Ring([(P.sb([128, 512], F32, "t2"), Buf()) for _ in range(2)])
    stg = Ring([(P.sb([128, 512], BF16, "stg"), Buf()) for _ in range(6)])
    psr = Ring([(T["ps"][i], T["psb"][i]) for i in range(8)])
    xT3 = T["xT"].rearrange("(c p) t -> p c t", p=128)

    def do_block(i):
        own = i < 8
        xb, xbB = xblk.next()
        P.dma("pool", lambda e, xb=xb, i=i: e.dma_start(out=xb[:], in_=xT3[:, :, i * 512:(i + 1) * 512]), writes=[xbB])
        cb, sb_, csB = csb.next()
        P.dma("sp", lambda e, cb=cb, i=i: e.dma_start(out=cb[:], in_=T["cosT"][:, i * 512:(i + 1) * 512]), writes=[csB])
        P.dma("sp", lambda e, sb_=sb_, i=i: e.dma_start(out=sb_[:], in_=T["sinT"][:, i * 512:(i + 1) * 512]), writes=[csB])

        def mm_fm(ps, psB, col):
            for k in range(8):
                P.op("pe", lambda e, k=k: e.matmul(ps[:], wfm[:, k, col:col + 128], xb[:, k, :],
                                                   start=(k == 0), stop=(k == 7)),
                     reads=[xbB], writes=[psB])

        def rope_tile(col0, col1, dst):
            psA, psAB = psr.next()
            psC, psCB = psr.next()
            mm_fm(psA, psAB, col0)
            mm_fm(psC, psCB, col1)
            t1, t1B = t1r.next()
            t2, t2B = t2r.next()
            P.op("dve", lambda e: e.scalar_tensor_tensor(out=t1[:], in0=psA[:], scalar=bfm[:, col0 // 128:col0 // 128 + 1],
                                                          in1=cb[:], op0=ALU.add, op1=ALU.mult),
                 reads=[psAB, csB], writes=[t1B])
            P.op("dve", lambda e: e.scalar_tensor_tensor(out=t2[:], in0=psC[:], scalar=bfm[:, col1 // 128:col1 // 128 + 1],
                                                          in1=sb_[:], op0=ALU.add, op1=ALU.mult),
                 reads=[psCB, csB], writes=[t2B])
            st, stB = stg.next()
            P.op("pool", lambda e: e.tensor_tensor(out=st[:], in0=t1[:], in1=t2[:], op=ALU.add),
                 reads=[t1B, t2B], writes=[stB])
            P.dma("sp", lambda e: e.dma_start(out=dst, in_=st[:]), reads=[stB])

        def plain_tile(col, dst):
            ps, psB = psr.next()
            mm_fm(ps, psB, col)
            st, stB = stg.next()
            P.op("act", lambda e: e.activation(out=st[:], in_=ps[:], func=AF.Identity,
                                               bias=bfm[:, col // 128:col // 128 + 1], scale=1.0),
                 reads=[psB], writes=[stB])
            P.dma("sp", lambda e: e.dma_start(out=dst, in_=st[:]), reads=[stB])

        sl = slice(i * 512, (i + 1) * 512)
        for h in range(4):
            if own:
                rope_tile(h * 128, 512 + h * 128, T["qT_da"][h, :, sl])
            rope_tile(1024 + h * 128, 1536 + h * 128, T["kT_da"][h, :, sl])
        for j in range(4):
            if own:
                plain_tile(2048 + j * 128, T["qT_na"][j, :, sl])
            plain_tile(2560 + j * 128, T["kT_na"][j, :, sl])
        for tt in range(4):
            for g, dst in ((0, T["v_da"]), (1, T["v_na"])):
                ps, psB = psr.next()
                for k in range(8):
                    P.op("pe", lambda e, k=k, ps=ps, g=g, tt=tt: e.matmul(
                        ps[:], xb[:, k, tt * 128:(tt + 1) * 128], wtm[:, k, g * 512:(g + 1) * 512],
                        start=(k == 0), stop=(k == 7)), reads=[xbB], writes=[psB])
                st, stB = stg.next()
                P.op("dve", lambda e, ps=ps, st=st, g=g: e.tensor_tensor(out=st[:], in0=ps[:], in1=btm[:, g * 512:(g + 1) * 512],
                                                                         op=ALU.add), reads=[psB], writes=[stB])
                r0 = i * 512 + tt * 128
                P.dma("sp", lambda e, st=st, dst=dst, r0=r0: e.dma_start(out=dst[r0:r0 + 128, :], in_=st[:]), reads=[stB])

    for i in range(16):
        do_block(i)
    P.barrier()
def phase2(P, nc, T, a_sb, a_B):
    P.sb_reset(T["arena0"])
    lamv = P.sb([128, 256], F32, "lamv")
    gbc = P.sb([128, 128], F32, "gbc")
    tmp64 = P.sb([128, 128], F32, "tmp64")
    sc = P.sb([128, 8], F32, "sc")
    neglam = P.sb([128, 1], F32, "neglam")
    cB = Buf()
    P.dma("sp", lambda e: e.dma_start(out=lamv[:], in_=T["lamv"]), writes=[cB])
    P.dma("sp", lambda e: e.dma_start(out=gbc[:], in_=T["subln_bc"]), writes=[cB])
    P.op("dve", lambda e: e.tensor_tensor(out=tmp64[:, 0:64], in0=lamv[:, 0:64], in1=lamv[:, 64:128], op=ALU.mult), reads=[cB], writes=[cB])
    P.op("dve", lambda e: e.tensor_tensor(out=tmp64[:, 64:128], in0=lamv[:, 128:192], in1=lamv[:, 192:256], op=ALU.mult), reads=[cB], writes=[cB])
    P.op("dve", lambda e: e.reduce_sum(out=sc[:, 0:1], in_=tmp64[:, 0:64], axis=AX.X), reads=[cB], writes=[cB])
    P.op("dve", lambda e: e.reduce_sum(out=sc[:, 1:2], in_=tmp64[:, 64:128], axis=AX.X), reads=[cB], writes=[cB])
    P.op("act", lambda e: e.activation(out=sc[:, 2:4], in_=sc[:, 0:2], func=AF.Exp), reads=[cB], writes=[cB])
    P.op("dve", lambda e: e.tensor_tensor(out=sc[:, 4:5], in0=sc[:, 3:4], in1=sc[:, 2:3], op=ALU.subtract), reads=[cB], writes=[cB])
    P.op("dve", lambda e: e.tensor_scalar(out=neglam[:], in0=sc[:, 4:5], scalar1=-LAM_INIT, scalar2=None, op0=ALU.add), reads=[cB], writes=[cB])
    P.op("dve", lambda e: e.tensor_scalar(out=gbc[:], in0=gbc[:], scalar1=1.0 - LAM_INIT, scalar2=None, op0=ALU.mult), reads=[cB], writes=[cB])

    hb = []
    for _ in range(2):
        KT = P.sb([128, 8192], BF16, "KT")
        V = P.sb([128, 64, 129], BF16, "V")
        QT = P.sb([128, 2, 4096], BF16, "QT")
        oB = Buf()
        P.op("pool", lambda e, V=V: e.memset(V[:, :, 128:129], 1.0), writes=[oB])
        P.op("pool", lambda e, QT=QT: e.memset(QT[64:128, 0, :], 0.0), writes=[oB])
        P.op("pool", lambda e, QT=QT: e.memset(QT[0:64, 1, :], 0.0), writes=[oB])
        hb.append((KT, V, QT, Buf(), Buf(), Buf(), oB))
    ptr = Ring([(P.sb([128, 512], BF16, "pt"), Buf()) for _ in range(4)])
    sps = Ring([(T["ps"][i], T["psb"][i]) for i in range(4, 8)])
    accs = {}
    lay = [(0, 0), (0, 1), (0, 2), (1, 0), (2, 0), (2, 1), (2, 2), (3, 0)]
    n = 0
    for c in range(2):
        for qt in range(4):
            bk, pos = lay[n]
            n += 1
            accs[(c, qt)] = (T["ps"][bk][:, pos * 129:(pos + 1) * 129], T["psb"][bk])
    small = Ring([(P.sb([128, 8], F32, "sm"), P.sb([128, 128], F32, "tt"), P.sb([128, 128], F32, "oo"),
                   P.sb([128, 128], F32, "sq"), Buf()) for _ in range(3)])
    v_da3 = T["v_da"].rearrange("(t p) e -> p t e", p=128)

    def load_head(h):
        KT, V, QT, kB, vB, qB, oB = hb[h % 2]
        for s4 in range(4):
            P.dma("sp", lambda e, s4=s4: e.dma_start(out=KT[:, s4 * 2048:(s4 + 1) * 2048],
                                                      in_=T["kT_da"][h, :, s4 * 2048:(s4 + 1) * 2048]), writes=[kB])
        for s8 in range(8):
            P.dma("sp", lambda e, s8=s8: e.dma_start(out=V[:, s8 * 8:(s8 + 1) * 8, 0:128],
                                                      in_=v_da3[:, s8 * 8:(s8 + 1) * 8, h * 128:(h + 1) * 128]), writes=[vB])
        P.dma("sp", lambda e: e.dma_start(out=QT[0:64, 0, :], in_=T["qT_da"][h, 0:64, :]), writes=[qB])
        P.dma("sp", lambda e: e.dma_start(out=QT[64:128, 1, :], in_=T["qT_da"][h, 64:128, :]), writes=[qB])

    def epilogue(h, qb, qt):
        O1, O1B = accs[(0, qt)]
        O2, O2B = accs[(1, qt)]
        sm, tt, oo, sq, sB = small.next()
        P.op("dve", lambda e: e.reciprocal(out=sm[:, 0:1], in_=O1[:, 128:129]), reads=[O1B], writes=[sB])
        P.op("dve", lambda e: e.reciprocal(out=sm[:, 1:2], in_=O2[:, 128:129]), reads=[O2B], writes=[sB])
        P.op("dve", lambda e: e.tensor_tensor(out=sm[:, 2:3], in0=sm[:, 1:2], in1=neglam[:], op=ALU.mult), reads=[sB, cB], writes=[sB])
        P.op("dve", lambda e: e.tensor_scalar(out=tt[:], in0=O2[:, 0:128], scalar1=sm[:, 2:3], scalar2=None, op0=ALU.mult),
             reads=[O2B, sB], writes=[sB])
        P.op("dve", lambda e: e.scalar_tensor_tensor(out=oo[:], in0=O1[:, 0:128], scalar=sm[:, 0:1], in1=tt[:],
                                                      op0=ALU.mult, op1=ALU.add), reads=[O1B, sB], writes=[sB])
        P.op("act", lambda e: e.activation(out=sq[:], in_=oo[:], func=AF.Square, accum_out=sm[:, 3:4]), reads=[sB], writes=[sB])
        P.op("act", lambda e: e.activation(out=sm[:, 4:5], in_=sm[:, 3:4], func=AF.Ln, scale=1.0 / 128.0, bias=1e-5),
             reads=[sB], writes=[sB])
        P.op("act", lambda e: e.activation(out=sm[:, 5:6], in_=sm[:, 4:5], func=AF.Exp, scale=-0.5), reads=[sB], writes=[sB])
        P.op("dve", lambda e: e.scalar_tensor_tensor(out=a_sb[:, qb * 4 + qt, h * 128:(h + 1) * 128], in0=oo[:], scalar=sm[:, 5:6],
                                                      in1=gbc[:], op0=ALU.mult, op1=ALU.mult), reads=[sB, cB], writes=[a_B])

    def qk(h, qb, c, kt):
        KT, V, QT, kB, vB, qB, oB = hb[h % 2]
        S, SB = sps.next()
        P.op("pe", lambda e: e.matmul(S[:], KT[:, kt * 128:(kt + 1) * 128],
                                      QT[:, c, qb * 512:(qb + 1) * 512],
                                      start=True, stop=True), reads=[kB, qB, oB], writes=[SB])
        Pt, PtB = ptr.next()
        P.op("act", lambda e: e.activation(out=Pt[:], in_=S[:], func=AF.Exp, scale=0.125),
             reads=[SB], writes=[PtB])
        return Pt, PtB

    def pv(h, qb, c, kt, Pt, PtB):
        KT, V, QT, kB, vB, qB, oB = hb[h % 2]
        for qt in range(4):
            O, OB = accs[(c, qt)]
            P.op("pe", lambda e, O=O, qt=qt: e.matmul(O, Pt[:, qt * 128:(qt + 1) * 128], V[:, kt, :],
                                                      start=(kt == 0 and qt in (0, 3)), stop=(kt == 63), skip_group_check=True),
                 reads=[PtB, vB, oB], writes=[OB])
        if c == 1 and kt == 63:
            for qt in range(4):
                epilogue(h, qb, qt)

    iters = [(h, qb, c, kt) for h in range(T.get("da_heads", 4)) for qb in range(8) for c in range(2) for kt in range(64)]
    LA = 2
    pend = {}
    NH = T.get("da_heads", 4)
    load_head(0)
    if NH > 1:
        load_head(1)
    for n in range(len(iters) + LA):
        if n < len(iters):
            h, qb, c, kt = iters[n]
            pend[n] = qk(h, qb, c, kt)
        m = n - LA
        if m >= 0:
            h, qb, c, kt = iters[m]
            Pt, PtB = pend.pop(m)
            pv(h, qb, c, kt, Pt, PtB)
            if qb == 7 and c == 1 and kt == 63 and h + 2 < NH:
                load_head(h + 2)
    P.barrier()
def phase3(P, nc, T, nb_sb, nb_B):
    P.sb_reset(T["arena0"])
    if T.get("xs_d") is not None:
        zt = P.sb([128, CAP // 128, 1024], BF16, "zt")
        zB = Buf()
        P.op("pool", lambda e: e.memset(zt[:], 0.0), writes=[zB])
        xs3 = T["xs_d"].rearrange("(e s p) d -> e p s d", p=128, s=CAP // 128)
        for e_ in range(NEXP):
            P.dma("sp", lambda e, e_=e_: e.dma_start(out=xs3[e_], in_=zt[:]), reads=[zB])
    Mt = P.sb([128, 18 * 256], BF16, "Mt")
    mB = Buf()
    for s in range(3):
        P.dma("pool", lambda e, s=s: e.dma_start(out=Mt[:, s * 1536:(s + 1) * 1536], in_=T["na_M"][:, s * 1536:(s + 1) * 1536]), writes=[mB])
    Rt = P.sb([128, 18 * 256], F32, "Rt")
    rB = Buf()
    hb = []
    for _ in range(2):
        KT = P.sb([64, 4608], BF16, "KTn")
        V = P.sb([128, 36, 65], BF16, "Vn")
        QT = P.sb([64, 4096], BF16, "QTn")
        E = P.sb([128, 18 * 256], BF16, "En")
        oB = Buf()
        P.op("pool", lambda e, V=V: e.memset(V[:, :, 64:65], 1.0), writes=[oB])
        hb.append((KT, V, QT, E, Buf(), Buf(), Buf(), Buf(), oB))
    per = Ring([(P.sb([128, 256], BF16, "pe_"), Buf()) for _ in range(4)])
    pmr = Ring([(P.sb([128, 256], BF16, "pm_"), Buf()) for _ in range(4)])
    sps = Ring([(T["ps"][i], T["psb"][i]) for i in range(2, 8)])
    accs = [(T["ps"][0][:, 0:65], T["psb"][0]), (T["ps"][0][:, 128:193], T["psb"][0]),
            (T["ps"][1][:, 0:65], T["psb"][1]), (T["ps"][1][:, 128:193], T["psb"][1])]
    smr = Ring([(P.sb([128, 2], F32, "smn"), Buf()) for _ in range(4)])
    v_na3 = T["v_na"].rearrange("(t p) e -> p t e", p=128)

    def load_head(hn):
        KT, V, QT, E, kB, vB, qB, eB, oB = hb[hn % 2]
        j, po = hn // 2, (hn % 2) * 64
        P.dma("sp", lambda e: e.dma_start(out=KT[:, 0:256], in_=T["kT_na"][j, po:po + 64, 7936:8192]), writes=[kB])
        P.dma("sp", lambda e: e.dma_start(out=KT[:, 256:4608], in_=T["kT_na"][j, po:po + 64, 0:4352]), writes=[kB])
        P.dma("sp", lambda e: e.dma_start(out=QT[:], in_=T["qT_na"][j, po:po + 64, :]), writes=[qB])
        P.dma("sp", lambda e: e.dma_start(out=V[:, 0:2, 0:64], in_=v_na3[:, 62:64, hn * 64:(hn + 1) * 64]), writes=[vB])
        for s in range(2):
            P.dma("sp", lambda e, s=s: e.dma_start(out=V[:, 2 + s * 17:2 + (s + 1) * 17, 0:64],
                                                    in_=v_na3[:, s * 17:(s + 1) * 17, hn * 64:(hn + 1) * 64]), writes=[vB])
        P.dma("sp", lambda e: e.dma_start(out=Rt[:], in_=T["na_R"][hn, :, :]), writes=[rB])
        for s in range(3):
            sl = slice(s * 1536, (s + 1) * 1536)
            P.op("act", lambda e, sl=sl: e.activation(out=Rt[:, sl], in_=Rt[:, sl], func=AF.Exp), reads=[rB], writes=[rB])
            P.op("dve", lambda e, sl=sl: e.tensor_tensor(out=E[:, sl], in0=Rt[:, sl], in1=Mt[:, sl], op=ALU.mult),
                 reads=[rB, mB], writes=[eB])

    def qk(hn, g, j):
        KT, V, QT, E, kB, vB, qB, eB, oB = hb[hn % 2]
        S, SB = sps.next()
        ti = 2 * g + j
        P.op("pe", lambda e: e.matmul(S[:, 0:256], KT[:, ti * 128:(ti + 1) * 128], QT[:, g * 256:(g + 1) * 256],
                                      start=True, stop=True), reads=[kB, qB], writes=[SB])
        Pe, PeB = per.next()
        P.op("act", lambda e: e.activation(out=Pe[:], in_=S[:, 0:256], func=AF.Exp, scale=0.125), reads=[SB], writes=[PeB])
        Pm, PmB = pmr.next()
        cls = 0 if g == 0 else (2 if g == 15 else 1)
        ei = (cls * 6 + j) * 256
        P.op("dve", lambda e: e.tensor_tensor(out=Pm[:], in0=Pe[:], in1=E[:, ei:ei + 256], op=ALU.mult),
             reads=[PeB, eB], writes=[PmB])
        return Pm, PmB

    def pv(hn, g, j, Pm, PmB):
        KT, V, QT, E, kB, vB, qB, eB, oB = hb[hn % 2]
        ti = 2 * g + j
        for qt in range(2):
            O, OB = accs[(g % 2) * 2 + qt]
            P.op("pe", lambda e, O=O, qt=qt: e.matmul(O, Pm[:, qt * 128:(qt + 1) * 128], V[:, ti, :],
                                                      start=(j == 0 and qt == 0), stop=(j == 5), skip_group_check=True), reads=[PmB, vB, oB], writes=[OB])
        if j == 5:
            for qt in range(2):
                O, OB = accs[(g % 2) * 2 + qt]
                sm, sB = smr.next()
                P.op("dve", lambda e, O=O, sm=sm: e.reciprocal(out=sm[:, 0:1], in_=O[:, 64:65]), reads=[OB], writes=[sB])
                P.op("dve", lambda e, O=O, sm=sm, qt=qt: e.tensor_scalar(
                    out=nb_sb[:, g * 2 + qt, hn * 64:(hn + 1) * 64], in0=O[:, 0:64], scalar1=sm[:, 0:1], scalar2=None,
                    op0=ALU.mult), reads=[OB, sB], writes=[nb_B])

    iters = [(hn, g, j) for hn in range(T.get("na_heads", 8)) for g in range(16) for j in range(6)]
    LA = 3
    pend = {}
    NH = T.get("na_heads", 8)
    load_head(0)
    if NH > 1:
        load_head(1)
    for n in range(len(iters) + LA):
        if n < len(iters):
            hn, g, j = iters[n]
            pend[n] = qk(hn, g, j)
        m = n - LA
        if m >= 0:
            hn, g, j = iters[m]
            Pm, PmB = pend.pop(m)
            pv(hn, g, j, Pm, PmB)
            if g == 15 and j == 5 and hn + 2 < NH:
                load_head(hn + 2)
    P.barrier()
def layer_norm_tile(P, y, yB, out, outB, gbc, bbc, cB, junk, jB, sm, sB):
    class _W:
        def __init__(self, t):
            self.t = t

        def __getitem__(self, k):
            return self.t if isinstance(self.t, bass.AP) else self.t[k]
    y = _W(y)
    out = _W(out)
    junk = _W(junk)
    P.op("act", lambda e: e.activation(out=junk[:], in_=y[:], func=AF.Identity, accum_out=sm[:, 0:1]), reads=[yB], writes=[jB, sB])
    P.op("act", lambda e: e.activation(out=junk[:], in_=y[:], func=AF.Square, accum_out=sm[:, 1:2]), reads=[yB], writes=[jB, sB])
    P.op("dve", lambda e: e.tensor_scalar(out=sm[:, 2:3], in0=sm[:, 0:1], scalar1=1.0 / 1024.0, scalar2=None, op0=ALU.mult), reads=[sB], writes=[sB])
    P.op("dve", lambda e: e.tensor_tensor(out=sm[:, 3:4], in0=sm[:, 2:3], in1=sm[:, 2:3], op=ALU.mult), reads=[sB], writes=[sB])
    P.op("dve", lambda e: e.scalar_tensor_tensor(out=sm[:, 4:5], in0=sm[:, 1:2], scalar=1.0 / 1024.0, in1=sm[:, 3:4],
                                                  op0=ALU.mult, op1=ALU.subtract), reads=[sB], writes=[sB])
    P.op("act", lambda e: e.activation(out=sm[:, 5:6], in_=sm[:, 4:5], func=AF.Ln, scale=1.0, bias=1e-5), reads=[sB], writes=[sB])
    P.op("act", lambda e: e.activation(out=sm[:, 6:7], in_=sm[:, 5:6], func=AF.Exp, scale=-0.5), reads=[sB], writes=[sB])
    P.op("dve", lambda e: e.tensor_scalar(out=out[:], in0=y[:], scalar1=sm[:, 2:3], scalar2=sm[:, 6:7], op0=ALU.subtract, op1=ALU.mult),
         reads=[yB, sB], writes=[outB])
    P.op("dve", lambda e: e.tensor_tensor(out=out[:], in0=out[:], in1=gbc[:], op=ALU.mult), reads=[outB, cB], writes=[outB])
    P.op("dve", lambda e: e.tensor_tensor(out=out[:], in0=out[:], in1=bbc[:], op=ALU.add), reads=[outB, cB], writes=[outB])


def phase4(P, nc, T, a_sb, a_B, nb_sb, nb_B, GT, GT_B, slots_all, gk_all, rt_B):
    P.sb_reset(T["arena0"])
    wg = P.sb([128, 8, 2048], BF16, "wg")
    wbd = P.sb([128, 4, 1024], BF16, "wbd")
    wbn = P.sb([128, 4, 1024], BF16, "wbn")
    wo = P.sb([128, 8, 1024], BF16, "wo")
    wr = P.sb([128, 8, 32], F32, "wr")
    bg = P.sb([128, 16], F32, "bg")
    g1bc = P.sb([128, 1024], F32, "g1bc")
    b1bc = P.sb([128, 1024], F32, "b1bc")
    brbc = P.sb([128, 32], F32, "brbc")
    identb = P.sb([128, 128], BF16, "identb")
    ltri = P.sb([128, 128], BF16, "ltri")
    ones = P.sb([128, 128], BF16, "ones")
    ecap = P.sb([128, 32], F32, "ecap")
    cnt = P.sb([128, 32], F32, "cnt")
    cntB = Buf()
    P.op("pool", lambda e: e.memset(cnt[:], 0.0), writes=[cntB])
    identf = P.sb([128, 128], F32, "identf")
    cB = Buf()
    toks = []
    for c in range(8):
        for g in range(4):
            toks.append(P.dma("pool", lambda e, c=c, g=g: e.dma_start(out=wg[:, c, g * 512:(g + 1) * 512],
                                                                      in_=T["w_gate"][c * 128:(c + 1) * 128, g * 512:(g + 1) * 512])))
        toks.append(P.dma("pool", lambda e, c=c: e.dma_start(out=wo[:, c, :], in_=T["w_out"][c * 128:(c + 1) * 128, :])))
        toks.append(P.dma("sp", lambda e, c=c: e.dma_start(out=wr[:, c, :], in_=T["w_router"][c * 128:(c + 1) * 128, :])))
    for c in range(4):
        toks.append(P.dma("pool", lambda e, c=c: e.dma_start(out=wbd[:, c, :], in_=T["w_bda"][c * 128:(c + 1) * 128, :])))
        toks.append(P.dma("pool", lambda e, c=c: e.dma_start(out=wbn[:, c, :], in_=T["w_bna"][c * 128:(c + 1) * 128, :])))
    for dst, src in ((bg, "b_gate"), (g1bc, "ln1_g_bc"), (b1bc, "ln1_b_bc"), (brbc, "b_router_bc"), (identf, "ident")):
        toks.append(P.dma("sp", lambda e, dst=dst, src=src: e.dma_start(out=dst[:], in_=T[src])))
    toks.append(P.dma("pool", lambda e: e.dma_start(out=identb[:], in_=T["ident"])))
    toks.append(P.dma("pool", lambda e: e.dma_start(out=ltri[:], in_=T["ltri"])))
    toks.append(P.dma("pool", lambda e: e.dma_start(out=ones[:], in_=T["ones128"])))
    toks.append(P.dma("sp", lambda e: e.dma_start(out=ecap[:], in_=T["ecap"])))
    for eng in ("pe", "act", "dve", "pool"):
        P.op(eng, None, extra=toks)

    xblk = Ring([(P.sb([128, 8, 512], BF16, "xblk4"), Buf()) for _ in range(1)])
    aT = P.sb([128, 4, 512], BF16, "aT"); aTB = Buf()
    nT = P.sb([128, 4, 512], BF16, "nT"); nTB = Buf()
    g_r = Ring([(P.sb([128, 2, 512], BF16, "g01"), Buf()) for _ in range(2)])
    mT = P.sb([128, 8, 512], BF16, "mT"); mTB = Buf()
    tr = Ring([(P.sb([128, 512], F32, "t4"), P.sb([128, 512], F32, "u4"), Buf()) for _ in range(1)])
    xt_r = Ring([(P.sb([128, 1024], F32, "xt"), Buf()) for _ in range(1)])
    y_r = Ring([(P.sb([128, 1024], F32, "y4"), Buf()) for _ in range(1)])
    x1_r = Ring([(P.sb([128, 1024], F32, "x1"), Buf()) for _ in range(1)])
    x1b_r = Ring([(P.sb([128, 1024], BF16, "x1b"), Buf()) for _ in range(1)])
    junk = P.sb([128, 1024], BF16, "junk"); jB = Buf()
    sm_r = Ring([(P.sb([128, 8], F32, "sm4"), Buf()) for _ in range(2)])
    x1Tf_r = Ring([(P.sb([128, 1024], F32, "x1Tf"), P.sb([128, 1024], BF16, "x1Tb"), Buf()) for _ in range(1)])
    rt_r = Ring([(P.sb([128, 32], F32, "lg"), P.sb([128, 8], F32, "t8"), P.sb([128, 32], F32, "mk"), P.sb([128, 32], F32, "ex"),
                  P.sb([128, 4], F32, "rs"), P.sb([128, 32], F32, "G"), Buf(),
                  P.sb([128, 32], BF16, "mkb"), P.sb([128, 32], F32, "rk"), P.sb([128, 32], F32, "tm"), P.sb([128, 8], F32, "slf")) for _ in range(2)])
    psr = Ring([(T["ps"][i], T["psb"][i]) for i in range(8)])
    xT3 = T["xT"].rearrange("(c p) t -> p c t", p=128)
    x1T_d3 = T["x1T_d"].rearrange("(c p) t -> p c t", p=128)

    def do_block(tb):
        xb, xbB = xblk.next()
        P.dma("pool", lambda e: e.dma_start(out=xb[:], in_=xT3[:, :, tb * 512:(tb + 1) * 512]), writes=[xbB])
        for src, sB_, dst, dB in ((a_sb, a_B, aT, aTB), (nb_sb, nb_B, nT, nTB)):
            for hc in range(4):
                ps, psB = psr.next()
                psb16 = ps.bitcast(BF16)
                for tt in range(4):
                    P.op("pe", lambda e, tt=tt, psb16=psb16, src=src, hc=hc: e.transpose(
                        psb16[:, tt * 128:(tt + 1) * 128], src[:, tb * 4 + tt, hc * 128:(hc + 1) * 128], identb[:]),
                        reads=[sB_], writes=[psB])
                P.op("act", lambda e, psb16=psb16, dst=dst, hc=hc: e.activation(out=dst[:, hc, :], in_=psb16[:, 0:512], func=AF.Identity),
                     reads=[psB], writes=[dB])
        for dt in range(8):
            g01, g01B = g_r.next()
            for gi, j in enumerate((dt, 8 + dt)):
                ps, psB = psr.next()
                for k in range(8):
                    P.op("pe", lambda e, k=k, ps=ps, j=j: e.matmul(ps[:], wg[:, k, j * 128:(j + 1) * 128], xb[:, k, :],
                                                                   start=(k == 0), stop=(k == 7)), reads=[xbB], writes=[psB])
                P.op("act", lambda e, ps=ps, j=j, gi=gi, g01=g01: e.activation(out=g01[:, gi, :], in_=ps[:], func=AF.Sigmoid,
                                                                              bias=bg[:, j:j + 1], scale=1.0),
                     reads=[psB], writes=[g01B])
            psa, psaB = psr.next()
            psn, psnB = psr.next()
            for ec in range(4):
                P.op("pe", lambda e, ec=ec, psa=psa, dt=dt: e.matmul(psa[:], wbd[:, ec, dt * 128:(dt + 1) * 128], aT[:, ec, :],
                                                                    start=(ec == 0), stop=(ec == 3)), reads=[aTB], writes=[psaB])
            for ec in range(4):
                P.op("pe", lambda e, ec=ec, psn=psn, dt=dt: e.matmul(psn[:], wbn[:, ec, dt * 128:(dt + 1) * 128], nT[:, ec, :],
                                                                    start=(ec == 0), stop=(ec == 3)), reads=[nTB], writes=[psnB])
            t4, u4, tB = tr.next()
            P.op("dve", lambda e, t4=t4, psa=psa, g01=g01: e.tensor_tensor(out=t4[:], in0=psa[:], in1=g01[:, 0, :], op=ALU.mult),
                 reads=[psaB, g01B], writes=[tB])
            P.op("dve", lambda e, u4=u4, psn=psn, g01=g01: e.tensor_tensor(out=u4[:], in0=psn[:], in1=g01[:, 1, :], op=ALU.mult),
                 reads=[psnB, g01B], writes=[tB])
            P.op("pool", lambda e, t4=t4, u4=u4, dt=dt: e.tensor_tensor(out=mT[:, dt, :], in0=t4[:], in1=u4[:], op=ALU.add),
                 reads=[tB], writes=[mTB])
        for tt in range(4):
            tok0 = tb * 512 + tt * 128
            xt, xtB = xt_r.next()
            P.dma("sp", lambda e, xt=xt, tok0=tok0: e.dma_start(out=xt[:], in_=T["x_own"][tok0:tok0 + 128, :]), writes=[xtB])
            y, yB = y_r.next()
            for half in range(2):
                ps, psB = psr.next()
                for k in range(8):
                    P.op("pe", lambda e, k=k, ps=ps, half=half, tt=tt: e.matmul(
                        ps[:], mT[:, k, tt * 128:(tt + 1) * 128], wo[:, k, half * 512:(half + 1) * 512],
                        start=(k == 0), stop=(k == 7)), reads=[mTB], writes=[psB])
                P.op("dve", lambda e, ps=ps, half=half, y=y, xt=xt: e.scalar_tensor_tensor(
                    out=y[:, half * 512:(half + 1) * 512], in0=xt[:, half * 512:(half + 1) * 512], scalar=ALPHA, in1=ps[:],
                    op0=ALU.mult, op1=ALU.add), reads=[psB, xtB], writes=[yB])
            x1, x1B = x1_r.next()
            sm, sB = sm_r.next()
            layer_norm_tile(P, y, yB, x1, x1B, g1bc, b1bc, cB, junk, jB, sm, sB)
            P.dma("sp", lambda e, x1=x1, tok0=tok0: e.dma_start(out=T["x1_d"][tok0:tok0 + 128, :], in_=x1[:]), reads=[x1B])
            x1Tf, x1Tb, xTB = x1Tf_r.next()
            for k2 in range(2):
                ps, psB = psr.next()
                for k4 in range(4):
                    k = k2 * 4 + k4
                    P.op("pe", lambda e, ps=ps, k=k, k4=k4, x1=x1: e.transpose(ps[:, k4 * 128:(k4 + 1) * 128], x1[:, k * 128:(k + 1) * 128],
                                                                        identf[:]), reads=[x1B], writes=[psB])
                P.op("act", lambda e, ps=ps, x1Tf=x1Tf, k2=k2: e.activation(out=x1Tf[:, k2 * 512:(k2 + 1) * 512], in_=ps[:], func=AF.Identity),
                     reads=[psB], writes=[xTB])
                P.op("dve", lambda e, ps=ps, x1Tb=x1Tb, k2=k2: e.tensor_copy(out=x1Tb[:, k2 * 512:(k2 + 1) * 512], in_=ps[:]),
                     reads=[psB], writes=[xTB])
            P.dma("sp", lambda e, x1Tb=x1Tb, tok0=tok0: e.dma_start(out=x1T_d3[:, :, tok0:tok0 + 128], in_=x1Tb[:].rearrange("p (c t) -> p c t", c=8)), reads=[xTB])
            ps, psB = psr.next()
            for k in range(8):
                P.op("pe", lambda e, ps=ps, k=k, x1Tf=x1Tf: e.matmul(ps[:, 0:32], x1Tf[:, k * 128:(k + 1) * 128], wr[:, k, :], start=(k == 0), stop=(k == 7)),
                     reads=[xTB], writes=[psB])
            lg, t8, mk, ex, rs, G, rB, mkb, rk, tm, slf = rt_r.next()
            P.op("dve", lambda e, ps=ps, lg=lg: e.tensor_tensor(out=lg[:], in0=ps[:, 0:32], in1=brbc[:], op=ALU.add), reads=[psB], writes=[rB])
            P.op("dve", lambda e, lg=lg, t8=t8: e.max(out=t8[:], in_=lg[:]), reads=[rB], writes=[rB])
            P.op("dve", lambda e, lg=lg, t8=t8, mk=mk: e.tensor_scalar(out=mk[:], in0=lg[:], scalar1=t8[:, 3:4], scalar2=None, op0=ALU.is_ge),
                 reads=[rB], writes=[rB])
            P.op("dve", lambda e, t8=t8, rs=rs: e.tensor_scalar(out=rs[:, 0:1], in0=t8[:, 0:1], scalar1=-1.0, scalar2=None, op0=ALU.mult),
                 reads=[rB], writes=[rB])
            P.op("act", lambda e, lg=lg, ex=ex, rs=rs: e.activation(out=ex[:], in_=lg[:], func=AF.Exp, bias=rs[:, 0:1], scale=1.0),
                 reads=[rB], writes=[rB])
            P.op("dve", lambda e, ex=ex, mk=mk: e.tensor_tensor(out=ex[:], in0=ex[:], in1=mk[:], op=ALU.mult), reads=[rB], writes=[rB])
            P.op("dve", lambda e, ex=ex, rs=rs: e.reduce_sum(out=rs[:, 1:2], in_=ex[:], axis=AX.X), reads=[rB], writes=[rB])
            P.op("dve", lambda e, rs=rs: e.reciprocal(out=rs[:, 2:3], in_=rs[:, 1:2]), reads=[rB], writes=[rB])
            P.op("dve", lambda e, ex=ex, rs=rs, G=G: e.tensor_scalar(out=G[:], in0=ex[:], scalar1=rs[:, 2:3], scalar2=None, op0=ALU.mult),
                 reads=[rB], writes=[rB])
            ps2, ps2B = psr.next()
            P.op("pe", lambda e, ps2=ps2, G=G: e.transpose(ps2[0:32, 0:128], G[:], identf[:]), reads=[rB], writes=[ps2B])
            P.op("act", lambda e, ps2=ps2, tok0=tok0: e.activation(out=GT[:, tok0:tok0 + 128], in_=ps2[0:32, 0:128], func=AF.Identity),
                 reads=[ps2B], writes=[GT_B])
            tix = tb * 4 + tt
            P.op("dve", lambda e, mk=mk, mkb=mkb: e.tensor_copy(out=mkb[:], in_=mk[:]), reads=[rB], writes=[rB])
            ps3, ps3B = psr.next()
            P.op("pe", lambda e, ps3=ps3, mkb=mkb: e.matmul(ps3[:, 0:32], ltri[:], mkb[:], start=True, stop=True, skip_group_check=True),
                 reads=[rB], writes=[ps3B])
            P.op("pe", lambda e, ps3=ps3, mkb=mkb: e.matmul(ps3[:, 64:96], ones[:], mkb[:], start=False, stop=True, skip_group_check=True),
                 reads=[rB], writes=[ps3B])
            P.op("dve", lambda e, ps3=ps3, rk=rk: e.tensor_tensor(out=rk[:], in0=ps3[:, 0:32], in1=cnt[:], op=ALU.add), reads=[ps3B, cntB], writes=[rB])
            P.op("dve", lambda e, ps3=ps3: e.tensor_tensor(out=cnt[:], in0=cnt[:], in1=ps3[:, 64:96], op=ALU.add), reads=[ps3B, cntB, rB], writes=[cntB])
            P.op("dve", lambda e, rk=rk: e.scalar_tensor_tensor(out=rk[:], in0=rk[:], scalar=float(CAP - 1), in1=ecap[:], op0=ALU.min, op1=ALU.add),
                 reads=[rB], writes=[rB])
            for k in range(4):
                P.op("dve", lambda e, k=k, lg=lg, t8=t8, rk=rk, tm=tm: e.scalar_tensor_tensor(out=tm[:], in0=lg[:], scalar=t8[:, k:k + 1], in1=rk[:],
                                                                                             op0=ALU.is_equal, op1=ALU.mult), reads=[rB], writes=[rB])
                P.op("dve", lambda e, k=k, tm=tm, slf=slf: e.reduce_sum(out=slf[:, k:k + 1], in_=tm[:], axis=AX.X), reads=[rB], writes=[rB])
            P.op("dve", lambda e, slf=slf, tix=tix: e.tensor_copy(out=slots_all[:, tix, :], in_=slf[:, 0:4]), reads=[rB], writes=[rB, rt_B])
            P.op("act", lambda e, t8=t8, slf=slf, rs=rs: e.activation(out=slf[:, 4:8], in_=t8[:, 0:4], func=AF.Exp, bias=rs[:, 0:1], scale=1.0),
                 reads=[rB], writes=[rB])
            P.op("dve", lambda e, slf=slf, rs=rs: e.reduce_sum(out=rs[:, 3:4], in_=slf[:, 4:8], axis=AX.X), reads=[rB], writes=[rB])
            P.op("dve", lambda e, rs=rs: e.reciprocal(out=rs[:, 3:4], in_=rs[:, 3:4]), reads=[rB], writes=[rB])
            P.op("dve", lambda e, slf=slf, rs=rs, tix=tix: e.tensor_scalar(out=gk_all[:, tix, :], in0=slf[:, 4:8], scalar1=rs[:, 3:4], scalar2=None,
                                                                          op0=ALU.mult), reads=[rB], writes=[rB, rt_B])
            x1b, x1bB = x1b_r.next()
            P.op("act", lambda e, x1=x1, x1b=x1b: e.activation(out=x1b[:], in_=x1[:], func=AF.Identity), reads=[x1B], writes=[x1bB])
            for k in range(4):
                P.dma("pool", lambda e, k=k, x1b=x1b, tix=tix: e.indirect_dma_start(
                    out=T["xs_d"], out_offset=bass.IndirectOffsetOnAxis(ap=slots_all[:, tix, k:k + 1], axis=0),
                    in_=x1b[:], in_offset=None, bounds_check=None, oob_is_err=False), reads=[x1bB, rB, rt_B])
            if T.get("dbg_G") is not None:
                P.dma("sp", lambda e, G=G, tok0=tok0: e.dma_start(out=T["dbg_G"][tok0:tok0 + 128, :], in_=G[:]), reads=[rB])

    for tb in range(8):
        do_block(tb)
    P.barrier()
def phase5s(P, nc, T, GT, GT_B, slots_all, gk_all, rt_B):
    P.sb_reset(T["arena5"])
    NS = CAP // 128
    CH = [(0, 512), (512, 512), (1024, 256)]
    NH_ = 512
    b1a = P.sb([128, 32, 16], F32, "b1a")
    identb = P.sb([128, 128], BF16, "identb5")
    toks = [P.dma("sp", lambda e: e.dma_start(out=b1a[:], in_=T["b_mlp1"])),
            P.dma("pool", lambda e: e.dma_start(out=identb[:], in_=T["ident"]))]
    for eng in ("pe", "act", "dve"):
        P.op(eng, None, extra=toks)
    wr_ = Ring([(P.sb([128, 8, 1024], BF16, "w1g"), P.sb([128, 8, 1024], BF16, "w1l"), P.sb([128, 8, 1024], BF16, "w2"),
                 P.sb([128, NS, 1024], BF16, "xs_tm"), Buf(), Buf(), Buf(), Buf()) for _ in range(2)])
    xsT = P.sb([128, 8, CAP], BF16, "xsT"); xsTB = Buf()
    actT = P.sb([128, 8, CAP], BF16, "actT"); aB = Buf()
    tmp_r = Ring([(P.sb([128, NH_], F32, "g5"), P.sb([128, NH_], F32, "s5"), P.sb([128, NH_], F32, "l5"), Buf()) for _ in range(2)])
    ys_r = Ring([(P.sb([128, 1024], F32, "ys"), Buf()) for _ in range(2)])
    psr = Ring([(T["ps"][i], T["psb"][i]) for i in range(8)])
    xs3 = T["xs_d"].rearrange("(e s p) d -> e p s d", p=128, s=NS)
    ys3 = T["ys_d"].rearrange("(e s p) d -> e s p d", p=128, s=NS)

    def load_w(e_):
        w1g, w1l, w2, xs_tm, gB, lB, wB, xB = wr_.next()
        P.dma("sp", lambda e: e.dma_start(out=xs_tm[:], in_=xs3[e_]), writes=[xB])
        for c in range(8):
            P.dma("pool", lambda e, c=c: e.dma_start(out=w1g[:, c, :], in_=T["w1g"][e_, c * 128:(c + 1) * 128, :]), writes=[gB])
            P.dma("pool", lambda e, c=c: e.dma_start(out=w1l[:, c, :], in_=T["w1l"][e_, c * 128:(c + 1) * 128, :]), writes=[lB])
        for c in range(8):
            P.dma("pool", lambda e, c=c: e.dma_start(out=w2[:, c, :], in_=T["w2"][e_, c * 128:(c + 1) * 128, :]), writes=[wB])
        return w1g, w1l, w2, xs_tm, gB, lB, wB, xB

    def tr_group(xs_tm, xB, k, s0, n):
        ps, psB = psr.next()
        psb16 = ps.bitcast(BF16)
        for i in range(n):
            P.op("pe", lambda e, i=i: e.transpose(psb16[:, i * 128:(i + 1) * 128], xs_tm[:, s0 + i, k * 128:(k + 1) * 128], identb[:]),
                 reads=[xB], writes=[psB])
        P.op("act", lambda e: e.activation(out=xsT[:, k, s0 * 128:(s0 + n) * 128], in_=psb16[:, 0:n * 128], func=AF.Identity),
             reads=[psB], writes=[xsTB])

    def mm1(e_, W, f, hf):
        w1g, w1l, w2, xs_tm, gB, lB, wB, xB = W
        c0, cn = CH[hf]
        sl = slice(c0, c0 + cn)
        pg, pgB = psr.next()
        pl, plB = psr.next()
        for k in range(8):
            P.op("pe", lambda e, k=k: e.matmul(pg[:, 0:cn], w1g[:, k, f * 128:(f + 1) * 128], xsT[:, k, sl],
                                               start=(k == 0), stop=(k == 7)), reads=[gB, xsTB], writes=[pgB])
        for k in range(8):
            P.op("pe", lambda e, k=k: e.matmul(pl[:, 0:cn], w1l[:, k, f * 128:(f + 1) * 128], xsT[:, k, sl],
                                               start=(k == 0), stop=(k == 7)), reads=[lB, xsTB], writes=[plB])
        g5, s5, l5, tB = tmp_r.next()
        P.op("act", lambda e: e.activation(out=l5[:, 0:cn], in_=pl[:, 0:cn], func=AF.Identity, bias=b1a[:, e_, 8 + f:9 + f], scale=1.0),
             reads=[plB], writes=[tB])
        P.op("dve", lambda e: e.tensor_scalar(out=g5[:, 0:cn], in0=pg[:, 0:cn], scalar1=b1a[:, e_, f:f + 1], scalar2=7.0,
                                              op0=ALU.add, op1=ALU.min), reads=[pgB], writes=[tB])
        P.op("act", lambda e: e.activation(out=s5[:, 0:cn], in_=g5[:, 0:cn], func=AF.Sigmoid, scale=1.702), reads=[tB], writes=[tB])
        P.op("dve", lambda e: e.tensor_scalar(out=l5[:, 0:cn], in0=l5[:, 0:cn], scalar1=-7.0, scalar2=7.0, op0=ALU.max, op1=ALU.min),
             reads=[tB], writes=[tB])
        P.op("dve", lambda e: e.tensor_tensor(out=g5[:, 0:cn], in0=g5[:, 0:cn], in1=s5[:, 0:cn], op=ALU.mult), reads=[tB], writes=[tB])
        P.op("dve", lambda e: e.scalar_tensor_tensor(out=actT[:, f, sl], in0=l5[:, 0:cn], scalar=1.0, in1=g5[:, 0:cn], op0=ALU.add, op1=ALU.mult),
             reads=[tB], writes=[aB])

    def mm2(e_, W, st):
        w1g, w1l, w2, xs_tm, gB, lB, wB, xB = W
        ys, yB = ys_r.next()
        for half in range(2):
            ps, psB = psr.next()
            for k in range(8):
                P.op("pe", lambda e, k=k, ps=ps, half=half: e.matmul(ps[:], actT[:, k, st * 128:(st + 1) * 128],
                                                                    w2[:, k, half * 512:(half + 1) * 512],
                                                                    start=(k == 0), stop=(k == 7)), reads=[aB, wB], writes=[psB])
            if half == 0:
                P.op("act", lambda e, ps=ps: e.activation(out=ys[:, 0:512], in_=ps[:], func=AF.Identity), reads=[psB], writes=[yB])
            else:
                P.op("dve", lambda e, ps=ps: e.tensor_copy(out=ys[:, 512:1024], in_=ps[:]), reads=[psB], writes=[yB])
        P.dma("sp", lambda e: e.dma_start(out=ys3[e_, st], in_=ys[:]), reads=[yB])

    NE = T.get("n_exp", 32)
    W = load_w(0)
    for e_ in range(NE):
        Wn = load_w(e_ + 1) if e_ + 1 < NE else None
        xs_tm, xB = W[3], W[7]
        for k in range(8):
            for s0 in range(0, NS, 4):
                tr_group(xs_tm, xB, k, s0, min(4, NS - s0))
        for f in range(8):
            for hf in range(len(CH)):
                mm1(e_, W, f, hf)
        for st in range(NS):
            mm2(e_, W, st)
        W = Wn
    P.barrier()

    P.sb_reset(T["arena5"])
    b2b = P.sb([32, 1024], BF16, "b2b")
    g2bc = P.sb([128, 1024], F32, "g2bc")
    b2bc = P.sb([128, 1024], F32, "b2bc")
    cB = Buf()
    toks = [P.dma("pool", lambda e: e.dma_start(out=b2b[:], in_=T["b_mlp2"])),
            P.dma("sp", lambda e: e.dma_start(out=g2bc[:], in_=T["ln2_g_bc"])),
            P.dma("sp", lambda e: e.dma_start(out=b2bc[:], in_=T["ln2_b_bc"]))]
    for eng in ("pe", "act", "dve"):
        P.op(eng, None, extra=toks)
    rows_r = Ring([(P.sb([128, 1024], F32, "rows"), Buf()) for _ in range(8)])
    x1_r = Ring([(P.sb([128, 1024], F32, "x1c"), Buf()) for _ in range(2)])
    acc_r = Ring([(P.sb([128, 1024], F32, "accc"), Buf()) for _ in range(2)])
    out_r = Ring([(P.sb([128, 1024], F32, "outc"), Buf()) for _ in range(2)])
    junk = P.sb([128, 1024], BF16, "junk6"); jB = Buf()
    sm_r = Ring([(P.sb([128, 8], F32, "sm6"), Buf()) for _ in range(2)])

    def comb_tile(ti):
        tok0 = ti * 128
        x1c, x1B = x1_r.next()
        P.dma("sp", lambda e: e.dma_start(out=x1c[:], in_=T["x1_d"][tok0:tok0 + 128, :]), writes=[x1B])
        acc, accB = acc_r.next()
        for half in range(2):
            ps, psB = psr.next()
            P.op("pe", lambda e, ps=ps, half=half: e.matmul(ps[:], GT[:, tok0:tok0 + 128], b2b[:, half * 512:(half + 1) * 512],
                                                            start=True, stop=True), reads=[GT_B], writes=[psB])
            P.op("dve", lambda e, ps=ps, half=half: e.scalar_tensor_tensor(out=acc[:, half * 512:(half + 1) * 512],
                                                                           in0=x1c[:, half * 512:(half + 1) * 512], scalar=ALPHA, in1=ps[:],
                                                                           op0=ALU.mult, op1=ALU.add), reads=[psB, x1B], writes=[accB])
        for k in range(4):
            rows, rwB = rows_r.next()
            P.dma("pool", lambda e, rows=rows, k=k: e.indirect_dma_start(
                out=rows[:], out_offset=None, in_=T["ys_d"],
                in_offset=bass.IndirectOffsetOnAxis(ap=slots_all[:, ti, k:k + 1], axis=0),
                bounds_check=None, oob_is_err=False), reads=[rt_B], writes=[rwB])
            P.op("dve", lambda e, rows=rows, k=k: e.scalar_tensor_tensor(out=acc[:], in0=rows[:], scalar=gk_all[:, ti, k:k + 1], in1=acc[:],
                                                                         op0=ALU.mult, op1=ALU.add), reads=[rwB, rt_B, accB], writes=[accB])
        o, oB = out_r.next()
        sm, sB = sm_r.next()
        layer_norm_tile(P, acc, accB, o, oB, g2bc, b2bc, cB, junk, jB, sm, sB)
        P.dma("sp", lambda e: e.dma_start(out=T["out"][tok0:tok0 + 128, :], in_=o[:]), reads=[oB])

    for ti in range(32):
        comb_tile(ti)
    P.barrier()
def build(upto=99, debug=False):
    nc = bass.Bass("TRN2", target_bir_lowering=False)
    T = {}

    def inp(name, shape, dt=F32):
        T[name] = nc.dram_tensor(name, list(shape), dt, kind="ExternalInput").ap()

    def scr(name, shape, dt=BF16):
        T[name] = nc.dram_tensor(name, list(shape), dt, kind=("ExternalOutput" if (debug and debug.get("dump_scr")) else "Internal")).ap()

    def outp(name, shape, dt=F32):
        T[name] = nc.dram_tensor(name, list(shape), dt, kind="ExternalOutput").ap()

    inp("xT", [1024, 8192]); inp("x_own", [4096, 1024])
    inp("w_fm", [1024, 3072]); inp("w_tm", [1024, 1024]); inp("b_fm", [128, 24]); inp("b_tm", [128, 1024])
    inp("cosT", [128, 8192]); inp("sinT", [128, 8192]); inp("lamv", [128, 256]); inp("subln_bc", [128, 128])
    scr("qT_da", [4, 128, 4096]); scr("kT_da", [4, 128, 8192]); scr("qT_na", [4, 128, 4096]); scr("kT_na", [4, 128, 8192])
    scr("v_da", [8192, 512]); scr("v_na", [8192, 512])
    if upto >= 3:
        inp("na_R", [8, 128, 18 * 256]); inp("na_M", [128, 18 * 256])
    if upto >= 4:
        inp("w_gate", [1024, 2048]); inp("b_gate", [128, 16]); inp("w_bda", [512, 1024]); inp("w_bna", [512, 1024])
        inp("w_out", [1024, 1024]); inp("w_router", [1024, 32]); inp("ln1_g_bc", [128, 1024]); inp("ln1_b_bc", [128, 1024])
        inp("b_router_bc", [128, 32]); inp("ident", [128, 128])
        scr("x1_d", [4096, 1024], F32); scr("x1T_d", [1024, 4096], BF16)
        inp("ltri", [128, 128]); inp("ones128", [128, 128]); inp("ecap", [128, 32])
        scr("xs_d", [NEXP * CAP, 1024], BF16); scr("ys_d", [NEXP * CAP, 1024], F32)
        if debug and debug.get("dump_scr"):
            outp("dbg_G", [4096, 32])
    if upto >= 5:
        inp("b_mlp1", [128, 32, 16]); inp("b_mlp2", [32, 1024])
        inp("ln2_g_bc", [128, 1024]); inp("ln2_b_bc", [128, 1024])
        inp("w1g", [32, 1024, 1024]); inp("w1l", [32, 1024, 1024]); inp("w2", [32, 1024, 1024])
        outp("out", [4096, 1024])
    T["ps"] = [nc.alloc_psum_tensor("ps%d" % i, [128, 512], F32) for i in range(8)]
    T["psb"] = [Buf() for _ in range(8)]
    P = Prog(nc)
    GT = P.sb([32, 4096], BF16, "GT")
    GT_B = Buf()
    slots_all = P.sb([128, 32, 4], I32, "slots_all")
    gk_all = P.sb([128, 32, 4], F32, "gk_all")
    rt_B = Buf()
    T["arena5"] = P.sb_off
    a_sb = P.sb([128, 32, 512], BF16, "a_sb")
    nb_sb = P.sb([128, 32, 512], BF16, "nb_sb")
    a_B, nb_B = Buf(), Buf()
    T["arena0"] = P.sb_off
    if debug:
        T["da_heads"] = debug.get("da_heads", 4)
        T["n_exp"] = debug.get("n_exp", 32)
    phase1(P, nc, T)
    if upto >= 2:
        phase2(P, nc, T, a_sb, a_B)
    if upto >= 3:
        phase3(P, nc, T, nb_sb, nb_B)
    if upto >= 4:
        phase4(P, nc, T, a_sb, a_B, nb_sb, nb_B, GT, GT_B, slots_all, gk_all, rt_B)
    if upto >= 5:
        phase5s(P, nc, T, GT, GT_B, slots_all, gk_all, rt_B)
    if upto < 4:
        outp("dbg_a", [4096, 512], BF16); outp("dbg_nb", [4096, 512], BF16)
        P.dma("sp", lambda e: e.dma_start(out=T["dbg_a"].rearrange("(t p) e -> p t e", p=128), in_=a_sb[:]), reads=[a_B])
        P.dma("sp", lambda e: e.dma_start(out=T["dbg_nb"].rearrange("(t p) e -> p t e", p=128), in_=nb_sb[:]), reads=[nb_B])
        if debug and debug.get("dump_scr"):
            for nm in ("qT_da", "kT_da", "v_da", "qT_na", "kT_na", "v_na"):
                pass
    P.barrier()
    P.emit()
    return nc, P


def rope_tables(pos):
    inv = (10000.0 ** (-np.arange(0, 64, 2, dtype=np.float32) / np.float32(64))).astype(np.float32)
    ang = pos.astype(np.float32)[:, None] * inv[None, :]
    ang = np.concatenate([ang, ang], axis=-1)
    cos = np.cos(ang).astype(np.float32)
    sin = np.sin(ang).astype(np.float32)
    sgn = np.concatenate([-np.ones(32, np.float32), np.ones(32, np.float32)])
    sin_s = sin * sgn[None, :]
    cosT = np.ascontiguousarray(np.concatenate([cos, cos], axis=1).T)
    sinT = np.ascontiguousarray(np.concatenate([sin_s, sin_s], axis=1).T)
    return cosT, sinT


def na_tables(rpb, h):
    R = np.zeros((8, 3, 6, 2, 64, 4, 64), np.float32)
    M = np.zeros((3, 6, 2, 64, 4, 64), np.float32)
    cc = np.arange(64)
    cs = np.clip(cc - 8, 0, 48)
    colvalid = (cc[:, None] >= cs[None, :]) & (cc[:, None] <= cs[None, :] + 15)
    coloff = np.clip(cc[:, None] - cc[None, :] + 15, 0, 30)
    for cls, g in ((0, 0), (1, 1 if h == 0 else 14), (2, 15)):
        for j in range(6):
            for a in range(2):
                for i in range(4):
                    r = 64 * h + 4 * g + i
                    kr = 64 * h + 4 * g + 2 * j - 4 + a
                    rs = min(max(r - 4, 0), 120)
                    if kr < rs or kr > rs + 7:
                        continue
                    M[cls, j, a, :, i, :] = colvalid
                    R[:, cls, j, a, :, i, :] = rpb[:, kr - r + 7][:, coloff] * colvalid[None]
    M2 = np.ascontiguousarray(M.transpose(2, 3, 0, 1, 4, 5).reshape(128, 18 * 256))
    R2 = np.ascontiguousarray(R.transpose(0, 3, 4, 1, 2, 5, 6).reshape(8, 128, 18 * 256))
    return R2, M2


def host_prep(inputs, upto=99):
    x = np.asarray(inputs["x"], np.float32)
    w_in = np.asarray(inputs["w_in"], np.float32)[0]
    b_in = np.asarray(inputs["b_in"], np.float32)[0]
    d = np.arange(64)
    swap = np.concatenate([(hh * 128 + c * 64 + (d + 32) % 64) for hh in range(4) for c in range(2)])
    qda, kda, vda = np.arange(0, 512), np.arange(512, 1024), np.arange(1024, 1536)
    qna, kna, vna = np.arange(1536, 2048), np.arange(2048, 2560), np.arange(2560, 3072)
    fm_cols = np.concatenate([qda, qda[swap], kda, kda[swap], qna, kna])
    tm_cols = np.concatenate([vda, vna])
    w_fm = np.ascontiguousarray(w_in[:, fm_cols])
    w_tm = np.ascontiguousarray(w_in[:, tm_cols])
    b_fm = np.ascontiguousarray(b_in[fm_cols].reshape(24, 128).T)
    b_tm = np.ascontiguousarray(np.broadcast_to(b_in[tm_cols][None, :], (128, 1024)))
    lamv = np.concatenate([np.asarray(inputs[k], np.float32)[0] for k in ("lambda_q1", "lambda_k1", "lambda_q2", "lambda_k2")])
    lamv = np.ascontiguousarray(np.broadcast_to(lamv[None, :], (128, 256)))
    subln_bc = np.ascontiguousarray(np.broadcast_to(np.asarray(inputs["subln_g"], np.float32)[0][None, :], (128, 128)))
    rpb = np.asarray(inputs["rpb"], np.float32)[0]
    shared = dict(w_fm=w_fm, w_tm=w_tm, b_fm=b_fm, b_tm=b_tm, lamv=lamv, subln_bc=subln_bc)
    f32 = lambda k: np.asarray(inputs[k], np.float32)[0]
    bc = lambda v, n=128: np.ascontiguousarray(np.broadcast_to(v[None, :], (n, v.shape[0])))
    if upto >= 4:
        shared.update(w_gate=np.ascontiguousarray(w_in[:, 3072:5120]), b_gate=np.ascontiguousarray(b_in[3072:5120].reshape(16, 128).T),
                      w_bda=f32("w_branch_da"), w_bna=f32("w_branch_na"), w_out=f32("w_out"), w_router=f32("w_router"),
                      ln1_g_bc=bc(f32("ln1_g")), ln1_b_bc=bc(f32("ln1_b")), b_router_bc=bc(f32("b_router")),
                      ident=np.eye(128, dtype=np.float32), ltri=np.triu(np.ones((128, 128), np.float32), k=1),
                      ones128=np.ones((128, 128), np.float32),
                      ecap=np.ascontiguousarray(np.broadcast_to((np.arange(32, dtype=np.float32) * CAP)[None, :], (128, 32))))
    if upto >= 5:
        w1 = f32("w_mlp1"); b1 = f32("b_mlp1")
        b1g = b1[:, 0::2].reshape(32, 8, 128).transpose(2, 0, 1)
        b1l = b1[:, 1::2].reshape(32, 8, 128).transpose(2, 0, 1)
        shared.update(b_mlp1=np.ascontiguousarray(np.concatenate([b1g, b1l], axis=2)),
                      b_mlp2=f32("b_mlp2"), ln2_g_bc=bc(f32("ln2_g")), ln2_b_bc=bc(f32("ln2_b")),
                      w1g=np.ascontiguousarray(w1[:, :, 0::2]), w1l=np.ascontiguousarray(w1[:, :, 1::2]), w2=f32("w_mlp2"))
    natab = [na_tables(rpb, h) for h in range(2)] if upto >= 3 else None
    maps = []
    for c in range(NCORES):
        b, h = c // 2, c % 2
        perm = np.concatenate([np.arange(h * 4096, (h + 1) * 4096), np.arange((1 - h) * 4096, (2 - h) * 4096)])
        xb = x[b]
        cosT, sinT = rope_tables(perm)
        m = dict(shared)
        m["xT"] = np.ascontiguousarray(xb[perm].T)
        m["x_own"] = np.ascontiguousarray(xb[h * 4096:(h + 1) * 4096])
        m["cosT"] = cosT
        m["sinT"] = sinT
        if upto >= 3:
            m["na_R"], m["na_M"] = natab[h]
        maps.append(m)
    return maps


def kernel(**inputs):
    from concourse.bass_utils import run_bass_kernel_spmd
    maps = host_prep(inputs)
    nc, P = build()
    res = run_bass_kernel_spmd(nc, maps, core_ids=list(range(NCORES)))
    out = np.zeros((4, SEQ, D), np.float32)
    for c in range(NCORES):
        b, h = c // 2, c % 2
        out[b, h * 4096:(h + 1) * 4096] = np.asarray(res.results[c]["out"], np.float32)
    return out
```

```python
import numpy as np
import concourse.bass as bass
import concourse.mybir as mybir

F32 = mybir.dt.float32
BF16 = mybir.dt.bfloat16
I32 = mybir.dt.int32
U32 = mybir.dt.uint32
AF = mybir.ActivationFunctionType
ALU = mybir.AluOpType
AX = mybir.AxisListType

ENGS = ("pe", "act", "dve", "pool", "sp")
KDMA = 8


class Tok:
    __slots__ = ("eng", "seq", "dma")

    def __init__(self, eng, seq, dma=None):
        self.eng = eng
        self.seq = seq
        self.dma = dma


class Buf:
    __slots__ = ("w", "r", "name")

    def __init__(self, name=""):
        self.w = None
        self.r = []
        self.name = name


class Prog:
    def __init__(self, nc):
        self.nc = nc
        self.ops = {e: [] for e in ENGS}
        self.ndma = {e: 0 for e in ENGS}
        self.all_dma = []
        self.sb_off = self.SB_BASE
        self.sb_hi = 0
        self.uid = 0

    SB_BASE = 16640
    SB_END = 229376

    def sb_reset(self, off=None):
        self.sb_off = self.SB_BASE if off is None else off

    def sb(self, shape, dtype, name=None):
        self.uid += 1
        nm = "%s_%d" % (name or "t", self.uid)
        nbytes = int(np.prod(shape[1:])) * mybir.dt.size(dtype)
        off = (self.sb_off + 63) // 64 * 64
        t = self.nc.alloc_sbuf_tensor_at(nm, list(shape), dtype, offset=off)
        self.sb_off = off + nbytes
        self.sb_hi = max(self.sb_hi, self.sb_off)
        assert self.sb_off <= self.SB_END, ("SBUF overflow", nm, self.sb_off)
        return t

    def _deps(self, reads, writes, extra):
        deps = {}
        for b in reads:
            if b.w is not None:
                deps[id(b.w)] = b.w
        for b in writes:
            if b.w is not None:
                deps[id(b.w)] = b.w
            for t in b.r:
                deps[id(t)] = t
        for t in extra:
            if t is not None:
                deps[id(t)] = t
        return list(deps.values())

    def _commit(self, tok, reads, writes):
        for b in writes:
            b.w = tok
            b.r = []
        for b in reads:
            if tok.dma is None:
                b.r = [t for t in b.r if not (t.dma is None and t.eng == tok.eng)]
            b.r.append(tok)

    def op(self, eng, fn, reads=(), writes=(), extra=()):
        deps = self._deps(reads, writes, extra)
        if eng == "pe":
            deps = [t for t in deps if not (t.eng == "pe" and t.dma is None)]
        tok = Tok(eng, len(self.ops[eng]))
        self.ops[eng].append(dict(fn=fn, waits=deps, tok=tok, dma=False))
        self._commit(tok, reads, writes)
        return tok

    def dma(self, eng, fn, reads=(), writes=(), extra=()):
        deps = self._deps(reads, writes, extra)
        i = self.ndma[eng]
        self.ndma[eng] += 1
        tok = Tok(eng, len(self.ops[eng]), dma=(i % KDMA, 16 * (i // KDMA + 1)))
        self.ops[eng].append(dict(fn=fn, waits=deps, tok=tok, dma=True, idx=i))
        self.all_dma.append(tok)
        self._commit(tok, reads, writes)
        return tok

    def barrier(self):
        lasts = []
        for e in ENGS:
            for o in reversed(self.ops[e]):
                if not o["dma"]:
                    lasts.append(o["tok"])
                    break
        dm = list(self.all_dma)
        self.all_dma = []
        for e in ENGS:
            if e == "sp":
                self.op(e, None, extra=lasts + dm)
            else:
                self.op(e, None, extra=lasts + dm)

    def emit(self):
        nc = self.nc
        needed = {e: set() for e in ENGS}
        for e in ENGS:
            for o in self.ops[e]:
                for t in o["waits"]:
                    if t.dma is None:
                        needed[t.eng].add(t.seq)
        tokval = {e: {} for e in ENGS}
        for e in ENGS:
            c = 0
            for o in self.ops[e]:
                if o["dma"]:
                    continue
                if o["tok"].seq in needed[e]:
                    c += 1
                    tokval[e][o["tok"].seq] = c
            self.maxcount = getattr(self, "maxcount", {})
            self.maxcount[e] = c
        engobj = {"pe": nc.tensor, "act": nc.scalar, "dve": nc.vector, "pool": nc.gpsimd, "sp": nc.sync}
        import contextlib
        with contextlib.ExitStack() as st:
            csem = {e: st.enter_context(nc.semaphore("c_" + e)) for e in ENGS}
            dsem = {e: [st.enter_context(nc.semaphore("d_%s%d" % (e, k))) for k in range(KDMA)]
                    for e in ENGS if self.ndma[e] > 0}
            block = st.enter_context(nc.Block())

            def run(e, engine):
                waited = {}
                for o in self.ops[e]:
                    for t in o["waits"]:
                        if t.dma is not None:
                            sem = dsem[t.eng][t.dma[0]]
                            val = t.dma[1]
                            key = ("d", t.eng, t.dma[0])
                        else:
                            sem = csem[t.eng]
                            val = tokval[t.eng][t.seq]
                            key = ("c", t.eng)
                        if waited.get(key, 0) >= val:
                            continue
                        waited[key] = val
                        engine.wait_ge(sem, val)
                    if o["dma"]:
                        i = o["idx"]
                        slot = i % KDMA
                        if i >= KDMA:
                            key = ("d", e, slot)
                            val = 16 * (i // KDMA)
                            if waited.get(key, 0) < val:
                                waited[key] = val
                                engine.wait_ge(dsem[e][slot], val)
                        ins = o["fn"](engine)
                        ins.then_inc(dsem[e][slot], 16)
                    else:
                        if o["fn"] is None:
                            if o["tok"].seq in needed[e]:
                                ins = engine.nop() if hasattr(engine, "nop") else None
                                ins.then_inc(csem[e], 1)
                            continue
                        ins = o["fn"](engine)
                        if o["tok"].seq in needed[e]:
                            ins.then_inc(csem[e], 1)

            @block.tensor
            def _(eng):
                run("pe", eng)

            @block.scalar
            def _(eng):
                run("act", eng)

            @block.vector
            def _(eng):
                run("dve", eng)

            @block.gpsimd
            def _(eng):
                run("pool", eng)

            @block.sync
            def _(eng):
                run("sp", eng)
D = 1024
SEQ = 8192
NOWN = 4096
NCORES = 8
CAP = 1280
NEXP = 32
LAM_INIT = 0.2
ALPHA = 2.0 ** 0.25


class Ring:
    def __init__(self, items):
        self.items = list(items)
        self.i = 0

    def next(self):
        it = self.items[self.i % len(self.items)]
        self.i += 1
        return it


def phase1(P, nc, T):
    P.sb_reset()
    wfm = P.sb([128, 8, 3072], BF16, "wfm")
    wtm = P.sb([128, 8, 1024], BF16, "wtm")
    bfm = P.sb([128, 24], F32, "bfm")
    btm = P.sb([128, 1024], F32, "btm")
    b_w = Buf()
    wtoks = []
    for c in range(8):
        for g in range(6):
            wtoks.append(P.dma("pool", lambda e, c=c, g=g: e.dma_start(
                out=wfm[:, c, g * 512:(g + 1) * 512], in_=T["w_fm"][c * 128:(c + 1) * 128, g * 512:(g + 1) * 512])))
        for g in range(2):
            wtoks.append(P.dma("pool", lambda e, c=c, g=g: e.dma_start(
                out=wtm[:, c, g * 512:(g + 1) * 512], in_=T["w_tm"][c * 128:(c + 1) * 128, g * 512:(g + 1) * 512])))
    wtoks.append(P.dma("sp", lambda e: e.dma_start(out=bfm[:], in_=T["b_fm"])))
    wtoks.append(P.dma("sp", lambda e: e.dma_start(out=btm[:], in_=T["b_tm"])))
    for eng in ("pe", "act", "dve"):
        P.op(eng, None, extra=wtoks)

    xblk = Ring([(P.sb([128, 8, 512], BF16, "xblk"), Buf()) for _ in range(2)])
    csb = Ring([(P.sb([128, 512], F32, "cos"), P.sb([128, 512], F32, "sin"), Buf()) for _ in range(2)])
    t1r = Ring([(P.sb([128, 512], F32, "t1"), Buf()) for _ in range(2)])
    t2r = Ring([(P.sb([128, 512], F32, "t2"), Buf()) for _ in range(2)])
    stg = Ring([(P.sb([128, 512], BF16, "stg"), Buf()) for _ in range(6)])
    psr = Ring([(T["ps"][i], T["psb"][i]) for i in range(8)])
    xT3 = T["xT"].rearrange("(c p) t -> p c t", p=128)

    def do_block(i):
        own = i < 8
        xb, xbB = xblk.next()
        P.dma("pool", lambda e, xb=xb, i=i: e.dma_start(out=xb[:], in_=xT3[:, :, i * 512:(i + 1) * 512]), writes=[xbB])
        cb, sb_, csB = csb.next()
        P.dma("sp", lambda e, cb=cb, i=i: e.dma_start(out=cb[:], in_=T["cosT"][:, i * 512:(i + 1) * 512]), writes=[csB])
        P.dma("sp", lambda e, sb_=sb_, i=i: e.dma_start(out=sb_[:], in_=T["sinT"][:, i * 512:(i + 1) * 512]), writes=[csB])

        def mm_fm(ps, psB, col):
            for k in range(8):
                P.op("pe", lambda e, k=k: e.matmul(ps[:], wfm[:, k, col:col + 128], xb[:, k, :],
                                                   start=(k == 0), stop=(k == 7)),
                     reads=[xbB], writes=[psB])

        def rope_tile(col0, col1, dst):
            psA, psAB = psr.next()
            psC, psCB = psr.next()
            mm_fm(psA, psAB, col0)
            mm_fm(psC, psCB, col1)
            t1, t1B = t1r.next()
            t2, t2B = t2r.next()
            P.op("dve", lambda e: e.scalar_tensor_tensor(out=t1[:], in0=psA[:], scalar=bfm[:, col0 // 128:col0 // 128 + 1],
                                                          in1=cb[:], op0=ALU.add, op1=ALU.mult),
                 reads=[psAB, csB], writes=[t1B])
            P.op("dve", lambda e: e.scalar_tensor_tensor(out=t2[:], in0=psC[:], scalar=bfm[:, col1 // 128:col1 // 128 + 1],
                                                          in1=sb_[:], op0=ALU.add, op1=ALU.mult),
                 reads=[psCB, csB], writes=[t2B])
            st, stB = stg.next()
            P.op("pool", lambda e: e.tensor_tensor(out=st[:], in0=t1[:], in1=t2[:], op=ALU.add),
                 reads=[t1B, t2B], writes=[stB])
            P.dma("sp", lambda e: e.dma_start(out=dst, in_=st[:]), reads=[stB])

        def plain_tile(col, dst):
            ps, psB = psr.next()
            mm_fm(ps, psB, col)
            st, stB = stg.next()
            P.op("act", lambda e: e.activation(out=st[:], in_=ps[:], func=AF.Identity,
                                               bias=bfm[:, col // 128:col // 128 + 1], scale=1.0),
                 reads=[psB], writes=[stB])
            P.dma("sp", lambda e: e.dma_start(out=dst, in_=st[:]), reads=[stB])

        sl = slice(i * 512, (i + 1) * 512)
        for h in range(4):
            if own:
                rope_tile(h * 128, 512 + h * 128, T["qT_da"][h, :, sl])
            rope_tile(1024 + h * 128, 1536 + h * 128, T["kT_da"][h, :, sl])
        na_kv = (i <= 8) or (i == 15)
        for j in range(4):
            if own:
                plain_tile(2048 + j * 128, T["qT_na"][j, :, sl])
            if na_kv:
                plain_tile(2560 + j * 128, T["kT_na"][j, :, sl])
        for tt in range(4):
            for g, dst in (((0, T["v_da"]), (1, T["v_na"])) if na_kv else ((0, T["v_da"]),)):
                ps, psB = psr.next()
                for k in range(8):
                    P.op("pe", lambda e, k=k, ps=ps, g=g, tt=tt: e.matmul(
                        ps[:], xb[:, k, tt * 128:(tt + 1) * 128], wtm[:, k, g * 512:(g + 1) * 512],
                        start=(k == 0), stop=(k == 7)), reads=[xbB], writes=[psB])
                st, stB = stg.next()
                P.op("dve", lambda e, ps=ps, st=st, g=g: e.tensor_tensor(out=st[:], in0=ps[:], in1=btm[:, g * 512:(g + 1) * 512],
                                                                         op=ALU.add), reads=[psB], writes=[stB])
                r0 = i * 512 + tt * 128
                P.dma("sp", lambda e, st=st, dst=dst, r0=r0: e.dma_start(out=dst[r0:r0 + 128, :], in_=st[:]), reads=[stB])

    for i in range(16):
        do_block(i)
    P.barrier()
def phase2(P, nc, T, a_sb, a_B):
    P.sb_reset(T["arena0"])
    lamv = P.sb([128, 256], F32, "lamv")
    gbc = P.sb([128, 128], F32, "gbc")
    tmp64 = P.sb([128, 128], F32, "tmp64")
    sc = P.sb([128, 8], F32, "sc")
    neglam = P.sb([128, 1], F32, "neglam")
    cB = Buf()
    P.dma("sp", lambda e: e.dma_start(out=lamv[:], in_=T["lamv"]), writes=[cB])
    P.dma("sp", lambda e: e.dma_start(out=gbc[:], in_=T["subln_bc"]), writes=[cB])
    P.op("dve", lambda e: e.tensor_tensor(out=tmp64[:, 0:64], in0=lamv[:, 0:64], in1=lamv[:, 64:128], op=ALU.mult), reads=[cB], writes=[cB])
    P.op("dve", lambda e: e.tensor_tensor(out=tmp64[:, 64:128], in0=lamv[:, 128:192], in1=lamv[:, 192:256], op=ALU.mult), reads=[cB], writes=[cB])
    P.op("dve", lambda e: e.reduce_sum(out=sc[:, 0:1], in_=tmp64[:, 0:64], axis=AX.X), reads=[cB], writes=[cB])
    P.op("dve", lambda e: e.reduce_sum(out=sc[:, 1:2], in_=tmp64[:, 64:128], axis=AX.X), reads=[cB], writes=[cB])
    P.op("act", lambda e: e.activation(out=sc[:, 2:4], in_=sc[:, 0:2], func=AF.Exp), reads=[cB], writes=[cB])
    P.op("dve", lambda e: e.tensor_tensor(out=sc[:, 4:5], in0=sc[:, 3:4], in1=sc[:, 2:3], op=ALU.subtract), reads=[cB], writes=[cB])
    P.op("dve", lambda e: e.tensor_scalar(out=neglam[:], in0=sc[:, 4:5], scalar1=-LAM_INIT, scalar2=None, op0=ALU.add), reads=[cB], writes=[cB])
    P.op("dve", lambda e: e.tensor_scalar(out=gbc[:], in0=gbc[:], scalar1=1.0 - LAM_INIT, scalar2=None, op0=ALU.mult), reads=[cB], writes=[cB])

    hb = []
    for _ in range(2):
        KT = P.sb([128, 8192], BF16, "KT")
        V = P.sb([128, 64, 129], BF16, "V")
        QT = P.sb([128, 2, 4096], BF16, "QT")
        oB = Buf()
        P.op("pool", lambda e, V=V: e.memset(V[:, :, 128:129], 1.0), writes=[oB])
        P.op("pool", lambda e, QT=QT: e.memset(QT[64:128, 0, :], 0.0), writes=[oB])
        P.op("pool", lambda e, QT=QT: e.memset(QT[0:64, 1, :], 0.0), writes=[oB])
        hb.append((KT, V, QT, Buf(), Buf(), Buf(), oB))
    ptr = Ring([(P.sb([128, 512], BF16, "pt"), Buf()) for _ in range(4)])
    sps = Ring([(T["ps"][i], T["psb"][i]) for i in range(4, 8)])
    accs = {}
    lay = [(0, 0), (0, 1), (0, 2), (1, 0), (2, 0), (2, 1), (2, 2), (3, 0)]
    n = 0
    for c in range(2):
        for qt in range(4):
            bk, pos = lay[n]
            n += 1
            accs[(c, qt)] = (T["ps"][bk][:, pos * 129:(pos + 1) * 129], T["psb"][bk])
    small = Ring([(P.sb([128, 8], F32, "sm"), P.sb([128, 128], F32, "tt"), P.sb([128, 128], F32, "oo"),
                   P.sb([128, 128], F32, "sq"), Buf()) for _ in range(3)])
    v_da3 = T["v_da"].rearrange("(t p) e -> p t e", p=128)

    def load_head(h):
        KT, V, QT, kB, vB, qB, oB = hb[h % 2]
        for s4 in range(4):
            P.dma("sp", lambda e, s4=s4: e.dma_start(out=KT[:, s4 * 2048:(s4 + 1) * 2048],
                                                      in_=T["kT_da"][h, :, s4 * 2048:(s4 + 1) * 2048]), writes=[kB])
        for s8 in range(8):
            P.dma("sp", lambda e, s8=s8: e.dma_start(out=V[:, s8 * 8:(s8 + 1) * 8, 0:128],
                                                      in_=v_da3[:, s8 * 8:(s8 + 1) * 8, h * 128:(h + 1) * 128]), writes=[vB])
        P.dma("sp", lambda e: e.dma_start(out=QT[0:64, 0, :], in_=T["qT_da"][h, 0:64, :]), writes=[qB])
        P.dma("sp", lambda e: e.dma_start(out=QT[64:128, 1, :], in_=T["qT_da"][h, 64:128, :]), writes=[qB])

    def epilogue(h, qb, qt):
        O1, O1B = accs[(0, qt)]
        O2, O2B = accs[(1, qt)]
        sm, tt, oo, sq, sB = small.next()
        P.op("dve", lambda e: e.reciprocal(out=sm[:, 0:1], in_=O1[:, 128:129]), reads=[O1B], writes=[sB])
        P.op("dve", lambda e: e.reciprocal(out=sm[:, 1:2], in_=O2[:, 128:129]), reads=[O2B], writes=[sB])
        P.op("dve", lambda e: e.tensor_tensor(out=sm[:, 2:3], in0=sm[:, 1:2], in1=neglam[:], op=ALU.mult), reads=[sB, cB], writes=[sB])
        P.op("dve", lambda e: e.tensor_scalar(out=tt[:], in0=O2[:, 0:128], scalar1=sm[:, 2:3], scalar2=None, op0=ALU.mult),
             reads=[O2B, sB], writes=[sB])
        P.op("dve", lambda e: e.scalar_tensor_tensor(out=oo[:], in0=O1[:, 0:128], scalar=sm[:, 0:1], in1=tt[:],
                                                      op0=ALU.mult, op1=ALU.add), reads=[O1B, sB], writes=[sB])
        P.op("act", lambda e: e.activation(out=sq[:], in_=oo[:], func=AF.Square, accum_out=sm[:, 3:4]), reads=[sB], writes=[sB])
        P.op("act", lambda e: e.activation(out=sm[:, 4:5], in_=sm[:, 3:4], func=AF.Ln, scale=1.0 / 128.0, bias=1e-5),
             reads=[sB], writes=[sB])
        P.op("act", lambda e: e.activation(out=sm[:, 5:6], in_=sm[:, 4:5], func=AF.Exp, scale=-0.5), reads=[sB], writes=[sB])
        P.op("dve", lambda e: e.scalar_tensor_tensor(out=a_sb[:, qb * 4 + qt, h * 128:(h + 1) * 128], in0=oo[:], scalar=sm[:, 5:6],
                                                      in1=gbc[:], op0=ALU.mult, op1=ALU.mult), reads=[sB, cB], writes=[a_B])

    def qk(h, qb, c, kt):
        KT, V, QT, kB, vB, qB, oB = hb[h % 2]
        S, SB = sps.next()
        P.op("pe", lambda e: e.matmul(S[:], KT[:, kt * 128:(kt + 1) * 128],
                                      QT[:, c, qb * 512:(qb + 1) * 512],
                                      start=True, stop=True), reads=[kB, qB, oB], writes=[SB])
        Pt, PtB = ptr.next()
        P.op("act", lambda e: e.activation(out=Pt[:], in_=S[:], func=AF.Exp, scale=0.125),
             reads=[SB], writes=[PtB])
        return Pt, PtB

    def pv(h, qb, c, kt, Pt, PtB):
        KT, V, QT, kB, vB, qB, oB = hb[h % 2]
        for qt in range(4):
            O, OB = accs[(c, qt)]
            P.op("pe", lambda e, O=O, qt=qt: e.matmul(O, Pt[:, qt * 128:(qt + 1) * 128], V[:, kt, :],
                                                      start=(kt == 0 and qt in (0, 3)), stop=(kt == 63), skip_group_check=True),
                 reads=[PtB, vB, oB], writes=[OB])
        if c == 1 and kt == 63:
            for qt in range(4):
                epilogue(h, qb, qt)

    iters = [(h, qb, c, kt) for h in range(T.get("da_heads", 4)) for qb in range(8) for c in range(2) for kt in range(64)]
    LA = 2
    pend = {}
    NH = T.get("da_heads", 4)
    load_head(0)
    if NH > 1:
        load_head(1)
    for n in range(len(iters) + LA):
        if n < len(iters):
            h, qb, c, kt = iters[n]
            pend[n] = qk(h, qb, c, kt)
        m = n - LA
        if m >= 0:
            h, qb, c, kt = iters[m]
            Pt, PtB = pend.pop(m)
            pv(h, qb, c, kt, Pt, PtB)
            if qb == 7 and c == 1 and kt == 63 and h + 2 < NH:
                load_head(h + 2)
    P.barrier()
def phase3(P, nc, T, nb_sb, nb_B):
    P.sb_reset(T["arena0"])
    if T.get("xs_d") is not None:
        zt = P.sb([128, CAP // 128, 1024], BF16, "zt")
        zB = Buf()
        P.op("pool", lambda e: e.memset(zt[:], 0.0), writes=[zB])
        xs3 = T["xs_d"].rearrange("(e s p) d -> e p s d", p=128, s=CAP // 128)
        for e_ in range(NEXP):
            P.dma("sp", lambda e, e_=e_: e.dma_start(out=xs3[e_], in_=zt[:]), reads=[zB])
    Mt = P.sb([128, 18 * 256], BF16, "Mt")
    mB = Buf()
    for s in range(3):
        P.dma("pool", lambda e, s=s: e.dma_start(out=Mt[:, s * 1536:(s + 1) * 1536], in_=T["na_M"][:, s * 1536:(s + 1) * 1536]), writes=[mB])
    Rt = P.sb([128, 18 * 256], F32, "Rt")
    rB = Buf()
    hb = []
    for _ in range(2):
        KT = P.sb([64, 4608], BF16, "KTn")
        V = P.sb([128, 36, 65], BF16, "Vn")
        QT = P.sb([64, 4096], BF16, "QTn")
        E = P.sb([128, 18 * 256], BF16, "En")
        oB = Buf()
        P.op("pool", lambda e, V=V: e.memset(V[:, :, 64:65], 1.0), writes=[oB])
        hb.append((KT, V, QT, E, Buf(), Buf(), Buf(), Buf(), oB))
    per = Ring([(P.sb([128, 256], BF16, "pe_"), Buf()) for _ in range(6)])
    pmr = Ring([(P.sb([128, 256], BF16, "pm_"), Buf()) for _ in range(6)])
    sps = Ring([(T["ps"][i], T["psb"][i]) for i in range(2, 8)])
    accs = [(T["ps"][0][:, 0:65], T["psb"][0]), (T["ps"][0][:, 128:193], T["psb"][0]),
            (T["ps"][1][:, 0:65], T["psb"][1]), (T["ps"][1][:, 128:193], T["psb"][1])]
    smr = Ring([(P.sb([128, 2], F32, "smn"), Buf()) for _ in range(4)])
    v_na3 = T["v_na"].rearrange("(t p) e -> p t e", p=128)

    def load_head(hn):
        KT, V, QT, E, kB, vB, qB, eB, oB = hb[hn % 2]
        j, po = hn // 2, (hn % 2) * 64
        P.dma("sp", lambda e: e.dma_start(out=KT[:, 0:256], in_=T["kT_na"][j, po:po + 64, 7936:8192]), writes=[kB])
        P.dma("sp", lambda e: e.dma_start(out=KT[:, 256:4608], in_=T["kT_na"][j, po:po + 64, 0:4352]), writes=[kB])
        P.dma("sp", lambda e: e.dma_start(out=QT[:], in_=T["qT_na"][j, po:po + 64, :]), writes=[qB])
        P.dma("sp", lambda e: e.dma_start(out=V[:, 0:2, 0:64], in_=v_na3[:, 62:64, hn * 64:(hn + 1) * 64]), writes=[vB])
        for s in range(2):
            P.dma("sp", lambda e, s=s: e.dma_start(out=V[:, 2 + s * 17:2 + (s + 1) * 17, 0:64],
                                                    in_=v_na3[:, s * 17:(s + 1) * 17, hn * 64:(hn + 1) * 64]), writes=[vB])
        P.dma("sp", lambda e: e.dma_start(out=Rt[:], in_=T["na_R"][hn, :, :]), writes=[rB])
        for s in range(3):
            sl = slice(s * 1536, (s + 1) * 1536)
            P.op("act", lambda e, sl=sl: e.activation(out=Rt[:, sl], in_=Rt[:, sl], func=AF.Exp), reads=[rB], writes=[rB])
            P.op("dve", lambda e, sl=sl: e.tensor_tensor(out=E[:, sl], in0=Rt[:, sl], in1=Mt[:, sl], op=ALU.mult),
                 reads=[rB, mB], writes=[eB])

    def qk(hn, g, j):
        KT, V, QT, E, kB, vB, qB, eB, oB = hb[hn % 2]
        S, SB = sps.next()
        ti = 2 * g + j
        P.op("pe", lambda e: e.matmul(S[:, 0:256], KT[:, ti * 128:(ti + 1) * 128], QT[:, g * 256:(g + 1) * 256],
                                      start=True, stop=True), reads=[kB, qB], writes=[SB])
        Pe, PeB = per.next()
        P.op("act", lambda e: e.activation(out=Pe[:], in_=S[:, 0:256], func=AF.Exp, scale=0.125), reads=[SB], writes=[PeB])
        Pm, PmB = pmr.next()
        cls = 0 if g == 0 else (2 if g == 15 else 1)
        ei = (cls * 6 + j) * 256
        P.op("dve", lambda e: e.tensor_tensor(out=Pm[:], in0=Pe[:], in1=E[:, ei:ei + 256], op=ALU.mult),
             reads=[PeB, eB], writes=[PmB])
        return Pm, PmB

    def pv(hn, g, j, Pm, PmB):
        KT, V, QT, E, kB, vB, qB, eB, oB = hb[hn % 2]
        ti = 2 * g + j
        for qt in range(2):
            O, OB = accs[(g % 2) * 2 + qt]
            P.op("pe", lambda e, O=O, qt=qt: e.matmul(O, Pm[:, qt * 128:(qt + 1) * 128], V[:, ti, :],
                                                      start=(j == 0 and qt == 0), stop=(j == 5), skip_group_check=True), reads=[PmB, vB, oB], writes=[OB])
        if j == 5:
            for qt in range(2):
                O, OB = accs[(g % 2) * 2 + qt]
                sm, sB = smr.next()
                P.op("dve", lambda e, O=O, sm=sm: e.reciprocal(out=sm[:, 0:1], in_=O[:, 64:65]), reads=[OB], writes=[sB])
                P.op("dve", lambda e, O=O, sm=sm, qt=qt: e.tensor_scalar(
                    out=nb_sb[:, g * 2 + qt, hn * 64:(hn + 1) * 64], in0=O[:, 0:64], scalar1=sm[:, 0:1], scalar2=None,
                    op0=ALU.mult), reads=[OB, sB], writes=[nb_B])

    iters = [(hn, g, j) for hn in range(T.get("na_heads", 8)) for g in range(16) for j in range(6)]
    LA = 4
    pend = {}
    NH = T.get("na_heads", 8)
    load_head(0)
    if NH > 1:
        load_head(1)
    for n in range(len(iters) + LA):
        if n < len(iters):
            hn, g, j = iters[n]
            pend[n] = qk(hn, g, j)
        m = n - LA
        if m >= 0:
            hn, g, j = iters[m]
            Pm, PmB = pend.pop(m)
            pv(hn, g, j, Pm, PmB)
            if g == 15 and j == 5 and hn + 2 < NH:
                load_head(hn + 2)
    P.barrier()
def layer_norm_tile(P, y, yB, out, outB, gbc, bbc, cB, junk, jB, sm, sB):
    class _W:
        def __init__(self, t):
            self.t = t

        def __getitem__(self, k):
            return self.t if isinstance(self.t, bass.AP) else self.t[k]
    y = _W(y)
    out = _W(out)
    junk = _W(junk)
    P.op("act", lambda e: e.activation(out=junk[:], in_=y[:], func=AF.Identity, accum_out=sm[:, 0:1]), reads=[yB], writes=[jB, sB])
    P.op("act", lambda e: e.activation(out=junk[:], in_=y[:], func=AF.Square, accum_out=sm[:, 1:2]), reads=[yB], writes=[jB, sB])
    P.op("dve", lambda e: e.tensor_scalar(out=sm[:, 2:3], in0=sm[:, 0:1], scalar1=1.0 / 1024.0, scalar2=None, op0=ALU.mult), reads=[sB], writes=[sB])
    P.op("dve", lambda e: e.tensor_tensor(out=sm[:, 3:4], in0=sm[:, 2:3], in1=sm[:, 2:3], op=ALU.mult), reads=[sB], writes=[sB])
    P.op("dve", lambda e: e.scalar_tensor_tensor(out=sm[:, 4:5], in0=sm[:, 1:2], scalar=1.0 / 1024.0, in1=sm[:, 3:4],
                                                  op0=ALU.mult, op1=ALU.subtract), reads=[sB], writes=[sB])
    P.op("act", lambda e: e.activation(out=sm[:, 5:6], in_=sm[:, 4:5], func=AF.Ln, scale=1.0, bias=1e-5), reads=[sB], writes=[sB])
    P.op("act", lambda e: e.activation(out=sm[:, 6:7], in_=sm[:, 5:6], func=AF.Exp, scale=-0.5), reads=[sB], writes=[sB])
    P.op("dve", lambda e: e.tensor_scalar(out=out[:], in0=y[:], scalar1=sm[:, 2:3], scalar2=sm[:, 6:7], op0=ALU.subtract, op1=ALU.mult),
         reads=[yB, sB], writes=[outB])
    P.op("dve", lambda e: e.tensor_tensor(out=out[:], in0=out[:], in1=gbc[:], op=ALU.mult), reads=[outB, cB], writes=[outB])
    P.op("dve", lambda e: e.tensor_tensor(out=out[:], in0=out[:], in1=bbc[:], op=ALU.add), reads=[outB, cB], writes=[outB])


def phase4(P, nc, T, a_sb, a_B, nb_sb, nb_B, GT, GT_B, slots_all, gk_all, rt_B):
    P.sb_reset(T["arena0"])
    wg = P.sb([128, 8, 2048], BF16, "wg")
    wbd = P.sb([128, 4, 1024], BF16, "wbd")
    wbn = P.sb([128, 4, 1024], BF16, "wbn")
    wo = P.sb([128, 8, 1024], BF16, "wo")
    wr = P.sb([128, 8, 32], F32, "wr")
    bg = P.sb([128, 16], F32, "bg")
    g1bc = P.sb([128, 1024], F32, "g1bc")
    b1bc = P.sb([128, 1024], F32, "b1bc")
    brbc = P.sb([128, 32], F32, "brbc")
    identb = P.sb([128, 128], BF16, "identb")
    ltri = P.sb([128, 128], BF16, "ltri")
    ones = P.sb([128, 128], BF16, "ones")
    ecap = P.sb([128, 32], F32, "ecap")
    cnt = P.sb([128, 32], F32, "cnt")
    cntB = Buf()
    P.op("pool", lambda e: e.memset(cnt[:], 0.0), writes=[cntB])
    identf = P.sb([128, 128], F32, "identf")
    cB = Buf()
    toks = []
    for c in range(8):
        for g in range(4):
            toks.append(P.dma("pool", lambda e, c=c, g=g: e.dma_start(out=wg[:, c, g * 512:(g + 1) * 512],
                                                                      in_=T["w_gate"][c * 128:(c + 1) * 128, g * 512:(g + 1) * 512])))
        toks.append(P.dma("pool", lambda e, c=c: e.dma_start(out=wo[:, c, :], in_=T["w_out"][c * 128:(c + 1) * 128, :])))
        toks.append(P.dma("sp", lambda e, c=c: e.dma_start(out=wr[:, c, :], in_=T["w_router"][c * 128:(c + 1) * 128, :])))
    for c in range(4):
        toks.append(P.dma("pool", lambda e, c=c: e.dma_start(out=wbd[:, c, :], in_=T["w_bda"][c * 128:(c + 1) * 128, :])))
        toks.append(P.dma("pool", lambda e, c=c: e.dma_start(out=wbn[:, c, :], in_=T["w_bna"][c * 128:(c + 1) * 128, :])))
    for dst, src in ((bg, "b_gate"), (g1bc, "ln1_g_bc"), (b1bc, "ln1_b_bc"), (brbc, "b_router_bc"), (identf, "ident")):
        toks.append(P.dma("sp", lambda e, dst=dst, src=src: e.dma_start(out=dst[:], in_=T[src])))
    toks.append(P.dma("pool", lambda e: e.dma_start(out=identb[:], in_=T["ident"])))
    toks.append(P.dma("pool", lambda e: e.dma_start(out=ltri[:], in_=T["ltri"])))
    toks.append(P.dma("pool", lambda e: e.dma_start(out=ones[:], in_=T["ones128"])))
    toks.append(P.dma("sp", lambda e: e.dma_start(out=ecap[:], in_=T["ecap"])))
    for eng in ("pe", "act", "dve", "pool"):
        P.op(eng, None, extra=toks)

    xblk = Ring([(P.sb([128, 8, 512], BF16, "xblk4"), Buf()) for _ in range(1)])
    aT = P.sb([128, 4, 512], BF16, "aT"); aTB = Buf()
    nT = P.sb([128, 4, 512], BF16, "nT"); nTB = Buf()
    g_r = Ring([(P.sb([128, 2, 512], BF16, "g01"), Buf()) for _ in range(2)])
    mT = P.sb([128, 8, 512], BF16, "mT"); mTB = Buf()
    tr = Ring([(P.sb([128, 512], F32, "t4"), P.sb([128, 512], F32, "u4"), Buf()) for _ in range(1)])
    y_r = Ring([(P.sb([128, 1024], F32, "y4"), Buf()) for _ in range(2)])
    x1_r = Ring([(P.sb([128, 1024], F32, "x1"), Buf()) for _ in range(2)])
    junk = P.sb([128, 1024], BF16, "junk"); jB = Buf()
    sm_r = Ring([(P.sb([128, 8], F32, "sm4"), Buf()) for _ in range(2)])
    x1Tf_r = Ring([(P.sb([128, 1024], F32, "x1Tf"), None, Buf()) for _ in range(1)])
    rt_r = Ring([(P.sb([128, 32], F32, "lg"), P.sb([128, 8], F32, "t8"), P.sb([128, 32], F32, "mk"), P.sb([128, 32], F32, "ex"),
                  P.sb([128, 4], F32, "rs"), P.sb([128, 32], F32, "G"), Buf(),
                  P.sb([128, 32], BF16, "mkb"), P.sb([128, 32], F32, "rk"), P.sb([128, 32], F32, "tm"), P.sb([128, 8], F32, "slf")) for _ in range(2)])
    psr = Ring([(T["ps"][i], T["psb"][i]) for i in range(8)])
    xT3 = T["xT"].rearrange("(c p) t -> p c t", p=128)

    def do_block(tb):
        xb, xbB = xblk.next()
        P.dma("pool", lambda e: e.dma_start(out=xb[:], in_=xT3[:, :, tb * 512:(tb + 1) * 512]), writes=[xbB])
        for src, sB_, dst, dB in ((a_sb, a_B, aT, aTB), (nb_sb, nb_B, nT, nTB)):
            for hc in range(4):
                ps, psB = psr.next()
                psb16 = ps.bitcast(BF16)
                for tt in range(4):
                    P.op("pe", lambda e, tt=tt, psb16=psb16, src=src, hc=hc: e.transpose(
                        psb16[:, tt * 128:(tt + 1) * 128], src[:, tb * 4 + tt, hc * 128:(hc + 1) * 128], identb[:]),
                        reads=[sB_], writes=[psB])
                P.op("act", lambda e, psb16=psb16, dst=dst, hc=hc: e.activation(out=dst[:, hc, :], in_=psb16[:, 0:512], func=AF.Identity),
                     reads=[psB], writes=[dB])
        for dt in range(8):
            g01, g01B = g_r.next()
            for gi, j in enumerate((dt, 8 + dt)):
                ps, psB = psr.next()
                for k in range(8):
                    P.op("pe", lambda e, k=k, ps=ps, j=j: e.matmul(ps[:], wg[:, k, j * 128:(j + 1) * 128], xb[:, k, :],
                                                                   start=(k == 0), stop=(k == 7)), reads=[xbB], writes=[psB])
                P.op("act", lambda e, ps=ps, j=j, gi=gi, g01=g01: e.activation(out=g01[:, gi, :], in_=ps[:], func=AF.Sigmoid,
                                                                              bias=bg[:, j:j + 1], scale=1.0),
                     reads=[psB], writes=[g01B])
            psa, psaB = psr.next()
            psn, psnB = psr.next()
            for ec in range(4):
                P.op("pe", lambda e, ec=ec, psa=psa, dt=dt: e.matmul(psa[:], wbd[:, ec, dt * 128:(dt + 1) * 128], aT[:, ec, :],
                                                                    start=(ec == 0), stop=(ec == 3)), reads=[aTB], writes=[psaB])
            for ec in range(4):
                P.op("pe", lambda e, ec=ec, psn=psn, dt=dt: e.matmul(psn[:], wbn[:, ec, dt * 128:(dt + 1) * 128], nT[:, ec, :],
                                                                    start=(ec == 0), stop=(ec == 3)), reads=[nTB], writes=[psnB])
            t4, u4, tB = tr.next()
            P.op("dve", lambda e, t4=t4, psa=psa, g01=g01: e.tensor_tensor(out=t4[:], in0=psa[:], in1=g01[:, 0, :], op=ALU.mult),
                 reads=[psaB, g01B], writes=[tB])
            P.op("dve", lambda e, u4=u4, psn=psn, g01=g01: e.tensor_tensor(out=u4[:], in0=psn[:], in1=g01[:, 1, :], op=ALU.mult),
                 reads=[psnB, g01B], writes=[tB])
            P.op("pool", lambda e, t4=t4, u4=u4, dt=dt: e.tensor_tensor(out=mT[:, dt, :], in0=t4[:], in1=u4[:], op=ALU.add),
                 reads=[tB], writes=[mTB])
        def stageA(tt):
            tok0 = tb * 512 + tt * 128
            y, yB = y_r.next()
            P.dma("sp", lambda e: e.dma_start(out=y[:], in_=T["x_own"][tok0:tok0 + 128, :]), writes=[yB])
            for half in range(2):
                ps, psB = psr.next()
                for k in range(8):
                    P.op("pe", lambda e, k=k, ps=ps, half=half: e.matmul(
                        ps[:], mT[:, k, tt * 128:(tt + 1) * 128], wo[:, k, half * 512:(half + 1) * 512],
                        start=(k == 0), stop=(k == 7)), reads=[mTB], writes=[psB])
                P.op("dve", lambda e, ps=ps, half=half: e.scalar_tensor_tensor(
                    out=y[:, half * 512:(half + 1) * 512], in0=y[:, half * 512:(half + 1) * 512], scalar=ALPHA, in1=ps[:],
                    op0=ALU.mult, op1=ALU.add), reads=[psB, yB], writes=[yB])
            x1, x1B = x1_r.next()
            sm, sB = sm_r.next()
            layer_norm_tile(P, y, yB, x1, x1B, g1bc, b1bc, cB, junk, jB, sm, sB)
            P.dma("sp", lambda e: e.dma_start(out=T["x1_d"][tok0:tok0 + 128, :], in_=x1[:]), reads=[x1B])
            return x1, x1B, tok0

        def stageB(tt, x1, x1B, tok0):
            x1Tf, x1Tb, xTB = x1Tf_r.next()
            for k2 in range(2):
                ps, psB = psr.next()
                for k4 in range(4):
                    k = k2 * 4 + k4
                    P.op("pe", lambda e, ps=ps, k=k, k4=k4, x1=x1: e.transpose(ps[:, k4 * 128:(k4 + 1) * 128], x1[:, k * 128:(k + 1) * 128],
                                                                        identf[:]), reads=[x1B], writes=[psB])
                P.op("act", lambda e, ps=ps, x1Tf=x1Tf, k2=k2: e.activation(out=x1Tf[:, k2 * 512:(k2 + 1) * 512], in_=ps[:], func=AF.Identity),
                     reads=[psB], writes=[xTB])
            ps, psB = psr.next()
            for k in range(8):
                P.op("pe", lambda e, ps=ps, k=k, x1Tf=x1Tf: e.matmul(ps[:, 0:32], x1Tf[:, k * 128:(k + 1) * 128], wr[:, k, :], start=(k == 0), stop=(k == 7)),
                     reads=[xTB], writes=[psB])
            lg, t8, mk, ex, rs, G, rB, mkb, rk, tm, slf = rt_r.next()
            P.op("dve", lambda e, ps=ps, lg=lg: e.tensor_tensor(out=lg[:], in0=ps[:, 0:32], in1=brbc[:], op=ALU.add), reads=[psB], writes=[rB])
            P.op("dve", lambda e, lg=lg, t8=t8: e.max(out=t8[:], in_=lg[:]), reads=[rB], writes=[rB])
            P.op("dve", lambda e, lg=lg, t8=t8, mk=mk: e.tensor_scalar(out=mk[:], in0=lg[:], scalar1=t8[:, 3:4], scalar2=None, op0=ALU.is_ge),
                 reads=[rB], writes=[rB])
            P.op("dve", lambda e, t8=t8, rs=rs: e.tensor_scalar(out=rs[:, 0:1], in0=t8[:, 0:1], scalar1=-1.0, scalar2=None, op0=ALU.mult),
                 reads=[rB], writes=[rB])
            P.op("act", lambda e, lg=lg, ex=ex, rs=rs: e.activation(out=ex[:], in_=lg[:], func=AF.Exp, bias=rs[:, 0:1], scale=1.0),
                 reads=[rB], writes=[rB])
            P.op("dve", lambda e, ex=ex, mk=mk: e.tensor_tensor(out=ex[:], in0=ex[:], in1=mk[:], op=ALU.mult), reads=[rB], writes=[rB])
            P.op("dve", lambda e, ex=ex, rs=rs: e.reduce_sum(out=rs[:, 1:2], in_=ex[:], axis=AX.X), reads=[rB], writes=[rB])
            P.op("dve", lambda e, rs=rs: e.reciprocal(out=rs[:, 2:3], in_=rs[:, 1:2]), reads=[rB], writes=[rB])
            P.op("dve", lambda e, ex=ex, rs=rs, G=G: e.tensor_scalar(out=G[:], in0=ex[:], scalar1=rs[:, 2:3], scalar2=None, op0=ALU.mult),
                 reads=[rB], writes=[rB])
            ps2, ps2B = psr.next()
            P.op("pe", lambda e, ps2=ps2, G=G: e.transpose(ps2[0:32, 0:128], G[:], identf[:]), reads=[rB], writes=[ps2B])
            P.op("act", lambda e, ps2=ps2, tok0=tok0: e.activation(out=GT[:, tok0:tok0 + 128], in_=ps2[0:32, 0:128], func=AF.Identity),
                 reads=[ps2B], writes=[GT_B])
            tix = tb * 4 + tt
            P.op("dve", lambda e, mk=mk, mkb=mkb: e.tensor_copy(out=mkb[:], in_=mk[:]), reads=[rB], writes=[rB])
            ps3, ps3B = psr.next()
            P.op("pe", lambda e, ps3=ps3, mkb=mkb: e.matmul(ps3[:, 0:32], ltri[:], mkb[:], start=True, stop=True, skip_group_check=True),
                 reads=[rB], writes=[ps3B])
            P.op("pe", lambda e, ps3=ps3, mkb=mkb: e.matmul(ps3[:, 64:96], ones[:], mkb[:], start=False, stop=True, skip_group_check=True),
                 reads=[rB], writes=[ps3B])
            P.op("dve", lambda e, ps3=ps3, rk=rk: e.tensor_tensor(out=rk[:], in0=ps3[:, 0:32], in1=cnt[:], op=ALU.add), reads=[ps3B, cntB], writes=[rB])
            P.op("dve", lambda e, ps3=ps3: e.tensor_tensor(out=cnt[:], in0=cnt[:], in1=ps3[:, 64:96], op=ALU.add), reads=[ps3B, cntB, rB], writes=[cntB])
            P.op("dve", lambda e, rk=rk: e.scalar_tensor_tensor(out=rk[:], in0=rk[:], scalar=float(CAP - 1), in1=ecap[:], op0=ALU.min, op1=ALU.add),
                 reads=[rB], writes=[rB])
            for k in range(4):
                P.op("dve", lambda e, k=k, lg=lg, t8=t8, rk=rk, tm=tm: e.scalar_tensor_tensor(out=tm[:], in0=lg[:], scalar=t8[:, k:k + 1], in1=rk[:],
                                                                                             op0=ALU.is_equal, op1=ALU.mult), reads=[rB], writes=[rB])
                P.op("dve", lambda e, k=k, tm=tm, slf=slf: e.reduce_sum(out=slf[:, k:k + 1], in_=tm[:], axis=AX.X), reads=[rB], writes=[rB])
            P.op("dve", lambda e, slf=slf, tix=tix: e.tensor_copy(out=slots_all[:, tix, :], in_=slf[:, 0:4]), reads=[rB], writes=[rB, rt_B])
            P.op("act", lambda e, t8=t8, slf=slf, rs=rs: e.activation(out=slf[:, 4:8], in_=t8[:, 0:4], func=AF.Exp, bias=rs[:, 0:1], scale=1.0),
                 reads=[rB], writes=[rB])
            P.op("dve", lambda e, slf=slf, rs=rs: e.reduce_sum(out=rs[:, 3:4], in_=slf[:, 4:8], axis=AX.X), reads=[rB], writes=[rB])
            P.op("dve", lambda e, rs=rs: e.reciprocal(out=rs[:, 3:4], in_=rs[:, 3:4]), reads=[rB], writes=[rB])
            P.op("dve", lambda e, slf=slf, rs=rs, tix=tix: e.tensor_scalar(out=gk_all[:, tix, :], in0=slf[:, 4:8], scalar1=rs[:, 3:4], scalar2=None,
                                                                          op0=ALU.mult), reads=[rB], writes=[rB, rt_B])
            if T.get("dbg_G") is not None:
                P.dma("sp", lambda e, G=G, tok0=tok0: e.dma_start(out=T["dbg_G"][tok0:tok0 + 128, :], in_=G[:]), reads=[rB])

        pend = stageA(0)
        for tt in range(4):
            nxt = stageA(tt + 1) if tt + 1 < 4 else None
            stageB(tt, *pend)
            pend = nxt

    for tb in range(8):
        do_block(tb)
    if T.get("dbg_cnt") is not None:
        P.dma("sp", lambda e: e.dma_start(out=T["dbg_cnt"], in_=cnt[:]), reads=[cntB])
    P.barrier()
def phase5s(P, nc, T, GT, GT_B, slots_all, gk_all, rt_B):
    P.sb_reset(T["arena5"])
    xb_r = Ring([(P.sb([128, 1024], BF16, "x1b"), Buf()) for _ in range(8)])
    for tix in range(32):
        x1b, x1bB = xb_r.next()
        P.dma("pool", lambda e, x1b=x1b, tix=tix: e.dma_start(out=x1b[:], in_=T["x1_d"][tix * 128:(tix + 1) * 128, :]), writes=[x1bB])
        for k in range(4):
            P.dma("pool", lambda e, k=k, x1b=x1b, tix=tix: e.indirect_dma_start(
                out=T["xs_d"], out_offset=bass.IndirectOffsetOnAxis(ap=slots_all[:, tix, k:k + 1], axis=0),
                in_=x1b[:], in_offset=None, bounds_check=None, oob_is_err=False), reads=[x1bB, rt_B])
    P.barrier()
    P.sb_reset(T["arena5"])
    NS = CAP // 128
    CH = [(0, 512), (512, 512), (1024, 256)]
    NH_ = 512
    b1a = P.sb([128, 32, 16], F32, "b1a")
    identb = P.sb([128, 128], BF16, "identb5")
    toks = [P.dma("sp", lambda e: e.dma_start(out=b1a[:], in_=T["b_mlp1"])),
            P.dma("pool", lambda e: e.dma_start(out=identb[:], in_=T["ident"]))]
    for eng in ("pe", "act", "dve"):
        P.op(eng, None, extra=toks)
    wr_ = Ring([(P.sb([128, 8, 1024], BF16, "w1g"), P.sb([128, 8, 1024], BF16, "w1l"), P.sb([128, 8, 1024], BF16, "w2"),
                 P.sb([128, NS, 1024], BF16, "xs_tm"), Buf(), Buf(), Buf(), Buf()) for _ in range(2)])
    xsT = P.sb([128, 8, CAP], BF16, "xsT"); xsTB = Buf()
    actT = P.sb([128, 8, CAP], BF16, "actT"); aB = Buf()
    tmp_r = Ring([(P.sb([128, NH_], F32, "g5"), P.sb([128, NH_], F32, "s5"), P.sb([128, NH_], F32, "l5"), Buf()) for _ in range(2)])
    ys_r = Ring([(P.sb([128, 1024], BF16, "ys"), Buf()) for _ in range(3)])
    psr = Ring([(T["ps"][i], T["psb"][i]) for i in range(8)])
    xs3 = T["xs_d"].rearrange("(e s p) d -> e p s d", p=128, s=NS)
    ys3 = T["ys_d"].rearrange("(e s p) d -> e s p d", p=128, s=NS)

    def load_w(e_):
        w1g, w1l, w2, xs_tm, gB, lB, wB, xB = wr_.next()
        P.dma("sp", lambda e: e.dma_start(out=xs_tm[:], in_=xs3[e_]), writes=[xB])
        for c in range(8):
            P.dma("pool", lambda e, c=c: e.dma_start(out=w1g[:, c, :], in_=T["w1g"][e_, c * 128:(c + 1) * 128, :]), writes=[gB])
            P.dma("pool", lambda e, c=c: e.dma_start(out=w1l[:, c, :], in_=T["w1l"][e_, c * 128:(c + 1) * 128, :]), writes=[lB])
        for c in range(8):
            P.dma("pool", lambda e, c=c: e.dma_start(out=w2[:, c, :], in_=T["w2"][e_, c * 128:(c + 1) * 128, :]), writes=[wB])
        return w1g, w1l, w2, xs_tm, gB, lB, wB, xB

    def tr_group(xs_tm, xB, k, s0, n):
        ps, psB = psr.next()
        psb16 = ps.bitcast(BF16)
        for i in range(n):
            P.op("pe", lambda e, i=i: e.transpose(psb16[:, i * 128:(i + 1) * 128], xs_tm[:, s0 + i, k * 128:(k + 1) * 128], identb[:]),
                 reads=[xB], writes=[psB])
        P.op("act", lambda e: e.activation(out=xsT[:, k, s0 * 128:(s0 + n) * 128], in_=psb16[:, 0:n * 128], func=AF.Identity),
             reads=[psB], writes=[xsTB])

    def mm1(e_, W, f, hf):
        w1g, w1l, w2, xs_tm, gB, lB, wB, xB = W
        c0, cn = CH[hf]
        sl = slice(c0, c0 + cn)
        pg, pgB = psr.next()
        pl, plB = psr.next()
        for k in range(8):
            P.op("pe", lambda e, k=k: e.matmul(pg[:, 0:cn], w1g[:, k, f * 128:(f + 1) * 128], xsT[:, k, sl],
                                               start=(k == 0), stop=(k == 7)), reads=[gB, xsTB], writes=[pgB])
        for k in range(8):
            P.op("pe", lambda e, k=k: e.matmul(pl[:, 0:cn], w1l[:, k, f * 128:(f + 1) * 128], xsT[:, k, sl],
                                               start=(k == 0), stop=(k == 7)), reads=[lB, xsTB], writes=[plB])
        g5, s5, l5, tB = tmp_r.next()
        P.op("act", lambda e: e.activation(out=l5[:, 0:cn], in_=pl[:, 0:cn], func=AF.Identity, bias=b1a[:, e_, 8 + f:9 + f], scale=1.0),
             reads=[plB], writes=[tB])
        P.op("dve", lambda e: e.tensor_scalar(out=g5[:, 0:cn], in0=pg[:, 0:cn], scalar1=b1a[:, e_, f:f + 1], scalar2=7.0,
                                              op0=ALU.add, op1=ALU.min), reads=[pgB], writes=[tB])
        P.op("act", lambda e: e.activation(out=s5[:, 0:cn], in_=g5[:, 0:cn], func=AF.Sigmoid, scale=1.702), reads=[tB], writes=[tB])
        P.op("dve", lambda e: e.tensor_scalar(out=l5[:, 0:cn], in0=l5[:, 0:cn], scalar1=-7.0, scalar2=7.0, op0=ALU.max, op1=ALU.min),
             reads=[tB], writes=[tB])
        P.op("dve", lambda e: e.tensor_tensor(out=g5[:, 0:cn], in0=g5[:, 0:cn], in1=s5[:, 0:cn], op=ALU.mult), reads=[tB], writes=[tB])
        P.op("dve", lambda e: e.scalar_tensor_tensor(out=actT[:, f, sl], in0=l5[:, 0:cn], scalar=1.0, in1=g5[:, 0:cn], op0=ALU.add, op1=ALU.mult),
             reads=[tB], writes=[aB])

    def mm2(e_, W, st):
        w1g, w1l, w2, xs_tm, gB, lB, wB, xB = W
        ys, yB = ys_r.next()
        for half in range(2):
            ps, psB = psr.next()
            for k in range(8):
                P.op("pe", lambda e, k=k, ps=ps, half=half: e.matmul(ps[:], actT[:, k, st * 128:(st + 1) * 128],
                                                                    w2[:, k, half * 512:(half + 1) * 512],
                                                                    start=(k == 0), stop=(k == 7)), reads=[aB, wB], writes=[psB])
            if half == 0:
                P.op("act", lambda e, ps=ps: e.activation(out=ys[:, 0:512], in_=ps[:], func=AF.Identity), reads=[psB], writes=[yB])
            else:
                P.op("dve", lambda e, ps=ps: e.tensor_copy(out=ys[:, 512:1024], in_=ps[:]), reads=[psB], writes=[yB])
        P.dma("sp", lambda e: e.dma_start(out=ys3[e_, st], in_=ys[:]), reads=[yB])

    NE = T.get("n_exp", 32)
    W = load_w(0)
    for e_ in range(NE):
        Wn = load_w(e_ + 1) if e_ + 1 < NE else None
        xs_tm, xB = W[3], W[7]
        for k in range(8):
            for s0 in range(0, NS, 4):
                tr_group(xs_tm, xB, k, s0, min(4, NS - s0))
        for f in range(8):
            for hf in range(len(CH)):
                mm1(e_, W, f, hf)
        for st in range(NS):
            mm2(e_, W, st)
        W = Wn
    P.barrier()

    P.sb_reset(T["arena5"])
    b2b = P.sb([32, 1024], BF16, "b2b")
    g2bc = P.sb([128, 1024], F32, "g2bc")
    b2bc = P.sb([128, 1024], F32, "b2bc")
    cB = Buf()
    toks = [P.dma("pool", lambda e: e.dma_start(out=b2b[:], in_=T["b_mlp2"])),
            P.dma("sp", lambda e: e.dma_start(out=g2bc[:], in_=T["ln2_g_bc"])),
            P.dma("sp", lambda e: e.dma_start(out=b2bc[:], in_=T["ln2_b_bc"]))]
    for eng in ("pe", "act", "dve"):
        P.op(eng, None, extra=toks)
    rows_r = Ring([(P.sb([128, 1024], BF16, "rows"), Buf()) for _ in range(16)])
    x1_r = Ring([(P.sb([128, 1024], F32, "x1c"), Buf()) for _ in range(4)])
    acc_r = Ring([(P.sb([128, 1024], F32, "accc"), Buf()) for _ in range(4)])
    out_r = Ring([(P.sb([128, 1024], F32, "outc"), Buf()) for _ in range(3)])
    junk = P.sb([128, 1024], BF16, "junk6"); jB = Buf()
    sm_r = Ring([(P.sb([128, 8], F32, "sm6"), Buf()) for _ in range(2)])

    def comb_tile(ti):
        tok0 = ti * 128
        x1c, x1B = x1_r.next()
        P.dma("sp", lambda e: e.dma_start(out=x1c[:], in_=T["x1_d"][tok0:tok0 + 128, :]), writes=[x1B])
        acc, accB = acc_r.next()
        for half in range(2):
            ps, psB = psr.next()
            P.op("pe", lambda e, ps=ps, half=half: e.matmul(ps[:], GT[:, tok0:tok0 + 128], b2b[:, half * 512:(half + 1) * 512],
                                                            start=True, stop=True), reads=[GT_B], writes=[psB])
            P.op("dve", lambda e, ps=ps, half=half: e.scalar_tensor_tensor(out=acc[:, half * 512:(half + 1) * 512],
                                                                           in0=x1c[:, half * 512:(half + 1) * 512], scalar=ALPHA, in1=ps[:],
                                                                           op0=ALU.mult, op1=ALU.add), reads=[psB, x1B], writes=[accB])
        for k in range(4):
            rows, rwB = rows_r.next()
            P.dma("pool", lambda e, rows=rows, k=k: e.indirect_dma_start(
                out=rows[:], out_offset=None, in_=T["ys_d"],
                in_offset=bass.IndirectOffsetOnAxis(ap=slots_all[:, ti, k:k + 1], axis=0),
                bounds_check=None, oob_is_err=False), reads=[rt_B], writes=[rwB])
            P.op("dve", lambda e, rows=rows, k=k: e.scalar_tensor_tensor(out=acc[:], in0=rows[:], scalar=gk_all[:, ti, k:k + 1], in1=acc[:],
                                                                         op0=ALU.mult, op1=ALU.add), reads=[rwB, rt_B, accB], writes=[accB])
        o, oB = out_r.next()
        sm, sB = sm_r.next()
        layer_norm_tile(P, acc, accB, o, oB, g2bc, b2bc, cB, junk, jB, sm, sB)
        P.dma("sp", lambda e: e.dma_start(out=T["out"][tok0:tok0 + 128, :], in_=o[:]), reads=[oB])

    for ti in range(32):
        comb_tile(ti)
    P.barrier()
def build(upto=99, debug=False):
    nc = bass.Bass("TRN2", target_bir_lowering=False)
    T = {}

    def inp(name, shape, dt=F32):
        T[name] = nc.dram_tensor(name, list(shape), dt, kind="ExternalInput").ap()

    def scr(name, shape, dt=BF16):
        T[name] = nc.dram_tensor(name, list(shape), dt, kind=("ExternalOutput" if (debug and debug.get("dump_scr")) else "Internal")).ap()

    def outp(name, shape, dt=F32):
        T[name] = nc.dram_tensor(name, list(shape), dt, kind="ExternalOutput").ap()

    inp("xT", [1024, 8192]); inp("x_own", [4096, 1024])
    inp("w_fm", [1024, 3072]); inp("w_tm", [1024, 1024]); inp("b_fm", [128, 24]); inp("b_tm", [128, 1024])
    inp("cosT", [128, 8192]); inp("sinT", [128, 8192]); inp("lamv", [128, 256]); inp("subln_bc", [128, 128])
    scr("qT_da", [4, 128, 4096]); scr("kT_da", [4, 128, 8192]); scr("qT_na", [4, 128, 4096]); scr("kT_na", [4, 128, 8192])
    scr("v_da", [8192, 512]); scr("v_na", [8192, 512])
    if upto >= 3:
        inp("na_R", [8, 128, 18 * 256]); inp("na_M", [128, 18 * 256])
    if upto >= 4:
        inp("w_gate", [1024, 2048]); inp("b_gate", [128, 16]); inp("w_bda", [512, 1024]); inp("w_bna", [512, 1024])
        inp("w_out", [1024, 1024]); inp("w_router", [1024, 32]); inp("ln1_g_bc", [128, 1024]); inp("ln1_b_bc", [128, 1024])
        inp("b_router_bc", [128, 32]); inp("ident", [128, 128])
        scr("x1_d", [4096, 1024], F32)
        inp("ltri", [128, 128]); inp("ones128", [128, 128]); inp("ecap", [128, 32])
        scr("xs_d", [NEXP * CAP, 1024], BF16); scr("ys_d", [NEXP * CAP, 1024], BF16)
        if debug and debug.get("dump_scr"):
            outp("dbg_G", [4096, 32])
        if debug and debug.get("dump_cnt"):
            outp("dbg_cnt", [128, 32])
    if upto >= 5:
        inp("b_mlp1", [128, 32, 16]); inp("b_mlp2", [32, 1024])
        inp("ln2_g_bc", [128, 1024]); inp("ln2_b_bc", [128, 1024])
        inp("w1g", [32, 1024, 1024]); inp("w1l", [32, 1024, 1024]); inp("w2", [32, 1024, 1024])
        outp("out", [4096, 1024])
    T["ps"] = [nc.alloc_psum_tensor("ps%d" % i, [128, 512], F32) for i in range(8)]
    T["psb"] = [Buf() for _ in range(8)]
    P = Prog(nc)
    GT = P.sb([32, 4096], BF16, "GT")
    GT_B = Buf()
    slots_all = P.sb([128, 32, 4], I32, "slots_all")
    gk_all = P.sb([128, 32, 4], F32, "gk_all")
    rt_B = Buf()
    T["arena5"] = P.sb_off
    a_sb = P.sb([128, 32, 512], BF16, "a_sb")
    nb_sb = P.sb([128, 32, 512], BF16, "nb_sb")
    a_B, nb_B = Buf(), Buf()
    T["arena0"] = P.sb_off
    if debug:
        T["da_heads"] = debug.get("da_heads", 4)
        T["n_exp"] = debug.get("n_exp", 32)
    phase1(P, nc, T)
    if upto >= 2:
        phase2(P, nc, T, a_sb, a_B)
    if upto >= 3:
        phase3(P, nc, T, nb_sb, nb_B)
    if upto >= 4:
        phase4(P, nc, T, a_sb, a_B, nb_sb, nb_B, GT, GT_B, slots_all, gk_all, rt_B)
    if upto >= 5:
        phase5s(P, nc, T, GT, GT_B, slots_all, gk_all, rt_B)
    if upto < 4:
        outp("dbg_a", [4096, 512], BF16); outp("dbg_nb", [4096, 512], BF16)
        P.dma("sp", lambda e: e.dma_start(out=T["dbg_a"].rearrange("(t p) e -> p t e", p=128), in_=a_sb[:]), reads=[a_B])
        P.dma("sp", lambda e: e.dma_start(out=T["dbg_nb"].rearrange("(t p) e -> p t e", p=128), in_=nb_sb[:]), reads=[nb_B])
        if debug and debug.get("dump_scr"):
            for nm in ("qT_da", "kT_da", "v_da", "qT_na", "kT_na", "v_na"):
                pass
    P.barrier()
    P.emit()
    return nc, P


def rope_tables(pos):
    inv = (10000.0 ** (-np.arange(0, 64, 2, dtype=np.float32) / np.float32(64))).astype(np.float32)
    ang = pos.astype(np.float32)[:, None] * inv[None, :]
    ang = np.concatenate([ang, ang], axis=-1)
    cos = np.cos(ang).astype(np.float32)
    sin = np.sin(ang).astype(np.float32)
    sgn = np.concatenate([-np.ones(32, np.float32), np.ones(32, np.float32)])
    sin_s = sin * sgn[None, :]
    cosT = np.ascontiguousarray(np.concatenate([cos, cos], axis=1).T)
    sinT = np.ascontiguousarray(np.concatenate([sin_s, sin_s], axis=1).T)
    return cosT, sinT


def na_tables(rpb, h):
    R = np.zeros((8, 3, 6, 2, 64, 4, 64), np.float32)
    M = np.zeros((3, 6, 2, 64, 4, 64), np.float32)
    cc = np.arange(64)
    cs = np.clip(cc - 8, 0, 48)
    colvalid = (cc[:, None] >= cs[None, :]) & (cc[:, None] <= cs[None, :] + 15)
    coloff = np.clip(cc[:, None] - cc[None, :] + 15, 0, 30)
    for cls, g in ((0, 0), (1, 1 if h == 0 else 14), (2, 15)):
        for j in range(6):
            for a in range(2):
                for i in range(4):
                    r = 64 * h + 4 * g + i
                    kr = 64 * h + 4 * g + 2 * j - 4 + a
                    rs = min(max(r - 4, 0), 120)
                    if kr < rs or kr > rs + 7:
                        continue
                    M[cls, j, a, :, i, :] = colvalid
                    R[:, cls, j, a, :, i, :] = rpb[:, kr - r + 7][:, coloff] * colvalid[None]
    M2 = np.ascontiguousarray(M.transpose(2, 3, 0, 1, 4, 5).reshape(128, 18 * 256))
    R2 = np.ascontiguousarray(R.transpose(0, 3, 4, 1, 2, 5, 6).reshape(8, 128, 18 * 256))
    return R2, M2


def host_prep(inputs, upto=99):
    x = np.asarray(inputs["x"], np.float32)
    w_in = np.asarray(inputs["w_in"], np.float32)[0]
    b_in = np.asarray(inputs["b_in"], np.float32)[0]
    d = np.arange(64)
    swap = np.concatenate([(hh * 128 + c * 64 + (d + 32) % 64) for hh in range(4) for c in range(2)])
    qda, kda, vda = np.arange(0, 512), np.arange(512, 1024), np.arange(1024, 1536)
    qna, kna, vna = np.arange(1536, 2048), np.arange(2048, 2560), np.arange(2560, 3072)
    fm_cols = np.concatenate([qda, qda[swap], kda, kda[swap], qna, kna])
    tm_cols = np.concatenate([vda, vna])
    w_fm = np.ascontiguousarray(w_in[:, fm_cols])
    w_tm = np.ascontiguousarray(w_in[:, tm_cols])
    b_fm = np.ascontiguousarray(b_in[fm_cols].reshape(24, 128).T)
    b_tm = np.ascontiguousarray(np.broadcast_to(b_in[tm_cols][None, :], (128, 1024)))
    lamv = np.concatenate([np.asarray(inputs[k], np.float32)[0] for k in ("lambda_q1", "lambda_k1", "lambda_q2", "lambda_k2")])
    lamv = np.ascontiguousarray(np.broadcast_to(lamv[None, :], (128, 256)))
    subln_bc = np.ascontiguousarray(np.broadcast_to(np.asarray(inputs["subln_g"], np.float32)[0][None, :], (128, 128)))
    rpb = np.asarray(inputs["rpb"], np.float32)[0]
    shared = dict(w_fm=w_fm, w_tm=w_tm, b_fm=b_fm, b_tm=b_tm, lamv=lamv, subln_bc=subln_bc)
    f32 = lambda k: np.asarray(inputs[k], np.float32)[0]
    bc = lambda v, n=128: np.ascontiguousarray(np.broadcast_to(v[None, :], (n, v.shape[0])))
    if upto >= 4:
        shared.update(w_gate=np.ascontiguousarray(w_in[:, 3072:5120]), b_gate=np.ascontiguousarray(b_in[3072:5120].reshape(16, 128).T),
                      w_bda=f32("w_branch_da"), w_bna=f32("w_branch_na"), w_out=f32("w_out"), w_router=f32("w_router"),
                      ln1_g_bc=bc(f32("ln1_g")), ln1_b_bc=bc(f32("ln1_b")), b_router_bc=bc(f32("b_router")),
                      ident=np.eye(128, dtype=np.float32), ltri=np.triu(np.ones((128, 128), np.float32), k=1),
                      ones128=np.ones((128, 128), np.float32),
                      ecap=np.ascontiguousarray(np.broadcast_to((np.arange(32, dtype=np.float32) * CAP)[None, :], (128, 32))))
    if upto >= 5:
        w1 = f32("w_mlp1"); b1 = f32("b_mlp1")
        b1g = b1[:, 0::2].reshape(32, 8, 128).transpose(2, 0, 1)
        b1l = b1[:, 1::2].reshape(32, 8, 128).transpose(2, 0, 1)
        shared.update(b_mlp1=np.ascontiguousarray(np.concatenate([b1g, b1l], axis=2)),
                      b_mlp2=f32("b_mlp2"), ln2_g_bc=bc(f32("ln2_g")), ln2_b_bc=bc(f32("ln2_b")),
                      w1g=np.ascontiguousarray(w1[:, :, 0::2]), w1l=np.ascontiguousarray(w1[:, :, 1::2]), w2=f32("w_mlp2"))
    natab = [na_tables(rpb, h) for h in range(2)] if upto >= 3 else None
    maps = []
    for c in range(NCORES):
        b, h = c // 2, c % 2
        perm = np.concatenate([np.arange(h * 4096, (h + 1) * 4096), np.arange((1 - h) * 4096, (2 - h) * 4096)])
        xb = x[b]
        cosT, sinT = rope_tables(perm)
        m = dict(shared)
        m["xT"] = np.ascontiguousarray(xb[perm].T)
        m["x_own"] = np.ascontiguousarray(xb[h * 4096:(h + 1) * 4096])
        m["cosT"] = cosT
        m["sinT"] = sinT
        if upto >= 3:
            m["na_R"], m["na_M"] = natab[h]
        maps.append(m)
    return maps


def kernel(**inputs):
    from concourse.bass_utils import run_bass_kernel_spmd
    maps = host_prep(inputs)
    nc, P = build()
    res = run_bass_kernel_spmd(nc, maps, core_ids=list(range(NCORES)))
    out = np.zeros((4, SEQ, D), np.float32)
    for c in range(NCORES):
        b, h = c // 2, c % 2
        out[b, h * 4096:(h + 1) * 4096] = np.asarray(res.results[c]["out"], np.float32)
    return out
```

```python
import numpy as np
import concourse.bass as bass
import concourse.mybir as mybir

F32 = mybir.dt.float32
BF16 = mybir.dt.bfloat16
I32 = mybir.dt.int32
U32 = mybir.dt.uint32
AF = mybir.ActivationFunctionType
ALU = mybir.AluOpType
AX = mybir.AxisListType

ENGS = ("pe", "act", "dve", "pool", "sp")
KDMA = 8


class Tok:
    __slots__ = ("eng", "seq", "dma")

    def __init__(self, eng, seq, dma=None):
        self.eng = eng
        self.seq = seq
        self.dma = dma


class Buf:
    __slots__ = ("w", "r", "name")

    def __init__(self, name=""):
        self.w = None
        self.r = []
        self.name = name


class Prog:
    def __init__(self, nc):
        self.nc = nc
        self.ops = {e: [] for e in ENGS}
        self.ndma = {e: 0 for e in ENGS}
        self.all_dma = []
        self.sb_off = self.SB_BASE
        self.sb_hi = 0
        self.uid = 0

    SB_BASE = 16640
    SB_END = 229376

    def sb_reset(self, off=None):
        self.sb_off = self.SB_BASE if off is None else off

    def sb(self, shape, dtype, name=None):
        self.uid += 1
        nm = "%s_%d" % (name or "t", self.uid)
        nbytes = int(np.prod(shape[1:])) * mybir.dt.size(dtype)
        off = (self.sb_off + 63) // 64 * 64
        t = self.nc.alloc_sbuf_tensor_at(nm, list(shape), dtype, offset=off)
        self.sb_off = off + nbytes
        self.sb_hi = max(self.sb_hi, self.sb_off)
        assert self.sb_off <= self.SB_END, ("SBUF overflow", nm, self.sb_off)
        return t

    def _deps(self, reads, writes, extra):
        deps = {}
        for b in reads:
            if b.w is not None:
                deps[id(b.w)] = b.w
        for b in writes:
            if b.w is not None:
                deps[id(b.w)] = b.w
            for t in b.r:
                deps[id(t)] = t
        for t in extra:
            if t is not None:
                deps[id(t)] = t
        return list(deps.values())

    def _commit(self, tok, reads, writes):
        for b in writes:
            b.w = tok
            b.r = []
        for b in reads:
            if tok.dma is None:
                b.r = [t for t in b.r if not (t.dma is None and t.eng == tok.eng)]
            b.r.append(tok)

    def op(self, eng, fn, reads=(), writes=(), extra=()):
        deps = self._deps(reads, writes, extra)
        if eng == "pe":
            deps = [t for t in deps if not (t.eng == "pe" and t.dma is None)]
        tok = Tok(eng, len(self.ops[eng]))
        self.ops[eng].append(dict(fn=fn, waits=deps, tok=tok, dma=False))
        self._commit(tok, reads, writes)
        return tok

    def dma(self, eng, fn, reads=(), writes=(), extra=()):
        deps = self._deps(reads, writes, extra)
        i = self.ndma[eng]
        self.ndma[eng] += 1
        tok = Tok(eng, len(self.ops[eng]), dma=(i % KDMA, 16 * (i // KDMA + 1)))
        self.ops[eng].append(dict(fn=fn, waits=deps, tok=tok, dma=True, idx=i))
        self.all_dma.append(tok)
        self._commit(tok, reads, writes)
        return tok

    def barrier(self):
        lasts = []
        for e in ENGS:
            for o in reversed(self.ops[e]):
                if not o["dma"]:
                    lasts.append(o["tok"])
                    break
        dm = list(self.all_dma)
        self.all_dma = []
        for e in ENGS:
            if e == "sp":
                self.op(e, None, extra=lasts + dm)
            else:
                self.op(e, None, extra=lasts + dm)

    def emit(self):
        nc = self.nc
        needed = {e: set() for e in ENGS}
        for e in ENGS:
            for o in self.ops[e]:
                for t in o["waits"]:
                    if t.dma is None:
                        needed[t.eng].add(t.seq)
        tokval = {e: {} for e in ENGS}
        for e in ENGS:
            c = 0
            for o in self.ops[e]:
                if o["dma"]:
                    continue
                if o["tok"].seq in needed[e]:
                    c += 1
                    tokval[e][o["tok"].seq] = c
            self.maxcount = getattr(self, "maxcount", {})
            self.maxcount[e] = c
        engobj = {"pe": nc.tensor, "act": nc.scalar, "dve": nc.vector, "pool": nc.gpsimd, "sp": nc.sync}
        import contextlib
        with contextlib.ExitStack() as st:
            csem = {e: st.enter_context(nc.semaphore("c_" + e)) for e in ENGS}
            dsem = {e: [st.enter_context(nc.semaphore("d_%s%d" % (e, k))) for k in range(KDMA)]
                    for e in ENGS if self.ndma[e] > 0}
            block = st.enter_context(nc.Block())

            def run(e, engine):
                waited = {}
                for o in self.ops[e]:
                    for t in o["waits"]:
                        if t.dma is not None:
                            sem = dsem[t.eng][t.dma[0]]
                            val = t.dma[1]
                            key = ("d", t.eng, t.dma[0])
                        else:
                            sem = csem[t.eng]
                            val = tokval[t.eng][t.seq]
                            key = ("c", t.eng)
                        if waited.get(key, 0) >= val:
                            continue
                        waited[key] = val
                        engine.wait_ge(sem, val)
                    if o["dma"]:
                        i = o["idx"]
                        slot = i % KDMA
                        if i >= KDMA:
                            key = ("d", e, slot)
                            val = 16 * (i // KDMA)
                            if waited.get(key, 0) < val:
                                waited[key] = val
                                engine.wait_ge(dsem[e][slot], val)
                        ins = o["fn"](engine)
                        ins.then_inc(dsem[e][slot], 16)
                    else:
                        if o["fn"] is None:
                            if o["tok"].seq in needed[e]:
                                ins = engine.nop() if hasattr(engine, "nop") else None
                                ins.then_inc(csem[e], 1)
                            continue
                        ins = o["fn"](engine)
                        if o["tok"].seq in needed[e]:
                            ins.then_inc(csem[e], 1)

            @block.tensor
            def _(eng):
                run("pe", eng)

            @block.scalar
            def _(eng):
                run("act", eng)

            @block.vector
            def _(eng):
                run("dve", eng)

            @block.gpsimd
            def _(eng):
                run("pool", eng)

            @block.sync
            def _(eng):
                run("sp", eng)
D = 1024
SEQ = 8192
NOWN = 4096
NCORES = 8
CAP = 1280
NEXP = 32
LAM_INIT = 0.2
ALPHA = 2.0 ** 0.25


class Ring:
    def __init__(self, items):
        self.items = list(items)
        self.i = 0

    def next(self):
        it = self.items[self.i % len(self.items)]
        self.i += 1
        return it


def phase1(P, nc, T):
    P.sb_reset()
    wfm = P.sb([128, 8, 3072], BF16, "wfm")
    wtm = P.sb([128, 8, 1024], BF16, "wtm")
    bfm = P.sb([128, 24], F32, "bfm")
    btm = P.sb([128, 1024], F32, "btm")
    b_w = Buf()
    wtoks = []
    for c in range(8):
        for g in range(6):
            wtoks.append(P.dma("pool", lambda e, c=c, g=g: e.dma_start(
                out=wfm[:, c, g * 512:(g + 1) * 512], in_=T["w_fm"][c * 128:(c + 1) * 128, g * 512:(g + 1) * 512])))
        for g in range(2):
            wtoks.append(P.dma("pool", lambda e, c=c, g=g: e.dma_start(
                out=wtm[:, c, g * 512:(g + 1) * 512], in_=T["w_tm"][c * 128:(c + 1) * 128, g * 512:(g + 1) * 512])))
    wtoks.append(P.dma("sp", lambda e: e.dma_start(out=bfm[:], in_=T["b_fm"])))
    wtoks.append(P.dma("sp", lambda e: e.dma_start(out=btm[:], in_=T["b_tm"])))
    for eng in ("pe", "act", "dve"):
        P.op(eng, None, extra=wtoks)

    xblk = Ring([(P.sb([128, 8, 512], BF16, "xblk"), Buf()) for _ in range(2)])
    csb = Ring([(P.sb([128, 512], F32, "cos"), P.sb([128, 512], F32, "sin"), Buf()) for _ in range(2)])
    t1r = Ring([(P.sb([128, 512], F32, "t1"), Buf()) for _ in range(2)])
    t2r = Ring([(P.sb([128, 512], F32, "t2"), Buf()) for _ in range(2)])
    stg = Ring([(P.sb([128, 512], BF16, "stg"), Buf()) for _ in range(6)])
    psr = Ring([(T["ps"][i], T["psb"][i]) for i in range(8)])
    xT3 = T["xT"].rearrange("(c p) t -> p c t", p=128)

    def do_block(i):
        own = i < 8
        xb, xbB = xblk.next()
        P.dma("pool", lambda e, xb=xb, i=i: e.dma_start(out=xb[:], in_=xT3[:, :, i * 512:(i + 1) * 512]), writes=[xbB])
        cb, sb_, csB = csb.next()
        P.dma("sp", lambda e, cb=cb, i=i: e.dma_start(out=cb[:], in_=T["cosT"][:, i * 512:(i + 1) * 512]), writes=[csB])
        P.dma("sp", lambda e, sb_=sb_, i=i: e.dma_start(out=sb_[:], in_=T["sinT"][:, i * 512:(i + 1) * 512]), writes=[csB])

        def mm_fm(ps, psB, col):
            for k in range(8):
                P.op("pe", lambda e, k=k: e.matmul(ps[:], wfm[:, k, col:col + 128], xb[:, k, :],
                                                   start=(k == 0), stop=(k == 7)),
                     reads=[xbB], writes=[psB])

        def rope_tile(col0, col1, dst):
            psA, psAB = psr.next()
            psC, psCB = psr.next()
            mm_fm(psA, psAB, col0)
            mm_fm(psC, psCB, col1)
            t1, t1B = t1r.next()
            t2, t2B = t2r.next()
            P.op("dve", lambda e: e.scalar_tensor_tensor(out=t1[:], in0=psA[:], scalar=bfm[:, col0 // 128:col0 // 128 + 1],
                                                          in1=cb[:], op0=ALU.add, op1=ALU.mult),
                 reads=[psAB, csB], writes=[t1B])
            P.op("dve", lambda e: e.scalar_tensor_tensor(out=t2[:], in0=psC[:], scalar=bfm[:, col1 // 128:col1 // 128 + 1],
                                                          in1=sb_[:], op0=ALU.add, op1=ALU.mult),
                 reads=[psCB, csB], writes=[t2B])
            st, stB = stg.next()
            P.op("pool", lambda e: e.tensor_tensor(out=st[:], in0=t1[:], in1=t2[:], op=ALU.add),
                 reads=[t1B, t2B], writes=[stB])
            P.dma("sp", lambda e: e.dma_start(out=dst, in_=st[:]), reads=[stB])

        def plain_tile(col, dst):
            ps, psB = psr.next()
            mm_fm(ps, psB, col)
            st, stB = stg.next()
            P.op("act", lambda e: e.activation(out=st[:], in_=ps[:], func=AF.Identity,
                                               bias=bfm[:, col // 128:col // 128 + 1], scale=1.0),
                 reads=[psB], writes=[stB])
            P.dma("sp", lambda e: e.dma_start(out=dst, in_=st[:]), reads=[stB])

        sl = slice(i * 512, (i + 1) * 512)
        for h in range(4):
            if own:
                rope_tile(h * 128, 512 + h * 128, T["qT_da"][h, :, sl])
            rope_tile(1024 + h * 128, 1536 + h * 128, T["kT_da"][h, :, sl])
        na_kv = (i <= 8) or (i == 15)
        for j in range(4):
            if own:
                plain_tile(2048 + j * 128, T["qT_na"][j, :, sl])
            if na_kv:
                plain_tile(2560 + j * 128, T["kT_na"][j, :, sl])
        for tt in range(4):
            for g, dst in (((0, T["v_da"]), (1, T["v_na"])) if na_kv else ((0, T["v_da"]),)):
                ps, psB = psr.next()
                for k in range(8):
                    P.op("pe", lambda e, k=k, ps=ps, g=g, tt=tt: e.matmul(
                        ps[:], xb[:, k, tt * 128:(tt + 1) * 128], wtm[:, k, g * 512:(g + 1) * 512],
                        start=(k == 0), stop=(k == 7)), reads=[xbB], writes=[psB])
                st, stB = stg.next()
                P.op("dve", lambda e, ps=ps, st=st, g=g: e.tensor_tensor(out=st[:], in0=ps[:], in1=btm[:, g * 512:(g + 1) * 512],
                                                                         op=ALU.add), reads=[psB], writes=[stB])
                r0 = i * 512 + tt * 128
                P.dma("sp", lambda e, st=st, dst=dst, r0=r0: e.dma_start(out=dst[r0:r0 + 128, :], in_=st[:]), reads=[stB])

    for i in range(16):
        do_block(i)
    P.barrier()
def phase2(P, nc, T, a_sb, a_B):
    P.sb_reset(T["arena0"])
    lamv = P.sb([128, 256], F32, "lamv")
    gbc = P.sb([128, 128], F32, "gbc")
    tmp64 = P.sb([128, 128], F32, "tmp64")
    sc = P.sb([128, 8], F32, "sc")
    neglam = P.sb([128, 1], F32, "neglam")
    cB = Buf()
    P.dma("sp", lambda e: e.dma_start(out=lamv[:], in_=T["lamv"]), writes=[cB])
    P.dma("sp", lambda e: e.dma_start(out=gbc[:], in_=T["subln_bc"]), writes=[cB])
    P.op("dve", lambda e: e.tensor_tensor(out=tmp64[:, 0:64], in0=lamv[:, 0:64], in1=lamv[:, 64:128], op=ALU.mult), reads=[cB], writes=[cB])
    P.op("dve", lambda e: e.tensor_tensor(out=tmp64[:, 64:128], in0=lamv[:, 128:192], in1=lamv[:, 192:256], op=ALU.mult), reads=[cB], writes=[cB])
    P.op("dve", lambda e: e.reduce_sum(out=sc[:, 0:1], in_=tmp64[:, 0:64], axis=AX.X), reads=[cB], writes=[cB])
    P.op("dve", lambda e: e.reduce_sum(out=sc[:, 1:2], in_=tmp64[:, 64:128], axis=AX.X), reads=[cB], writes=[cB])
    P.op("act", lambda e: e.activation(out=sc[:, 2:4], in_=sc[:, 0:2], func=AF.Exp), reads=[cB], writes=[cB])
    P.op("dve", lambda e: e.tensor_tensor(out=sc[:, 4:5], in0=sc[:, 3:4], in1=sc[:, 2:3], op=ALU.subtract), reads=[cB], writes=[cB])
    P.op("dve", lambda e: e.tensor_scalar(out=neglam[:], in0=sc[:, 4:5], scalar1=-LAM_INIT, scalar2=None, op0=ALU.add), reads=[cB], writes=[cB])
    P.op("dve", lambda e: e.tensor_scalar(out=gbc[:], in0=gbc[:], scalar1=1.0 - LAM_INIT, scalar2=None, op0=ALU.mult), reads=[cB], writes=[cB])

    hb = []
    for _ in range(2):
        KT = P.sb([128, 8192], BF16, "KT")
        V = P.sb([128, 64, 129], BF16, "V")
        QT = P.sb([128, 2, 4096], BF16, "QT")
        oB = Buf()
        P.op("pool", lambda e, V=V: e.memset(V[:, :, 128:129], 1.0), writes=[oB])
        P.op("pool", lambda e, QT=QT: e.memset(QT[64:128, 0, :], 0.0), writes=[oB])
        P.op("pool", lambda e, QT=QT: e.memset(QT[0:64, 1, :], 0.0), writes=[oB])
        hb.append((KT, V, QT, Buf(), Buf(), Buf(), oB))
    ptr = Ring([(P.sb([128, 512], BF16, "pt"), Buf()) for _ in range(4)])
    sps = Ring([(T["ps"][i], T["psb"][i]) for i in range(4, 8)])
    accs = {}
    lay = [(0, 0), (0, 1), (0, 2), (1, 0), (2, 0), (2, 1), (2, 2), (3, 0)]
    n = 0
    for c in range(2):
        for qt in range(4):
            bk, pos = lay[n]
            n += 1
            accs[(c, qt)] = (T["ps"][bk][:, pos * 129:(pos + 1) * 129], T["psb"][bk])
    small = Ring([(P.sb([128, 8], F32, "sm"), P.sb([128, 128], F32, "tt"), P.sb([128, 128], F32, "oo"),
                   P.sb([128, 128], F32, "sq"), Buf()) for _ in range(3)])
    v_da3 = T["v_da"].rearrange("(t p) e -> p t e", p=128)

    def load_head(h):
        KT, V, QT, kB, vB, qB, oB = hb[h % 2]
        for s4 in range(4):
            P.dma("sp", lambda e, s4=s4: e.dma_start(out=KT[:, s4 * 2048:(s4 + 1) * 2048],
                                                      in_=T["kT_da"][h, :, s4 * 2048:(s4 + 1) * 2048]), writes=[kB])
        for s8 in range(8):
            P.dma("sp", lambda e, s8=s8: e.dma_start(out=V[:, s8 * 8:(s8 + 1) * 8, 0:128],
                                                      in_=v_da3[:, s8 * 8:(s8 + 1) * 8, h * 128:(h + 1) * 128]), writes=[vB])
        P.dma("sp", lambda e: e.dma_start(out=QT[0:64, 0, :], in_=T["qT_da"][h, 0:64, :]), writes=[qB])
        P.dma("sp", lambda e: e.dma_start(out=QT[64:128, 1, :], in_=T["qT_da"][h, 64:128, :]), writes=[qB])

    def epilogue(h, qb, qt):
        O1, O1B = accs[(0, qt)]
        O2, O2B = accs[(1, qt)]
        sm, tt, oo, sq, sB = small.next()
        P.op("dve", lambda e: e.reciprocal(out=sm[:, 0:1], in_=O1[:, 128:129]), reads=[O1B], writes=[sB])
        P.op("dve", lambda e: e.reciprocal(out=sm[:, 1:2], in_=O2[:, 128:129]), reads=[O2B], writes=[sB])
        P.op("dve", lambda e: e.tensor_tensor(out=sm[:, 2:3], in0=sm[:, 1:2], in1=neglam[:], op=ALU.mult), reads=[sB, cB], writes=[sB])
        P.op("dve", lambda e: e.tensor_scalar(out=tt[:], in0=O2[:, 0:128], scalar1=sm[:, 2:3], scalar2=None, op0=ALU.mult),
             reads=[O2B, sB], writes=[sB])
        P.op("dve", lambda e: e.scalar_tensor_tensor(out=oo[:], in0=O1[:, 0:128], scalar=sm[:, 0:1], in1=tt[:],
                                                      op0=ALU.mult, op1=ALU.add), reads=[O1B, sB], writes=[sB])
        P.op("act", lambda e: e.activation(out=sq[:], in_=oo[:], func=AF.Square, accum_out=sm[:, 3:4]), reads=[sB], writes=[sB])
        P.op("act", lambda e: e.activation(out=sm[:, 4:5], in_=sm[:, 3:4], func=AF.Ln, scale=1.0 / 128.0, bias=1e-5),
             reads=[sB], writes=[sB])
        P.op("act", lambda e: e.activation(out=sm[:, 5:6], in_=sm[:, 4:5], func=AF.Exp, scale=-0.5), reads=[sB], writes=[sB])
        P.op("dve", lambda e: e.scalar_tensor_tensor(out=a_sb[:, qb * 4 + qt, h * 128:(h + 1) * 128], in0=oo[:], scalar=sm[:, 5:6],
                                                      in1=gbc[:], op0=ALU.mult, op1=ALU.mult), reads=[sB, cB], writes=[a_B])

    def qk(h, qb, c, kt):
        KT, V, QT, kB, vB, qB, oB = hb[h % 2]
        S, SB = sps.next()
        P.op("pe", lambda e: e.matmul(S[:], KT[:, kt * 128:(kt + 1) * 128],
                                      QT[:, c, qb * 512:(qb + 1) * 512],
                                      start=True, stop=True), reads=[kB, qB, oB], writes=[SB])
        Pt, PtB = ptr.next()
        P.op("act", lambda e: e.activation(out=Pt[:], in_=S[:], func=AF.Exp, scale=0.125),
             reads=[SB], writes=[PtB])
        return Pt, PtB

    def pv(h, qb, c, kt, Pt, PtB):
        KT, V, QT, kB, vB, qB, oB = hb[h % 2]
        for qt in range(4):
            O, OB = accs[(c, qt)]
            P.op("pe", lambda e, O=O, qt=qt: e.matmul(O, Pt[:, qt * 128:(qt + 1) * 128], V[:, kt, :],
                                                      start=(kt == 0 and qt in (0, 3)), stop=(kt == 63), skip_group_check=True),
                 reads=[PtB, vB, oB], writes=[OB])
        if c == 1 and kt == 63:
            for qt in range(4):
                epilogue(h, qb, qt)

    iters = [(h, qb, c, kt) for h in range(T.get("da_heads", 4)) for qb in range(8) for c in range(2) for kt in range(64)]
    LA = 2
    pend = {}
    NH = T.get("da_heads", 4)
    load_head(0)
    if NH > 1:
        load_head(1)
    for n in range(len(iters) + LA):
        if n < len(iters):
            h, qb, c, kt = iters[n]
            pend[n] = qk(h, qb, c, kt)
        m = n - LA
        if m >= 0:
            h, qb, c, kt = iters[m]
            Pt, PtB = pend.pop(m)
            pv(h, qb, c, kt, Pt, PtB)
            if qb == 7 and c == 1 and kt == 63 and h + 2 < NH:
                load_head(h + 2)
    P.barrier()
def phase3(P, nc, T, nb_sb, nb_B):
    P.sb_reset(T["arena0"])
    if T.get("xs_d") is not None:
        zt = P.sb([128, CAP // 128, 1024], BF16, "zt")
        zB = Buf()
        P.op("pool", lambda e: e.memset(zt[:], 0.0), writes=[zB])
        xs3 = T["xs_d"].rearrange("(e s p) d -> e p s d", p=128, s=CAP // 128)
        for e_ in range(NEXP):
            P.dma("sp", lambda e, e_=e_: e.dma_start(out=xs3[e_], in_=zt[:]), reads=[zB])
    Mt = P.sb([128, 18 * 256], BF16, "Mt")
    mB = Buf()
    for s in range(3):
        P.dma("pool", lambda e, s=s: e.dma_start(out=Mt[:, s * 1536:(s + 1) * 1536], in_=T["na_M"][:, s * 1536:(s + 1) * 1536]), writes=[mB])
    Rt = P.sb([128, 18 * 256], F32, "Rt")
    rB = Buf()
    hb = []
    for _ in range(2):
        KT = P.sb([64, 4608], BF16, "KTn")
        V = P.sb([128, 36, 65], BF16, "Vn")
        QT = P.sb([64, 4096], BF16, "QTn")
        E = P.sb([128, 18 * 256], BF16, "En")
        oB = Buf()
        P.op("pool", lambda e, V=V: e.memset(V[:, :, 64:65], 1.0), writes=[oB])
        hb.append((KT, V, QT, E, Buf(), Buf(), Buf(), Buf(), oB))
    per = Ring([(P.sb([128, 512], BF16, "pe_"), Buf()) for _ in range(4)])
    pmr = Ring([(P.sb([128, 512], BF16, "pm_"), Buf()) for _ in range(4)])
    sps = Ring([(T["ps"][i], T["psb"][i]) for i in range(2, 8)])
    accs = [(T["ps"][0][:, 0:65], T["psb"][0]), (T["ps"][0][:, 128:193], T["psb"][0]),
            (T["ps"][1][:, 0:65], T["psb"][1]), (T["ps"][1][:, 128:193], T["psb"][1])]
    smr = Ring([(P.sb([128, 2], F32, "smn"), Buf()) for _ in range(4)])
    v_na3 = T["v_na"].rearrange("(t p) e -> p t e", p=128)

    def load_head(hn):
        KT, V, QT, E, kB, vB, qB, eB, oB = hb[hn % 2]
        j, po = hn // 2, (hn % 2) * 64
        P.dma("sp", lambda e: e.dma_start(out=KT[:, 0:256], in_=T["kT_na"][j, po:po + 64, 7936:8192]), writes=[kB])
        P.dma("sp", lambda e: e.dma_start(out=KT[:, 256:4608], in_=T["kT_na"][j, po:po + 64, 0:4352]), writes=[kB])
        P.dma("sp", lambda e: e.dma_start(out=QT[:], in_=T["qT_na"][j, po:po + 64, :]), writes=[qB])
        P.dma("sp", lambda e: e.dma_start(out=V[:, 0:2, 0:64], in_=v_na3[:, 62:64, hn * 64:(hn + 1) * 64]), writes=[vB])
        for s in range(2):
            P.dma("sp", lambda e, s=s: e.dma_start(out=V[:, 2 + s * 17:2 + (s + 1) * 17, 0:64],
                                                    in_=v_na3[:, s * 17:(s + 1) * 17, hn * 64:(hn + 1) * 64]), writes=[vB])
        P.dma("sp", lambda e: e.dma_start(out=Rt[:], in_=T["na_R"][hn, :, :]), writes=[rB])
        for s in range(3):
            sl = slice(s * 1536, (s + 1) * 1536)
            P.op("act", lambda e, sl=sl: e.activation(out=Rt[:, sl], in_=Rt[:, sl], func=AF.Exp), reads=[rB], writes=[rB])
            P.op("dve", lambda e, sl=sl: e.tensor_tensor(out=E[:, sl], in0=Rt[:, sl], in1=Mt[:, sl], op=ALU.mult),
                 reads=[rB, mB], writes=[eB])

    def qk(hn, g, jp):
        KT, V, QT, E, kB, vB, qB, eB, oB = hb[hn % 2]
        S, SB = sps.next()
        for hf in range(2):
            ti = 2 * g + 2 * jp + hf
            P.op("pe", lambda e, hf=hf, ti=ti: e.matmul(S[:, hf * 256:(hf + 1) * 256], KT[:, ti * 128:(ti + 1) * 128],
                                                        QT[:, g * 256:(g + 1) * 256],
                                                        start=(hf == 0), stop=True, skip_group_check=True), reads=[kB, qB], writes=[SB])
        Pe, PeB = per.next()
        P.op("act", lambda e: e.activation(out=Pe[:], in_=S[:, 0:512], func=AF.Exp, scale=0.125), reads=[SB], writes=[PeB])
        Pm, PmB = pmr.next()
        cls = 0 if g == 0 else (2 if g == 15 else 1)
        ei = (cls * 6 + 2 * jp) * 256
        P.op("dve", lambda e: e.tensor_tensor(out=Pm[:], in0=Pe[:], in1=E[:, ei:ei + 512], op=ALU.mult),
             reads=[PeB, eB], writes=[PmB])
        return Pm, PmB

    def pv(hn, g, jp, Pm, PmB):
        KT, V, QT, E, kB, vB, qB, eB, oB = hb[hn % 2]
        for hf in range(2):
            ti = 2 * g + 2 * jp + hf
            for qt in range(2):
                O, OB = accs[(g % 2) * 2 + qt]
                P.op("pe", lambda e, O=O, qt=qt, hf=hf, ti=ti: e.matmul(
                    O, Pm[:, hf * 256 + qt * 128:hf * 256 + (qt + 1) * 128], V[:, ti, :],
                    start=(jp == 0 and hf == 0 and qt == 0), stop=(jp == 2 and hf == 1), skip_group_check=True),
                    reads=[PmB, vB, oB], writes=[OB])
        if jp == 2:
            for qt in range(2):
                O, OB = accs[(g % 2) * 2 + qt]
                sm, sB = smr.next()
                P.op("dve", lambda e, O=O, sm=sm: e.reciprocal(out=sm[:, 0:1], in_=O[:, 64:65]), reads=[OB], writes=[sB])
                P.op("dve", lambda e, O=O, sm=sm, qt=qt: e.tensor_scalar(
                    out=nb_sb[:, g * 2 + qt, hn * 64:(hn + 1) * 64], in0=O[:, 0:64], scalar1=sm[:, 0:1], scalar2=None,
                    op0=ALU.mult), reads=[OB, sB], writes=[nb_B])

    iters = [(hn, g, j) for hn in range(T.get("na_heads", 8)) for g in range(16) for j in range(3)]
    LA = 2
    pend = {}
    NH = T.get("na_heads", 8)
    load_head(0)
    if NH > 1:
        load_head(1)
    for n in range(len(iters) + LA):
        if n < len(iters):
            hn, g, j = iters[n]
            pend[n] = qk(hn, g, j)
        m = n - LA
        if m >= 0:
            hn, g, j = iters[m]
            Pm, PmB = pend.pop(m)
            pv(hn, g, j, Pm, PmB)
            if g == 15 and j == 2 and hn + 2 < NH:
                load_head(hn + 2)
    P.barrier()
def layer_norm_tile(P, y, yB, out, outB, gbc, bbc, cB, junk, jB, sm, sB):
    class _W:
        def __init__(self, t):
            self.t = t

        def __getitem__(self, k):
            return self.t if isinstance(self.t, bass.AP) else self.t[k]
    y = _W(y)
    out = _W(out)
    junk = _W(junk)
    P.op("act", lambda e: e.activation(out=junk[:], in_=y[:], func=AF.Identity, accum_out=sm[:, 0:1]), reads=[yB], writes=[jB, sB])
    P.op("act", lambda e: e.activation(out=junk[:], in_=y[:], func=AF.Square, accum_out=sm[:, 1:2]), reads=[yB], writes=[jB, sB])
    P.op("dve", lambda e: e.tensor_scalar(out=sm[:, 2:3], in0=sm[:, 0:1], scalar1=1.0 / 1024.0, scalar2=None, op0=ALU.mult), reads=[sB], writes=[sB])
    P.op("dve", lambda e: e.tensor_tensor(out=sm[:, 3:4], in0=sm[:, 2:3], in1=sm[:, 2:3], op=ALU.mult), reads=[sB], writes=[sB])
    P.op("dve", lambda e: e.scalar_tensor_tensor(out=sm[:, 4:5], in0=sm[:, 1:2], scalar=1.0 / 1024.0, in1=sm[:, 3:4],
                                                  op0=ALU.mult, op1=ALU.subtract), reads=[sB], writes=[sB])
    P.op("act", lambda e: e.activation(out=sm[:, 5:6], in_=sm[:, 4:5], func=AF.Ln, scale=1.0, bias=1e-5), reads=[sB], writes=[sB])
    P.op("act", lambda e: e.activation(out=sm[:, 6:7], in_=sm[:, 5:6], func=AF.Exp, scale=-0.5), reads=[sB], writes=[sB])
    P.op("dve", lambda e: e.tensor_scalar(out=out[:], in0=y[:], scalar1=sm[:, 2:3], scalar2=sm[:, 6:7], op0=ALU.subtract, op1=ALU.mult),
         reads=[yB, sB], writes=[outB])
    P.op("dve", lambda e: e.tensor_tensor(out=out[:], in0=out[:], in1=gbc[:], op=ALU.mult), reads=[outB, cB], writes=[outB])
    P.op("dve", lambda e: e.tensor_tensor(out=out[:], in0=out[:], in1=bbc[:], op=ALU.add), reads=[outB, cB], writes=[outB])


def phase4(P, nc, T, a_sb, a_B, nb_sb, nb_B, GT, GT_B, slots_all, gk_all, rt_B):
    P.sb_reset(T["arena0"])
    wg = P.sb([128, 8, 2048], BF16, "wg")
    wbd = P.sb([128, 4, 1024], BF16, "wbd")
    wbn = P.sb([128, 4, 1024], BF16, "wbn")
    wo = P.sb([128, 8, 1024], BF16, "wo")
    wr = P.sb([128, 8, 32], F32, "wr")
    bg = P.sb([128, 16], F32, "bg")
    g1bc = P.sb([128, 1024], F32, "g1bc")
    b1bc = P.sb([128, 1024], F32, "b1bc")
    brbc = P.sb([128, 32], F32, "brbc")
    identb = P.sb([128, 128], BF16, "identb")
    ltri = P.sb([128, 128], BF16, "ltri")
    ones = P.sb([128, 128], BF16, "ones")
    ecap = P.sb([128, 32], F32, "ecap")
    cnt = P.sb([128, 32], F32, "cnt")
    cntB = Buf()
    P.op("pool", lambda e: e.memset(cnt[:], 0.0), writes=[cntB])
    identf = P.sb([128, 128], F32, "identf")
    cB = Buf()
    toks = []
    for c in range(8):
        for g in range(4):
            toks.append(P.dma("pool", lambda e, c=c, g=g: e.dma_start(out=wg[:, c, g * 512:(g + 1) * 512],
                                                                      in_=T["w_gate"][c * 128:(c + 1) * 128, g * 512:(g + 1) * 512])))
        toks.append(P.dma("pool", lambda e, c=c: e.dma_start(out=wo[:, c, :], in_=T["w_out"][c * 128:(c + 1) * 128, :])))
        toks.append(P.dma("sp", lambda e, c=c: e.dma_start(out=wr[:, c, :], in_=T["w_router"][c * 128:(c + 1) * 128, :])))
    for c in range(4):
        toks.append(P.dma("pool", lambda e, c=c: e.dma_start(out=wbd[:, c, :], in_=T["w_bda"][c * 128:(c + 1) * 128, :])))
        toks.append(P.dma("pool", lambda e, c=c: e.dma_start(out=wbn[:, c, :], in_=T["w_bna"][c * 128:(c + 1) * 128, :])))
    for dst, src in ((bg, "b_gate"), (g1bc, "ln1_g_bc"), (b1bc, "ln1_b_bc"), (brbc, "b_router_bc"), (identf, "ident")):
        toks.append(P.dma("sp", lambda e, dst=dst, src=src: e.dma_start(out=dst[:], in_=T[src])))
    toks.append(P.dma("pool", lambda e: e.dma_start(out=identb[:], in_=T["ident"])))
    toks.append(P.dma("pool", lambda e: e.dma_start(out=ltri[:], in_=T["ltri"])))
    toks.append(P.dma("pool", lambda e: e.dma_start(out=ones[:], in_=T["ones128"])))
    toks.append(P.dma("sp", lambda e: e.dma_start(out=ecap[:], in_=T["ecap"])))
    for eng in ("pe", "act", "dve", "pool"):
        P.op(eng, None, extra=toks)

    xblk = Ring([(P.sb([128, 8, 512], BF16, "xblk4"), Buf()) for _ in range(1)])
    aT = P.sb([128, 4, 512], BF16, "aT"); aTB = Buf()
    nT = P.sb([128, 4, 512], BF16, "nT"); nTB = Buf()
    g_r = Ring([(P.sb([128, 2, 512], BF16, "g01"), Buf()) for _ in range(2)])
    mT = P.sb([128, 8, 512], BF16, "mT"); mTB = Buf()
    tr = Ring([(P.sb([128, 512], F32, "t4"), P.sb([128, 512], F32, "u4"), Buf()) for _ in range(1)])
    y_r = Ring([(P.sb([128, 1024], F32, "y4"), Buf()) for _ in range(2)])
    x1_r = Ring([(P.sb([128, 1024], F32, "x1"), Buf()) for _ in range(2)])
    junk = P.sb([128, 1024], BF16, "junk"); jB = Buf()
    sm_r = Ring([(P.sb([128, 8], F32, "sm4"), Buf()) for _ in range(2)])
    x1Tf_r = Ring([(P.sb([128, 1024], F32, "x1Tf"), None, Buf()) for _ in range(1)])
    rt_r = Ring([(P.sb([128, 32], F32, "lg"), P.sb([128, 8], F32, "t8"), P.sb([128, 32], F32, "mk"), P.sb([128, 32], F32, "ex"),
                  P.sb([128, 4], F32, "rs"), P.sb([128, 32], F32, "G"), Buf(),
                  P.sb([128, 32], BF16, "mkb"), P.sb([128, 32], F32, "rk"), P.sb([128, 32], F32, "tm"), P.sb([128, 8], F32, "slf")) for _ in range(2)])
    psr = Ring([(T["ps"][i], T["psb"][i]) for i in range(8)])
    xT3 = T["xT"].rearrange("(c p) t -> p c t", p=128)

    def do_block(tb):
        xb, xbB = xblk.next()
        P.dma("pool", lambda e: e.dma_start(out=xb[:], in_=xT3[:, :, tb * 512:(tb + 1) * 512]), writes=[xbB])
        for src, sB_, dst, dB in ((a_sb, a_B, aT, aTB), (nb_sb, nb_B, nT, nTB)):
            for hc in range(4):
                ps, psB = psr.next()
                psb16 = ps.bitcast(BF16)
                for tt in range(4):
                    P.op("pe", lambda e, tt=tt, psb16=psb16, src=src, hc=hc: e.transpose(
                        psb16[:, tt * 128:(tt + 1) * 128], src[:, tb * 4 + tt, hc * 128:(hc + 1) * 128], identb[:]),
                        reads=[sB_], writes=[psB])
                P.op("act", lambda e, psb16=psb16, dst=dst, hc=hc: e.activation(out=dst[:, hc, :], in_=psb16[:, 0:512], func=AF.Identity),
                     reads=[psB], writes=[dB])
        for dt in range(8):
            g01, g01B = g_r.next()
            for gi, j in enumerate((dt, 8 + dt)):
                ps, psB = psr.next()
                for k in range(8):
                    P.op("pe", lambda e, k=k, ps=ps, j=j: e.matmul(ps[:], wg[:, k, j * 128:(j + 1) * 128], xb[:, k, :],
                                                                   start=(k == 0), stop=(k == 7)), reads=[xbB], writes=[psB])
                P.op("act", lambda e, ps=ps, j=j, gi=gi, g01=g01: e.activation(out=g01[:, gi, :], in_=ps[:], func=AF.Sigmoid,
                                                                              bias=bg[:, j:j + 1], scale=1.0),
                     reads=[psB], writes=[g01B])
            psa, psaB = psr.next()
            psn, psnB = psr.next()
            for ec in range(4):
                P.op("pe", lambda e, ec=ec, psa=psa, dt=dt: e.matmul(psa[:], wbd[:, ec, dt * 128:(dt + 1) * 128], aT[:, ec, :],
                                                                    start=(ec == 0), stop=(ec == 3)), reads=[aTB], writes=[psaB])
            for ec in range(4):
                P.op("pe", lambda e, ec=ec, psn=psn, dt=dt: e.matmul(psn[:], wbn[:, ec, dt * 128:(dt + 1) * 128], nT[:, ec, :],
                                                                    start=(ec == 0), stop=(ec == 3)), reads=[nTB], writes=[psnB])
            t4, u4, tB = tr.next()
            P.op("dve", lambda e, t4=t4, psa=psa, g01=g01: e.tensor_tensor(out=t4[:], in0=psa[:], in1=g01[:, 0, :], op=ALU.mult),
                 reads=[psaB, g01B], writes=[tB])
            P.op("dve", lambda e, u4=u4, psn=psn, g01=g01: e.tensor_tensor(out=u4[:], in0=psn[:], in1=g01[:, 1, :], op=ALU.mult),
                 reads=[psnB, g01B], writes=[tB])
            P.op("pool", lambda e, t4=t4, u4=u4, dt=dt: e.tensor_tensor(out=mT[:, dt, :], in0=t4[:], in1=u4[:], op=ALU.add),
                 reads=[tB], writes=[mTB])
        def stageA(tt):
            tok0 = tb * 512 + tt * 128
            y, yB = y_r.next()
            P.dma("sp", lambda e: e.dma_start(out=y[:], in_=T["x_own"][tok0:tok0 + 128, :]), writes=[yB])
            for half in range(2):
                ps, psB = psr.next()
                for k in range(8):
                    P.op("pe", lambda e, k=k, ps=ps, half=half: e.matmul(
                        ps[:], mT[:, k, tt * 128:(tt + 1) * 128], wo[:, k, half * 512:(half + 1) * 512],
                        start=(k == 0), stop=(k == 7)), reads=[mTB], writes=[psB])
                P.op("dve", lambda e, ps=ps, half=half: e.scalar_tensor_tensor(
                    out=y[:, half * 512:(half + 1) * 512], in0=y[:, half * 512:(half + 1) * 512], scalar=ALPHA, in1=ps[:],
                    op0=ALU.mult, op1=ALU.add), reads=[psB, yB], writes=[yB])
            x1, x1B = x1_r.next()
            sm, sB = sm_r.next()
            layer_norm_tile(P, y, yB, x1, x1B, g1bc, b1bc, cB, junk, jB, sm, sB)
            P.dma("sp", lambda e: e.dma_start(out=T["x1_d"][tok0:tok0 + 128, :], in_=x1[:]), reads=[x1B])
            return x1, x1B, tok0

        def stageB(tt, x1, x1B, tok0):
            x1Tf, x1Tb, xTB = x1Tf_r.next()
            for k2 in range(2):
                ps, psB = psr.next()
                for k4 in range(4):
                    k = k2 * 4 + k4
                    P.op("pe", lambda e, ps=ps, k=k, k4=k4, x1=x1: e.transpose(ps[:, k4 * 128:(k4 + 1) * 128], x1[:, k * 128:(k + 1) * 128],
                                                                        identf[:]), reads=[x1B], writes=[psB])
                P.op("act", lambda e, ps=ps, x1Tf=x1Tf, k2=k2: e.activation(out=x1Tf[:, k2 * 512:(k2 + 1) * 512], in_=ps[:], func=AF.Identity),
                     reads=[psB], writes=[xTB])
            ps, psB = psr.next()
            for k in range(8):
                P.op("pe", lambda e, ps=ps, k=k, x1Tf=x1Tf: e.matmul(ps[:, 0:32], x1Tf[:, k * 128:(k + 1) * 128], wr[:, k, :], start=(k == 0), stop=(k == 7)),
                     reads=[xTB], writes=[psB])
            lg, t8, mk, ex, rs, G, rB, mkb, rk, tm, slf = rt_r.next()
            P.op("dve", lambda e, ps=ps, lg=lg: e.tensor_tensor(out=lg[:], in0=ps[:, 0:32], in1=brbc[:], op=ALU.add), reads=[psB], writes=[rB])
            P.op("dve", lambda e, lg=lg, t8=t8: e.max(out=t8[:], in_=lg[:]), reads=[rB], writes=[rB])
            P.op("dve", lambda e, lg=lg, t8=t8, mk=mk: e.tensor_scalar(out=mk[:], in0=lg[:], scalar1=t8[:, 3:4], scalar2=None, op0=ALU.is_ge),
                 reads=[rB], writes=[rB])
            P.op("dve", lambda e, t8=t8, rs=rs: e.tensor_scalar(out=rs[:, 0:1], in0=t8[:, 0:1], scalar1=-1.0, scalar2=None, op0=ALU.mult),
                 reads=[rB], writes=[rB])
            P.op("act", lambda e, lg=lg, ex=ex, rs=rs: e.activation(out=ex[:], in_=lg[:], func=AF.Exp, bias=rs[:, 0:1], scale=1.0),
                 reads=[rB], writes=[rB])
            P.op("dve", lambda e, ex=ex, mk=mk: e.tensor_tensor(out=ex[:], in0=ex[:], in1=mk[:], op=ALU.mult), reads=[rB], writes=[rB])
            P.op("dve", lambda e, ex=ex, rs=rs: e.reduce_sum(out=rs[:, 1:2], in_=ex[:], axis=AX.X), reads=[rB], writes=[rB])
            P.op("dve", lambda e, rs=rs: e.reciprocal(out=rs[:, 2:3], in_=rs[:, 1:2]), reads=[rB], writes=[rB])
            P.op("dve", lambda e, ex=ex, rs=rs, G=G: e.tensor_scalar(out=G[:], in0=ex[:], scalar1=rs[:, 2:3], scalar2=None, op0=ALU.mult),
                 reads=[rB], writes=[rB])
            ps2, ps2B = psr.next()
            P.op("pe", lambda e, ps2=ps2, G=G: e.transpose(ps2[0:32, 0:128], G[:], identf[:]), reads=[rB], writes=[ps2B])
            P.op("act", lambda e, ps2=ps2, tok0=tok0: e.activation(out=GT[:, tok0:tok0 + 128], in_=ps2[0:32, 0:128], func=AF.Identity),
                 reads=[ps2B], writes=[GT_B])
            tix = tb * 4 + tt
            P.op("dve", lambda e, mk=mk, mkb=mkb: e.tensor_copy(out=mkb[:], in_=mk[:]), reads=[rB], writes=[rB])
            ps3, ps3B = psr.next()
            P.op("pe", lambda e, ps3=ps3, mkb=mkb: e.matmul(ps3[:, 0:32], ltri[:], mkb[:], start=True, stop=True, skip_group_check=True),
                 reads=[rB], writes=[ps3B])
            P.op("pe", lambda e, ps3=ps3, mkb=mkb: e.matmul(ps3[:, 64:96], ones[:], mkb[:], start=False, stop=True, skip_group_check=True),
                 reads=[rB], writes=[ps3B])
            P.op("dve", lambda e, ps3=ps3, rk=rk: e.tensor_tensor(out=rk[:], in0=ps3[:, 0:32], in1=cnt[:], op=ALU.add), reads=[ps3B, cntB], writes=[rB])
            P.op("dve", lambda e, ps3=ps3: e.tensor_tensor(out=cnt[:], in0=cnt[:], in1=ps3[:, 64:96], op=ALU.add), reads=[ps3B, cntB, rB], writes=[cntB])
            P.op("dve", lambda e, rk=rk: e.scalar_tensor_tensor(out=rk[:], in0=rk[:], scalar=float(CAP - 1), in1=ecap[:], op0=ALU.min, op1=ALU.add),
                 reads=[rB], writes=[rB])
            for k in range(4):
                P.op("dve", lambda e, k=k, lg=lg, t8=t8, rk=rk, tm=tm: e.scalar_tensor_tensor(out=tm[:], in0=lg[:], scalar=t8[:, k:k + 1], in1=rk[:],
                                                                                             op0=ALU.is_equal, op1=ALU.mult), reads=[rB], writes=[rB])
                P.op("dve", lambda e, k=k, tm=tm, slf=slf: e.reduce_sum(out=slf[:, k:k + 1], in_=tm[:], axis=AX.X), reads=[rB], writes=[rB])
            P.op("dve", lambda e, slf=slf, tix=tix: e.tensor_copy(out=slots_all[:, tix, :], in_=slf[:, 0:4]), reads=[rB], writes=[rB, rt_B])
            P.op("act", lambda e, t8=t8, slf=slf, rs=rs: e.activation(out=slf[:, 4:8], in_=t8[:, 0:4], func=AF.Exp, bias=rs[:, 0:1], scale=1.0),
                 reads=[rB], writes=[rB])
            P.op("dve", lambda e, slf=slf, rs=rs: e.reduce_sum(out=rs[:, 3:4], in_=slf[:, 4:8], axis=AX.X), reads=[rB], writes=[rB])
            P.op("dve", lambda e, rs=rs: e.reciprocal(out=rs[:, 3:4], in_=rs[:, 3:4]), reads=[rB], writes=[rB])
            P.op("dve", lambda e, slf=slf, rs=rs, tix=tix: e.tensor_scalar(out=gk_all[:, tix, :], in0=slf[:, 4:8], scalar1=rs[:, 3:4], scalar2=None,
                                                                          op0=ALU.mult), reads=[rB], writes=[rB, rt_B])
            if T.get("dbg_G") is not None:
                P.dma("sp", lambda e, G=G, tok0=tok0: e.dma_start(out=T["dbg_G"][tok0:tok0 + 128, :], in_=G[:]), reads=[rB])

        pend = stageA(0)
        for tt in range(4):
            nxt = stageA(tt + 1) if tt + 1 < 4 else None
            stageB(tt, *pend)
            pend = nxt

    for tb in range(8):
        do_block(tb)
    if T.get("dbg_cnt") is not None:
        P.dma("sp", lambda e: e.dma_start(out=T["dbg_cnt"], in_=cnt[:]), reads=[cntB])
    P.barrier()
def phase5s(P, nc, T, GT, GT_B, slots_all, gk_all, rt_B):
    P.sb_reset(T["arena5"])
    xb_r = Ring([(P.sb([128, 1024], BF16, "x1b"), Buf()) for _ in range(8)])
    for tix in range(32):
        x1b, x1bB = xb_r.next()
        P.dma("pool", lambda e, x1b=x1b, tix=tix: e.dma_start(out=x1b[:], in_=T["x1_d"][tix * 128:(tix + 1) * 128, :]), writes=[x1bB])
        for k in range(4):
            P.dma("pool", lambda e, k=k, x1b=x1b, tix=tix: e.indirect_dma_start(
                out=T["xs_d"], out_offset=bass.IndirectOffsetOnAxis(ap=slots_all[:, tix, k:k + 1], axis=0),
                in_=x1b[:], in_offset=None, bounds_check=None, oob_is_err=False), reads=[x1bB, rt_B])
    P.barrier()
    P.sb_reset(T["arena5"])
    NS = CAP // 128
    CH = [(0, 512), (512, 512), (1024, 256)]
    NH_ = 512
    b1a = P.sb([128, 32, 16], F32, "b1a")
    identb = P.sb([128, 128], BF16, "identb5")
    toks = [P.dma("sp", lambda e: e.dma_start(out=b1a[:], in_=T["b_mlp1"])),
            P.dma("pool", lambda e: e.dma_start(out=identb[:], in_=T["ident"]))]
    for eng in ("pe", "act", "dve"):
        P.op(eng, None, extra=toks)
    wr_ = Ring([(P.sb([128, 8, 1024], BF16, "w1g"), P.sb([128, 8, 1024], BF16, "w1l"), P.sb([128, 8, 1024], BF16, "w2"),
                 None, Buf(), Buf(), Buf(), None) for _ in range(2)])
    xs_tm = P.sb([128, NS, 1024], BF16, "xs_tm"); xB = Buf()
    xsT_r = [(P.sb([128, 8, CAP], BF16, "xsT"), Buf()) for _ in range(2)]
    actT = P.sb([128, 8, CAP], BF16, "actT"); aB = Buf()
    tmp_r = Ring([(P.sb([128, NH_], F32, "g5"), P.sb([128, NH_], F32, "s5"), P.sb([128, NH_], F32, "l5"), Buf()) for _ in range(2)])
    ys_r = Ring([(P.sb([128, 1024], BF16, "ys"), Buf()) for _ in range(3)])
    psr = Ring([(T["ps"][i], T["psb"][i]) for i in range(8)])
    xs3 = T["xs_d"].rearrange("(e s p) d -> e p s d", p=128, s=NS)
    ys3 = T["ys_d"].rearrange("(e s p) d -> e s p d", p=128, s=NS)

    def load_w(e_):
        w1g, w1l, w2, _x, gB, lB, wB, _b = wr_.next()
        for c in range(8):
            P.dma("pool", lambda e, c=c: e.dma_start(out=w1g[:, c, :], in_=T["w1g"][e_, c * 128:(c + 1) * 128, :]), writes=[gB])
            P.dma("pool", lambda e, c=c: e.dma_start(out=w1l[:, c, :], in_=T["w1l"][e_, c * 128:(c + 1) * 128, :]), writes=[lB])
        for c in range(8):
            P.dma("pool", lambda e, c=c: e.dma_start(out=w2[:, c, :], in_=T["w2"][e_, c * 128:(c + 1) * 128, :]), writes=[wB])
        return w1g, w1l, w2, None, gB, lB, wB, None

    def load_xs(e_):
        P.dma("sp", lambda e: e.dma_start(out=xs_tm[:], in_=xs3[e_]), writes=[xB])

    def tr_group(xsT, xsTB, k, s0, n):
        ps, psB = psr.next()
        psb16 = ps.bitcast(BF16)
        for i in range(n):
            P.op("pe", lambda e, i=i: e.transpose(psb16[:, i * 128:(i + 1) * 128], xs_tm[:, s0 + i, k * 128:(k + 1) * 128], identb[:]),
                 reads=[xB], writes=[psB])
        P.op("act", lambda e: e.activation(out=xsT[:, k, s0 * 128:(s0 + n) * 128], in_=psb16[:, 0:n * 128], func=AF.Identity),
             reads=[psB], writes=[xsTB])

    def mm1(e_, W, f, hf, xsT, xsTB):
        w1g, w1l, w2, _x, gB, lB, wB, _b = W
        c0, cn = CH[hf]
        sl = slice(c0, c0 + cn)
        pg, pgB = psr.next()
        pl, plB = psr.next()
        for k in range(8):
            P.op("pe", lambda e, k=k: e.matmul(pg[:, 0:cn], w1g[:, k, f * 128:(f + 1) * 128], xsT[:, k, sl],
                                               start=(k == 0), stop=(k == 7)), reads=[gB, xsTB], writes=[pgB])
        for k in range(8):
            P.op("pe", lambda e, k=k: e.matmul(pl[:, 0:cn], w1l[:, k, f * 128:(f + 1) * 128], xsT[:, k, sl],
                                               start=(k == 0), stop=(k == 7)), reads=[lB, xsTB], writes=[plB])
        g5, s5, l5, tB = tmp_r.next()
        P.op("act", lambda e: e.activation(out=l5[:, 0:cn], in_=pl[:, 0:cn], func=AF.Identity, bias=b1a[:, e_, 8 + f:9 + f], scale=1.0),
             reads=[plB], writes=[tB])
        P.op("dve", lambda e: e.tensor_scalar(out=g5[:, 0:cn], in0=pg[:, 0:cn], scalar1=b1a[:, e_, f:f + 1], scalar2=7.0,
                                              op0=ALU.add, op1=ALU.min), reads=[pgB], writes=[tB])
        P.op("act", lambda e: e.activation(out=s5[:, 0:cn], in_=g5[:, 0:cn], func=AF.Sigmoid, scale=1.702), reads=[tB], writes=[tB])
        P.op("dve", lambda e: e.tensor_scalar(out=l5[:, 0:cn], in0=l5[:, 0:cn], scalar1=-7.0, scalar2=7.0, op0=ALU.max, op1=ALU.min),
             reads=[tB], writes=[tB])
        P.op("dve", lambda e: e.tensor_tensor(out=g5[:, 0:cn], in0=g5[:, 0:cn], in1=s5[:, 0:cn], op=ALU.mult), reads=[tB], writes=[tB])
        P.op("dve", lambda e: e.scalar_tensor_tensor(out=actT[:, f, sl], in0=l5[:, 0:cn], scalar=1.0, in1=g5[:, 0:cn], op0=ALU.add, op1=ALU.mult),
             reads=[tB], writes=[aB])

    def mm2(e_, W, st):
        w1g, w1l, w2, _x, gB, lB, wB, _b = W
        ys, yB = ys_r.next()
        for half in range(2):
            ps, psB = psr.next()
            for k in range(8):
                P.op("pe", lambda e, k=k, ps=ps, half=half: e.matmul(ps[:], actT[:, k, st * 128:(st + 1) * 128],
                                                                    w2[:, k, half * 512:(half + 1) * 512],
                                                                    start=(k == 0), stop=(k == 7)), reads=[aB, wB], writes=[psB])
            if half == 0:
                P.op("act", lambda e, ps=ps: e.activation(out=ys[:, 0:512], in_=ps[:], func=AF.Identity), reads=[psB], writes=[yB])
            else:
                P.op("dve", lambda e, ps=ps: e.tensor_copy(out=ys[:, 512:1024], in_=ps[:]), reads=[psB], writes=[yB])
        P.dma("sp", lambda e: e.dma_start(out=ys3[e_, st], in_=ys[:]), reads=[yB])

    NE = T.get("n_exp", 32)

    def transposes(e_):
        xsT, xsTB = xsT_r[e_ % 2]
        for k in range(8):
            for s0 in range(0, NS, 4):
                tr_group(xsT, xsTB, k, s0, min(4, NS - s0))

    W = load_w(0)
    load_xs(0)
    transposes(0)
    for e_ in range(NE):
        Wn = load_w(e_ + 1) if e_ + 1 < NE else None
        if e_ + 1 < NE:
            load_xs(e_ + 1)
        xsT, xsTB = xsT_r[e_ % 2]
        for f in range(8):
            for hf in range(len(CH)):
                mm1(e_, W, f, hf, xsT, xsTB)
        if e_ + 1 < NE:
            transposes(e_ + 1)
        for st in range(NS):
            mm2(e_, W, st)
        W = Wn
    P.barrier()

    P.sb_reset(T["arena5"])
    b2b = P.sb([32, 1024], BF16, "b2b")
    g2bc = P.sb([128, 1024], F32, "g2bc")
    b2bc = P.sb([128, 1024], F32, "b2bc")
    cB = Buf()
    toks = [P.dma("pool", lambda e: e.dma_start(out=b2b[:], in_=T["b_mlp2"])),
            P.dma("sp", lambda e: e.dma_start(out=g2bc[:], in_=T["ln2_g_bc"])),
            P.dma("sp", lambda e: e.dma_start(out=b2bc[:], in_=T["ln2_b_bc"]))]
    for eng in ("pe", "act", "dve"):
        P.op(eng, None, extra=toks)
    rows_r = Ring([(P.sb([128, 1024], BF16, "rows"), Buf()) for _ in range(16)])
    x1_r = Ring([(P.sb([128, 1024], F32, "x1c"), Buf()) for _ in range(4)])
    acc_r = Ring([(P.sb([128, 1024], F32, "accc"), Buf()) for _ in range(4)])
    out_r = Ring([(P.sb([128, 1024], F32, "outc"), Buf()) for _ in range(3)])
    junk = P.sb([128, 1024], BF16, "junk6"); jB = Buf()
    sm_r = Ring([(P.sb([128, 8], F32, "sm6"), Buf()) for _ in range(2)])

    def comb_tile(ti):
        tok0 = ti * 128
        x1c, x1B = x1_r.next()
        P.dma("sp", lambda e: e.dma_start(out=x1c[:], in_=T["x1_d"][tok0:tok0 + 128, :]), writes=[x1B])
        acc, accB = acc_r.next()
        for half in range(2):
            ps, psB = psr.next()
            P.op("pe", lambda e, ps=ps, half=half: e.matmul(ps[:], GT[:, tok0:tok0 + 128], b2b[:, half * 512:(half + 1) * 512],
                                                            start=True, stop=True), reads=[GT_B], writes=[psB])
            P.op("dve", lambda e, ps=ps, half=half: e.scalar_tensor_tensor(out=acc[:, half * 512:(half + 1) * 512],
                                                                           in0=x1c[:, half * 512:(half + 1) * 512], scalar=ALPHA, in1=ps[:],
                                                                           op0=ALU.mult, op1=ALU.add), reads=[psB, x1B], writes=[accB])
        for k in range(4):
            rows, rwB = rows_r.next()
            P.dma("pool", lambda e, rows=rows, k=k: e.indirect_dma_start(
                out=rows[:], out_offset=None, in_=T["ys_d"],
                in_offset=bass.IndirectOffsetOnAxis(ap=slots_all[:, ti, k:k + 1], axis=0),
                bounds_check=None, oob_is_err=False), reads=[rt_B], writes=[rwB])
            P.op("dve", lambda e, rows=rows, k=k: e.scalar_tensor_tensor(out=acc[:], in0=rows[:], scalar=gk_all[:, ti, k:k + 1], in1=acc[:],
                                                                         op0=ALU.mult, op1=ALU.add), reads=[rwB, rt_B, accB], writes=[accB])
        o, oB = out_r.next()
        sm, sB = sm_r.next()
        layer_norm_tile(P, acc, accB, o, oB, g2bc, b2bc, cB, junk, jB, sm, sB)
        P.dma("sp", lambda e: e.dma_start(out=T["out"][tok0:tok0 + 128, :], in_=o[:]), reads=[oB])

    for ti in range(32):
        comb_tile(ti)
    P.barrier()
def build(upto=99, debug=False):
    nc = bass.Bass("TRN2", target_bir_lowering=False)
    T = {}

    def inp(name, shape, dt=F32):
        T[name] = nc.dram_tensor(name, list(shape), dt, kind="ExternalInput").ap()

    def scr(name, shape, dt=BF16):
        T[name] = nc.dram_tensor(name, list(shape), dt, kind=("ExternalOutput" if (debug and debug.get("dump_scr")) else "Internal")).ap()

    def outp(name, shape, dt=F32):
        T[name] = nc.dram_tensor(name, list(shape), dt, kind="ExternalOutput").ap()

    inp("xT", [1024, 8192]); inp("x_own", [4096, 1024])
    inp("w_fm", [1024, 3072]); inp("w_tm", [1024, 1024]); inp("b_fm", [128, 24]); inp("b_tm", [128, 1024])
    inp("cosT", [128, 8192]); inp("sinT", [128, 8192]); inp("lamv", [128, 256]); inp("subln_bc", [128, 128])
    scr("qT_da", [4, 128, 4096]); scr("kT_da", [4, 128, 8192]); scr("qT_na", [4, 128, 4096]); scr("kT_na", [4, 128, 8192])
    scr("v_da", [8192, 512]); scr("v_na", [8192, 512])
    if upto >= 3:
        inp("na_R", [8, 128, 18 * 256]); inp("na_M", [128, 18 * 256])
    if upto >= 4:
        inp("w_gate", [1024, 2048]); inp("b_gate", [128, 16]); inp("w_bda", [512, 1024]); inp("w_bna", [512, 1024])
        inp("w_out", [1024, 1024]); inp("w_router", [1024, 32]); inp("ln1_g_bc", [128, 1024]); inp("ln1_b_bc", [128, 1024])
        inp("b_router_bc", [128, 32]); inp("ident", [128, 128])
        scr("x1_d", [4096, 1024], F32)
        inp("ltri", [128, 128]); inp("ones128", [128, 128]); inp("ecap", [128, 32])
        scr("xs_d", [NEXP * CAP, 1024], BF16); scr("ys_d", [NEXP * CAP, 1024], BF16)
        if debug and debug.get("dump_scr"):
            outp("dbg_G", [4096, 32])
        if debug and debug.get("dump_cnt"):
            outp("dbg_cnt", [128, 32])
    if upto >= 5:
        inp("b_mlp1", [128, 32, 16]); inp("b_mlp2", [32, 1024])
        inp("ln2_g_bc", [128, 1024]); inp("ln2_b_bc", [128, 1024])
        inp("w1g", [32, 1024, 1024]); inp("w1l", [32, 1024, 1024]); inp("w2", [32, 1024, 1024])
        outp("out", [4096, 1024])
    psall = nc.alloc_psum_tensor("psall", [128, 4096], F32)
    T["ps"] = [psall[:, i * 512:(i + 1) * 512] for i in range(8)]
    T["ps2"] = [psall[:, 2048:3072], psall[:, 3072:4096]]
    T["psb"] = [Buf() for _ in range(8)]
    P = Prog(nc)
    GT = P.sb([32, 4096], BF16, "GT")
    GT_B = Buf()
    slots_all = P.sb([128, 32, 4], I32, "slots_all")
    gk_all = P.sb([128, 32, 4], F32, "gk_all")
    rt_B = Buf()
    T["arena5"] = P.sb_off
    a_sb = P.sb([128, 32, 512], BF16, "a_sb")
    nb_sb = P.sb([128, 32, 512], BF16, "nb_sb")
    a_B, nb_B = Buf(), Buf()
    T["arena0"] = P.sb_off
    if debug:
        T["da_heads"] = debug.get("da_heads", 4)
        T["n_exp"] = debug.get("n_exp", 32)
    phase1(P, nc, T)
    if upto >= 2:
        phase2(P, nc, T, a_sb, a_B)
    if upto >= 3:
        phase3(P, nc, T, nb_sb, nb_B)
    if upto >= 4:
        phase4(P, nc, T, a_sb, a_B, nb_sb, nb_B, GT, GT_B, slots_all, gk_all, rt_B)
    if upto >= 5:
        phase5s(P, nc, T, GT, GT_B, slots_all, gk_all, rt_B)
    if upto < 4:
        outp("dbg_a", [4096, 512], BF16); outp("dbg_nb", [4096, 512], BF16)
        P.dma("sp", lambda e: e.dma_start(out=T["dbg_a"].rearrange("(t p) e -> p t e", p=128), in_=a_sb[:]), reads=[a_B])
        P.dma("sp", lambda e: e.dma_start(out=T["dbg_nb"].rearrange("(t p) e -> p t e", p=128), in_=nb_sb[:]), reads=[nb_B])
        if debug and debug.get("dump_scr"):
            for nm in ("qT_da", "kT_da", "v_da", "qT_na", "kT_na", "v_na"):
                pass
    P.barrier()
    P.emit()
    return nc, P


def rope_tables(pos):
    inv = (10000.0 ** (-np.arange(0, 64, 2, dtype=np.float32) / np.float32(64))).astype(np.float32)
    ang = pos.astype(np.float32)[:, None] * inv[None, :]
    ang = np.concatenate([ang, ang], axis=-1)
    cos = np.cos(ang).astype(np.float32)
    sin = np.sin(ang).astype(np.float32)
    sgn = np.concatenate([-np.ones(32, np.float32), np.ones(32, np.float32)])
    sin_s = sin * sgn[None, :]
    cosT = np.ascontiguousarray(np.concatenate([cos, cos], axis=1).T)
    sinT = np.ascontiguousarray(np.concatenate([sin_s, sin_s], axis=1).T)
    return cosT, sinT


def na_tables(rpb, h):
    R = np.zeros((8, 3, 6, 2, 64, 4, 64), np.float32)
    M = np.zeros((3, 6, 2, 64, 4, 64), np.float32)
    cc = np.arange(64)
    cs = np.clip(cc - 8, 0, 48)
    colvalid = (cc[:, None] >= cs[None, :]) & (cc[:, None] <= cs[None, :] + 15)
    coloff = np.clip(cc[:, None] - cc[None, :] + 15, 0, 30)
    for cls, g in ((0, 0), (1, 1 if h == 0 else 14), (2, 15)):
        for j in range(6):
            for a in range(2):
                for i in range(4):
                    r = 64 * h + 4 * g + i
                    kr = 64 * h + 4 * g + 2 * j - 4 + a
                    rs = min(max(r - 4, 0), 120)
                    if kr < rs or kr > rs + 7:
                        continue
                    M[cls, j, a, :, i, :] = colvalid
                    R[:, cls, j, a, :, i, :] = rpb[:, kr - r + 7][:, coloff] * colvalid[None]
    M2 = np.ascontiguousarray(M.transpose(2, 3, 0, 1, 4, 5).reshape(128, 18 * 256))
    R2 = np.ascontiguousarray(R.transpose(0, 3, 4, 1, 2, 5, 6).reshape(8, 128, 18 * 256))
    return R2, M2


def host_prep(inputs, upto=99):
    x = np.asarray(inputs["x"], np.float32)
    w_in = np.asarray(inputs["w_in"], np.float32)[0]
    b_in = np.asarray(inputs["b_in"], np.float32)[0]
    d = np.arange(64)
    swap = np.concatenate([(hh * 128 + c * 64 + (d + 32) % 64) for hh in range(4) for c in range(2)])
    qda, kda, vda = np.arange(0, 512), np.arange(512, 1024), np.arange(1024, 1536)
    qna, kna, vna = np.arange(1536, 2048), np.arange(2048, 2560), np.arange(2560, 3072)
    fm_cols = np.concatenate([qda, qda[swap], kda, kda[swap], qna, kna])
    tm_cols = np.concatenate([vda, vna])
    w_fm = np.ascontiguousarray(w_in[:, fm_cols])
    w_tm = np.ascontiguousarray(w_in[:, tm_cols])
    b_fm = np.ascontiguousarray(b_in[fm_cols].reshape(24, 128).T)
    b_tm = np.ascontiguousarray(np.broadcast_to(b_in[tm_cols][None, :], (128, 1024)))
    lamv = np.concatenate([np.asarray(inputs[k], np.float32)[0] for k in ("lambda_q1", "lambda_k1", "lambda_q2", "lambda_k2")])
    lamv = np.ascontiguousarray(np.broadcast_to(lamv[None, :], (128, 256)))
    subln_bc = np.ascontiguousarray(np.broadcast_to(np.asarray(inputs["subln_g"], np.float32)[0][None, :], (128, 128)))
    rpb = np.asarray(inputs["rpb"], np.float32)[0]
    shared = dict(w_fm=w_fm, w_tm=w_tm, b_fm=b_fm, b_tm=b_tm, lamv=lamv, subln_bc=subln_bc)
    f32 = lambda k: np.asarray(inputs[k], np.float32)[0]
    bc = lambda v, n=128: np.ascontiguousarray(np.broadcast_to(v[None, :], (n, v.shape[0])))
    if upto >= 4:
        shared.update(w_gate=np.ascontiguousarray(w_in[:, 3072:5120]), b_gate=np.ascontiguousarray(b_in[3072:5120].reshape(16, 128).T),
                      w_bda=f32("w_branch_da"), w_bna=f32("w_branch_na"), w_out=f32("w_out"), w_router=f32("w_router"),
                      ln1_g_bc=bc(f32("ln1_g")), ln1_b_bc=bc(f32("ln1_b")), b_router_bc=bc(f32("b_router")),
                      ident=np.eye(128, dtype=np.float32), ltri=np.triu(np.ones((128, 128), np.float32), k=1),
                      ones128=np.ones((128, 128), np.float32),
                      ecap=np.ascontiguousarray(np.broadcast_to((np.arange(32, dtype=np.float32) * CAP)[None, :], (128, 32))))
    if upto >= 5:
        w1 = f32("w_mlp1"); b1 = f32("b_mlp1")
        b1g = b1[:, 0::2].reshape(32, 8, 128).transpose(2, 0, 1)
        b1l = b1[:, 1::2].reshape(32, 8, 128).transpose(2, 0, 1)
        shared.update(b_mlp1=np.ascontiguousarray(np.concatenate([b1g, b1l], axis=2)),
                      b_mlp2=f32("b_mlp2"), ln2_g_bc=bc(f32("ln2_g")), ln2_b_bc=bc(f32("ln2_b")),
                      w1g=np.ascontiguousarray(w1[:, :, 0::2]), w1l=np.ascontiguousarray(w1[:, :, 1::2]), w2=f32("w_mlp2"))
    natab = [na_tables(rpb, h) for h in range(2)] if upto >= 3 else None
    maps = []
    for c in range(NCORES):
        b, h = c // 2, c % 2
        perm = np.concatenate([np.arange(h * 4096, (h + 1) * 4096), np.arange((1 - h) * 4096, (2 - h) * 4096)])
        xb = x[b]
        cosT, sinT = rope_tables(perm)
        m = dict(shared)
        m["xT"] = np.ascontiguousarray(xb[perm].T)
        m["x_own"] = np.ascontiguousarray(xb[h * 4096:(h + 1) * 4096])
        m["cosT"] = cosT
        m["sinT"] = sinT
        if upto >= 3:
            m["na_R"], m["na_M"] = natab[h]
        maps.append(m)
    return maps


def kernel(**inputs):
    from concourse.bass_utils import run_bass_kernel_spmd
    maps = host_prep(inputs)
    nc, P = build()
    res = run_bass_kernel_spmd(nc, maps, core_ids=list(range(NCORES)))
    out = np.zeros((4, SEQ, D), np.float32)
    for c in range(NCORES):
        b, h = c // 2, c % 2
        out[b, h * 4096:(h + 1) * 4096] = np.asarray(res.results[c]["out"], np.float32)
    return out
```

```python
import numpy as np
import concourse.bass as bass
import concourse.mybir as mybir

F32 = mybir.dt.float32
BF16 = mybir.dt.bfloat16
I32 = mybir.dt.int32
U32 = mybir.dt.uint32
AF = mybir.ActivationFunctionType
ALU = mybir.AluOpType
AX = mybir.AxisListType

ENGS = ("pe", "act", "dve", "pool", "sp")
KDMA = 8


class Tok:
    __slots__ = ("eng", "seq", "dma")

    def __init__(self, eng, seq, dma=None):
        self.eng = eng
        self.seq = seq
        self.dma = dma


class Buf:
    __slots__ = ("w", "r", "name")

    def __init__(self, name=""):
        self.w = None
        self.r = []
        self.name = name


class Prog:
    def __init__(self, nc):
        self.nc = nc
        self.ops = {e: [] for e in ENGS}
        self.ndma = {e: 0 for e in ENGS}
        self.all_dma = []
        self.sb_off = self.SB_BASE
        self.sb_hi = 0
        self.uid = 0

    SB_BASE = 16640
    SB_END = 229376

    def sb_reset(self, off=None):
        self.sb_off = self.SB_BASE if off is None else off

    def sb(self, shape, dtype, name=None):
        self.uid += 1
        nm = "%s_%d" % (name or "t", self.uid)
        nbytes = int(np.prod(shape[1:])) * mybir.dt.size(dtype)
        off = (self.sb_off + 63) // 64 * 64
        t = self.nc.alloc_sbuf_tensor_at(nm, list(shape), dtype, offset=off)
        self.sb_off = off + nbytes
        self.sb_hi = max(self.sb_hi, self.sb_off)
        assert self.sb_off <= self.SB_END, ("SBUF overflow", nm, self.sb_off)
        return t

    def _deps(self, reads, writes, extra):
        deps = {}
        for b in reads:
            if b.w is not None:
                deps[id(b.w)] = b.w
        for b in writes:
            if b.w is not None:
                deps[id(b.w)] = b.w
            for t in b.r:
                deps[id(t)] = t
        for t in extra:
            if t is not None:
                deps[id(t)] = t
        return list(deps.values())

    def _commit(self, tok, reads, writes):
        for b in writes:
            b.w = tok
            b.r = []
        for b in reads:
            if tok.dma is None:
                b.r = [t for t in b.r if not (t.dma is None and t.eng == tok.eng)]
            b.r.append(tok)

    def op(self, eng, fn, reads=(), writes=(), extra=()):
        deps = self._deps(reads, writes, extra)
        if eng == "pe":
            deps = [t for t in deps if not (t.eng == "pe" and t.dma is None)]
        tok = Tok(eng, len(self.ops[eng]))
        self.ops[eng].append(dict(fn=fn, waits=deps, tok=tok, dma=False))
        self._commit(tok, reads, writes)
        return tok

    def dma(self, eng, fn, reads=(), writes=(), extra=()):
        deps = self._deps(reads, writes, extra)
        i = self.ndma[eng]
        self.ndma[eng] += 1
        tok = Tok(eng, len(self.ops[eng]), dma=(i % KDMA, 16 * (i // KDMA + 1)))
        self.ops[eng].append(dict(fn=fn, waits=deps, tok=tok, dma=True, idx=i))
        self.all_dma.append(tok)
        self._commit(tok, reads, writes)
        return tok

    def barrier(self):
        lasts = []
        for e in ENGS:
            for o in reversed(self.ops[e]):
                if not o["dma"]:
                    lasts.append(o["tok"])
                    break
        dm = list(self.all_dma)
        self.all_dma = []
        for e in ENGS:
            if e == "sp":
                self.op(e, None, extra=lasts + dm)
            else:
                self.op(e, None, extra=lasts + dm)

    def emit(self):
        nc = self.nc
        needed = {e: set() for e in ENGS}
        for e in ENGS:
            for o in self.ops[e]:
                for t in o["waits"]:
                    if t.dma is None:
                        needed[t.eng].add(t.seq)
        tokval = {e: {} for e in ENGS}
        for e in ENGS:
            c = 0
            for o in self.ops[e]:
                if o["dma"]:
                    continue
                if o["tok"].seq in needed[e]:
                    c += 1
                    tokval[e][o["tok"].seq] = c
            self.maxcount = getattr(self, "maxcount", {})
            self.maxcount[e] = c
        engobj = {"pe": nc.tensor, "act": nc.scalar, "dve": nc.vector, "pool": nc.gpsimd, "sp": nc.sync}
        import contextlib
        with contextlib.ExitStack() as st:
            csem = {e: st.enter_context(nc.semaphore("c_" + e)) for e in ENGS}
            dsem = {e: [st.enter_context(nc.semaphore("d_%s%d" % (e, k))) for k in range(KDMA)]
                    for e in ENGS if self.ndma[e] > 0}
            block = st.enter_context(nc.Block())

            def run(e, engine):
                waited = {}
                for o in self.ops[e]:
                    for t in o["waits"]:
                        if t.dma is not None:
                            sem = dsem[t.eng][t.dma[0]]
                            val = t.dma[1]
                            key = ("d", t.eng, t.dma[0])
                        else:
                            sem = csem[t.eng]
                            val = tokval[t.eng][t.seq]
                            key = ("c", t.eng)
                        if waited.get(key, 0) >= val:
                            continue
                        waited[key] = val
                        engine.wait_ge(sem, val)
                    if o["dma"]:
                        i = o["idx"]
                        slot = i % KDMA
                        if i >= KDMA:
                            key = ("d", e, slot)
                            val = 16 * (i // KDMA)
                            if waited.get(key, 0) < val:
                                waited[key] = val
                                engine.wait_ge(dsem[e][slot], val)
                        ins = o["fn"](engine)
                        ins.then_inc(dsem[e][slot], 16)
                    else:
                        if o["fn"] is None:
                            if o["tok"].seq in needed[e]:
                                ins = engine.nop() if hasattr(engine, "nop") else None
                                ins.then_inc(csem[e], 1)
                            continue
                        ins = o["fn"](engine)
                        if o["tok"].seq in needed[e]:
                            ins.then_inc(csem[e], 1)

            @block.tensor
            def _(eng):
                run("pe", eng)

            @block.scalar
            def _(eng):
                run("act", eng)

            @block.vector
            def _(eng):
                run("dve", eng)

            @block.gpsimd
            def _(eng):
                run("pool", eng)

            @block.sync
            def _(eng):
                run("sp", eng)
D = 1024
SEQ = 8192
NOWN = 4096
NCORES = 8
CAP = 1280
NEXP = 32
LAM_INIT = 0.2
ALPHA = 2.0 ** 0.25


class Ring:
    def __init__(self, items):
        self.items = list(items)
        self.i = 0

    def next(self):
        it = self.items[self.i % len(self.items)]
        self.i += 1
        return it


def phase1(P, nc, T):
    P.sb_reset()
    wfm = P.sb([128, 8, 3072], BF16, "wfm")
    wtm = P.sb([128, 8, 1024], BF16, "wtm")
    bfm = P.sb([128, 24], F32, "bfm")
    btm = P.sb([128, 1024], F32, "btm")
    b_w = Buf()
    wtoks = []
    for c in range(8):
        for g in range(6):
            wtoks.append(P.dma("pool", lambda e, c=c, g=g: e.dma_start(
                out=wfm[:, c, g * 512:(g + 1) * 512], in_=T["w_fm"][c * 128:(c + 1) * 128, g * 512:(g + 1) * 512])))
        for g in range(2):
            wtoks.append(P.dma("pool", lambda e, c=c, g=g: e.dma_start(
                out=wtm[:, c, g * 512:(g + 1) * 512], in_=T["w_tm"][c * 128:(c + 1) * 128, g * 512:(g + 1) * 512])))
    wtoks.append(P.dma("sp", lambda e: e.dma_start(out=bfm[:], in_=T["b_fm"])))
    wtoks.append(P.dma("sp", lambda e: e.dma_start(out=btm[:], in_=T["b_tm"])))
    for eng in ("pe", "act", "dve"):
        P.op(eng, None, extra=wtoks)

    xblk = Ring([(P.sb([128, 8, 512], BF16, "xblk"), Buf()) for _ in range(2)])
    csb = Ring([(P.sb([128, 512], F32, "cos"), P.sb([128, 512], F32, "sin"), Buf()) for _ in range(2)])
    t1r = Ring([(P.sb([128, 512], F32, "t1"), Buf()) for _ in range(2)])
    t2r = Ring([(P.sb([128, 512], F32, "t2"), Buf()) for _ in range(2)])
    stg = Ring([(P.sb([128, 512], BF16, "stg"), Buf()) for _ in range(6)])
    psr = Ring([(T["ps"][i], T["psb"][i]) for i in range(8)])
    xT3 = T["xT"].rearrange("(c p) t -> p c t", p=128)

    def do_block(i):
        own = i < 8
        xb, xbB = xblk.next()
        P.dma("pool", lambda e, xb=xb, i=i: e.dma_start(out=xb[:], in_=xT3[:, :, i * 512:(i + 1) * 512]), writes=[xbB])
        cb, sb_, csB = csb.next()
        P.dma("sp", lambda e, cb=cb, i=i: e.dma_start(out=cb[:], in_=T["cosT"][:, i * 512:(i + 1) * 512]), writes=[csB])
        P.dma("sp", lambda e, sb_=sb_, i=i: e.dma_start(out=sb_[:], in_=T["sinT"][:, i * 512:(i + 1) * 512]), writes=[csB])

        def mm_fm(ps, psB, col):
            for k in range(8):
                P.op("pe", lambda e, k=k: e.matmul(ps[:], wfm[:, k, col:col + 128], xb[:, k, :],
                                                   start=(k == 0), stop=(k == 7)),
                     reads=[xbB], writes=[psB])

        def rope_tile(col0, col1, dst):
            psA, psAB = psr.next()
            psC, psCB = psr.next()
            mm_fm(psA, psAB, col0)
            mm_fm(psC, psCB, col1)
            t1, t1B = t1r.next()
            t2, t2B = t2r.next()
            P.op("dve", lambda e: e.scalar_tensor_tensor(out=t1[:], in0=psA[:], scalar=bfm[:, col0 // 128:col0 // 128 + 1],
                                                          in1=cb[:], op0=ALU.add, op1=ALU.mult),
                 reads=[psAB, csB], writes=[t1B])
            P.op("dve", lambda e: e.scalar_tensor_tensor(out=t2[:], in0=psC[:], scalar=bfm[:, col1 // 128:col1 // 128 + 1],
                                                          in1=sb_[:], op0=ALU.add, op1=ALU.mult),
                 reads=[psCB, csB], writes=[t2B])
            st, stB = stg.next()
            P.op("pool", lambda e: e.tensor_tensor(out=st[:], in0=t1[:], in1=t2[:], op=ALU.add),
                 reads=[t1B, t2B], writes=[stB])
            P.dma("sp", lambda e: e.dma_start(out=dst, in_=st[:]), reads=[stB])

        def plain_tile(col, dst):
            ps, psB = psr.next()
            mm_fm(ps, psB, col)
            st, stB = stg.next()
            P.op("act", lambda e: e.activation(out=st[:], in_=ps[:], func=AF.Identity,
                                               bias=bfm[:, col // 128:col // 128 + 1], scale=1.0),
                 reads=[psB], writes=[stB])
            P.dma("sp", lambda e: e.dma_start(out=dst, in_=st[:]), reads=[stB])

        sl = slice(i * 512, (i + 1) * 512)
        for h in range(4):
            if own:
                rope_tile(h * 128, 512 + h * 128, T["qT_da"][h, :, sl])
            rope_tile(1024 + h * 128, 1536 + h * 128, T["kT_da"][h, :, sl])
        na_kv = (i <= 8) or (i == 15)
        for j in range(4):
            if own:
                plain_tile(2048 + j * 128, T["qT_na"][j, :, sl])
            if na_kv:
                plain_tile(2560 + j * 128, T["kT_na"][j, :, sl])
        for tt in range(4):
            for g, dst in (((0, T["v_da"]), (1, T["v_na"])) if na_kv else ((0, T["v_da"]),)):
                ps, psB = psr.next()
                for k in range(8):
                    P.op("pe", lambda e, k=k, ps=ps, g=g, tt=tt: e.matmul(
                        ps[:], xb[:, k, tt * 128:(tt + 1) * 128], wtm[:, k, g * 512:(g + 1) * 512],
                        start=(k == 0), stop=(k == 7)), reads=[xbB], writes=[psB])
                st, stB = stg.next()
                P.op("dve", lambda e, ps=ps, st=st, g=g: e.tensor_tensor(out=st[:], in0=ps[:], in1=btm[:, g * 512:(g + 1) * 512],
                                                                         op=ALU.add), reads=[psB], writes=[stB])
                r0 = i * 512 + tt * 128
                P.dma("sp", lambda e, st=st, dst=dst, r0=r0: e.dma_start(out=dst[r0:r0 + 128, :], in_=st[:]), reads=[stB])

    for i in range(16):
        do_block(i)
    P.barrier()
def phase2(P, nc, T, a_sb, a_B):
    P.sb_reset(T["arena0"])
    lamv = P.sb([128, 256], F32, "lamv")
    gbc = P.sb([128, 128], F32, "gbc")
    tmp64 = P.sb([128, 128], F32, "tmp64")
    sc = P.sb([128, 8], F32, "sc")
    neglam = P.sb([128, 1], F32, "neglam")
    cB = Buf()
    P.dma("sp", lambda e: e.dma_start(out=lamv[:], in_=T["lamv"]), writes=[cB])
    P.dma("sp", lambda e: e.dma_start(out=gbc[:], in_=T["subln_bc"]), writes=[cB])
    P.op("dve", lambda e: e.tensor_tensor(out=tmp64[:, 0:64], in0=lamv[:, 0:64], in1=lamv[:, 64:128], op=ALU.mult), reads=[cB], writes=[cB])
    P.op("dve", lambda e: e.tensor_tensor(out=tmp64[:, 64:128], in0=lamv[:, 128:192], in1=lamv[:, 192:256], op=ALU.mult), reads=[cB], writes=[cB])
    P.op("dve", lambda e: e.reduce_sum(out=sc[:, 0:1], in_=tmp64[:, 0:64], axis=AX.X), reads=[cB], writes=[cB])
    P.op("dve", lambda e: e.reduce_sum(out=sc[:, 1:2], in_=tmp64[:, 64:128], axis=AX.X), reads=[cB], writes=[cB])
    P.op("act", lambda e: e.activation(out=sc[:, 2:4], in_=sc[:, 0:2], func=AF.Exp), reads=[cB], writes=[cB])
    P.op("dve", lambda e: e.tensor_tensor(out=sc[:, 4:5], in0=sc[:, 3:4], in1=sc[:, 2:3], op=ALU.subtract), reads=[cB], writes=[cB])
    P.op("dve", lambda e: e.tensor_scalar(out=neglam[:], in0=sc[:, 4:5], scalar1=-LAM_INIT, scalar2=None, op0=ALU.add), reads=[cB], writes=[cB])
    P.op("dve", lambda e: e.tensor_scalar(out=gbc[:], in0=gbc[:], scalar1=1.0 - LAM_INIT, scalar2=None, op0=ALU.mult), reads=[cB], writes=[cB])

    hb = []
    for _ in range(2):
        KT = P.sb([128, 8192], BF16, "KT")
        V = P.sb([128, 64, 129], BF16, "V")
        QT = P.sb([128, 2, 4096], BF16, "QT")
        oB = Buf()
        P.op("pool", lambda e, V=V: e.memset(V[:, :, 128:129], 1.0), writes=[oB])
        P.op("pool", lambda e, QT=QT: e.memset(QT[64:128, 0, :], 0.0), writes=[oB])
        P.op("pool", lambda e, QT=QT: e.memset(QT[0:64, 1, :], 0.0), writes=[oB])
        hb.append((KT, V, QT, Buf(), Buf(), Buf(), oB))
    ptr = Ring([(P.sb([128, 512], BF16, "pt"), Buf()) for _ in range(4)])
    sps = Ring([(T["ps"][i], T["psb"][i]) for i in range(4, 8)])
    accs = {}
    lay = [(0, 0), (0, 1), (0, 2), (1, 0), (2, 0), (2, 1), (2, 2), (3, 0)]
    n = 0
    for c in range(2):
        for qt in range(4):
            bk, pos = lay[n]
            n += 1
            accs[(c, qt)] = (T["ps"][bk][:, pos * 129:(pos + 1) * 129], T["psb"][bk])
    small = Ring([(P.sb([128, 8], F32, "sm"), P.sb([128, 128], F32, "tt"), P.sb([128, 128], F32, "oo"),
                   P.sb([128, 128], F32, "sq"), Buf()) for _ in range(4)])
    v_da3 = T["v_da"].rearrange("(t p) e -> p t e", p=128)

    def load_head(h):
        KT, V, QT, kB, vB, qB, oB = hb[h % 2]
        for s4 in range(4):
            P.dma("sp", lambda e, s4=s4: e.dma_start(out=KT[:, s4 * 2048:(s4 + 1) * 2048],
                                                      in_=T["kT_da"][h, :, s4 * 2048:(s4 + 1) * 2048]), writes=[kB])
        for s8 in range(8):
            P.dma("sp", lambda e, s8=s8: e.dma_start(out=V[:, s8 * 8:(s8 + 1) * 8, 0:128],
                                                      in_=v_da3[:, s8 * 8:(s8 + 1) * 8, h * 128:(h + 1) * 128]), writes=[vB])
        P.dma("sp", lambda e: e.dma_start(out=QT[0:64, 0, :], in_=T["qT_da"][h, 0:64, :]), writes=[qB])
        P.dma("sp", lambda e: e.dma_start(out=QT[64:128, 1, :], in_=T["qT_da"][h, 64:128, :]), writes=[qB])

    def epilogue(h, qb, qt):
        O1, O1B = accs[(0, qt)]
        O2, O2B = accs[(1, qt)]
        sm, tt, oo, sq, sB = small.next()
        P.op("dve", lambda e: e.reciprocal(out=sm[:, 0:1], in_=O1[:, 128:129]), reads=[O1B], writes=[sB])
        yield
        P.op("dve", lambda e: e.reciprocal(out=sm[:, 1:2], in_=O2[:, 128:129]), reads=[O2B], writes=[sB])
        yield
        P.op("dve", lambda e: e.tensor_tensor(out=sm[:, 2:3], in0=sm[:, 1:2], in1=neglam[:], op=ALU.mult), reads=[sB, cB], writes=[sB])
        yield
        P.op("dve", lambda e: e.tensor_scalar(out=tt[:], in0=O2[:, 0:128], scalar1=sm[:, 2:3], scalar2=None, op0=ALU.mult),
             reads=[O2B, sB], writes=[sB])
        yield
        P.op("dve", lambda e: e.scalar_tensor_tensor(out=oo[:], in0=O1[:, 0:128], scalar=sm[:, 0:1], in1=tt[:],
                                                      op0=ALU.mult, op1=ALU.add), reads=[O1B, sB], writes=[sB])
        yield
        P.op("act", lambda e: e.activation(out=sq[:], in_=oo[:], func=AF.Square, accum_out=sm[:, 3:4]), reads=[sB], writes=[sB])
        yield
        P.op("act", lambda e: e.activation(out=sm[:, 4:5], in_=sm[:, 3:4], func=AF.Ln, scale=1.0 / 128.0, bias=1e-5),
             reads=[sB], writes=[sB])
        yield
        P.op("act", lambda e: e.activation(out=sm[:, 5:6], in_=sm[:, 4:5], func=AF.Exp, scale=-0.5), reads=[sB], writes=[sB])
        yield
        P.op("dve", lambda e: e.scalar_tensor_tensor(out=a_sb[:, qb * 4 + qt, h * 128:(h + 1) * 128], in0=oo[:], scalar=sm[:, 5:6],
                                                      in1=gbc[:], op0=ALU.mult, op1=ALU.mult), reads=[sB, cB], writes=[a_B])
        yield

    def qk(h, qb, c, kt):
        KT, V, QT, kB, vB, qB, oB = hb[h % 2]
        S, SB = sps.next()
        P.op("pe", lambda e: e.matmul(S[:], KT[:, kt * 128:(kt + 1) * 128],
                                      QT[:, c, qb * 512:(qb + 1) * 512],
                                      start=True, stop=True), reads=[kB, qB, oB], writes=[SB])
        Pt, PtB = ptr.next()
        P.op("act", lambda e: e.activation(out=Pt[:], in_=S[:], func=AF.Exp, scale=0.125),
             reads=[SB], writes=[PtB])
        return Pt, PtB

    def pv(h, qb, c, kt, Pt, PtB):
        KT, V, QT, kB, vB, qB, oB = hb[h % 2]
        for qt in range(4):
            O, OB = accs[(c, qt)]
            P.op("pe", lambda e, O=O, qt=qt: e.matmul(O, Pt[:, qt * 128:(qt + 1) * 128], V[:, kt, :],
                                                      start=(kt == 0 and qt in (0, 3)), stop=(kt == 63), skip_group_check=True),
                 reads=[PtB, vB, oB], writes=[OB])
        if c == 1 and kt == 63:
            interleave([epilogue(h, qb, qt) for qt in range(4)])

    iters = [(h, qb, c, kt) for h in range(T.get("da_heads", 4)) for qb in range(8) for c in range(2) for kt in range(64)]
    LA = 2
    pend = {}
    NH = T.get("da_heads", 4)
    load_head(0)
    if NH > 1:
        load_head(1)
    for n in range(len(iters) + LA):
        if n < len(iters):
            h, qb, c, kt = iters[n]
            pend[n] = qk(h, qb, c, kt)
        m = n - LA
        if m >= 0:
            h, qb, c, kt = iters[m]
            Pt, PtB = pend.pop(m)
            pv(h, qb, c, kt, Pt, PtB)
            if qb == 7 and c == 1 and kt == 63 and h + 2 < NH:
                load_head(h + 2)
    P.barrier()
def phase3(P, nc, T, nb_sb, nb_B):
    P.sb_reset(T["arena0"])
    if T.get("xs_d") is not None:
        zt = P.sb([128, CAP // 128, 1024], BF16, "zt")
        zB = Buf()
        P.op("pool", lambda e: e.memset(zt[:], 0.0), writes=[zB])
        xs3 = T["xs_d"].rearrange("(e s p) d -> e p s d", p=128, s=CAP // 128)
        for e_ in range(NEXP):
            P.dma("sp", lambda e, e_=e_: e.dma_start(out=xs3[e_], in_=zt[:]), reads=[zB])
    Mt = P.sb([128, 18 * 256], BF16, "Mt")
    mB = Buf()
    for s in range(3):
        P.dma("pool", lambda e, s=s: e.dma_start(out=Mt[:, s * 1536:(s + 1) * 1536], in_=T["na_M"][:, s * 1536:(s + 1) * 1536]), writes=[mB])
    Rt = P.sb([128, 18 * 256], F32, "Rt")
    rB = Buf()
    hb = []
    for _ in range(2):
        KT = P.sb([64, 4608], BF16, "KTn")
        V = P.sb([128, 36, 65], BF16, "Vn")
        QT = P.sb([64, 4096], BF16, "QTn")
        E = P.sb([128, 18 * 256], BF16, "En")
        oB = Buf()
        P.op("pool", lambda e, V=V: e.memset(V[:, :, 64:65], 1.0), writes=[oB])
        hb.append((KT, V, QT, E, Buf(), Buf(), Buf(), Buf(), oB))
    per = Ring([(P.sb([128, 512], BF16, "pe_"), Buf()) for _ in range(4)])
    pmr = Ring([(P.sb([128, 512], BF16, "pm_"), Buf()) for _ in range(4)])
    sps = Ring([(T["ps"][i], T["psb"][i]) for i in range(2, 8)])
    accs = [(T["ps"][0][:, 0:65], T["psb"][0]), (T["ps"][0][:, 128:193], T["psb"][0]),
            (T["ps"][1][:, 0:65], T["psb"][1]), (T["ps"][1][:, 128:193], T["psb"][1])]
    smr = Ring([(P.sb([128, 2], F32, "smn"), Buf()) for _ in range(4)])
    v_na3 = T["v_na"].rearrange("(t p) e -> p t e", p=128)

    def load_head(hn):
        KT, V, QT, E, kB, vB, qB, eB, oB = hb[hn % 2]
        j, po = hn // 2, (hn % 2) * 64
        P.dma("sp", lambda e: e.dma_start(out=KT[:, 0:256], in_=T["kT_na"][j, po:po + 64, 7936:8192]), writes=[kB])
        P.dma("sp", lambda e: e.dma_start(out=KT[:, 256:4608], in_=T["kT_na"][j, po:po + 64, 0:4352]), writes=[kB])
        P.dma("sp", lambda e: e.dma_start(out=QT[:], in_=T["qT_na"][j, po:po + 64, :]), writes=[qB])
        P.dma("sp", lambda e: e.dma_start(out=V[:, 0:2, 0:64], in_=v_na3[:, 62:64, hn * 64:(hn + 1) * 64]), writes=[vB])
        for s in range(2):
            P.dma("sp", lambda e, s=s: e.dma_start(out=V[:, 2 + s * 17:2 + (s + 1) * 17, 0:64],
                                                    in_=v_na3[:, s * 17:(s + 1) * 17, hn * 64:(hn + 1) * 64]), writes=[vB])
        P.dma("sp", lambda e: e.dma_start(out=Rt[:], in_=T["na_R"][hn, :, :]), writes=[rB])
        for s in range(3):
            sl = slice(s * 1536, (s + 1) * 1536)
            P.op("act", lambda e, sl=sl: e.activation(out=Rt[:, sl], in_=Rt[:, sl], func=AF.Exp), reads=[rB], writes=[rB])
            P.op("dve", lambda e, sl=sl: e.tensor_tensor(out=E[:, sl], in0=Rt[:, sl], in1=Mt[:, sl], op=ALU.mult),
                 reads=[rB, mB], writes=[eB])

    def qk(hn, g, jp):
        KT, V, QT, E, kB, vB, qB, eB, oB = hb[hn % 2]
        S, SB = sps.next()
        for hf in range(2):
            ti = 2 * g + 2 * jp + hf
            P.op("pe", lambda e, hf=hf, ti=ti: e.matmul(S[:, hf * 256:(hf + 1) * 256], KT[:, ti * 128:(ti + 1) * 128],
                                                        QT[:, g * 256:(g + 1) * 256],
                                                        start=(hf == 0), stop=True, skip_group_check=True), reads=[kB, qB], writes=[SB])
        Pe, PeB = per.next()
        P.op("act", lambda e: e.activation(out=Pe[:], in_=S[:, 0:512], func=AF.Exp, scale=0.125), reads=[SB], writes=[PeB])
        Pm, PmB = pmr.next()
        cls = 0 if g == 0 else (2 if g == 15 else 1)
        ei = (cls * 6 + 2 * jp) * 256
        P.op("dve", lambda e: e.tensor_tensor(out=Pm[:], in0=Pe[:], in1=E[:, ei:ei + 512], op=ALU.mult),
             reads=[PeB, eB], writes=[PmB])
        return Pm, PmB

    def pv(hn, g, jp, Pm, PmB):
        KT, V, QT, E, kB, vB, qB, eB, oB = hb[hn % 2]
        for hf in range(2):
            ti = 2 * g + 2 * jp + hf
            for qt in range(2):
                O, OB = accs[(g % 2) * 2 + qt]
                P.op("pe", lambda e, O=O, qt=qt, hf=hf, ti=ti: e.matmul(
                    O, Pm[:, hf * 256 + qt * 128:hf * 256 + (qt + 1) * 128], V[:, ti, :],
                    start=(jp == 0 and hf == 0 and qt == 0), stop=(jp == 2 and hf == 1), skip_group_check=True),
                    reads=[PmB, vB, oB], writes=[OB])
        if jp == 2:
            for qt in range(2):
                O, OB = accs[(g % 2) * 2 + qt]
                sm, sB = smr.next()
                P.op("dve", lambda e, O=O, sm=sm: e.reciprocal(out=sm[:, 0:1], in_=O[:, 64:65]), reads=[OB], writes=[sB])
                P.op("dve", lambda e, O=O, sm=sm, qt=qt: e.tensor_scalar(
                    out=nb_sb[:, g * 2 + qt, hn * 64:(hn + 1) * 64], in0=O[:, 0:64], scalar1=sm[:, 0:1], scalar2=None,
                    op0=ALU.mult), reads=[OB, sB], writes=[nb_B])

    iters = [(hn, g, j) for hn in range(T.get("na_heads", 8)) for g in range(16) for j in range(3)]
    LA = 2
    pend = {}
    NH = T.get("na_heads", 8)
    load_head(0)
    if NH > 1:
        load_head(1)
    for n in range(len(iters) + LA):
        if n < len(iters):
            hn, g, j = iters[n]
            pend[n] = qk(hn, g, j)
        m = n - LA
        if m >= 0:
            hn, g, j = iters[m]
            Pm, PmB = pend.pop(m)
            pv(hn, g, j, Pm, PmB)
            if g == 15 and j == 2 and hn + 2 < NH:
                load_head(hn + 2)
    P.barrier()
def interleave(gens):
    gens = list(gens)
    while gens:
        nxt = []
        for g in gens:
            try:
                next(g)
                nxt.append(g)
            except StopIteration:
                pass
        gens = nxt


def layer_norm_tile(P, y, yB, out, outB, gbc, bbc, cB, junk, jB, sm, sB):
    for _ in layer_norm_gen(P, y, yB, out, outB, gbc, bbc, cB, junk, jB, sm, sB):
        pass


def layer_norm_gen(P, y, yB, out, outB, gbc, bbc, cB, junk, jB, sm, sB):
    class _W:
        def __init__(self, t):
            self.t = t

        def __getitem__(self, k):
            return self.t if isinstance(self.t, bass.AP) else self.t[k]
    y = _W(y)
    out = _W(out)
    junk = _W(junk)
    P.op("act", lambda e: e.activation(out=junk[:], in_=y[:], func=AF.Identity, accum_out=sm[:, 0:1]), reads=[yB], writes=[jB, sB])
    yield
    P.op("act", lambda e: e.activation(out=junk[:], in_=y[:], func=AF.Square, accum_out=sm[:, 1:2]), reads=[yB], writes=[jB, sB])
    yield
    P.op("dve", lambda e: e.tensor_scalar(out=sm[:, 2:3], in0=sm[:, 0:1], scalar1=1.0 / 1024.0, scalar2=None, op0=ALU.mult), reads=[sB], writes=[sB])
    yield
    P.op("dve", lambda e: e.tensor_tensor(out=sm[:, 3:4], in0=sm[:, 2:3], in1=sm[:, 2:3], op=ALU.mult), reads=[sB], writes=[sB])
    yield
    P.op("dve", lambda e: e.scalar_tensor_tensor(out=sm[:, 4:5], in0=sm[:, 1:2], scalar=1.0 / 1024.0, in1=sm[:, 3:4],
                                                  op0=ALU.mult, op1=ALU.subtract), reads=[sB], writes=[sB])
    yield
    P.op("act", lambda e: e.activation(out=sm[:, 5:6], in_=sm[:, 4:5], func=AF.Ln, scale=1.0, bias=1e-5), reads=[sB], writes=[sB])
    yield
    P.op("act", lambda e: e.activation(out=sm[:, 6:7], in_=sm[:, 5:6], func=AF.Exp, scale=-0.5), reads=[sB], writes=[sB])
    yield
    P.op("dve", lambda e: e.tensor_scalar(out=out[:], in0=y[:], scalar1=sm[:, 2:3], scalar2=sm[:, 6:7], op0=ALU.subtract, op1=ALU.mult),
         reads=[yB, sB], writes=[outB])
    yield
    P.op("dve", lambda e: e.tensor_tensor(out=out[:], in0=out[:], in1=gbc[:], op=ALU.mult), reads=[outB, cB], writes=[outB])
    yield
    P.op("dve", lambda e: e.tensor_tensor(out=out[:], in0=out[:], in1=bbc[:], op=ALU.add), reads=[outB, cB], writes=[outB])
    yield


def phase4(P, nc, T, a_sb, a_B, nb_sb, nb_B, GT, GT_B, slots_all, gk_all, rt_B):
    P.sb_reset(T["arena0"])
    wg = P.sb([128, 8, 2048], BF16, "wg")
    wbd = P.sb([128, 4, 1024], BF16, "wbd")
    wbn = P.sb([128, 4, 1024], BF16, "wbn")
    wo = P.sb([128, 8, 1024], BF16, "wo")
    wr = P.sb([128, 8, 32], F32, "wr")
    bg = P.sb([128, 16], F32, "bg")
    g1bc = P.sb([128, 1024], F32, "g1bc")
    b1bc = P.sb([128, 1024], F32, "b1bc")
    brbc = P.sb([128, 32], F32, "brbc")
    identb = P.sb([128, 128], BF16, "identb")
    ltri = P.sb([128, 128], BF16, "ltri")
    ones = P.sb([128, 128], BF16, "ones")
    ecap = P.sb([128, 32], F32, "ecap")
    cnt = P.sb([128, 32], F32, "cnt")
    cntB = Buf()
    P.op("pool", lambda e: e.memset(cnt[:], 0.0), writes=[cntB])
    identf = P.sb([128, 128], F32, "identf")
    cB = Buf()
    toks = []
    for c in range(8):
        for g in range(4):
            toks.append(P.dma("pool", lambda e, c=c, g=g: e.dma_start(out=wg[:, c, g * 512:(g + 1) * 512],
                                                                      in_=T["w_gate"][c * 128:(c + 1) * 128, g * 512:(g + 1) * 512])))
        toks.append(P.dma("pool", lambda e, c=c: e.dma_start(out=wo[:, c, :], in_=T["w_out"][c * 128:(c + 1) * 128, :])))
        toks.append(P.dma("sp", lambda e, c=c: e.dma_start(out=wr[:, c, :], in_=T["w_router"][c * 128:(c + 1) * 128, :])))
    for c in range(4):
        toks.append(P.dma("pool", lambda e, c=c: e.dma_start(out=wbd[:, c, :], in_=T["w_bda"][c * 128:(c + 1) * 128, :])))
        toks.append(P.dma("pool", lambda e, c=c: e.dma_start(out=wbn[:, c, :], in_=T["w_bna"][c * 128:(c + 1) * 128, :])))
    for dst, src in ((bg, "b_gate"), (g1bc, "ln1_g_bc"), (b1bc, "ln1_b_bc"), (brbc, "b_router_bc"), (identf, "ident")):
        toks.append(P.dma("sp", lambda e, dst=dst, src=src: e.dma_start(out=dst[:], in_=T[src])))
    toks.append(P.dma("pool", lambda e: e.dma_start(out=identb[:], in_=T["ident"])))
    toks.append(P.dma("pool", lambda e: e.dma_start(out=ltri[:], in_=T["ltri"])))
    toks.append(P.dma("pool", lambda e: e.dma_start(out=ones[:], in_=T["ones128"])))
    toks.append(P.dma("sp", lambda e: e.dma_start(out=ecap[:], in_=T["ecap"])))
    for eng in ("pe", "act", "dve", "pool"):
        P.op(eng, None, extra=toks)

    xblk = Ring([(P.sb([128, 8, 512], BF16, "xblk4"), Buf()) for _ in range(1)])
    aT = P.sb([128, 4, 512], BF16, "aT"); aTB = Buf()
    nT = P.sb([128, 4, 512], BF16, "nT"); nTB = Buf()
    g_r = Ring([(P.sb([128, 2, 512], BF16, "g01"), Buf()) for _ in range(2)])
    mT = P.sb([128, 8, 512], BF16, "mT"); mTB = Buf()
    tr = Ring([(P.sb([128, 512], F32, "t4"), P.sb([128, 512], F32, "u4"), Buf()) for _ in range(1)])
    y_r = Ring([(P.sb([128, 1024], F32, "y4"), Buf()) for _ in range(2)])
    x1_r = Ring([(P.sb([128, 1024], F32, "x1"), Buf()) for _ in range(2)])
    junk = P.sb([128, 1024], BF16, "junk"); jB = Buf()
    sm_r = Ring([(P.sb([128, 8], F32, "sm4"), Buf()) for _ in range(2)])
    x1Tf_r = Ring([(P.sb([128, 1024], F32, "x1Tf"), None, Buf()) for _ in range(1)])
    rt_r = Ring([(P.sb([128, 32], F32, "lg"), P.sb([128, 8], F32, "t8"), P.sb([128, 32], F32, "mk"), P.sb([128, 32], F32, "ex"),
                  P.sb([128, 4], F32, "rs"), P.sb([128, 32], F32, "G"), Buf(),
                  P.sb([128, 32], BF16, "mkb"), P.sb([128, 32], F32, "rk"), P.sb([128, 32], F32, "tm"), P.sb([128, 8], F32, "slf")) for _ in range(2)])
    psr = Ring([(T["ps"][i], T["psb"][i]) for i in range(8)])
    xT3 = T["xT"].rearrange("(c p) t -> p c t", p=128)

    def do_block(tb):
        xb, xbB = xblk.next()
        P.dma("pool", lambda e: e.dma_start(out=xb[:], in_=xT3[:, :, tb * 512:(tb + 1) * 512]), writes=[xbB])
        for src, sB_, dst, dB in ((a_sb, a_B, aT, aTB), (nb_sb, nb_B, nT, nTB)):
            for hc in range(4):
                ps, psB = psr.next()
                psb16 = ps.bitcast(BF16)
                for tt in range(4):
                    P.op("pe", lambda e, tt=tt, psb16=psb16, src=src, hc=hc: e.transpose(
                        psb16[:, tt * 128:(tt + 1) * 128], src[:, tb * 4 + tt, hc * 128:(hc + 1) * 128], identb[:]),
                        reads=[sB_], writes=[psB])
                P.op("act", lambda e, psb16=psb16, dst=dst, hc=hc: e.activation(out=dst[:, hc, :], in_=psb16[:, 0:512], func=AF.Identity),
                     reads=[psB], writes=[dB])
        for dt in range(8):
            g01, g01B = g_r.next()
            for gi, j in enumerate((dt, 8 + dt)):
                ps, psB = psr.next()
                for k in range(8):
                    P.op("pe", lambda e, k=k, ps=ps, j=j: e.matmul(ps[:], wg[:, k, j * 128:(j + 1) * 128], xb[:, k, :],
                                                                   start=(k == 0), stop=(k == 7)), reads=[xbB], writes=[psB])
                P.op("act", lambda e, ps=ps, j=j, gi=gi, g01=g01: e.activation(out=g01[:, gi, :], in_=ps[:], func=AF.Sigmoid,
                                                                              bias=bg[:, j:j + 1], scale=1.0),
                     reads=[psB], writes=[g01B])
            psa, psaB = psr.next()
            psn, psnB = psr.next()
            for ec in range(4):
                P.op("pe", lambda e, ec=ec, psa=psa, dt=dt: e.matmul(psa[:], wbd[:, ec, dt * 128:(dt + 1) * 128], aT[:, ec, :],
                                                                    start=(ec == 0), stop=(ec == 3)), reads=[aTB], writes=[psaB])
            for ec in range(4):
                P.op("pe", lambda e, ec=ec, psn=psn, dt=dt: e.matmul(psn[:], wbn[:, ec, dt * 128:(dt + 1) * 128], nT[:, ec, :],
                                                                    start=(ec == 0), stop=(ec == 3)), reads=[nTB], writes=[psnB])
            t4, u4, tB = tr.next()
            P.op("dve", lambda e, t4=t4, psa=psa, g01=g01: e.tensor_tensor(out=t4[:], in0=psa[:], in1=g01[:, 0, :], op=ALU.mult),
                 reads=[psaB, g01B], writes=[tB])
            P.op("dve", lambda e, u4=u4, psn=psn, g01=g01: e.tensor_tensor(out=u4[:], in0=psn[:], in1=g01[:, 1, :], op=ALU.mult),
                 reads=[psnB, g01B], writes=[tB])
            P.op("pool", lambda e, t4=t4, u4=u4, dt=dt: e.tensor_tensor(out=mT[:, dt, :], in0=t4[:], in1=u4[:], op=ALU.add),
                 reads=[tB], writes=[mTB])
        def stageA(tt):
            tok0 = tb * 512 + tt * 128
            y, yB = y_r.next()
            P.dma("sp", lambda e: e.dma_start(out=y[:], in_=T["x_own"][tok0:tok0 + 128, :]), writes=[yB])
            for half in range(2):
                ps, psB = psr.next()
                for k in range(8):
                    P.op("pe", lambda e, k=k, ps=ps, half=half: e.matmul(
                        ps[:], mT[:, k, tt * 128:(tt + 1) * 128], wo[:, k, half * 512:(half + 1) * 512],
                        start=(k == 0), stop=(k == 7)), reads=[mTB], writes=[psB])
                P.op("dve", lambda e, ps=ps, half=half: e.scalar_tensor_tensor(
                    out=y[:, half * 512:(half + 1) * 512], in0=y[:, half * 512:(half + 1) * 512], scalar=ALPHA, in1=ps[:],
                    op0=ALU.mult, op1=ALU.add), reads=[psB, yB], writes=[yB])
            x1, x1B = x1_r.next()
            sm, sB = sm_r.next()
            layer_norm_tile(P, y, yB, x1, x1B, g1bc, b1bc, cB, junk, jB, sm, sB)
            P.dma("sp", lambda e: e.dma_start(out=T["x1_d"][tok0:tok0 + 128, :], in_=x1[:]), reads=[x1B])
            return x1, x1B, tok0

        def stageB(tt, x1, x1B, tok0):
            x1Tf, x1Tb, xTB = x1Tf_r.next()
            for k2 in range(2):
                ps, psB = psr.next()
                for k4 in range(4):
                    k = k2 * 4 + k4
                    P.op("pe", lambda e, ps=ps, k=k, k4=k4, x1=x1: e.transpose(ps[:, k4 * 128:(k4 + 1) * 128], x1[:, k * 128:(k + 1) * 128],
                                                                        identf[:]), reads=[x1B], writes=[psB])
                P.op("act", lambda e, ps=ps, x1Tf=x1Tf, k2=k2: e.activation(out=x1Tf[:, k2 * 512:(k2 + 1) * 512], in_=ps[:], func=AF.Identity),
                     reads=[psB], writes=[xTB])
            ps, psB = psr.next()
            for k in range(8):
                P.op("pe", lambda e, ps=ps, k=k, x1Tf=x1Tf: e.matmul(ps[:, 0:32], x1Tf[:, k * 128:(k + 1) * 128], wr[:, k, :], start=(k == 0), stop=(k == 7)),
                     reads=[xTB], writes=[psB])
            lg, t8, mk, ex, rs, G, rB, mkb, rk, tm, slf = rt_r.next()
            P.op("dve", lambda e, ps=ps, lg=lg: e.tensor_tensor(out=lg[:], in0=ps[:, 0:32], in1=brbc[:], op=ALU.add), reads=[psB], writes=[rB])
            P.op("dve", lambda e, lg=lg, t8=t8: e.max(out=t8[:], in_=lg[:]), reads=[rB], writes=[rB])
            P.op("dve", lambda e, lg=lg, t8=t8, mk=mk: e.tensor_scalar(out=mk[:], in0=lg[:], scalar1=t8[:, 3:4], scalar2=None, op0=ALU.is_ge),
                 reads=[rB], writes=[rB])
            P.op("dve", lambda e, t8=t8, rs=rs: e.tensor_scalar(out=rs[:, 0:1], in0=t8[:, 0:1], scalar1=-1.0, scalar2=None, op0=ALU.mult),
                 reads=[rB], writes=[rB])
            P.op("act", lambda e, lg=lg, ex=ex, rs=rs: e.activation(out=ex[:], in_=lg[:], func=AF.Exp, bias=rs[:, 0:1], scale=1.0),
                 reads=[rB], writes=[rB])
            P.op("dve", lambda e, ex=ex, mk=mk: e.tensor_tensor(out=ex[:], in0=ex[:], in1=mk[:], op=ALU.mult), reads=[rB], writes=[rB])
            P.op("dve", lambda e, ex=ex, rs=rs: e.reduce_sum(out=rs[:, 1:2], in_=ex[:], axis=AX.X), reads=[rB], writes=[rB])
            P.op("dve", lambda e, rs=rs: e.reciprocal(out=rs[:, 2:3], in_=rs[:, 1:2]), reads=[rB], writes=[rB])
            P.op("dve", lambda e, ex=ex, rs=rs, G=G: e.tensor_scalar(out=G[:], in0=ex[:], scalar1=rs[:, 2:3], scalar2=None, op0=ALU.mult),
                 reads=[rB], writes=[rB])
            ps2, ps2B = psr.next()
            P.op("pe", lambda e, ps2=ps2, G=G: e.transpose(ps2[0:32, 0:128], G[:], identf[:]), reads=[rB], writes=[ps2B])
            P.op("act", lambda e, ps2=ps2, tok0=tok0: e.activation(out=GT[:, tok0:tok0 + 128], in_=ps2[0:32, 0:128], func=AF.Identity),
                 reads=[ps2B], writes=[GT_B])
            tix = tb * 4 + tt
            P.op("dve", lambda e, mk=mk, mkb=mkb: e.tensor_copy(out=mkb[:], in_=mk[:]), reads=[rB], writes=[rB])
            ps3, ps3B = psr.next()
            P.op("pe", lambda e, ps3=ps3, mkb=mkb: e.matmul(ps3[:, 0:32], ltri[:], mkb[:], start=True, stop=True, skip_group_check=True),
                 reads=[rB], writes=[ps3B])
            P.op("pe", lambda e, ps3=ps3, mkb=mkb: e.matmul(ps3[:, 64:96], ones[:], mkb[:], start=False, stop=True, skip_group_check=True),
                 reads=[rB], writes=[ps3B])
            P.op("dve", lambda e, ps3=ps3, rk=rk: e.tensor_tensor(out=rk[:], in0=ps3[:, 0:32], in1=cnt[:], op=ALU.add), reads=[ps3B, cntB], writes=[rB])
            P.op("dve", lambda e, ps3=ps3: e.tensor_tensor(out=cnt[:], in0=cnt[:], in1=ps3[:, 64:96], op=ALU.add), reads=[ps3B, cntB, rB], writes=[cntB])
            P.op("dve", lambda e, rk=rk: e.scalar_tensor_tensor(out=rk[:], in0=rk[:], scalar=float(CAP - 1), in1=ecap[:], op0=ALU.min, op1=ALU.add),
                 reads=[rB], writes=[rB])
            for k in range(4):
                P.op("dve", lambda e, k=k, lg=lg, t8=t8, rk=rk, tm=tm: e.scalar_tensor_tensor(out=tm[:], in0=lg[:], scalar=t8[:, k:k + 1], in1=rk[:],
                                                                                             op0=ALU.is_equal, op1=ALU.mult), reads=[rB], writes=[rB])
                P.op("dve", lambda e, k=k, tm=tm, slf=slf: e.reduce_sum(out=slf[:, k:k + 1], in_=tm[:], axis=AX.X), reads=[rB], writes=[rB])
            P.op("dve", lambda e, slf=slf, tix=tix: e.tensor_copy(out=slots_all[:, tix, :], in_=slf[:, 0:4]), reads=[rB], writes=[rB, rt_B])
            P.op("act", lambda e, t8=t8, slf=slf, rs=rs: e.activation(out=slf[:, 4:8], in_=t8[:, 0:4], func=AF.Exp, bias=rs[:, 0:1], scale=1.0),
                 reads=[rB], writes=[rB])
            P.op("dve", lambda e, slf=slf, rs=rs: e.reduce_sum(out=rs[:, 3:4], in_=slf[:, 4:8], axis=AX.X), reads=[rB], writes=[rB])
            P.op("dve", lambda e, rs=rs: e.reciprocal(out=rs[:, 3:4], in_=rs[:, 3:4]), reads=[rB], writes=[rB])
            P.op("dve", lambda e, slf=slf, rs=rs, tix=tix: e.tensor_scalar(out=gk_all[:, tix, :], in0=slf[:, 4:8], scalar1=rs[:, 3:4], scalar2=None,
                                                                          op0=ALU.mult), reads=[rB], writes=[rB, rt_B])
            if T.get("dbg_G") is not None:
                P.dma("sp", lambda e, G=G, tok0=tok0: e.dma_start(out=T["dbg_G"][tok0:tok0 + 128, :], in_=G[:]), reads=[rB])

        pend = stageA(0)
        for tt in range(4):
            nxt = stageA(tt + 1) if tt + 1 < 4 else None
            stageB(tt, *pend)
            pend = nxt

    for tb in range(8):
        do_block(tb)
    if T.get("dbg_cnt") is not None:
        P.dma("sp", lambda e: e.dma_start(out=T["dbg_cnt"], in_=cnt[:]), reads=[cntB])
    P.barrier()
def phase5s(P, nc, T, GT, GT_B, slots_all, gk_all, rt_B):
    P.sb_reset(T["arena5"])
    xb_r = Ring([(P.sb([128, 1024], BF16, "x1b"), Buf()) for _ in range(8)])
    for tix in range(32):
        x1b, x1bB = xb_r.next()
        P.dma("pool", lambda e, x1b=x1b, tix=tix: e.dma_start(out=x1b[:], in_=T["x1_d"][tix * 128:(tix + 1) * 128, :]), writes=[x1bB])
        for k in range(4):
            P.dma("pool", lambda e, k=k, x1b=x1b, tix=tix: e.indirect_dma_start(
                out=T["xs_d"], out_offset=bass.IndirectOffsetOnAxis(ap=slots_all[:, tix, k:k + 1], axis=0),
                in_=x1b[:], in_offset=None, bounds_check=None, oob_is_err=False), reads=[x1bB, rt_B])
    P.barrier()
    P.sb_reset(T["arena5"])
    NS = CAP // 128
    CH = [(0, 512), (512, 512), (1024, 256)]
    NH_ = 512
    b1a = P.sb([128, 32, 16], F32, "b1a")
    identb = P.sb([128, 128], BF16, "identb5")
    toks = [P.dma("sp", lambda e: e.dma_start(out=b1a[:], in_=T["b_mlp1"])),
            P.dma("pool", lambda e: e.dma_start(out=identb[:], in_=T["ident"]))]
    for eng in ("pe", "act", "dve"):
        P.op(eng, None, extra=toks)
    wr_ = Ring([(P.sb([128, 8, 1024], BF16, "w1g"), P.sb([128, 8, 1024], BF16, "w1l"), P.sb([128, 8, 1024], BF16, "w2"),
                 None, Buf(), Buf(), Buf(), None) for _ in range(2)])
    xs_tm = P.sb([128, NS, 1024], BF16, "xs_tm"); xB = Buf()
    xsT_r = [(P.sb([128, 8, CAP], BF16, "xsT"), Buf()) for _ in range(2)]
    actT = P.sb([128, 8, CAP], BF16, "actT"); aB = Buf()
    tmp_r = Ring([(P.sb([128, NH_], F32, "g5"), P.sb([128, NH_], F32, "s5"), P.sb([128, NH_], F32, "l5"), Buf()) for _ in range(2)])
    ys_r = Ring([(P.sb([128, 1024], BF16, "ys"), Buf()) for _ in range(3)])
    psr = Ring([(T["ps"][i], T["psb"][i]) for i in range(8)])
    xs3 = T["xs_d"].rearrange("(e s p) d -> e p s d", p=128, s=NS)
    ys3 = T["ys_d"].rearrange("(e s p) d -> e s p d", p=128, s=NS)

    def load_w(e_):
        w1g, w1l, w2, _x, gB, lB, wB, _b = wr_.next()
        for c in range(8):
            P.dma("pool", lambda e, c=c: e.dma_start(out=w1g[:, c, :], in_=T["w1g"][e_, c * 128:(c + 1) * 128, :]), writes=[gB])
            P.dma("pool", lambda e, c=c: e.dma_start(out=w1l[:, c, :], in_=T["w1l"][e_, c * 128:(c + 1) * 128, :]), writes=[lB])
        for c in range(8):
            P.dma("pool", lambda e, c=c: e.dma_start(out=w2[:, c, :], in_=T["w2"][e_, c * 128:(c + 1) * 128, :]), writes=[wB])
        return w1g, w1l, w2, None, gB, lB, wB, None

    def load_xs(e_):
        P.dma("sp", lambda e: e.dma_start(out=xs_tm[:], in_=xs3[e_]), writes=[xB])

    def tr_group(xsT, xsTB, k, s0, n):
        ps, psB = psr.next()
        psb16 = ps.bitcast(BF16)
        for i in range(n):
            P.op("pe", lambda e, i=i: e.transpose(psb16[:, i * 128:(i + 1) * 128], xs_tm[:, s0 + i, k * 128:(k + 1) * 128], identb[:]),
                 reads=[xB], writes=[psB])
        P.op("act", lambda e: e.activation(out=xsT[:, k, s0 * 128:(s0 + n) * 128], in_=psb16[:, 0:n * 128], func=AF.Identity),
             reads=[psB], writes=[xsTB])

    def mm1(e_, W, f, hf, xsT, xsTB):
        w1g, w1l, w2, _x, gB, lB, wB, _b = W
        c0, cn = CH[hf]
        sl = slice(c0, c0 + cn)
        pg, pgB = psr.next()
        pl, plB = psr.next()
        for k in range(8):
            P.op("pe", lambda e, k=k: e.matmul(pg[:, 0:cn], w1g[:, k, f * 128:(f + 1) * 128], xsT[:, k, sl],
                                               start=(k == 0), stop=(k == 7)), reads=[gB, xsTB], writes=[pgB])
        for k in range(8):
            P.op("pe", lambda e, k=k: e.matmul(pl[:, 0:cn], w1l[:, k, f * 128:(f + 1) * 128], xsT[:, k, sl],
                                               start=(k == 0), stop=(k == 7)), reads=[lB, xsTB], writes=[plB])
        g5, s5, l5, tB = tmp_r.next()
        P.op("act", lambda e: e.activation(out=l5[:, 0:cn], in_=pl[:, 0:cn], func=AF.Identity, bias=b1a[:, e_, 8 + f:9 + f], scale=1.0),
             reads=[plB], writes=[tB])
        P.op("dve", lambda e: e.tensor_scalar(out=g5[:, 0:cn], in0=pg[:, 0:cn], scalar1=b1a[:, e_, f:f + 1], scalar2=7.0,
                                              op0=ALU.add, op1=ALU.min), reads=[pgB], writes=[tB])
        P.op("act", lambda e: e.activation(out=s5[:, 0:cn], in_=g5[:, 0:cn], func=AF.Sigmoid, scale=1.702), reads=[tB], writes=[tB])
        P.op("dve", lambda e: e.tensor_scalar(out=l5[:, 0:cn], in0=l5[:, 0:cn], scalar1=-7.0, scalar2=7.0, op0=ALU.max, op1=ALU.min),
             reads=[tB], writes=[tB])
        P.op("dve", lambda e: e.tensor_tensor(out=g5[:, 0:cn], in0=g5[:, 0:cn], in1=s5[:, 0:cn], op=ALU.mult), reads=[tB], writes=[tB])
        P.op("dve", lambda e: e.scalar_tensor_tensor(out=actT[:, f, sl], in0=l5[:, 0:cn], scalar=1.0, in1=g5[:, 0:cn], op0=ALU.add, op1=ALU.mult),
             reads=[tB], writes=[aB])

    def mm2(e_, W, st):
        w1g, w1l, w2, _x, gB, lB, wB, _b = W
        ys, yB = ys_r.next()
        for half in range(2):
            ps, psB = psr.next()
            for k in range(8):
                P.op("pe", lambda e, k=k, ps=ps, half=half: e.matmul(ps[:], actT[:, k, st * 128:(st + 1) * 128],
                                                                    w2[:, k, half * 512:(half + 1) * 512],
                                                                    start=(k == 0), stop=(k == 7)), reads=[aB, wB], writes=[psB])
            if half == 0:
                P.op("act", lambda e, ps=ps: e.activation(out=ys[:, 0:512], in_=ps[:], func=AF.Identity), reads=[psB], writes=[yB])
            else:
                P.op("dve", lambda e, ps=ps: e.tensor_copy(out=ys[:, 512:1024], in_=ps[:]), reads=[psB], writes=[yB])
        P.dma("sp", lambda e: e.dma_start(out=ys3[e_, st], in_=ys[:]), reads=[yB])

    NE = T.get("n_exp", 32)

    def transposes(e_):
        xsT, xsTB = xsT_r[e_ % 2]
        for k in range(8):
            for s0 in range(0, NS, 4):
                tr_group(xsT, xsTB, k, s0, min(4, NS - s0))

    W = load_w(0)
    load_xs(0)
    transposes(0)
    for e_ in range(NE):
        Wn = load_w(e_ + 1) if e_ + 1 < NE else None
        if e_ + 1 < NE:
            load_xs(e_ + 1)
        xsT, xsTB = xsT_r[e_ % 2]
        for f in range(8):
            for hf in range(len(CH)):
                mm1(e_, W, f, hf, xsT, xsTB)
        if e_ + 1 < NE:
            transposes(e_ + 1)
        for st in range(NS):
            mm2(e_, W, st)
        W = Wn
    P.barrier()

    P.sb_reset(T["arena5"])
    b2b = P.sb([32, 1024], BF16, "b2b")
    g2bc = P.sb([128, 1024], F32, "g2bc")
    b2bc = P.sb([128, 1024], F32, "b2bc")
    cB = Buf()
    toks = [P.dma("pool", lambda e: e.dma_start(out=b2b[:], in_=T["b_mlp2"])),
            P.dma("sp", lambda e: e.dma_start(out=g2bc[:], in_=T["ln2_g_bc"])),
            P.dma("sp", lambda e: e.dma_start(out=b2bc[:], in_=T["ln2_b_bc"]))]
    for eng in ("pe", "act", "dve"):
        P.op(eng, None, extra=toks)
    rows_r = Ring([(P.sb([128, 1024], BF16, "rows"), Buf()) for _ in range(16)])
    x1_r = Ring([(P.sb([128, 1024], F32, "x1c"), Buf()) for _ in range(4)])
    acc_r = Ring([(P.sb([128, 1024], F32, "accc"), Buf()) for _ in range(4)])
    out_r = Ring([(P.sb([128, 1024], F32, "outc"), Buf()) for _ in range(4)])
    junk_r = Ring([(P.sb([128, 1024], BF16, "junk6"), Buf()) for _ in range(4)])
    sm_r = Ring([(P.sb([128, 8], F32, "sm6"), Buf()) for _ in range(4)])

    def comb_tile(ti):
        tok0 = ti * 128
        x1c, x1B = x1_r.next()
        P.dma("sp", lambda e: e.dma_start(out=x1c[:], in_=T["x1_d"][tok0:tok0 + 128, :]), writes=[x1B])
        yield
        acc, accB = acc_r.next()
        for half in range(2):
            ps, psB = psr.next()
            P.op("pe", lambda e, ps=ps, half=half: e.matmul(ps[:], GT[:, tok0:tok0 + 128], b2b[:, half * 512:(half + 1) * 512],
                                                            start=True, stop=True), reads=[GT_B], writes=[psB])
            yield
            P.op("dve", lambda e, ps=ps, half=half: e.scalar_tensor_tensor(out=acc[:, half * 512:(half + 1) * 512],
                                                                           in0=x1c[:, half * 512:(half + 1) * 512], scalar=ALPHA, in1=ps[:],
                                                                           op0=ALU.mult, op1=ALU.add), reads=[psB, x1B], writes=[accB])
            yield
        for k in range(4):
            rows, rwB = rows_r.next()
            P.dma("pool", lambda e, rows=rows, k=k: e.indirect_dma_start(
                out=rows[:], out_offset=None, in_=T["ys_d"],
                in_offset=bass.IndirectOffsetOnAxis(ap=slots_all[:, ti, k:k + 1], axis=0),
                bounds_check=None, oob_is_err=False), reads=[rt_B], writes=[rwB])
            yield
            P.op("dve", lambda e, rows=rows, k=k: e.scalar_tensor_tensor(out=acc[:], in0=rows[:], scalar=gk_all[:, ti, k:k + 1], in1=acc[:],
                                                                         op0=ALU.mult, op1=ALU.add), reads=[rwB, rt_B, accB], writes=[accB])
            yield
        o, oB = out_r.next()
        sm, sB = sm_r.next()
        jk, jkB = junk_r.next()
        yield from layer_norm_gen(P, acc, accB, o, oB, g2bc, b2bc, cB, jk, jkB, sm, sB)
        P.dma("sp", lambda e: e.dma_start(out=T["out"][tok0:tok0 + 128, :], in_=o[:]), reads=[oB])

    for ti in range(0, 32, 4):
        interleave([comb_tile(ti + j) for j in range(4)])
    P.barrier()
def build(upto=99, debug=False):
    nc = bass.Bass("TRN2", target_bir_lowering=False)
    T = {}

    def inp(name, shape, dt=F32):
        T[name] = nc.dram_tensor(name, list(shape), dt, kind="ExternalInput").ap()

    def scr(name, shape, dt=BF16):
        T[name] = nc.dram_tensor(name, list(shape), dt, kind=("ExternalOutput" if (debug and debug.get("dump_scr")) else "Internal")).ap()

    def outp(name, shape, dt=F32):
        T[name] = nc.dram_tensor(name, list(shape), dt, kind="ExternalOutput").ap()

    inp("xT", [1024, 8192]); inp("x_own", [4096, 1024])
    inp("w_fm", [1024, 3072]); inp("w_tm", [1024, 1024]); inp("b_fm", [128, 24]); inp("b_tm", [128, 1024])
    inp("cosT", [128, 8192]); inp("sinT", [128, 8192]); inp("lamv", [128, 256]); inp("subln_bc", [128, 128])
    scr("qT_da", [4, 128, 4096]); scr("kT_da", [4, 128, 8192]); scr("qT_na", [4, 128, 4096]); scr("kT_na", [4, 128, 8192])
    scr("v_da", [8192, 512]); scr("v_na", [8192, 512])
    if upto >= 3:
        inp("na_R", [8, 128, 18 * 256]); inp("na_M", [128, 18 * 256])
    if upto >= 4:
        inp("w_gate", [1024, 2048]); inp("b_gate", [128, 16]); inp("w_bda", [512, 1024]); inp("w_bna", [512, 1024])
        inp("w_out", [1024, 1024]); inp("w_router", [1024, 32]); inp("ln1_g_bc", [128, 1024]); inp("ln1_b_bc", [128, 1024])
        inp("b_router_bc", [128, 32]); inp("ident", [128, 128])
        scr("x1_d", [4096, 1024], F32)
        inp("ltri", [128, 128]); inp("ones128", [128, 128]); inp("ecap", [128, 32])
        scr("xs_d", [NEXP * CAP, 1024], BF16); scr("ys_d", [NEXP * CAP, 1024], BF16)
        if debug and debug.get("dump_scr"):
            outp("dbg_G", [4096, 32])
        if debug and debug.get("dump_cnt"):
            outp("dbg_cnt", [128, 32])
    if upto >= 5:
        inp("b_mlp1", [128, 32, 16]); inp("b_mlp2", [32, 1024])
        inp("ln2_g_bc", [128, 1024]); inp("ln2_b_bc", [128, 1024])
        inp("w1g", [32, 1024, 1024]); inp("w1l", [32, 1024, 1024]); inp("w2", [32, 1024, 1024])
        outp("out", [4096, 1024])
    psall = nc.alloc_psum_tensor("psall", [128, 4096], F32)
    T["ps"] = [psall[:, i * 512:(i + 1) * 512] for i in range(8)]
    T["ps2"] = [psall[:, 2048:3072], psall[:, 3072:4096]]
    T["psb"] = [Buf() for _ in range(8)]
    P = Prog(nc)
    GT = P.sb([32, 4096], BF16, "GT")
    GT_B = Buf()
    slots_all = P.sb([128, 32, 4], I32, "slots_all")
    gk_all = P.sb([128, 32, 4], F32, "gk_all")
    rt_B = Buf()
    T["arena5"] = P.sb_off
    a_sb = P.sb([128, 32, 512], BF16, "a_sb")
    nb_sb = P.sb([128, 32, 512], BF16, "nb_sb")
    a_B, nb_B = Buf(), Buf()
    T["arena0"] = P.sb_off
    if debug:
        T["da_heads"] = debug.get("da_heads", 4)
        T["n_exp"] = debug.get("n_exp", 32)
    phase1(P, nc, T)
    if upto >= 2:
        phase2(P, nc, T, a_sb, a_B)
    if upto >= 3:
        phase3(P, nc, T, nb_sb, nb_B)
    if upto >= 4:
        phase4(P, nc, T, a_sb, a_B, nb_sb, nb_B, GT, GT_B, slots_all, gk_all, rt_B)
    if upto >= 5:
        phase5s(P, nc, T, GT, GT_B, slots_all, gk_all, rt_B)
    if upto < 4:
        outp("dbg_a", [4096, 512], BF16); outp("dbg_nb", [4096, 512], BF16)
        P.dma("sp", lambda e: e.dma_start(out=T["dbg_a"].rearrange("(t p) e -> p t e", p=128), in_=a_sb[:]), reads=[a_B])
        P.dma("sp", lambda e: e.dma_start(out=T["dbg_nb"].rearrange("(t p) e -> p t e", p=128), in_=nb_sb[:]), reads=[nb_B])
        if debug and debug.get("dump_scr"):
            for nm in ("qT_da", "kT_da", "v_da", "qT_na", "kT_na", "v_na"):
                pass
    P.barrier()
    P.emit()
    return nc, P


def rope_tables(pos):
    inv = (10000.0 ** (-np.arange(0, 64, 2, dtype=np.float32) / np.float32(64))).astype(np.float32)
    ang = pos.astype(np.float32)[:, None] * inv[None, :]
    ang = np.concatenate([ang, ang], axis=-1)
    cos = np.cos(ang).astype(np.float32)
    sin = np.sin(ang).astype(np.float32)
    sgn = np.concatenate([-np.ones(32, np.float32), np.ones(32, np.float32)])
    sin_s = sin * sgn[None, :]
    cosT = np.ascontiguousarray(np.concatenate([cos, cos], axis=1).T)
    sinT = np.ascontiguousarray(np.concatenate([sin_s, sin_s], axis=1).T)
    return cosT, sinT


def na_tables(rpb, h):
    R = np.zeros((8, 3, 6, 2, 64, 4, 64), np.float32)
    M = np.zeros((3, 6, 2, 64, 4, 64), np.float32)
    cc = np.arange(64)
    cs = np.clip(cc - 8, 0, 48)
    colvalid = (cc[:, None] >= cs[None, :]) & (cc[:, None] <= cs[None, :] + 15)
    coloff = np.clip(cc[:, None] - cc[None, :] + 15, 0, 30)
    for cls, g in ((0, 0), (1, 1 if h == 0 else 14), (2, 15)):
        for j in range(6):
            for a in range(2):
                for i in range(4):
                    r = 64 * h + 4 * g + i
                    kr = 64 * h + 4 * g + 2 * j - 4 + a
                    rs = min(max(r - 4, 0), 120)
                    if kr < rs or kr > rs + 7:
                        continue
                    M[cls, j, a, :, i, :] = colvalid
                    R[:, cls, j, a, :, i, :] = rpb[:, kr - r + 7][:, coloff] * colvalid[None]
    M2 = np.ascontiguousarray(M.transpose(2, 3, 0, 1, 4, 5).reshape(128, 18 * 256))
    R2 = np.ascontiguousarray(R.transpose(0, 3, 4, 1, 2, 5, 6).reshape(8, 128, 18 * 256))
    return R2, M2


def host_prep(inputs, upto=99):
    x = np.asarray(inputs["x"], np.float32)
    w_in = np.asarray(inputs["w_in"], np.float32)[0]
    b_in = np.asarray(inputs["b_in"], np.float32)[0]
    d = np.arange(64)
    swap = np.concatenate([(hh * 128 + c * 64 + (d + 32) % 64) for hh in range(4) for c in range(2)])
    qda, kda, vda = np.arange(0, 512), np.arange(512, 1024), np.arange(1024, 1536)
    qna, kna, vna = np.arange(1536, 2048), np.arange(2048, 2560), np.arange(2560, 3072)
    fm_cols = np.concatenate([qda, qda[swap], kda, kda[swap], qna, kna])
    tm_cols = np.concatenate([vda, vna])
    w_fm = np.ascontiguousarray(w_in[:, fm_cols])
    w_tm = np.ascontiguousarray(w_in[:, tm_cols])
    b_fm = np.ascontiguousarray(b_in[fm_cols].reshape(24, 128).T)
    b_tm = np.ascontiguousarray(np.broadcast_to(b_in[tm_cols][None, :], (128, 1024)))
    lamv = np.concatenate([np.asarray(inputs[k], np.float32)[0] for k in ("lambda_q1", "lambda_k1", "lambda_q2", "lambda_k2")])
    lamv = np.ascontiguousarray(np.broadcast_to(lamv[None, :], (128, 256)))
    subln_bc = np.ascontiguousarray(np.broadcast_to(np.asarray(inputs["subln_g"], np.float32)[0][None, :], (128, 128)))
    rpb = np.asarray(inputs["rpb"], np.float32)[0]
    shared = dict(w_fm=w_fm, w_tm=w_tm, b_fm=b_fm, b_tm=b_tm, lamv=lamv, subln_bc=subln_bc)
    f32 = lambda k: np.asarray(inputs[k], np.float32)[0]
    bc = lambda v, n=128: np.ascontiguousarray(np.broadcast_to(v[None, :], (n, v.shape[0])))
    if upto >= 4:
        shared.update(w_gate=np.ascontiguousarray(w_in[:, 3072:5120]), b_gate=np.ascontiguousarray(b_in[3072:5120].reshape(16, 128).T),
                      w_bda=f32("w_branch_da"), w_bna=f32("w_branch_na"), w_out=f32("w_out"), w_router=f32("w_router"),
                      ln1_g_bc=bc(f32("ln1_g")), ln1_b_bc=bc(f32("ln1_b")), b_router_bc=bc(f32("b_router")),
                      ident=np.eye(128, dtype=np.float32), ltri=np.triu(np.ones((128, 128), np.float32), k=1),
                      ones128=np.ones((128, 128), np.float32),
                      ecap=np.ascontiguousarray(np.broadcast_to((np.arange(32, dtype=np.float32) * CAP)[None, :], (128, 32))))
    if upto >= 5:
        w1 = f32("w_mlp1"); b1 = f32("b_mlp1")
        b1g = b1[:, 0::2].reshape(32, 8, 128).transpose(2, 0, 1)
        b1l = b1[:, 1::2].reshape(32, 8, 128).transpose(2, 0, 1)
        shared.update(b_mlp1=np.ascontiguousarray(np.concatenate([b1g, b1l], axis=2)),
                      b_mlp2=f32("b_mlp2"), ln2_g_bc=bc(f32("ln2_g")), ln2_b_bc=bc(f32("ln2_b")),
                      w1g=np.ascontiguousarray(w1[:, :, 0::2]), w1l=np.ascontiguousarray(w1[:, :, 1::2]), w2=f32("w_mlp2"))
    natab = [na_tables(rpb, h) for h in range(2)] if upto >= 3 else None
    maps = []
    for c in range(NCORES):
        b, h = c // 2, c % 2
        perm = np.concatenate([np.arange(h * 4096, (h + 1) * 4096), np.arange((1 - h) * 4096, (2 - h) * 4096)])
        xb = x[b]
        cosT, sinT = rope_tables(perm)
        m = dict(shared)
        m["xT"] = np.ascontiguousarray(xb[perm].T)
        m["x_own"] = np.ascontiguousarray(xb[h * 4096:(h + 1) * 4096])
        m["cosT"] = cosT
        m["sinT"] = sinT
        if upto >= 3:
            m["na_R"], m["na_M"] = natab[h]
        maps.append(m)
    return maps


def kernel(**inputs):
    from concourse.bass_utils import run_bass_kernel_spmd
    maps = host_prep(inputs)
    nc, P = build()
    res = run_bass_kernel_spmd(nc, maps, core_ids=list(range(NCORES)))
    out = np.zeros((4, SEQ, D), np.float32)
    for c in range(NCORES):
        b, h = c // 2, c % 2
        out[b, h * 4096:(h + 1) * 4096] = np.asarray(res.results[c]["out"], np.float32)
    return out
```

```python
import numpy as np
import concourse.bass as bass
import concourse.mybir as mybir

F32 = mybir.dt.float32
BF16 = mybir.dt.bfloat16
I32 = mybir.dt.int32
U32 = mybir.dt.uint32
AF = mybir.ActivationFunctionType
ALU = mybir.AluOpType
AX = mybir.AxisListType

ENGS = ("pe", "act", "dve", "pool", "sp")
KDMA = 8


class Tok:
    __slots__ = ("eng", "seq", "dma")

    def __init__(self, eng, seq, dma=None):
        self.eng = eng
        self.seq = seq
        self.dma = dma


class Buf:
    __slots__ = ("w", "r", "name")

    def __init__(self, name=""):
        self.w = None
        self.r = []
        self.name = name


class Prog:
    def __init__(self, nc):
        self.nc = nc
        self.ops = {e: [] for e in ENGS}
        self.ndma = {e: 0 for e in ENGS}
        self.all_dma = []
        self.sb_off = self.SB_BASE
        self.sb_hi = 0
        self.uid = 0

    SB_BASE = 16640
    SB_END = 229376

    def sb_reset(self, off=None):
        self.sb_off = self.SB_BASE if off is None else off

    def sb(self, shape, dtype, name=None):
        self.uid += 1
        nm = "%s_%d" % (name or "t", self.uid)
        nbytes = int(np.prod(shape[1:])) * mybir.dt.size(dtype)
        off = (self.sb_off + 63) // 64 * 64
        t = self.nc.alloc_sbuf_tensor_at(nm, list(shape), dtype, offset=off)
        self.sb_off = off + nbytes
        self.sb_hi = max(self.sb_hi, self.sb_off)
        assert self.sb_off <= self.SB_END, ("SBUF overflow", nm, self.sb_off)
        return t

    def _deps(self, reads, writes, extra):
        deps = {}
        for b in reads:
            if b.w is not None:
                deps[id(b.w)] = b.w
        for b in writes:
            if b.w is not None:
                deps[id(b.w)] = b.w
            for t in b.r:
                deps[id(t)] = t
        for t in extra:
            if t is not None:
                deps[id(t)] = t
        return list(deps.values())

    def _commit(self, tok, reads, writes):
        for b in writes:
            b.w = tok
            b.r = []
        for b in reads:
            if tok.dma is None:
                b.r = [t for t in b.r if not (t.dma is None and t.eng == tok.eng)]
            b.r.append(tok)

    def op(self, eng, fn, reads=(), writes=(), extra=()):
        deps = self._deps(reads, writes, extra)
        if eng == "pe":
            deps = [t for t in deps if not (t.eng == "pe" and t.dma is None)]
        tok = Tok(eng, len(self.ops[eng]))
        self.ops[eng].append(dict(fn=fn, waits=deps, tok=tok, dma=False))
        self._commit(tok, reads, writes)
        return tok

    def dma(self, eng, fn, reads=(), writes=(), extra=()):
        deps = self._deps(reads, writes, extra)
        i = self.ndma[eng]
        self.ndma[eng] += 1
        tok = Tok(eng, len(self.ops[eng]), dma=(i % KDMA, 16 * (i // KDMA + 1)))
        self.ops[eng].append(dict(fn=fn, waits=deps, tok=tok, dma=True, idx=i))
        self.all_dma.append(tok)
        self._commit(tok, reads, writes)
        return tok

    def barrier(self):
        lasts = []
        for e in ENGS:
            for o in reversed(self.ops[e]):
                if not o["dma"]:
                    lasts.append(o["tok"])
                    break
        dm = list(self.all_dma)
        self.all_dma = []
        for e in ENGS:
            if e == "sp":
                self.op(e, None, extra=lasts + dm)
            else:
                self.op(e, None, extra=lasts + dm)

    def emit(self):
        nc = self.nc
        needed = {e: set() for e in ENGS}
        for e in ENGS:
            for o in self.ops[e]:
                for t in o["waits"]:
                    if t.dma is None:
                        needed[t.eng].add(t.seq)
        tokval = {e: {} for e in ENGS}
        for e in ENGS:
            c = 0
            for o in self.ops[e]:
                if o["dma"]:
                    continue
                if o["tok"].seq in needed[e]:
                    c += 1
                    tokval[e][o["tok"].seq] = c
            self.maxcount = getattr(self, "maxcount", {})
            self.maxcount[e] = c
        engobj = {"pe": nc.tensor, "act": nc.scalar, "dve": nc.vector, "pool": nc.gpsimd, "sp": nc.sync}
        import contextlib
        with contextlib.ExitStack() as st:
            csem = {e: st.enter_context(nc.semaphore("c_" + e)) for e in ENGS}
            dsem = {e: [st.enter_context(nc.semaphore("d_%s%d" % (e, k))) for k in range(KDMA)]
                    for e in ENGS if self.ndma[e] > 0}
            block = st.enter_context(nc.Block())

            def run(e, engine):
                waited = {}
                for o in self.ops[e]:
                    for t in o["waits"]:
                        if t.dma is not None:
                            sem = dsem[t.eng][t.dma[0]]
                            val = t.dma[1]
                            key = ("d", t.eng, t.dma[0])
                        else:
                            sem = csem[t.eng]
                            val = tokval[t.eng][t.seq]
                            key = ("c", t.eng)
                        if waited.get(key, 0) >= val:
                            continue
                        waited[key] = val
                        engine.wait_ge(sem, val)
                    if o["dma"]:
                        i = o["idx"]
                        slot = i % KDMA
                        if i >= KDMA:
                            key = ("d", e, slot)
                            val = 16 * (i // KDMA)
                            if waited.get(key, 0) < val:
                                waited[key] = val
                                engine.wait_ge(dsem[e][slot], val)
                        ins = o["fn"](engine)
                        ins.then_inc(dsem[e][slot], 16)
                    else:
                        if o["fn"] is None:
                            if o["tok"].seq in needed[e]:
                                ins = engine.nop() if hasattr(engine, "nop") else None
                                ins.then_inc(csem[e], 1)
                            continue
                        ins = o["fn"](engine)
                        if o["tok"].seq in needed[e]:
                            ins.then_inc(csem[e], 1)

            @block.tensor
            def _(eng):
                run("pe", eng)

            @block.scalar
            def _(eng):
                run("act", eng)

            @block.vector
            def _(eng):
                run("dve", eng)

            @block.gpsimd
            def _(eng):
                run("pool", eng)

            @block.sync
            def _(eng):
                run("sp", eng)
D = 1024
SEQ = 8192
NOWN = 4096
NCORES = 8
CAP = 1280
NEXP = 32
LAM_INIT = 0.2
ALPHA = 2.0 ** 0.25


class Ring:
    def __init__(self, items):
        self.items = list(items)
        self.i = 0

    def next(self):
        it = self.items[self.i % len(self.items)]
        self.i += 1
        return it


def phase1(P, nc, T):
    P.sb_reset()
    wfm = P.sb([128, 8, 3072], BF16, "wfm")
    wtm = P.sb([128, 8, 1024], BF16, "wtm")
    bfm = P.sb([128, 24], F32, "bfm")
    btm = P.sb([128, 1024], F32, "btm")
    b_w = Buf()
    wtoks = []
    for c in range(8):
        for g in range(6):
            wtoks.append(P.dma("pool", lambda e, c=c, g=g: e.dma_start(
                out=wfm[:, c, g * 512:(g + 1) * 512], in_=T["w_fm"][c * 128:(c + 1) * 128, g * 512:(g + 1) * 512])))
        for g in range(2):
            wtoks.append(P.dma("pool", lambda e, c=c, g=g: e.dma_start(
                out=wtm[:, c, g * 512:(g + 1) * 512], in_=T["w_tm"][c * 128:(c + 1) * 128, g * 512:(g + 1) * 512])))
    wtoks.append(P.dma("sp", lambda e: e.dma_start(out=bfm[:], in_=T["b_fm"])))
    wtoks.append(P.dma("sp", lambda e: e.dma_start(out=btm[:], in_=T["b_tm"])))
    for eng in ("pe", "act", "dve"):
        P.op(eng, None, extra=wtoks)

    xblk = Ring([(P.sb([128, 8, 512], BF16, "xblk"), Buf()) for _ in range(2)])
    csb = Ring([(P.sb([128, 512], F32, "cos"), P.sb([128, 512], F32, "sin"), Buf()) for _ in range(2)])
    t1r = Ring([(P.sb([128, 512], F32, "t1"), Buf()) for _ in range(2)])
    t2r = Ring([(P.sb([128, 512], F32, "t2"), Buf()) for _ in range(2)])
    stg = Ring([(P.sb([128, 512], BF16, "stg"), Buf()) for _ in range(6)])
    psr = Ring([(T["ps"][i], T["psb"][i]) for i in range(8)])
    xT3 = T["xT"].rearrange("(c p) t -> p c t", p=128)

    def do_block(i):
        own = i < 8
        xb, xbB = xblk.next()
        P.dma("pool", lambda e, xb=xb, i=i: e.dma_start(out=xb[:], in_=xT3[:, :, i * 512:(i + 1) * 512]), writes=[xbB])
        cb, sb_, csB = csb.next()
        P.dma("sp", lambda e, cb=cb, i=i: e.dma_start(out=cb[:], in_=T["cosT"][:, i * 512:(i + 1) * 512]), writes=[csB])
        P.dma("sp", lambda e, sb_=sb_, i=i: e.dma_start(out=sb_[:], in_=T["sinT"][:, i * 512:(i + 1) * 512]), writes=[csB])

        def mm_fm(ps, psB, col):
            for k in range(8):
                P.op("pe", lambda e, k=k: e.matmul(ps[:], wfm[:, k, col:col + 128], xb[:, k, :],
                                                   start=(k == 0), stop=(k == 7)),
                     reads=[xbB], writes=[psB])

        def rope_tile(col0, col1, dst):
            psA, psAB = psr.next()
            psC, psCB = psr.next()
            mm_fm(psA, psAB, col0)
            mm_fm(psC, psCB, col1)
            t1, t1B = t1r.next()
            t2, t2B = t2r.next()
            P.op("dve", lambda e: e.scalar_tensor_tensor(out=t1[:], in0=psA[:], scalar=bfm[:, col0 // 128:col0 // 128 + 1],
                                                          in1=cb[:], op0=ALU.add, op1=ALU.mult),
                 reads=[psAB, csB], writes=[t1B])
            P.op("dve", lambda e: e.scalar_tensor_tensor(out=t2[:], in0=psC[:], scalar=bfm[:, col1 // 128:col1 // 128 + 1],
                                                          in1=sb_[:], op0=ALU.add, op1=ALU.mult),
                 reads=[psCB, csB], writes=[t2B])
            st, stB = stg.next()
            P.op("pool", lambda e: e.tensor_tensor(out=st[:], in0=t1[:], in1=t2[:], op=ALU.add),
                 reads=[t1B, t2B], writes=[stB])
            P.dma("sp", lambda e: e.dma_start(out=dst, in_=st[:]), reads=[stB])

        def plain_tile(col, dst):
            ps, psB = psr.next()
            mm_fm(ps, psB, col)
            st, stB = stg.next()
            P.op("act", lambda e: e.activation(out=st[:], in_=ps[:], func=AF.Identity,
                                               bias=bfm[:, col // 128:col // 128 + 1], scale=1.0),
                 reads=[psB], writes=[stB])
            P.dma("sp", lambda e: e.dma_start(out=dst, in_=st[:]), reads=[stB])

        sl = slice(i * 512, (i + 1) * 512)
        for h in range(4):
            if own:
                rope_tile(h * 128, 512 + h * 128, T["qT_da"][h, :, sl])
            rope_tile(1024 + h * 128, 1536 + h * 128, T["kT_da"][h, :, sl])
        na_kv = (i <= 8) or (i == 15)
        for j in range(4):
            if own:
                plain_tile(2048 + j * 128, T["qT_na"][j, :, sl])
            if na_kv:
                plain_tile(2560 + j * 128, T["kT_na"][j, :, sl])
        for tt in range(4):
            for g, dst in (((0, T["v_da"]), (1, T["v_na"])) if na_kv else ((0, T["v_da"]),)):
                ps, psB = psr.next()
                for k in range(8):
                    P.op("pe", lambda e, k=k, ps=ps, g=g, tt=tt: e.matmul(
                        ps[:], xb[:, k, tt * 128:(tt + 1) * 128], wtm[:, k, g * 512:(g + 1) * 512],
                        start=(k == 0), stop=(k == 7)), reads=[xbB], writes=[psB])
                st, stB = stg.next()
                P.op("dve", lambda e, ps=ps, st=st, g=g: e.tensor_tensor(out=st[:], in0=ps[:], in1=btm[:, g * 512:(g + 1) * 512],
                                                                         op=ALU.add), reads=[psB], writes=[stB])
                r0 = i * 512 + tt * 128
                P.dma("sp", lambda e, st=st, dst=dst, r0=r0: e.dma_start(out=dst[r0:r0 + 128, :], in_=st[:]), reads=[stB])

    for i in range(16):
        do_block(i)
    P.barrier()
def phase2(P, nc, T, a_sb, a_B):
    P.sb_reset(T["arena0"])
    lamv = P.sb([128, 256], F32, "lamv")
    gbc = P.sb([128, 128], F32, "gbc")
    tmp64 = P.sb([128, 128], F32, "tmp64")
    sc = P.sb([128, 8], F32, "sc")
    neglam = P.sb([128, 1], F32, "neglam")
    cB = Buf()
    P.dma("sp", lambda e: e.dma_start(out=lamv[:], in_=T["lamv"]), writes=[cB])
    P.dma("sp", lambda e: e.dma_start(out=gbc[:], in_=T["subln_bc"]), writes=[cB])
    P.op("dve", lambda e: e.tensor_tensor(out=tmp64[:, 0:64], in0=lamv[:, 0:64], in1=lamv[:, 64:128], op=ALU.mult), reads=[cB], writes=[cB])
    P.op("dve", lambda e: e.tensor_tensor(out=tmp64[:, 64:128], in0=lamv[:, 128:192], in1=lamv[:, 192:256], op=ALU.mult), reads=[cB], writes=[cB])
    P.op("dve", lambda e: e.reduce_sum(out=sc[:, 0:1], in_=tmp64[:, 0:64], axis=AX.X), reads=[cB], writes=[cB])
    P.op("dve", lambda e: e.reduce_sum(out=sc[:, 1:2], in_=tmp64[:, 64:128], axis=AX.X), reads=[cB], writes=[cB])
    P.op("act", lambda e: e.activation(out=sc[:, 2:4], in_=sc[:, 0:2], func=AF.Exp), reads=[cB], writes=[cB])
    P.op("dve", lambda e: e.tensor_tensor(out=sc[:, 4:5], in0=sc[:, 3:4], in1=sc[:, 2:3], op=ALU.subtract), reads=[cB], writes=[cB])
    P.op("dve", lambda e: e.tensor_scalar(out=neglam[:], in0=sc[:, 4:5], scalar1=-LAM_INIT, scalar2=None, op0=ALU.add), reads=[cB], writes=[cB])
    P.op("dve", lambda e: e.tensor_scalar(out=gbc[:], in0=gbc[:], scalar1=1.0 - LAM_INIT, scalar2=None, op0=ALU.mult), reads=[cB], writes=[cB])

    hb = []
    for _ in range(2):
        KT = P.sb([128, 8192], BF16, "KT")
        V = P.sb([128, 64, 129], BF16, "V")
        QT = P.sb([128, 2, 4096], BF16, "QT")
        oB = Buf()
        P.op("pool", lambda e, V=V: e.memset(V[:, :, 128:129], 1.0), writes=[oB])
        P.op("pool", lambda e, QT=QT: e.memset(QT[64:128, 0, :], 0.0), writes=[oB])
        P.op("pool", lambda e, QT=QT: e.memset(QT[0:64, 1, :], 0.0), writes=[oB])
        hb.append((KT, V, QT, Buf(), Buf(), Buf(), oB))
    ptr = Ring([(P.sb([128, 512], BF16, "pt"), Buf()) for _ in range(4)])
    sps = Ring([(T["ps"][i], T["psb"][i]) for i in range(4, 8)])
    accs = {}
    lay = [(0, 0), (0, 1), (0, 2), (1, 0), (2, 0), (2, 1), (2, 2), (3, 0)]
    n = 0
    for c in range(2):
        for qt in range(4):
            bk, pos = lay[n]
            n += 1
            accs[(c, qt)] = (T["ps"][bk][:, pos * 129:(pos + 1) * 129], T["psb"][bk])
    small = Ring([(P.sb([128, 8], F32, "sm"), P.sb([128, 128], F32, "tt"), P.sb([128, 128], F32, "oo"),
                   P.sb([128, 128], F32, "sq"), Buf()) for _ in range(4)])
    v_da3 = T["v_da"].rearrange("(t p) e -> p t e", p=128)

    def load_head(h):
        KT, V, QT, kB, vB, qB, oB = hb[h % 2]
        for s4 in range(4):
            P.dma("sp", lambda e, s4=s4: e.dma_start(out=KT[:, s4 * 2048:(s4 + 1) * 2048],
                                                      in_=T["kT_da"][h, :, s4 * 2048:(s4 + 1) * 2048]), writes=[kB])
        for s8 in range(8):
            P.dma("sp", lambda e, s8=s8: e.dma_start(out=V[:, s8 * 8:(s8 + 1) * 8, 0:128],
                                                      in_=v_da3[:, s8 * 8:(s8 + 1) * 8, h * 128:(h + 1) * 128]), writes=[vB])
        P.dma("sp", lambda e: e.dma_start(out=QT[0:64, 0, :], in_=T["qT_da"][h, 0:64, :]), writes=[qB])
        P.dma("sp", lambda e: e.dma_start(out=QT[64:128, 1, :], in_=T["qT_da"][h, 64:128, :]), writes=[qB])

    def epilogue(h, qb, qt):
        O1, O1B = accs[(0, qt)]
        O2, O2B = accs[(1, qt)]
        sm, tt, oo, sq, sB = small.next()
        P.op("dve", lambda e: e.reciprocal(out=sm[:, 0:1], in_=O1[:, 128:129]), reads=[O1B], writes=[sB])
        yield
        P.op("dve", lambda e: e.reciprocal(out=sm[:, 1:2], in_=O2[:, 128:129]), reads=[O2B], writes=[sB])
        yield
        P.op("dve", lambda e: e.tensor_tensor(out=sm[:, 2:3], in0=sm[:, 1:2], in1=neglam[:], op=ALU.mult), reads=[sB, cB], writes=[sB])
        yield
        P.op("dve", lambda e: e.tensor_scalar(out=tt[:], in0=O2[:, 0:128], scalar1=sm[:, 2:3], scalar2=None, op0=ALU.mult),
             reads=[O2B, sB], writes=[sB])
        yield
        P.op("dve", lambda e: e.scalar_tensor_tensor(out=oo[:], in0=O1[:, 0:128], scalar=sm[:, 0:1], in1=tt[:],
                                                      op0=ALU.mult, op1=ALU.add), reads=[O1B, sB], writes=[sB])
        yield
        P.op("act", lambda e: e.activation(out=sq[:], in_=oo[:], func=AF.Square, accum_out=sm[:, 3:4]), reads=[sB], writes=[sB])
        yield
        P.op("act", lambda e: e.activation(out=sm[:, 4:5], in_=sm[:, 3:4], func=AF.Ln, scale=1.0 / 128.0, bias=1e-5),
             reads=[sB], writes=[sB])
        yield
        P.op("act", lambda e: e.activation(out=sm[:, 5:6], in_=sm[:, 4:5], func=AF.Exp, scale=-0.5), reads=[sB], writes=[sB])
        yield
        P.op("dve", lambda e: e.scalar_tensor_tensor(out=a_sb[:, qb * 4 + qt, h * 128:(h + 1) * 128], in0=oo[:], scalar=sm[:, 5:6],
                                                      in1=gbc[:], op0=ALU.mult, op1=ALU.mult), reads=[sB, cB], writes=[a_B])
        yield

    def qk(h, qb, c, kt):
        KT, V, QT, kB, vB, qB, oB = hb[h % 2]
        S, SB = sps.next()
        P.op("pe", lambda e: e.matmul(S[:], KT[:, kt * 128:(kt + 1) * 128],
                                      QT[:, c, qb * 512:(qb + 1) * 512],
                                      start=True, stop=True), reads=[kB, qB, oB], writes=[SB])
        Pt, PtB = ptr.next()
        P.op("act", lambda e: e.activation(out=Pt[:], in_=S[:], func=AF.Exp, scale=0.125),
             reads=[SB], writes=[PtB])
        return Pt, PtB

    def pv(h, qb, c, kt, Pt, PtB):
        KT, V, QT, kB, vB, qB, oB = hb[h % 2]
        for qt in range(4):
            O, OB = accs[(c, qt)]
            P.op("pe", lambda e, O=O, qt=qt: e.matmul(O, Pt[:, qt * 128:(qt + 1) * 128], V[:, kt, :],
                                                      start=(kt == 0 and qt in (0, 3)), stop=(kt == 63), skip_group_check=True),
                 reads=[PtB, vB, oB], writes=[OB])
        if c == 1 and kt == 63:
            interleave([epilogue(h, qb, qt) for qt in range(4)])

    iters = [(h, qb, c, kt) for h in range(T.get("da_heads", 4)) for qb in range(8) for c in range(2) for kt in range(64)]
    LA = 2
    pend = {}
    NH = T.get("da_heads", 4)
    load_head(0)
    if NH > 1:
        load_head(1)
    for n in range(len(iters) + LA):
        if n < len(iters):
            h, qb, c, kt = iters[n]
            pend[n] = qk(h, qb, c, kt)
        m = n - LA
        if m >= 0:
            h, qb, c, kt = iters[m]
            Pt, PtB = pend.pop(m)
            pv(h, qb, c, kt, Pt, PtB)
            if qb == 7 and c == 1 and kt == 63 and h + 2 < NH:
                load_head(h + 2)
    P.barrier()
def phase3(P, nc, T, nb_sb, nb_B):
    P.sb_reset(T["arena0"])
    if T.get("xs_d") is not None:
        zt = P.sb([128, CAP // 128, 1024], BF16, "zt")
        zB = Buf()
        P.op("pool", lambda e: e.memset(zt[:], 0.0), writes=[zB])
        xs3 = T["xs_d"].rearrange("(e s p) d -> e p s d", p=128, s=CAP // 128)
        for e_ in range(NEXP):
            P.dma("sp", lambda e, e_=e_: e.dma_start(out=xs3[e_], in_=zt[:]), reads=[zB])
    Mt = P.sb([128, 18 * 256], BF16, "Mt")
    mB = Buf()
    for s in range(3):
        P.dma("pool", lambda e, s=s: e.dma_start(out=Mt[:, s * 1536:(s + 1) * 1536], in_=T["na_M"][:, s * 1536:(s + 1) * 1536]), writes=[mB])
    Rt = P.sb([128, 18 * 256], F32, "Rt")
    rB = Buf()
    hb = []
    for _ in range(2):
        KT = P.sb([64, 4608], BF16, "KTn")
        V = P.sb([128, 36, 65], BF16, "Vn")
        QT = P.sb([64, 4096], BF16, "QTn")
        E = P.sb([128, 18 * 256], BF16, "En")
        oB = Buf()
        P.op("pool", lambda e, V=V: e.memset(V[:, :, 64:65], 1.0), writes=[oB])
        hb.append((KT, V, QT, E, Buf(), Buf(), Buf(), Buf(), oB))
    per = Ring([(P.sb([128, 512], BF16, "pe_"), Buf()) for _ in range(4)])
    pmr = Ring([(P.sb([128, 512], BF16, "pm_"), Buf()) for _ in range(4)])
    sps = Ring([(T["ps"][i], T["psb"][i]) for i in range(2, 8)])
    accs = [(T["ps"][0][:, 0:65], T["psb"][0]), (T["ps"][0][:, 128:193], T["psb"][0]),
            (T["ps"][1][:, 0:65], T["psb"][1]), (T["ps"][1][:, 128:193], T["psb"][1])]
    smr = Ring([(P.sb([128, 2], F32, "smn"), Buf()) for _ in range(4)])
    v_na3 = T["v_na"].rearrange("(t p) e -> p t e", p=128)

    def load_head(hn):
        KT, V, QT, E, kB, vB, qB, eB, oB = hb[hn % 2]
        j, po = hn // 2, (hn % 2) * 64
        P.dma("sp", lambda e: e.dma_start(out=KT[:, 0:256], in_=T["kT_na"][j, po:po + 64, 7936:8192]), writes=[kB])
        P.dma("sp", lambda e: e.dma_start(out=KT[:, 256:4608], in_=T["kT_na"][j, po:po + 64, 0:4352]), writes=[kB])
        P.dma("sp", lambda e: e.dma_start(out=QT[:], in_=T["qT_na"][j, po:po + 64, :]), writes=[qB])
        P.dma("sp", lambda e: e.dma_start(out=V[:, 0:2, 0:64], in_=v_na3[:, 62:64, hn * 64:(hn + 1) * 64]), writes=[vB])
        for s in range(2):
            P.dma("sp", lambda e, s=s: e.dma_start(out=V[:, 2 + s * 17:2 + (s + 1) * 17, 0:64],
                                                    in_=v_na3[:, s * 17:(s + 1) * 17, hn * 64:(hn + 1) * 64]), writes=[vB])
        P.dma("sp", lambda e: e.dma_start(out=Rt[:], in_=T["na_R"][hn, :, :]), writes=[rB])
        for s in range(3):
            sl = slice(s * 1536, (s + 1) * 1536)
            P.op("act", lambda e, sl=sl: e.activation(out=Rt[:, sl], in_=Rt[:, sl], func=AF.Exp), reads=[rB], writes=[rB])
            P.op("dve", lambda e, sl=sl: e.tensor_tensor(out=E[:, sl], in0=Rt[:, sl], in1=Mt[:, sl], op=ALU.mult),
                 reads=[rB, mB], writes=[eB])

    def qk(hn, g, jp):
        KT, V, QT, E, kB, vB, qB, eB, oB = hb[hn % 2]
        S, SB = sps.next()
        for hf in range(2):
            ti = 2 * g + 2 * jp + hf
            P.op("pe", lambda e, hf=hf, ti=ti: e.matmul(S[:, hf * 256:(hf + 1) * 256], KT[:, ti * 128:(ti + 1) * 128],
                                                        QT[:, g * 256:(g + 1) * 256],
                                                        start=(hf == 0), stop=True, skip_group_check=True), reads=[kB, qB], writes=[SB])
        Pe, PeB = per.next()
        P.op("act", lambda e: e.activation(out=Pe[:], in_=S[:, 0:512], func=AF.Exp, scale=0.125), reads=[SB], writes=[PeB])
        Pm, PmB = pmr.next()
        cls = 0 if g == 0 else (2 if g == 15 else 1)
        ei = (cls * 6 + 2 * jp) * 256
        P.op("dve", lambda e: e.tensor_tensor(out=Pm[:], in0=Pe[:], in1=E[:, ei:ei + 512], op=ALU.mult),
             reads=[PeB, eB], writes=[PmB])
        return Pm, PmB

    def pv(hn, g, jp, Pm, PmB):
        KT, V, QT, E, kB, vB, qB, eB, oB = hb[hn % 2]
        for hf in range(2):
            ti = 2 * g + 2 * jp + hf
            for qt in range(2):
                O, OB = accs[(g % 2) * 2 + qt]
                P.op("pe", lambda e, O=O, qt=qt, hf=hf, ti=ti: e.matmul(
                    O, Pm[:, hf * 256 + qt * 128:hf * 256 + (qt + 1) * 128], V[:, ti, :],
                    start=(jp == 0 and hf == 0 and qt == 0), stop=(jp == 2 and hf == 1), skip_group_check=True),
                    reads=[PmB, vB, oB], writes=[OB])
        if jp == 2:
            for qt in range(2):
                O, OB = accs[(g % 2) * 2 + qt]
                sm, sB = smr.next()
                P.op("dve", lambda e, O=O, sm=sm: e.reciprocal(out=sm[:, 0:1], in_=O[:, 64:65]), reads=[OB], writes=[sB])
                P.op("dve", lambda e, O=O, sm=sm, qt=qt: e.tensor_scalar(
                    out=nb_sb[:, g * 2 + qt, hn * 64:(hn + 1) * 64], in0=O[:, 0:64], scalar1=sm[:, 0:1], scalar2=None,
                    op0=ALU.mult), reads=[OB, sB], writes=[nb_B])

    iters = [(hn, g, j) for hn in range(T.get("na_heads", 8)) for g in range(16) for j in range(3)]
    LA = 2
    pend = {}
    NH = T.get("na_heads", 8)
    load_head(0)
    if NH > 1:
        load_head(1)
    for n in range(len(iters) + LA):
        if n < len(iters):
            hn, g, j = iters[n]
            pend[n] = qk(hn, g, j)
        m = n - LA
        if m >= 0:
            hn, g, j = iters[m]
            Pm, PmB = pend.pop(m)
            pv(hn, g, j, Pm, PmB)
            if g == 15 and j == 2 and hn + 2 < NH:
                load_head(hn + 2)
    P.barrier()
def interleave(gens):
    gens = list(gens)
    while gens:
        nxt = []
        for g in gens:
            try:
                next(g)
                nxt.append(g)
            except StopIteration:
                pass
        gens = nxt


def layer_norm_tile(P, y, yB, out, outB, gbc, bbc, cB, junk, jB, sm, sB):
    for _ in layer_norm_gen(P, y, yB, out, outB, gbc, bbc, cB, junk, jB, sm, sB):
        pass


def layer_norm_gen(P, y, yB, out, outB, gbc, bbc, cB, junk, jB, sm, sB):
    class _W:
        def __init__(self, t):
            self.t = t

        def __getitem__(self, k):
            return self.t if isinstance(self.t, bass.AP) else self.t[k]
    y = _W(y)
    out = _W(out)
    junk = _W(junk)
    P.op("act", lambda e: e.activation(out=junk[:], in_=y[:], func=AF.Identity, accum_out=sm[:, 0:1]), reads=[yB], writes=[jB, sB])
    yield
    P.op("act", lambda e: e.activation(out=junk[:], in_=y[:], func=AF.Square, accum_out=sm[:, 1:2]), reads=[yB], writes=[jB, sB])
    yield
    P.op("dve", lambda e: e.tensor_scalar(out=sm[:, 2:3], in0=sm[:, 0:1], scalar1=1.0 / 1024.0, scalar2=None, op0=ALU.mult), reads=[sB], writes=[sB])
    yield
    P.op("dve", lambda e: e.tensor_tensor(out=sm[:, 3:4], in0=sm[:, 2:3], in1=sm[:, 2:3], op=ALU.mult), reads=[sB], writes=[sB])
    yield
    P.op("dve", lambda e: e.scalar_tensor_tensor(out=sm[:, 4:5], in0=sm[:, 1:2], scalar=1.0 / 1024.0, in1=sm[:, 3:4],
                                                  op0=ALU.mult, op1=ALU.subtract), reads=[sB], writes=[sB])
    yield
    P.op("act", lambda e: e.activation(out=sm[:, 5:6], in_=sm[:, 4:5], func=AF.Ln, scale=1.0, bias=1e-5), reads=[sB], writes=[sB])
    yield
    P.op("act", lambda e: e.activation(out=sm[:, 6:7], in_=sm[:, 5:6], func=AF.Exp, scale=-0.5), reads=[sB], writes=[sB])
    yield
    P.op("dve", lambda e: e.tensor_scalar(out=out[:], in0=y[:], scalar1=sm[:, 2:3], scalar2=sm[:, 6:7], op0=ALU.subtract, op1=ALU.mult),
         reads=[yB, sB], writes=[outB])
    yield
    P.op("dve", lambda e: e.tensor_tensor(out=out[:], in0=out[:], in1=gbc[:], op=ALU.mult), reads=[outB, cB], writes=[outB])
    yield
    P.op("dve", lambda e: e.tensor_tensor(out=out[:], in0=out[:], in1=bbc[:], op=ALU.add), reads=[outB, cB], writes=[outB])
    yield


def phase4(P, nc, T, a_sb, a_B, nb_sb, nb_B, GT, GT_B, slots_all, gk_all, rt_B):
    P.sb_reset(T["arena0"])
    wg = P.sb([128, 8, 2048], BF16, "wg")
    wbd = P.sb([128, 4, 1024], BF16, "wbd")
    wbn = P.sb([128, 4, 1024], BF16, "wbn")
    wo = P.sb([128, 8, 1024], BF16, "wo")
    wr = P.sb([128, 8, 32], F32, "wr")
    bg = P.sb([128, 16], F32, "bg")
    g1bc = P.sb([128, 1024], F32, "g1bc")
    b1bc = P.sb([128, 1024], F32, "b1bc")
    brbc = P.sb([128, 32], F32, "brbc")
    identb = P.sb([128, 128], BF16, "identb")
    ltri = P.sb([128, 128], BF16, "ltri")
    ones = P.sb([128, 128], BF16, "ones")
    ecap = P.sb([128, 32], F32, "ecap")
    cnt = P.sb([128, 32], F32, "cnt")
    cntB = Buf()
    P.op("pool", lambda e: e.memset(cnt[:], 0.0), writes=[cntB])
    identf = P.sb([128, 128], F32, "identf")
    cB = Buf()
    toks = []
    for c in range(8):
        for g in range(4):
            toks.append(P.dma("pool", lambda e, c=c, g=g: e.dma_start(out=wg[:, c, g * 512:(g + 1) * 512],
                                                                      in_=T["w_gate"][c * 128:(c + 1) * 128, g * 512:(g + 1) * 512])))
        toks.append(P.dma("pool", lambda e, c=c: e.dma_start(out=wo[:, c, :], in_=T["w_out"][c * 128:(c + 1) * 128, :])))
        toks.append(P.dma("sp", lambda e, c=c: e.dma_start(out=wr[:, c, :], in_=T["w_router"][c * 128:(c + 1) * 128, :])))
    for c in range(4):
        toks.append(P.dma("pool", lambda e, c=c: e.dma_start(out=wbd[:, c, :], in_=T["w_bda"][c * 128:(c + 1) * 128, :])))
        toks.append(P.dma("pool", lambda e, c=c: e.dma_start(out=wbn[:, c, :], in_=T["w_bna"][c * 128:(c + 1) * 128, :])))
    for dst, src in ((bg, "b_gate"), (g1bc, "ln1_g_bc"), (b1bc, "ln1_b_bc"), (brbc, "b_router_bc"), (identf, "ident")):
        toks.append(P.dma("sp", lambda e, dst=dst, src=src: e.dma_start(out=dst[:], in_=T[src])))
    toks.append(P.dma("pool", lambda e: e.dma_start(out=identb[:], in_=T["ident"])))
    toks.append(P.dma("pool", lambda e: e.dma_start(out=ltri[:], in_=T["ltri"])))
    toks.append(P.dma("pool", lambda e: e.dma_start(out=ones[:], in_=T["ones128"])))
    toks.append(P.dma("sp", lambda e: e.dma_start(out=ecap[:], in_=T["ecap"])))
    for eng in ("pe", "act", "dve", "pool"):
        P.op(eng, None, extra=toks)

    xblk = Ring([(P.sb([128, 8, 512], BF16, "xblk4"), Buf()) for _ in range(1)])
    aT = P.sb([128, 4, 512], BF16, "aT"); aTB = Buf()
    nT = P.sb([128, 4, 512], BF16, "nT"); nTB = Buf()
    g_r = Ring([(P.sb([128, 2, 512], BF16, "g01"), Buf()) for _ in range(2)])
    mT = P.sb([128, 8, 512], BF16, "mT"); mTB = Buf()
    tr = Ring([(P.sb([128, 512], F32, "t4"), P.sb([128, 512], F32, "u4"), Buf()) for _ in range(1)])
    y_r = Ring([(P.sb([128, 1024], F32, "y4"), Buf()) for _ in range(2)])
    x1_r = Ring([(P.sb([128, 1024], F32, "x1"), Buf()) for _ in range(2)])
    junk = P.sb([128, 1024], BF16, "junk"); jB = Buf()
    sm_r = Ring([(P.sb([128, 8], F32, "sm4"), Buf()) for _ in range(2)])
    x1Tf_r = Ring([(P.sb([128, 1024], F32, "x1Tf"), None, Buf()) for _ in range(1)])
    rt_r = Ring([(P.sb([128, 32], F32, "lg"), P.sb([128, 8], F32, "t8"), P.sb([128, 32], F32, "mk"), P.sb([128, 32], F32, "ex"),
                  P.sb([128, 4], F32, "rs"), P.sb([128, 32], F32, "G"), Buf(),
                  P.sb([128, 32], BF16, "mkb"), P.sb([128, 32], F32, "rk"), P.sb([128, 32], F32, "tm"), P.sb([128, 8], F32, "slf")) for _ in range(2)])
    psr = Ring([(T["ps"][i], T["psb"][i]) for i in range(8)])
    xT3 = T["xT"].rearrange("(c p) t -> p c t", p=128)

    def do_block(tb):
        xb, xbB = xblk.next()
        P.dma("pool", lambda e: e.dma_start(out=xb[:], in_=xT3[:, :, tb * 512:(tb + 1) * 512]), writes=[xbB])
        for src, sB_, dst, dB in ((a_sb, a_B, aT, aTB), (nb_sb, nb_B, nT, nTB)):
            for hc in range(4):
                ps, psB = psr.next()
                psb16 = ps.bitcast(BF16)
                for tt in range(4):
                    P.op("pe", lambda e, tt=tt, psb16=psb16, src=src, hc=hc: e.transpose(
                        psb16[:, tt * 128:(tt + 1) * 128], src[:, tb * 4 + tt, hc * 128:(hc + 1) * 128], identb[:]),
                        reads=[sB_], writes=[psB])
                P.op("act", lambda e, psb16=psb16, dst=dst, hc=hc: e.activation(out=dst[:, hc, :], in_=psb16[:, 0:512], func=AF.Identity),
                     reads=[psB], writes=[dB])
        for dt in range(8):
            g01, g01B = g_r.next()
            for gi, j in enumerate((dt, 8 + dt)):
                ps, psB = psr.next()
                for k in range(8):
                    P.op("pe", lambda e, k=k, ps=ps, j=j: e.matmul(ps[:], wg[:, k, j * 128:(j + 1) * 128], xb[:, k, :],
                                                                   start=(k == 0), stop=(k == 7)), reads=[xbB], writes=[psB])
                P.op("act", lambda e, ps=ps, j=j, gi=gi, g01=g01: e.activation(out=g01[:, gi, :], in_=ps[:], func=AF.Sigmoid,
                                                                              bias=bg[:, j:j + 1], scale=1.0),
                     reads=[psB], writes=[g01B])
            psa, psaB = psr.next()
            psn, psnB = psr.next()
            for ec in range(4):
                P.op("pe", lambda e, ec=ec, psa=psa, dt=dt: e.matmul(psa[:], wbd[:, ec, dt * 128:(dt + 1) * 128], aT[:, ec, :],
                                                                    start=(ec == 0), stop=(ec == 3)), reads=[aTB], writes=[psaB])
            for ec in range(4):
                P.op("pe", lambda e, ec=ec, psn=psn, dt=dt: e.matmul(psn[:], wbn[:, ec, dt * 128:(dt + 1) * 128], nT[:, ec, :],
                                                                    start=(ec == 0), stop=(ec == 3)), reads=[nTB], writes=[psnB])
            t4, u4, tB = tr.next()
            P.op("dve", lambda e, t4=t4, psa=psa, g01=g01: e.tensor_tensor(out=t4[:], in0=psa[:], in1=g01[:, 0, :], op=ALU.mult),
                 reads=[psaB, g01B], writes=[tB])
            P.op("dve", lambda e, u4=u4, psn=psn, g01=g01: e.tensor_tensor(out=u4[:], in0=psn[:], in1=g01[:, 1, :], op=ALU.mult),
                 reads=[psnB, g01B], writes=[tB])
            P.op("pool", lambda e, t4=t4, u4=u4, dt=dt: e.tensor_tensor(out=mT[:, dt, :], in0=t4[:], in1=u4[:], op=ALU.add),
                 reads=[tB], writes=[mTB])
        def stageA(tt, x1, x1B, tok0):
            y, yB = y_r.next()
            P.dma("sp", lambda e: e.dma_start(out=y[:], in_=T["x_own"][tok0:tok0 + 128, :]), writes=[yB])
            yield
            for half in range(2):
                ps, psB = psr.next()
                for k in range(8):
                    P.op("pe", lambda e, k=k, ps=ps, half=half: e.matmul(
                        ps[:], mT[:, k, tt * 128:(tt + 1) * 128], wo[:, k, half * 512:(half + 1) * 512],
                        start=(k == 0), stop=(k == 7)), reads=[mTB], writes=[psB])
                    yield
                P.op("dve", lambda e, ps=ps, half=half: e.scalar_tensor_tensor(
                    out=y[:, half * 512:(half + 1) * 512], in0=y[:, half * 512:(half + 1) * 512], scalar=ALPHA, in1=ps[:],
                    op0=ALU.mult, op1=ALU.add), reads=[psB, yB], writes=[yB])
                yield
            sm, sB = sm_r.next()
            yield from layer_norm_gen(P, y, yB, x1, x1B, g1bc, b1bc, cB, junk, jB, sm, sB)
            P.dma("sp", lambda e: e.dma_start(out=T["x1_d"][tok0:tok0 + 128, :], in_=x1[:]), reads=[x1B])
            yield

        def stageB(tt, x1, x1B, tok0):
            x1Tf, x1Tb, xTB = x1Tf_r.next()
            for k2 in range(2):
                ps, psB = psr.next()
                for k4 in range(4):
                    k = k2 * 4 + k4
                    P.op("pe", lambda e, ps=ps, k=k, k4=k4, x1=x1: e.transpose(ps[:, k4 * 128:(k4 + 1) * 128], x1[:, k * 128:(k + 1) * 128],
                                                                        identf[:]), reads=[x1B], writes=[psB])
                    yield
                P.op("act", lambda e, ps=ps, x1Tf=x1Tf, k2=k2: e.activation(out=x1Tf[:, k2 * 512:(k2 + 1) * 512], in_=ps[:], func=AF.Identity),
                     reads=[psB], writes=[xTB])
                yield
            ps, psB = psr.next()
            for k in range(8):
                P.op("pe", lambda e, ps=ps, k=k, x1Tf=x1Tf: e.matmul(ps[:, 0:32], x1Tf[:, k * 128:(k + 1) * 128], wr[:, k, :], start=(k == 0), stop=(k == 7)),
                     reads=[xTB], writes=[psB])
                yield
            lg, t8, mk, ex, rs, G, rB, mkb, rk, tm, slf = rt_r.next()
            P.op("dve", lambda e, ps=ps, lg=lg: e.tensor_tensor(out=lg[:], in0=ps[:, 0:32], in1=brbc[:], op=ALU.add), reads=[psB], writes=[rB])
            yield
            P.op("dve", lambda e, lg=lg, t8=t8: e.max(out=t8[:], in_=lg[:]), reads=[rB], writes=[rB])
            yield
            P.op("dve", lambda e, lg=lg, t8=t8, mk=mk: e.tensor_scalar(out=mk[:], in0=lg[:], scalar1=t8[:, 3:4], scalar2=None, op0=ALU.is_ge),
                 reads=[rB], writes=[rB])
            yield
            P.op("dve", lambda e, t8=t8, rs=rs: e.tensor_scalar(out=rs[:, 0:1], in0=t8[:, 0:1], scalar1=-1.0, scalar2=None, op0=ALU.mult),
                 reads=[rB], writes=[rB])
            yield
            P.op("act", lambda e, lg=lg, ex=ex, rs=rs: e.activation(out=ex[:], in_=lg[:], func=AF.Exp, bias=rs[:, 0:1], scale=1.0),
                 reads=[rB], writes=[rB])
            yield
            P.op("dve", lambda e, ex=ex, mk=mk: e.tensor_tensor(out=ex[:], in0=ex[:], in1=mk[:], op=ALU.mult), reads=[rB], writes=[rB])
            yield
            P.op("dve", lambda e, ex=ex, rs=rs: e.reduce_sum(out=rs[:, 1:2], in_=ex[:], axis=AX.X), reads=[rB], writes=[rB])
            yield
            P.op("dve", lambda e, rs=rs: e.reciprocal(out=rs[:, 2:3], in_=rs[:, 1:2]), reads=[rB], writes=[rB])
            yield
            P.op("dve", lambda e, ex=ex, rs=rs, G=G: e.tensor_scalar(out=G[:], in0=ex[:], scalar1=rs[:, 2:3], scalar2=None, op0=ALU.mult),
                 reads=[rB], writes=[rB])
            yield
            ps2, ps2B = psr.next()
            P.op("pe", lambda e, ps2=ps2, G=G: e.transpose(ps2[0:32, 0:128], G[:], identf[:]), reads=[rB], writes=[ps2B])
            yield
            P.op("act", lambda e, ps2=ps2, tok0=tok0: e.activation(out=GT[:, tok0:tok0 + 128], in_=ps2[0:32, 0:128], func=AF.Identity),
                 reads=[ps2B], writes=[GT_B])
            yield
            tix = tb * 4 + tt
            P.op("dve", lambda e, mk=mk, mkb=mkb: e.tensor_copy(out=mkb[:], in_=mk[:]), reads=[rB], writes=[rB])
            yield
            ps3, ps3B = psr.next()
            P.op("pe", lambda e, ps3=ps3, mkb=mkb: e.matmul(ps3[:, 0:32], ltri[:], mkb[:], start=True, stop=True, skip_group_check=True),
                 reads=[rB], writes=[ps3B])
            yield
            P.op("pe", lambda e, ps3=ps3, mkb=mkb: e.matmul(ps3[:, 64:96], ones[:], mkb[:], start=False, stop=True, skip_group_check=True),
                 reads=[rB], writes=[ps3B])
            yield
            P.op("dve", lambda e, ps3=ps3, rk=rk: e.tensor_tensor(out=rk[:], in0=ps3[:, 0:32], in1=cnt[:], op=ALU.add), reads=[ps3B, cntB], writes=[rB])
            yield
            P.op("dve", lambda e, ps3=ps3: e.tensor_tensor(out=cnt[:], in0=cnt[:], in1=ps3[:, 64:96], op=ALU.add), reads=[ps3B, cntB, rB], writes=[cntB])
            yield
            P.op("dve", lambda e, rk=rk: e.scalar_tensor_tensor(out=rk[:], in0=rk[:], scalar=float(CAP - 1), in1=ecap[:], op0=ALU.min, op1=ALU.add),
                 reads=[rB], writes=[rB])
            yield
            for k in range(4):
                P.op("dve", lambda e, k=k, lg=lg, t8=t8, rk=rk, tm=tm: e.scalar_tensor_tensor(out=tm[:], in0=lg[:], scalar=t8[:, k:k + 1], in1=rk[:],
                                                                                             op0=ALU.is_equal, op1=ALU.mult), reads=[rB], writes=[rB])
                yield
                P.op("dve", lambda e, k=k, tm=tm, slf=slf: e.reduce_sum(out=slf[:, k:k + 1], in_=tm[:], axis=AX.X), reads=[rB], writes=[rB])
                yield
            P.op("dve", lambda e, slf=slf, tix=tix: e.tensor_copy(out=slots_all[:, tix, :], in_=slf[:, 0:4]), reads=[rB], writes=[rB, rt_B])
            yield
            P.op("act", lambda e, t8=t8, slf=slf, rs=rs: e.activation(out=slf[:, 4:8], in_=t8[:, 0:4], func=AF.Exp, bias=rs[:, 0:1], scale=1.0),
                 reads=[rB], writes=[rB])
            yield
            P.op("dve", lambda e, slf=slf, rs=rs: e.reduce_sum(out=rs[:, 3:4], in_=slf[:, 4:8], axis=AX.X), reads=[rB], writes=[rB])
            yield
            P.op("dve", lambda e, rs=rs: e.reciprocal(out=rs[:, 3:4], in_=rs[:, 3:4]), reads=[rB], writes=[rB])
            yield
            P.op("dve", lambda e, slf=slf, rs=rs, tix=tix: e.tensor_scalar(out=gk_all[:, tix, :], in0=slf[:, 4:8], scalar1=rs[:, 3:4], scalar2=None,
                                                                          op0=ALU.mult), reads=[rB], writes=[rB, rt_B])
            yield
            if T.get("dbg_G") is not None:
                P.dma("sp", lambda e, G=G, tok0=tok0: e.dma_start(out=T["dbg_G"][tok0:tok0 + 128, :], in_=G[:]), reads=[rB])
                yield

        bufs = []
        for tt in range(4):
            x1_, x1B_ = x1_r.next()
            bufs.append((x1_, x1B_, tb * 512 + tt * 128))
        interleave([stageA(0, *bufs[0])])
        for tt in range(4):
            gens = []
            if tt + 1 < 4:
                gens.append(stageA(tt + 1, *bufs[tt + 1]))
            gens.append(stageB(tt, *bufs[tt]))
            interleave(gens)

    for tb in range(8):
        do_block(tb)
    if T.get("dbg_cnt") is not None:
        P.dma("sp", lambda e: e.dma_start(out=T["dbg_cnt"], in_=cnt[:]), reads=[cntB])
    P.barrier()
def phase5s(P, nc, T, GT, GT_B, slots_all, gk_all, rt_B):
    P.sb_reset(T["arena5"])
    xb_r = Ring([(P.sb([128, 1024], BF16, "x1b"), Buf()) for _ in range(8)])
    for tix in range(32):
        x1b, x1bB = xb_r.next()
        P.dma("pool", lambda e, x1b=x1b, tix=tix: e.dma_start(out=x1b[:], in_=T["x1_d"][tix * 128:(tix + 1) * 128, :]), writes=[x1bB])
        for k in range(4):
            P.dma("pool", lambda e, k=k, x1b=x1b, tix=tix: e.indirect_dma_start(
                out=T["xs_d"], out_offset=bass.IndirectOffsetOnAxis(ap=slots_all[:, tix, k:k + 1], axis=0),
                in_=x1b[:], in_offset=None, bounds_check=None, oob_is_err=False), reads=[x1bB, rt_B])
    P.barrier()
    P.sb_reset(T["arena5"])
    NS = CAP // 128
    CH = [(0, 512), (512, 512), (1024, 256)]
    NH_ = 512
    b1a = P.sb([128, 32, 16], F32, "b1a")
    identb = P.sb([128, 128], BF16, "identb5")
    toks = [P.dma("sp", lambda e: e.dma_start(out=b1a[:], in_=T["b_mlp1"])),
            P.dma("pool", lambda e: e.dma_start(out=identb[:], in_=T["ident"]))]
    for eng in ("pe", "act", "dve"):
        P.op(eng, None, extra=toks)
    wr_ = Ring([(P.sb([128, 8, 1024], BF16, "w1g"), P.sb([128, 8, 1024], BF16, "w1l"), P.sb([128, 8, 1024], BF16, "w2"),
                 None, Buf(), Buf(), Buf(), None) for _ in range(2)])
    xs_tm = P.sb([128, NS, 1024], BF16, "xs_tm"); xB = Buf()
    xsT_r = [(P.sb([128, 8, CAP], BF16, "xsT"), Buf()) for _ in range(2)]
    actT = P.sb([128, 8, CAP], BF16, "actT"); aB = Buf()
    tmp_r = Ring([(P.sb([128, NH_], F32, "g5"), P.sb([128, NH_], F32, "s5"), P.sb([128, NH_], F32, "l5"), Buf()) for _ in range(2)])
    ys_r = Ring([(P.sb([128, 1024], BF16, "ys"), Buf()) for _ in range(3)])
    psr = Ring([(T["ps"][i], T["psb"][i]) for i in range(8)])
    xs3 = T["xs_d"].rearrange("(e s p) d -> e p s d", p=128, s=NS)
    ys3 = T["ys_d"].rearrange("(e s p) d -> e s p d", p=128, s=NS)

    def load_w(e_):
        w1g, w1l, w2, _x, gB, lB, wB, _b = wr_.next()
        for c in range(8):
            P.dma("pool", lambda e, c=c: e.dma_start(out=w1g[:, c, :], in_=T["w1g"][e_, c * 128:(c + 1) * 128, :]), writes=[gB])
            P.dma("pool", lambda e, c=c: e.dma_start(out=w1l[:, c, :], in_=T["w1l"][e_, c * 128:(c + 1) * 128, :]), writes=[lB])
        for c in range(8):
            P.dma("pool", lambda e, c=c: e.dma_start(out=w2[:, c, :], in_=T["w2"][e_, c * 128:(c + 1) * 128, :]), writes=[wB])
        return w1g, w1l, w2, None, gB, lB, wB, None

    def load_xs(e_):
        P.dma("sp", lambda e: e.dma_start(out=xs_tm[:], in_=xs3[e_]), writes=[xB])

    def tr_group(xsT, xsTB, k, s0, n):
        ps, psB = psr.next()
        psb16 = ps.bitcast(BF16)
        for i in range(n):
            P.op("pe", lambda e, i=i: e.transpose(psb16[:, i * 128:(i + 1) * 128], xs_tm[:, s0 + i, k * 128:(k + 1) * 128], identb[:]),
                 reads=[xB], writes=[psB])
        P.op("act", lambda e: e.activation(out=xsT[:, k, s0 * 128:(s0 + n) * 128], in_=psb16[:, 0:n * 128], func=AF.Identity),
             reads=[psB], writes=[xsTB])

    def mm1(e_, W, f, hf, xsT, xsTB):
        w1g, w1l, w2, _x, gB, lB, wB, _b = W
        c0, cn = CH[hf]
        sl = slice(c0, c0 + cn)
        pg, pgB = psr.next()
        pl, plB = psr.next()
        for k in range(8):
            P.op("pe", lambda e, k=k: e.matmul(pg[:, 0:cn], w1g[:, k, f * 128:(f + 1) * 128], xsT[:, k, sl],
                                               start=(k == 0), stop=(k == 7)), reads=[gB, xsTB], writes=[pgB])
        for k in range(8):
            P.op("pe", lambda e, k=k: e.matmul(pl[:, 0:cn], w1l[:, k, f * 128:(f + 1) * 128], xsT[:, k, sl],
                                               start=(k == 0), stop=(k == 7)), reads=[lB, xsTB], writes=[plB])
        g5, s5, l5, tB = tmp_r.next()
        P.op("act", lambda e: e.activation(out=l5[:, 0:cn], in_=pl[:, 0:cn], func=AF.Identity, bias=b1a[:, e_, 8 + f:9 + f], scale=1.0),
             reads=[plB], writes=[tB])
        P.op("dve", lambda e: e.tensor_scalar(out=g5[:, 0:cn], in0=pg[:, 0:cn], scalar1=b1a[:, e_, f:f + 1], scalar2=7.0,
                                              op0=ALU.add, op1=ALU.min), reads=[pgB], writes=[tB])
        P.op("act", lambda e: e.activation(out=s5[:, 0:cn], in_=g5[:, 0:cn], func=AF.Sigmoid, scale=1.702), reads=[tB], writes=[tB])
        P.op("dve", lambda e: e.tensor_scalar(out=l5[:, 0:cn], in0=l5[:, 0:cn], scalar1=-7.0, scalar2=7.0, op0=ALU.max, op1=ALU.min),
             reads=[tB], writes=[tB])
        P.op("dve", lambda e: e.tensor_tensor(out=g5[:, 0:cn], in0=g5[:, 0:cn], in1=s5[:, 0:cn], op=ALU.mult), reads=[tB], writes=[tB])
        P.op("dve", lambda e: e.scalar_tensor_tensor(out=actT[:, f, sl], in0=l5[:, 0:cn], scalar=1.0, in1=g5[:, 0:cn], op0=ALU.add, op1=ALU.mult),
             reads=[tB], writes=[aB])

    def mm2(e_, W, st):
        w1g, w1l, w2, _x, gB, lB, wB, _b = W
        ys, yB = ys_r.next()
        for half in range(2):
            ps, psB = psr.next()
            for k in range(8):
                P.op("pe", lambda e, k=k, ps=ps, half=half: e.matmul(ps[:], actT[:, k, st * 128:(st + 1) * 128],
                                                                    w2[:, k, half * 512:(half + 1) * 512],
                                                                    start=(k == 0), stop=(k == 7)), reads=[aB, wB], writes=[psB])
            if half == 0:
                P.op("act", lambda e, ps=ps: e.activation(out=ys[:, 0:512], in_=ps[:], func=AF.Identity), reads=[psB], writes=[yB])
            else:
                P.op("dve", lambda e, ps=ps: e.tensor_copy(out=ys[:, 512:1024], in_=ps[:]), reads=[psB], writes=[yB])
        P.dma("sp", lambda e: e.dma_start(out=ys3[e_, st], in_=ys[:]), reads=[yB])

    NE = T.get("n_exp", 32)

    def transposes(e_):
        xsT, xsTB = xsT_r[e_ % 2]
        for k in range(8):
            for s0 in range(0, NS, 4):
                tr_group(xsT, xsTB, k, s0, min(4, NS - s0))

    W = load_w(0)
    load_xs(0)
    transposes(0)
    for e_ in range(NE):
        Wn = load_w(e_ + 1) if e_ + 1 < NE else None
        if e_ + 1 < NE:
            load_xs(e_ + 1)
        xsT, xsTB = xsT_r[e_ % 2]
        for f in range(8):
            for hf in range(len(CH)):
                mm1(e_, W, f, hf, xsT, xsTB)
        if e_ + 1 < NE:
            transposes(e_ + 1)
        for st in range(NS):
            mm2(e_, W, st)
        W = Wn
    P.barrier()

    P.sb_reset(T["arena5"])
    b2b = P.sb([32, 1024], BF16, "b2b")
    g2bc = P.sb([128, 1024], F32, "g2bc")
    b2bc = P.sb([128, 1024], F32, "b2bc")
    cB = Buf()
    toks = [P.dma("pool", lambda e: e.dma_start(out=b2b[:], in_=T["b_mlp2"])),
            P.dma("sp", lambda e: e.dma_start(out=g2bc[:], in_=T["ln2_g_bc"])),
            P.dma("sp", lambda e: e.dma_start(out=b2bc[:], in_=T["ln2_b_bc"]))]
    for eng in ("pe", "act", "dve"):
        P.op(eng, None, extra=toks)
    rows_r = Ring([(P.sb([128, 1024], BF16, "rows"), Buf()) for _ in range(16)])
    x1_r = Ring([(P.sb([128, 1024], F32, "x1c"), Buf()) for _ in range(4)])
    acc_r = Ring([(P.sb([128, 1024], F32, "accc"), Buf()) for _ in range(4)])
    out_r = Ring([(P.sb([128, 1024], F32, "outc"), Buf()) for _ in range(4)])
    junk_r = Ring([(P.sb([128, 1024], BF16, "junk6"), Buf()) for _ in range(4)])
    sm_r = Ring([(P.sb([128, 8], F32, "sm6"), Buf()) for _ in range(4)])

    def comb_tile(ti):
        tok0 = ti * 128
        x1c, x1B = x1_r.next()
        P.dma("sp", lambda e: e.dma_start(out=x1c[:], in_=T["x1_d"][tok0:tok0 + 128, :]), writes=[x1B])
        yield
        acc, accB = acc_r.next()
        for half in range(2):
            ps, psB = psr.next()
            P.op("pe", lambda e, ps=ps, half=half: e.matmul(ps[:], GT[:, tok0:tok0 + 128], b2b[:, half * 512:(half + 1) * 512],
                                                            start=True, stop=True), reads=[GT_B], writes=[psB])
            yield
            P.op("dve", lambda e, ps=ps, half=half: e.scalar_tensor_tensor(out=acc[:, half * 512:(half + 1) * 512],
                                                                           in0=x1c[:, half * 512:(half + 1) * 512], scalar=ALPHA, in1=ps[:],
                                                                           op0=ALU.mult, op1=ALU.add), reads=[psB, x1B], writes=[accB])
            yield
        for k in range(4):
            rows, rwB = rows_r.next()
            P.dma("pool", lambda e, rows=rows, k=k: e.indirect_dma_start(
                out=rows[:], out_offset=None, in_=T["ys_d"],
                in_offset=bass.IndirectOffsetOnAxis(ap=slots_all[:, ti, k:k + 1], axis=0),
                bounds_check=None, oob_is_err=False), reads=[rt_B], writes=[rwB])
            yield
            P.op("dve", lambda e, rows=rows, k=k: e.scalar_tensor_tensor(out=acc[:], in0=rows[:], scalar=gk_all[:, ti, k:k + 1], in1=acc[:],
                                                                         op0=ALU.mult, op1=ALU.add), reads=[rwB, rt_B, accB], writes=[accB])
            yield
        o, oB = out_r.next()
        sm, sB = sm_r.next()
        jk, jkB = junk_r.next()
        yield from layer_norm_gen(P, acc, accB, o, oB, g2bc, b2bc, cB, jk, jkB, sm, sB)
        P.dma("sp", lambda e: e.dma_start(out=T["out"][tok0:tok0 + 128, :], in_=o[:]), reads=[oB])

    for ti in range(0, 32, 4):
        interleave([comb_tile(ti + j) for j in range(4)])
    P.barrier()
def build(upto=99, debug=False):
    nc = bass.Bass("TRN2", target_bir_lowering=False)
    T = {}

    def inp(name, shape, dt=F32):
        T[name] = nc.dram_tensor(name, list(shape), dt, kind="ExternalInput").ap()

    def scr(name, shape, dt=BF16):
        T[name] = nc.dram_tensor(name, list(shape), dt, kind=("ExternalOutput" if (debug and debug.get("dump_scr")) else "Internal")).ap()

    def outp(name, shape, dt=F32):
        T[name] = nc.dram_tensor(name, list(shape), dt, kind="ExternalOutput").ap()

    inp("xT", [1024, 8192]); inp("x_own", [4096, 1024])
    inp("w_fm", [1024, 3072]); inp("w_tm", [1024, 1024]); inp("b_fm", [128, 24]); inp("b_tm", [128, 1024])
    inp("cosT", [128, 8192]); inp("sinT", [128, 8192]); inp("lamv", [128, 256]); inp("subln_bc", [128, 128])
    scr("qT_da", [4, 128, 4096]); scr("kT_da", [4, 128, 8192]); scr("qT_na", [4, 128, 4096]); scr("kT_na", [4, 128, 8192])
    scr("v_da", [8192, 512]); scr("v_na", [8192, 512])
    if upto >= 3:
        inp("na_R", [8, 128, 18 * 256]); inp("na_M", [128, 18 * 256])
    if upto >= 4:
        inp("w_gate", [1024, 2048]); inp("b_gate", [128, 16]); inp("w_bda", [512, 1024]); inp("w_bna", [512, 1024])
        inp("w_out", [1024, 1024]); inp("w_router", [1024, 32]); inp("ln1_g_bc", [128, 1024]); inp("ln1_b_bc", [128, 1024])
        inp("b_router_bc", [128, 32]); inp("ident", [128, 128])
        scr("x1_d", [4096, 1024], F32)
        inp("ltri", [128, 128]); inp("ones128", [128, 128]); inp("ecap", [128, 32])
        scr("xs_d", [NEXP * CAP, 1024], BF16); scr("ys_d", [NEXP * CAP, 1024], BF16)
        if debug and debug.get("dump_scr"):
            outp("dbg_G", [4096, 32])
        if debug and debug.get("dump_cnt"):
            outp("dbg_cnt", [128, 32])
    if upto >= 5:
        inp("b_mlp1", [128, 32, 16]); inp("b_mlp2", [32, 1024])
        inp("ln2_g_bc", [128, 1024]); inp("ln2_b_bc", [128, 1024])
        inp("w1g", [32, 1024, 1024]); inp("w1l", [32, 1024, 1024]); inp("w2", [32, 1024, 1024])
        outp("out", [4096, 1024])
    psall = nc.alloc_psum_tensor("psall", [128, 4096], F32)
    T["ps"] = [psall[:, i * 512:(i + 1) * 512] for i in range(8)]
    T["ps2"] = [psall[:, 2048:3072], psall[:, 3072:4096]]
    T["psb"] = [Buf() for _ in range(8)]
    P = Prog(nc)
    GT = P.sb([32, 4096], BF16, "GT")
    GT_B = Buf()
    slots_all = P.sb([128, 32, 4], I32, "slots_all")
    gk_all = P.sb([128, 32, 4], F32, "gk_all")
    rt_B = Buf()
    T["arena5"] = P.sb_off
    a_sb = P.sb([128, 32, 512], BF16, "a_sb")
    nb_sb = P.sb([128, 32, 512], BF16, "nb_sb")
    a_B, nb_B = Buf(), Buf()
    T["arena0"] = P.sb_off
    if debug:
        T["da_heads"] = debug.get("da_heads", 4)
        T["n_exp"] = debug.get("n_exp", 32)
    phase1(P, nc, T)
    if upto >= 2:
        phase2(P, nc, T, a_sb, a_B)
    if upto >= 3:
        phase3(P, nc, T, nb_sb, nb_B)
    if upto >= 4:
        phase4(P, nc, T, a_sb, a_B, nb_sb, nb_B, GT, GT_B, slots_all, gk_all, rt_B)
    if upto >= 5:
        phase5s(P, nc, T, GT, GT_B, slots_all, gk_all, rt_B)
    if upto < 4:
        outp("dbg_a", [4096, 512], BF16); outp("dbg_nb", [4096, 512], BF16)
        P.dma("sp", lambda e: e.dma_start(out=T["dbg_a"].rearrange("(t p) e -> p t e", p=128), in_=a_sb[:]), reads=[a_B])
        P.dma("sp", lambda e: e.dma_start(out=T["dbg_nb"].rearrange("(t p) e -> p t e", p=128), in_=nb_sb[:]), reads=[nb_B])
        if debug and debug.get("dump_scr"):
            for nm in ("qT_da", "kT_da", "v_da", "qT_na", "kT_na", "v_na"):
                pass
    P.barrier()
    P.emit()
    return nc, P


def rope_tables(pos):
    inv = (10000.0 ** (-np.arange(0, 64, 2, dtype=np.float32) / np.float32(64))).astype(np.float32)
    ang = pos.astype(np.float32)[:, None] * inv[None, :]
    ang = np.concatenate([ang, ang], axis=-1)
    cos = np.cos(ang).astype(np.float32)
    sin = np.sin(ang).astype(np.float32)
    sgn = np.concatenate([-np.ones(32, np.float32), np.ones(32, np.float32)])
    sin_s = sin * sgn[None, :]
    cosT = np.ascontiguousarray(np.concatenate([cos, cos], axis=1).T)
    sinT = np.ascontiguousarray(np.concatenate([sin_s, sin_s], axis=1).T)
    return cosT, sinT


def na_tables(rpb, h):
    R = np.zeros((8, 3, 6, 2, 64, 4, 64), np.float32)
    M = np.zeros((3, 6, 2, 64, 4, 64), np.float32)
    cc = np.arange(64)
    cs = np.clip(cc - 8, 0, 48)
    colvalid = (cc[:, None] >= cs[None, :]) & (cc[:, None] <= cs[None, :] + 15)
    coloff = np.clip(cc[:, None] - cc[None, :] + 15, 0, 30)
    for cls, g in ((0, 0), (1, 1 if h == 0 else 14), (2, 15)):
        for j in range(6):
            for a in range(2):
                for i in range(4):
                    r = 64 * h + 4 * g + i
                    kr = 64 * h + 4 * g + 2 * j - 4 + a
                    rs = min(max(r - 4, 0), 120)
                    if kr < rs or kr > rs + 7:
                        continue
                    M[cls, j, a, :, i, :] = colvalid
                    R[:, cls, j, a, :, i, :] = rpb[:, kr - r + 7][:, coloff] * colvalid[None]
    M2 = np.ascontiguousarray(M.transpose(2, 3, 0, 1, 4, 5).reshape(128, 18 * 256))
    R2 = np.ascontiguousarray(R.transpose(0, 3, 4, 1, 2, 5, 6).reshape(8, 128, 18 * 256))
    return R2, M2


def host_prep(inputs, upto=99):
    x = np.asarray(inputs["x"], np.float32)
    w_in = np.asarray(inputs["w_in"], np.float32)[0]
    b_in = np.asarray(inputs["b_in"], np.float32)[0]
    d = np.arange(64)
    swap = np.concatenate([(hh * 128 + c * 64 + (d + 32) % 64) for hh in range(4) for c in range(2)])
    qda, kda, vda = np.arange(0, 512), np.arange(512, 1024), np.arange(1024, 1536)
    qna, kna, vna = np.arange(1536, 2048), np.arange(2048, 2560), np.arange(2560, 3072)
    fm_cols = np.concatenate([qda, qda[swap], kda, kda[swap], qna, kna])
    tm_cols = np.concatenate([vda, vna])
    w_fm = np.ascontiguousarray(w_in[:, fm_cols])
    w_tm = np.ascontiguousarray(w_in[:, tm_cols])
    b_fm = np.ascontiguousarray(b_in[fm_cols].reshape(24, 128).T)
    b_tm = np.ascontiguousarray(np.broadcast_to(b_in[tm_cols][None, :], (128, 1024)))
    lamv = np.concatenate([np.asarray(inputs[k], np.float32)[0] for k in ("lambda_q1", "lambda_k1", "lambda_q2", "lambda_k2")])
    lamv = np.ascontiguousarray(np.broadcast_to(lamv[None, :], (128, 256)))
    subln_bc = np.ascontiguousarray(np.broadcast_to(np.asarray(inputs["subln_g"], np.float32)[0][None, :], (128, 128)))
    rpb = np.asarray(inputs["rpb"], np.float32)[0]
    shared = dict(w_fm=w_fm, w_tm=w_tm, b_fm=b_fm, b_tm=b_tm, lamv=lamv, subln_bc=subln_bc)
    f32 = lambda k: np.asarray(inputs[k], np.float32)[0]
    bc = lambda v, n=128: np.ascontiguousarray(np.broadcast_to(v[None, :], (n, v.shape[0])))
    if upto >= 4:
        shared.update(w_gate=np.ascontiguousarray(w_in[:, 3072:5120]), b_gate=np.ascontiguousarray(b_in[3072:5120].reshape(16, 128).T),
                      w_bda=f32("w_branch_da"), w_bna=f32("w_branch_na"), w_out=f32("w_out"), w_router=f32("w_router"),
                      ln1_g_bc=bc(f32("ln1_g")), ln1_b_bc=bc(f32("ln1_b")), b_router_bc=bc(f32("b_router")),
                      ident=np.eye(128, dtype=np.float32), ltri=np.triu(np.ones((128, 128), np.float32), k=1),
                      ones128=np.ones((128, 128), np.float32),
                      ecap=np.ascontiguousarray(np.broadcast_to((np.arange(32, dtype=np.float32) * CAP)[None, :], (128, 32))))
    if upto >= 5:
        w1 = f32("w_mlp1"); b1 = f32("b_mlp1")
        b1g = b1[:, 0::2].reshape(32, 8, 128).transpose(2, 0, 1)
        b1l = b1[:, 1::2].reshape(32, 8, 128).transpose(2, 0, 1)
        shared.update(b_mlp1=np.ascontiguousarray(np.concatenate([b1g, b1l], axis=2)),
                      b_mlp2=f32("b_mlp2"), ln2_g_bc=bc(f32("ln2_g")), ln2_b_bc=bc(f32("ln2_b")),
                      w1g=np.ascontiguousarray(w1[:, :, 0::2]), w1l=np.ascontiguousarray(w1[:, :, 1::2]), w2=f32("w_mlp2"))
    natab = [na_tables(rpb, h) for h in range(2)] if upto >= 3 else None
    maps = []
    for c in range(NCORES):
        b, h = c // 2, c % 2
        perm = np.concatenate([np.arange(h * 4096, (h + 1) * 4096), np.arange((1 - h) * 4096, (2 - h) * 4096)])
        xb = x[b]
        cosT, sinT = rope_tables(perm)
        m = dict(shared)
        m["xT"] = np.ascontiguousarray(xb[perm].T)
        m["x_own"] = np.ascontiguousarray(xb[h * 4096:(h + 1) * 4096])
        m["cosT"] = cosT
        m["sinT"] = sinT
        if upto >= 3:
            m["na_R"], m["na_M"] = natab[h]
        maps.append(m)
    return maps


def kernel(**inputs):
    from concourse.bass_utils import run_bass_kernel_spmd
    maps = host_prep(inputs)
    nc, P = build()
    res = run_bass_kernel_spmd(nc, maps, core_ids=list(range(NCORES)))
    out = np.zeros((4, SEQ, D), np.float32)
    for c in range(NCORES):
        b, h = c // 2, c % 2
        out[b, h * 4096:(h + 1) * 4096] = np.asarray(res.results[c]["out"], np.float32)
    return out
```
